# Optimizing a Trainium2 kernel written in Bass

```python
import math
import jax
import jax.numpy as jnp
from jax import lax
import numpy as np

D_MODEL = 1024
BATCH = 4
SEQ = 4096
DEPTH = 2

GRID_W = 64
CTX_LEN = 256
MIX_W = D_MODEL
ML_H = 4
ML_W = MIX_W // 4
ML_D = ML_W // ML_H
ML_CHUNK = 64
HG_H = 4
HG_W = MIX_W // 4
HG_K = HG_W // HG_H
HG_V = HG_W // HG_H
HG_CHUNK = 16
DA_H = 4
DA_W = MIX_W // 2
DA_V = DA_W // DA_H
DA_D = DA_V // 2
Q_BLOCK = 128
ROPE_BASE = 10000.0
FF_DENSE = ((8 * D_MODEL // 3 + 255) // 256) * 256
N_EXPERTS = 8
TOP_K = 2
FF_EXPERT = 7 * D_MODEL // 2
N_DENSE = (DEPTH + 1) // 2
N_MOE = DEPTH // 2
EPS = 1e-6
MASK_NEG = -1e30
LB_FLOOR = 1e-30
f32 = jnp.float32

PROJ_LAYOUT = (
    ('ml_q', ML_W), ('ml_k', ML_W), ('ml_v', ML_W), ('ml_o', ML_W), ('ml_gates', 4 * ML_H),
    ('hg_q', HG_W), ('hg_f', 2 * HG_W), ('hg_i', HG_W), ('hg_g', HG_W),
    ('da_q', DA_W), ('da_k', DA_W), ('da_v', DA_W),
)
PROJ_W = sum(w for _, w in PROJ_LAYOUT)

kernel_name = 'hybrid_mlstm_hgrn2_diffattn_prefix_dit'


def rms_norm(x, w):
    x32 = x.astype(f32)
    y = x32 * lax.rsqrt(jnp.mean(x32 * x32, axis=-1, keepdims=True) + EPS)
    return (y * w.astype(f32)).astype(x.dtype)


def head_rms_norm(x, w, n_heads):
    b, l, _ = x.shape
    return rms_norm(x.reshape(b, l, n_heads, -1), w).reshape(b, l, -1)


def modulate(h, shift, scale):
    return h * (1 + scale) + shift


def split_proj(p):
    out = {}
    start = 0
    for name, width in PROJ_LAYOUT:
        out[name] = p[..., start:start + width]
        start += width
    return out


def to_heads(t, n_heads):
    b, l, w = t.shape
    return t.astype(f32).reshape(b, l, n_heads, w // n_heads).transpose(0, 2, 1, 3)


def from_heads(t):
    b, h, l, d = t.shape
    return t.transpose(0, 2, 1, 3).reshape(b, l, h * d)


def chunk_time(a, lc):
    b, h, l = a.shape[:3]
    return a.reshape(b, h, l // lc, lc, *a.shape[3:])


def unchunk_time(a):
    b, h, n, lc = a.shape[:4]
    return a.reshape(b, h, n * lc, *a.shape[4:])


def scan_chunks(step, init, xs):
    xs = jax.tree_util.tree_map(lambda a: jnp.moveaxis(a, 2, 0), xs)
    final, starts = lax.scan(step, init, xs)
    starts = jax.tree_util.tree_map(lambda a: jnp.moveaxis(a, 0, 2), starts)
    return starts, final


def run_direction(states_fn, outputs_fn, lc, ctx_in, lat_in, init, reverse, need_ctx):
    def prep(arrs):
        return tuple(chunk_time(jnp.flip(a, axis=2) if reverse else a, lc) for a in arrs)

    def post(o):
        o = unchunk_time(o)
        return jnp.flip(o, axis=2) if reverse else o

    c_in = prep(ctx_in)
    c_starts, c_final = states_fn(c_in, init)
    out_c = post(outputs_fn(c_in, c_starts)) if need_ctx else None
    x_in = prep(lat_in)
    x_starts, _ = states_fn(x_in, c_final)
    out_x = post(outputs_fn(x_in, x_starts))
    return out_x, out_c


def mlstm_states(inputs, init):
    q, k, v, li, lf = inputs
    b = jnp.cumsum(lf, axis=-1)
    b_last = b[..., -1]
    a = b_last[..., None] - b + li
    m_loc = jnp.max(a, axis=-1)
    w = jnp.exp(a - m_loc[..., None])
    dC = jnp.einsum('bhncd,bhnce->bhnde', k * w[..., None], v)
    dn = jnp.einsum('bhncd,bhnc->bhnd', k, w)

    def step(carry, inp):
        C, n, m = carry
        dC_j, dn_j, bl_j, ml_j = inp
        m_new = jnp.maximum(bl_j + m, ml_j)
        s_old = jnp.exp(bl_j + m - m_new)
        s_new = jnp.exp(ml_j - m_new)
        C_new = s_old[..., None, None] * C + s_new[..., None, None] * dC_j
        n_new = s_old[..., None] * n + s_new[..., None] * dn_j
        return (C_new, n_new, m_new), (C, n, m)

    return scan_chunks(step, init, (dC, dn, b_last, m_loc))


def mlstm_outputs(inputs, starts):
    q, k, v, li, lf = inputs
    C0, n0, m0 = starts
    lc = q.shape[3]
    b = jnp.cumsum(lf, axis=-1)
    causal = jnp.tril(jnp.ones((lc, lc), dtype=bool))
    d = jnp.where(causal, b[..., :, None] - b[..., None, :] + li[..., None, :], MASK_NEG)
    inter = b + m0[..., None]
    m = jnp.maximum(inter, jnp.max(d, axis=-1))
    w_intra = jnp.exp(d - m[..., None])
    w_inter = jnp.exp(inter - m)
    s = jnp.einsum('bhntd,bhnsd->bhnts', q, k) * w_intra
    num = jnp.einsum('bhnts,bhnse->bhnte', s, v) + w_inter[..., None] * jnp.einsum('bhntd,bhnde->bhnte', q, C0)
    den = jnp.sum(s, axis=-1) + w_inter * jnp.einsum('bhntd,bhnd->bhnt', q, n0)
    return num / jnp.maximum(jnp.abs(den), jnp.exp(-m))[..., None]


def mlstm_mixer(pc, px, gate_b, norm_w, need_ctx):
    def prep(p):
        b, l, _ = p['ml_q'].shape
        q = to_heads(p['ml_q'], ML_H)
        k = to_heads(p['ml_k'], ML_H) * (ML_D ** -0.5)
        v = to_heads(p['ml_v'], ML_H)
        g = (p['ml_gates'].astype(f32) + gate_b.astype(f32)).reshape(b, l, 4, ML_H).transpose(2, 0, 3, 1)
        fwd = (q, k, v, g[0], jax.nn.log_sigmoid(g[1]))
        bwd = (q, k, v, g[2], jax.nn.log_sigmoid(g[3]))
        return fwd, bwd

    c_f, c_b = prep(pc)
    x_f, x_b = prep(px)
    bsz = px['ml_q'].shape[0]
    init = (jnp.zeros((bsz, ML_H, ML_D, ML_D), f32), jnp.zeros((bsz, ML_H, ML_D), f32), jnp.zeros((bsz, ML_H), f32))
    xf, cf = run_direction(mlstm_states, mlstm_outputs, ML_CHUNK, c_f, x_f, init, False, need_ctx)
    xb, cb = run_direction(mlstm_states, mlstm_outputs, ML_CHUNK, c_b, x_b, init, True, need_ctx)
    dtype = px['ml_q'].dtype

    def finish(h, p):
        return (jax.nn.sigmoid(p['ml_o'].astype(f32)) * head_rms_norm(from_heads(h), norm_w.reshape(ML_H, ML_D), ML_H)).astype(dtype)

    return finish(xf + xb, px), (finish(cf + cb, pc) if need_ctx else None)


def gla_states(inputs, init):
    q, k, v, g = inputs
    G = jnp.cumsum(g, axis=3)
    G_last = G[:, :, :, -1]
    dS = jnp.einsum('bhnck,bhnce->bhnke', k * jnp.exp(G_last[:, :, :, None] - G), v)

    def step(S, inp):
        dS_j, gl_j = inp
        return jnp.exp(gl_j)[..., None] * S + dS_j, S

    return scan_chunks(step, init, (dS, G_last))


def gla_outputs(inputs, S0):
    q, k, v, g = inputs
    lc = q.shape[3]
    G = jnp.cumsum(g, axis=3)
    causal = jnp.tril(jnp.ones((lc, lc), dtype=bool))
    decay = jnp.exp(jnp.where(causal[:, :, None], G[:, :, :, :, None, :] - G[:, :, :, None, :, :], MASK_NEG))
    a = jnp.einsum('bhntk,bhntsk,bhnsk->bhnts', q, decay, k)
    return jnp.einsum('bhnts,bhnse->bhnte', a, v) + jnp.einsum('bhntk,bhnke->bhnte', q * jnp.exp(G), S0)


def hgrn2_mixer(pc, px, lb, norm_w, need_ctx):
    lb = lb.astype(f32)
    log_lb = jnp.log(jnp.maximum(lb, LB_FLOOR))
    log_1m_lb = jnp.log1p(-lb)

    def prep(p):
        q = to_heads(jax.nn.silu(p['hg_q'].astype(f32)), HG_H)
        v = to_heads(p['hg_i'], HG_H)
        fr = p['hg_f'].astype(f32)
        dirs = []
        for fr_d in (fr[..., :HG_W], fr[..., HG_W:]):
            log_f = jnp.logaddexp(log_lb, log_1m_lb + jax.nn.log_sigmoid(fr_d))
            k = (1.0 - lb) * jax.nn.sigmoid(-fr_d)
            dirs.append((q, to_heads(k, HG_H), v, to_heads(log_f, HG_H)))
        return dirs

    c_f, c_b = prep(pc)
    x_f, x_b = prep(px)
    bsz = px['hg_q'].shape[0]
    init = jnp.zeros((bsz, HG_H, HG_K, HG_V), f32)
    xf, cf = run_direction(gla_states, gla_outputs, HG_CHUNK, c_f, x_f, init, False, need_ctx)
    xb, cb = run_direction(gla_states, gla_outputs, HG_CHUNK, c_b, x_b, init, True, need_ctx)
    dtype = px['hg_q'].dtype

    def finish(o, p):
        return (head_rms_norm(from_heads(o), norm_w.reshape(HG_H, HG_V), HG_H) * jax.nn.silu(p['hg_g'].astype(f32))).astype(dtype)

    return finish(xf + xb, px), (finish(cf + cb, pc) if need_ctx else None)


def axial_rope_tables(n_tokens):
    rows = n_tokens // GRID_W
    row = jnp.repeat(jnp.arange(rows), GRID_W).astype(f32)
    col = jnp.tile(jnp.arange(GRID_W), rows).astype(f32)
    n_freq = DA_D // 4
    inv = ROPE_BASE ** (-jnp.arange(n_freq, dtype=f32) / n_freq)
    ang = jnp.stack([row[:, None] * inv, col[:, None] * inv], axis=1)
    return jnp.cos(ang), jnp.sin(ang)


def apply_axial_rope(t, cos, sin):
    shape = t.shape
    t = t.reshape(*shape[:-1], 2, 2, DA_D // 4)
    t1, t2 = t[..., 0, :], t[..., 1, :]
    out = jnp.stack([t1 * cos - t2 * sin, t1 * sin + t2 * cos], axis=-2)
    return out.reshape(shape)


def diff_softmax_attend(q, k, v, lam):
    s = jnp.einsum('ibhqd,ibhkd->ibhqk', q, k).astype(f32) * (DA_D ** -0.5)
    p = jax.nn.softmax(s, axis=-1)
    return jnp.einsum('bhqk,bhkv->bhqv', p[0] - lam * p[1], v.astype(f32))


def diff_attn_blocks(q, k, v, lam):
    _, b, h, l, d = q.shape
    nb = l // Q_BLOCK
    qb = jnp.moveaxis(q.reshape(2, b, h, nb, Q_BLOCK, d), 3, 0)
    out = lax.map(lambda blk: diff_softmax_attend(blk, k, v, lam), qb)
    return jnp.moveaxis(out, 0, 2).reshape(b, h, l, -1)


def diff_attn_mixer(pc, px, lam_p, norm_w, lam_init, need_ctx):
    def qkv(p):
        b, l, _ = p['da_q'].shape
        q = p['da_q'].astype(f32).reshape(b, l, DA_H, 2, DA_D).transpose(3, 0, 2, 1, 4)
        k = p['da_k'].astype(f32).reshape(b, l, DA_H, 2, DA_D).transpose(3, 0, 2, 1, 4)
        v = to_heads(p['da_v'], DA_H)
        return q, k, v

    qc, kc, vc = qkv(pc)
    qx, kx, vx = qkv(px)
    cos, sin = axial_rope_tables(qx.shape[3])
    qx = apply_axial_rope(qx, cos, sin)
    kx = apply_axial_rope(kx, cos, sin)
    lp = lam_p.astype(f32)
    lam = jnp.exp(jnp.sum(lp[0] * lp[1])) - jnp.exp(jnp.sum(lp[2] * lp[3])) + lam_init
    k_all = jnp.concatenate([kx, kc], axis=3)
    v_all = jnp.concatenate([vx, vc], axis=2)
    dtype = px['da_q'].dtype

    def finish(o):
        return (head_rms_norm(from_heads(o), norm_w, DA_H) * (1.0 - lam_init)).astype(dtype)

    out_x = finish(diff_attn_blocks(qx, k_all, v_all, lam))
    out_c = finish(diff_softmax_attend(qc, kc, vc, lam)) if need_ctx else None
    return out_x, out_c


def swiglu(h, w_gate, w_up, w_down):
    return (jax.nn.silu(h @ w_gate) * (h @ w_up)) @ w_down


def moe_swiglu(h, router_w, router_b, w_gate, w_up, w_down):
    shape = h.shape
    t = h.reshape(-1, shape[-1])
    logits = (t @ router_w + router_b).astype(f32)
    top_v, top_i = lax.top_k(logits, TOP_K)
    top_g = jax.nn.softmax(top_v, axis=-1)
    gates = jnp.sum(jax.nn.one_hot(top_i, N_EXPERTS, dtype=f32) * top_g[..., None], axis=1)
    y = jnp.zeros(t.shape, f32)
    for e in range(N_EXPERTS):
        y = y + gates[:, e:e + 1] * swiglu(t, w_gate[e], w_up[e], w_down[e]).astype(f32)
    return y.reshape(shape).astype(h.dtype)


def setup_inputs(seed: int = 0) -> dict:
    key = jax.random.key(seed)
    ks = jax.random.split(key, 32)
    nrm = jax.random.normal
    D = D_MODEL
    fb = jnp.linspace(3.0, 6.0, ML_H)
    gate_base = jnp.stack([jnp.zeros(ML_H), fb, jnp.zeros(ML_H), fb])
    return {
        'x': nrm(ks[0], (BATCH, SEQ, D), f32),
        'c': nrm(ks[1], (BATCH, D), f32),
        'ctx': nrm(ks[2], (BATCH, CTX_LEN, D), f32),
        'c_ctx': nrm(ks[3], (D,), f32),
        'w_mod': nrm(ks[4], (DEPTH, D, 6 * D), f32) * (0.5 * D ** -0.5),
        'b_mod': nrm(ks[5], (DEPTH, 6 * D), f32) * 0.02,
        'norm_pre_mix': 1.0 + 0.05 * nrm(ks[6], (DEPTH, D), f32),
        'norm_post_mix': 1.0 + 0.05 * nrm(ks[7], (DEPTH, D), f32),
        'norm_pre_ffn': 1.0 + 0.05 * nrm(ks[8], (DEPTH, D), f32),
        'norm_post_ffn': 1.0 + 0.05 * nrm(ks[9], (DEPTH, D), f32),
        'w_in': nrm(ks[10], (DEPTH, D, PROJ_W), f32) * D ** -0.5,
        'w_out': nrm(ks[11], (DEPTH, MIX_W, D), f32) * MIX_W ** -0.5,
        'ml_gate_b': (gate_base[None] + 0.1 * nrm(ks[12], (DEPTH, 4, ML_H), f32)).reshape(DEPTH, 4 * ML_H),
        'ml_norm': 1.0 + 0.05 * nrm(ks[13], (DEPTH, ML_W), f32),
        'hg_lb_logits': nrm(ks[14], (DEPTH, HG_W), f32),
        'hg_norm': 1.0 + 0.05 * nrm(ks[15], (DEPTH, HG_W), f32),
        'da_lambda': 0.1 * nrm(ks[16], (DEPTH, 4, DA_D), f32),
        'da_norm': 1.0 + 0.05 * nrm(ks[17], (DEPTH, DA_V), f32),
        'ffn_w_gate': nrm(ks[18], (N_DENSE, D, FF_DENSE), f32) * D ** -0.5,
        'ffn_w_up': nrm(ks[19], (N_DENSE, D, FF_DENSE), f32) * D ** -0.5,
        'ffn_w_down': nrm(ks[20], (N_DENSE, FF_DENSE, D), f32) * FF_DENSE ** -0.5,
        'router_w': nrm(ks[21], (N_MOE, D, N_EXPERTS), f32) * D ** -0.5,
        'router_b': 0.01 * nrm(ks[22], (N_MOE, N_EXPERTS), f32),
        'moe_w_gate': nrm(ks[23], (N_MOE, N_EXPERTS, D, FF_EXPERT), f32) * D ** -0.5,
        'moe_w_up': nrm(ks[24], (N_MOE, N_EXPERTS, D, FF_EXPERT), f32) * D ** -0.5,
        'moe_w_down': nrm(ks[25], (N_MOE, N_EXPERTS, FF_EXPERT, D), f32) * FF_EXPERT ** -0.5,
    }


def reference(x, c, ctx, c_ctx, w_mod, b_mod, norm_pre_mix, norm_post_mix, norm_pre_ffn, norm_post_ffn,
              w_in, w_out, ml_gate_b, ml_norm, hg_lb_logits, hg_norm, da_lambda, da_norm,
              ffn_w_gate, ffn_w_up, ffn_w_down, router_w, router_b, moe_w_gate, moe_w_up, moe_w_down):
    lb_soft = jax.nn.softmax(hg_lb_logits.astype(f32), axis=0)
    lower_bounds = jnp.cumsum(lb_soft, axis=0) - lb_soft[0]
    for layer in range(DEPTH):
        need_ctx = layer < DEPTH - 1
        mod_x = jnp.split((jax.nn.silu(c) @ w_mod[layer] + b_mod[layer])[:, None, :], 6, axis=-1)
        mod_c = jnp.split(jax.nn.silu(c_ctx) @ w_mod[layer] + b_mod[layer], 6, axis=-1)
        lam_init = 0.8 - 0.6 * math.exp(-0.3 * layer)

        hx = modulate(rms_norm(x, norm_pre_mix[layer]), mod_x[0], mod_x[1])
        hc = modulate(rms_norm(ctx, norm_pre_mix[layer]), mod_c[0], mod_c[1])
        px = split_proj(hx @ w_in[layer])
        pc = split_proj(hc @ w_in[layer])
        ml_x, ml_c = mlstm_mixer(pc, px, ml_gate_b[layer], ml_norm[layer], need_ctx)
        hg_x, hg_c = hgrn2_mixer(pc, px, lower_bounds[layer], hg_norm[layer], need_ctx)
        da_x, da_c = diff_attn_mixer(pc, px, da_lambda[layer], da_norm[layer], lam_init, need_ctx)
        mix_x = jnp.concatenate([ml_x, hg_x, da_x], axis=-1) @ w_out[layer]
        x = x + mod_x[2] * rms_norm(mix_x, norm_post_mix[layer])
        if need_ctx:
            mix_c = jnp.concatenate([ml_c, hg_c, da_c], axis=-1) @ w_out[layer]
            ctx = ctx + mod_c[2] * rms_norm(mix_c, norm_post_mix[layer])

        def channel_mixer(h, layer=layer):
            i = layer // 2
            if layer % 2 == 0:
                return swiglu(h, ffn_w_gate[i], ffn_w_up[i], ffn_w_down[i])
            return moe_swiglu(h, router_w[i], router_b[i], moe_w_gate[i], moe_w_up[i], moe_w_down[i])

        hx = modulate(rms_norm(x, norm_pre_ffn[layer]), mod_x[3], mod_x[4])
        x = x + mod_x[5] * rms_norm(channel_mixer(hx), norm_post_ffn[layer])
        if need_ctx:
            hc = modulate(rms_norm(ctx, norm_pre_ffn[layer]), mod_c[3], mod_c[4])
            ctx = ctx + mod_c[5] * rms_norm(channel_mixer(hc), norm_post_ffn[layer])
    return x
```

```python
import numpy as np
from contextlib import ExitStack
import concourse.bass as bass
import concourse.mybir as mybir

F32 = mybir.dt.float32
BF16 = mybir.dt.bfloat16
AF = mybir.ActivationFunctionType
ALU = mybir.AluOpType
AX = mybir.AxisListType

SAME_ENG_SYNC = True


class Buf:
    def __init__(self, k, t, name, space):
        self.k, self.t, self.name, self.space = k, t, name, space
        self.w = {}
        self.r = {}
        self.dsem = None

    def __getitem__(self, idx):
        return self.t[idx]


class Eng:
    def __init__(self, k, name, h):
        self.k, self.name, self.h = k, name, h
        self.sem = k.es.enter_context(k.nc.semaphore("e_" + name))
        self.cnt = 0
        self.waited = {}


class _Scope:
    def __init__(self, k):
        self.k = k

    def __enter__(self):
        k = self.k
        self.prev = k.es
        self.st = ExitStack()
        k.es = self.st
        k.scopes.append([])
        return self

    def __exit__(self, *a):
        k = self.k
        k.barrier()
        for b in k.scopes.pop():
            if b.dsem is not None and getattr(b, "own_dsem", False):
                k.dpool.append(b.dsem)
        self.st.close()
        k.es = self.prev
        return False


class K:
    def __init__(self):
        self.nc = bass.Bass("TRN2", target_bir_lowering=False)
        self.es = ExitStack()
        nc = self.nc
        self.pe = Eng(self, "pe", nc.tensor)
        self.act = Eng(self, "act", nc.scalar)
        self.dve = Eng(self, "dve", nc.vector)
        self.pool = Eng(self, "pool", nc.gpsimd)
        self.sp = Eng(self, "sp", nc.sync)
        self.sems = {}
        self.totals = {}
        for e in (self.pe, self.act, self.dve, self.pool, self.sp):
            self.sems[e.sem.name] = e.sem
        self.ndsem = 0
        self.dq = 0
        self.dpool = []
        for i in range(88):
            s = self.es.enter_context(self.nc.semaphore("d%d" % i))
            self.sems[s.name] = s
            self.totals[s.name] = 0
            self.dpool.append(s)
        self.scopes = []

    def sb(self, name, shape, dt, dshare=None):
        self.nuniq = getattr(self, "nuniq", 0) + 1
        t = self.es.enter_context(self.nc.sbuf_tensor("%s_%d" % (name, self.nuniq), list(shape), dt))
        b = Buf(self, t, name, "sb")
        if dshare is not None:
            b.dsem = self._dsem(dshare)
        if self.scopes:
            self.scopes[-1].append(b)
        return b

    def ps(self, name, shape, dt=F32):
        self.nuniq = getattr(self, "nuniq", 0) + 1
        t = self.es.enter_context(self.nc.psum_tensor("%s_%d" % (name, self.nuniq), list(shape), dt))
        return Buf(self, t, name, "ps")

    def scope(self):
        return _Scope(self)

    def barrier(self):
        engs = (self.pe, self.act, self.dve, self.pool, self.sp)
        for e in engs:
            for o in engs:
                if o is e or o.cnt == 0:
                    continue
                if e.waited.get(o.sem.name, 0) < o.cnt:
                    e.h.wait_ge(o.sem, o.cnt)
                    e.waited[o.sem.name] = o.cnt
            for s, v in self.totals.items():
                if v and e.waited.get(s, 0) < v:
                    e.h.wait_ge(self.sems[s], v)
                    e.waited[s] = v

    def dram(self, name, shape, dt, kind="Internal"):
        t = self.nc.dram_tensor(name, list(shape), dt, kind=kind)
        return Buf(self, t.ap(), name, "dram")

    def _dsem(self, b):
        if b.dsem is None:
            b.dsem = self.dpool.pop()
            b.own_dsem = True
        return b.dsem

    def _deps(self, reads, writes):
        deps = {}
        self._raw = {}
        for b in reads:
            for s, v in b.w.items():
                deps[s] = max(deps.get(s, 0), v)
                self._raw[s] = max(self._raw.get(s, 0), v)
        for b in writes:
            for s, v in b.w.items():
                deps[s] = max(deps.get(s, 0), v)
            for s, v in b.r.items():
                deps[s] = max(deps.get(s, 0), v)
        return deps

    def _wait(self, eng, deps):
        for s, v in deps.items():
            if s in self.totals:
                v = self.totals[s]
            if s == eng.sem.name and not SAME_ENG_SYNC:
                continue
            if s == eng.sem.name and eng is self.pe:
                continue
            if s == eng.sem.name:
                if s not in self._raw:
                    continue
                v = self._raw[s]
            if eng.waited.get(s, 0) >= v:
                continue
            eng.h.wait_ge(self.sems[s], v)
            eng.waited[s] = v

    def _mark(self, tick, reads, writes):
        s, v = tick
        for b in writes:
            b.w[s] = max(b.w.get(s, 0), v)
            b.r = {}
        for b in reads:
            b.r[s] = max(b.r.get(s, 0), v)

    def op(self, eng, fn, reads=(), writes=()):
        self._wait(eng, self._deps(reads, writes))
        ins = fn(eng.h)
        eng.cnt += 1
        ins.then_inc(eng.sem, 1)
        self._mark((eng.sem.name, eng.cnt), reads, writes)
        return ins

    def mm(self, fns, reads=(), writes=()):
        eng = self.pe
        self._wait(eng, self._deps(reads, writes))
        ins = None
        for fn in fns:
            ins = fn(eng.h)
        eng.cnt += 1
        ins.then_inc(eng.sem, 1)
        self._mark((eng.sem.name, eng.cnt), reads, writes)

    def dma(self, out_b, out_ap, in_b, in_ap, q=None, **kw):
        if q is None:
            q = (self.sp, self.pool)[self.dq % 2]
            self.dq += 1
        sb = out_b if out_b.space != "dram" else in_b
        if sb.space == "dram":
            sb = out_b
        s = self._dsem(sb)
        self._wait(q, self._deps([in_b], [out_b]))
        ins = q.h.dma_start(out=out_ap, in_=in_ap, **kw)
        ins.then_inc(s, 16)
        self.totals[s.name] += 16
        self._mark((s.name, self.totals[s.name]), [in_b], [out_b])
        return ins

    def finish(self, bufs):
        deps = {}
        for b in bufs:
            for s, v in b.w.items():
                deps[s] = max(deps.get(s, 0), v)
        self._wait(self.sp, deps)
        for e in (self.pe, self.act, self.dve, self.pool):
            if e.cnt:
                self.sp.h.wait_ge(e.sem, e.cnt)
        for s, v in self.totals.items():
            if v and self.sp.waited.get(s, 0) < v:
                self.sp.h.wait_ge(self.sems[s], v)
        self.es.close()


D = 1024
KC = 8
SEQ = 4096
CTX = 256
T = SEQ + CTX
NT = T // 128
PROJ_W = 3856
FF_DENSE = 2816
FF_EXP = 3584
NEXP = 8
EPS = 1e-6
C_ML_Q, C_ML_K, C_ML_V, C_ML_O, C_ML_G = 0, 256, 512, 768, 1024
C_HG_Q, C_HG_F, C_HG_I, C_HG_G = 1040, 1296, 1808, 2064
C_DA_Q, C_DA_K, C_DA_V = 2320, 2832, 3344


def tok_blocks(n, bs=512):
    out = []
    o = 0
    while o < n:
        out.append((o, min(bs, n - o)))
        o += bs
    return out


class Prog:
    def __init__(self, dbg=None):
        self.k = K()
        self.dbg = dbg or {}
        self.ext = {}

    def ext_in(self, name, shape, dt=F32):
        b = self.k.dram(name, shape, dt, kind="ExternalInput")
        self.ext[name] = b
        return b

    def ext_out(self, name, shape, dt=F32):
        b = self.k.dram(name, shape, dt, kind="ExternalOutput")
        self.ext[name] = b
        return b


def stage_mod(P, l):
    k = P.k
    with k.scope():
        cT = k.sb("cT", [128, KC, 2], F32)
        for kk in range(KC):
            k.dma(cT, cT[:, kk, :], P.c2, P.c2.t[:, kk * 128:(kk + 1) * 128].rearrange("r p -> p r"),
                  allow_slow_non_contiguous=True)
        k.op(k.act, lambda h: h.activation(out=cT[:], in_=cT[:], func=AF.Silu), [cT], [cT])
        modv = k.sb("modv", [2, 6, 1024], F32)
        bm = k.sb("bm", [2, 6, 1024], F32)
        k.dma(bm, bm[:], P.b_mod,
              P.b_mod.t[l:l + 1, :].rearrange("o (s d) -> o s d", s=6).to_broadcast([2, 6, 1024]))
        nrm = k.sb("nrm", [2, 4, 1024], F32)
        for j, nb in enumerate((P.norm_pre_mix, P.norm_post_mix, P.norm_pre_ffn, P.norm_post_ffn)):
            k.dma(nrm, nrm[:, j, :], nb, nb.t[l:l + 1, :].to_broadcast([2, 1024]))
        wst = [k.sb("wst%d" % i, [128, KC, 512], F32) for i in range(4)]
        pm = [k.ps("pm%d" % i, [2, 512]) for i in range(4)]
        for cb in range(12):
            w = wst[cb % 4]
            p = pm[cb % 4]
            k.dma(w, w[:], P.w_mod, P.w_mod.t[l, :, cb * 512:(cb + 1) * 512].rearrange("(k p) n -> p k n", p=128))
            k.mm([(lambda h, kk=kk: h.matmul(p[:], lhsT=cT[:, kk, :], rhs=w[:, kk, :],
                                             start=(kk == 0), stop=(kk == KC - 1))) for kk in range(KC)],
                 [cT, w], [p])
            s, o = divmod(cb * 512, 1024)
            k.op(k.dve, lambda h: h.tensor_tensor(out=modv[:, s, o:o + 512], in0=p[:], in1=bm[:, s, o:o + 512],
                                                  op=ALU.add), [p, bm], [modv])
        modc = k.sb("modc", [2, 6, 1024], F32)
        for (dst, msc, nj) in ((0, 1, 0), (3, 4, 2)):
            k.op(k.dve, lambda h: h.scalar_tensor_tensor(out=modc[:, dst, :], in0=modv[:, msc, :], scalar=1.0,
                                                         in1=nrm[:, nj, :], op0=ALU.add, op1=ALU.mult),
                 [modv, nrm], [modc])
        for (dst, msh) in ((1, 0), (4, 3)):
            k.op(k.dve, lambda h: h.tensor_copy(out=modc[:, dst, :], in_=modv[:, msh, :]), [modv], [modc])
        for (dst, mg, nj) in ((2, 2, 1), (5, 5, 3)):
            k.op(k.dve, lambda h: h.tensor_tensor(out=modc[:, dst, :], in0=modv[:, mg, :], in1=nrm[:, nj, :],
                                                  op=ALU.mult), [modv, nrm], [modc])
        k.dma(P.modc[l], P.modc[l].t[:, :, :], modc, modc[:])


def stage_norm(P, src, l, which, hxT, tiles, all_latent=False):
    k = P.k
    gi, si = (0, 1) if which == 0 else (3, 4)
    with k.scope():
        gb = k.sb("gb", [128, 2, 1024], F32)
        shb = k.sb("shb", [128, 2, 1024], F32)
        for kind in range(2):
            k.dma(gb, gb[:, kind, :], P.modc[l], P.modc[l].t[kind:kind + 1, gi, :].to_broadcast([128, 1024]))
            k.dma(shb, shb[:, kind, :], P.modc[l], P.modc[l].t[kind:kind + 1, si, :].to_broadcast([128, 1024]))
        xt = [k.sb("xt%d" % i, [128, 1024], F32) for i in range(2)]
        junk = k.sb("junk", [128, 1024], BF16)
        ss = [k.sb("ss%d" % i, [128, 2], F32) for i in range(2)]
        yf = k.sb("yf", [128, 1024], F32)
        yb = [k.sb("yb%d" % i, [128, 1024], BF16) for i in range(2)]
        pt = [k.ps("pt%d" % i, [128, KC, 128], BF16) for i in range(2)]
        for n, i in enumerate(tiles):
            kind = 1 if (i < 2 and not all_latent) else 0
            x, s, y, p = xt[n % 2], ss[n % 2], yb[n % 2], pt[n % 2]
            k.dma(x, x[:], src, src.t[i * 128:(i + 1) * 128, :])
            k.op(k.dve, lambda h: h.memset(s[:], 0.0), [], [s])
            k.op(k.act, lambda h: h.activation(out=junk[:], in_=x[:], func=AF.Square, accum_out=s[:, 0:1]),
                 [x, s], [junk, s])
            k.op(k.dve, lambda h: h.tensor_scalar(out=s[:, 1:2], in0=s[:, 0:1], scalar1=1.0 / D, scalar2=EPS,
                                                  op0=ALU.mult, op1=ALU.add), [s], [s])
            k.op(k.act, lambda h: h.sqrt(out=s[:, 1:2], in_=s[:, 1:2]), [s], [s])
            k.op(k.dve, lambda h: h.reciprocal(out=s[:, 1:2], in_=s[:, 1:2]), [s], [s])
            k.op(k.dve, lambda h: h.scalar_tensor_tensor(out=yf[:], in0=x[:], scalar=s[:, 1:2], in1=gb[:, kind, :],
                                                         op0=ALU.mult, op1=ALU.mult), [x, s, gb], [yf])
            k.op(k.dve, lambda h: h.tensor_tensor(out=y[:], in0=yf[:], in1=shb[:, kind, :], op=ALU.add),
                 [yf, shb], [y])
            k.mm([(lambda h, kk=kk: h.transpose(out=p[:, kk, :], in_=y[:, kk * 128:(kk + 1) * 128],
                                                identity=P.ident[:])) for kk in range(KC)], [y, P.ident], [p])
            k.op(k.act, lambda h: h.copy(out=hxT[:, :, n * 128:(n + 1) * 128], in_=p[:]), [p], [hxT])


def setup_consts(P):
    k = P.k
    idf = k.sb("idf", [128, 128], F32)
    k.dma(idf, idf[:], P.ext["ident"], P.ext["ident"].t[:, :])
    P.ident = k.sb("ident", [128, 128], BF16)
    k.op(k.dve, lambda h: h.tensor_copy(out=P.ident[:], in_=idf[:]), [idf], [P.ident])


class WLoader:
    def __init__(self, k, nbuf=2, width=512, kc=KC, name="wl"):
        self.k = k
        self.kc = kc
        self.wb = [k.sb(name + "b%d" % i, [128, kc, width], BF16) for i in range(nbuf)]
        self.i = 0
        self.nbuf = nbuf

    def get(self, wbuf, w_ap, n):
        k = self.k
        wb = self.wb[self.i % self.nbuf]
        self.i += 1
        k.dma(wb, wb[:, :, :n], wbuf, w_ap.rearrange("(k p) n -> p k n", p=128), q=k.pool)
        return wb


def lin_fm(k, wb, ncol, src, ntok, evac, ps_pool, kc=KC, bs=512):
    for bi, (t0, n) in enumerate(tok_blocks(ntok, bs)):
        p = ps_pool[bi % len(ps_pool)]
        k.mm([(lambda h, kk=kk: h.matmul(p[:ncol, :n], lhsT=wb[:, kk, :ncol], rhs=src[:, kk, t0:t0 + n],
                                         start=(kk == 0), stop=(kk == kc - 1))) for kk in range(kc)],
             [wb, src], [p])
        evac(p, t0, n)


def lin_tm(k, wb, ncol, src, tiles, evac, ps_pool, kc=KC):
    for ti, i in enumerate(tiles):
        p = ps_pool[ti % len(ps_pool)]
        k.mm([(lambda h, kk=kk: h.matmul(p[:, :ncol], lhsT=src[:, kk, i * 128:(i + 1) * 128], rhs=wb[:, kk, :ncol],
                                         start=(kk == 0), stop=(kk == kc - 1))) for kk in range(kc)],
             [wb, src], [p])
        evac(p, i)


def stage_attn(P, l, hxT, mixT, q0, nq, lam_init, heads=(0, 1, 2, 3), qblocks=None, mixT_d=None, own=False):
    k = P.k
    w_in = P.w_in
    with k.scope():
        wl = WLoader(k, nbuf=2, width=128, name="aw")
        ropeC = k.sb("ropeC", [128, T], BF16)
        ropeS = k.sb("ropeS", [128, T], BF16)
        k.dma(ropeC, ropeC[:], P.ext["rope_c"], P.ext["rope_c"].t[:, :])
        k.dma(ropeS, ropeS[:], P.ext["rope_s"], P.ext["rope_s"].t[:, :])
        lp = k.sb("lp", [128, 4, 64], F32)
        k.dma(lp, lp[:], P.da_lambda, P.da_lambda.t[l:l + 1, :, :].to_broadcast([128, 4, 64]))
        lt = k.sb("lt", [128, 2, 64], F32)
        lam = k.sb("lam", [128, 4], F32)
        k.op(k.dve, lambda h: h.tensor_tensor(out=lt[:, 0, :], in0=lp[:, 0, :], in1=lp[:, 1, :], op=ALU.mult), [lp], [lt])
        k.op(k.dve, lambda h: h.tensor_tensor(out=lt[:, 1, :], in0=lp[:, 2, :], in1=lp[:, 3, :], op=ALU.mult), [lp], [lt])
        k.op(k.dve, lambda h: h.reduce_sum(out=lam[:, 0:2], in_=lt[:], axis=AX.X), [lt], [lam])
        k.op(k.act, lambda h: h.activation(out=lam[:, 0:2], in_=lam[:, 0:2], func=AF.Exp), [lam], [lam])
        k.op(k.dve, lambda h: h.tensor_tensor(out=lam[:, 2:3], in0=lam[:, 1:2], in1=lam[:, 0:1], op=ALU.subtract), [lam], [lam])
        k.op(k.dve, lambda h: h.tensor_scalar(out=lam[:, 3:4], in0=lam[:, 2:3], scalar1=-float(lam_init), scalar2=None,
                                              op0=ALU.add), [lam], [lam])
        dn = k.sb("dn", [128, 128], F32)
        k.dma(dn, dn[:], P.da_norm, P.da_norm.t[l:l + 1, :].to_broadcast([128, 128]))
        k.op(k.dve, lambda h: h.tensor_scalar(out=dn[:], in0=dn[:], scalar1=float(1.0 - lam_init), scalar2=None,
                                              op0=ALU.mult), [dn], [dn])

        qT = k.sb("qT", [128, T], BF16)
        kT = k.sb("kT", [128, T], BF16)
        if own:
            qTo = k.sb("qTo", [128, SEQ // 2], BF16)
            selt = k.sb("selt", [128, 2], F32)
            k.dma(selt, selt[:], P.ext["sel"], P.ext["sel"].t[:, :])
        va = k.sb("va", [128, NT, 132], BF16)
        k.op(k.dve, lambda h: h.memset(va[:, :, 128:132], 1.0), [], [va])
        t1 = k.sb("t1", [128, 512], F32)
        t2 = k.sb("t2", [128, 512], F32)
        sT2 = [[k.ps("sT%d_%d" % (i, j), [128, 512]) for j in range(2)] for i in range(2)]
        pp = [sT2[0][0], sT2[0][1]]
        osbs = [k.sb("osb%d" % i, [128, 3, 3, 132], F32) for i in range(2)]
        blk_i = [0]
        oacc = [k.ps("oacc%d" % i, [128, 3, 132]) for i in range(3)]
        pT = [[k.sb("pT%d_%d" % (i, j), [128, 512], BF16) for j in range(2)] for i in range(2)]
        fin = k.sb("fin", [128, 8], F32)
        o0 = k.sb("o0", [128, 128], F32)
        o1 = k.sb("o1", [128, 128], F32)
        junk = k.sb("ajunk", [128, 128], F32)
        ob = k.sb("ob", [128, 128], BF16)
        ptr = k.ps("ptr", [128, 128], BF16)
        ost = k.sb("ost", [128, 512], BF16)
        nkc = NT
        for hd in heads:
            for (dst, c_main, c_sw) in ((qT, C_DA_Q + hd * 128, hd * 128), (kT, C_DA_K + hd * 128, 512 + hd * 128)):
                wm = wl.get(w_in, w_in.t[l, :, c_main:c_main + 128], 128)
                ws = wl.get(P.ext["w_da_sw"], P.ext["w_da_sw"].t[l, :, c_sw:c_sw + 128], 128)
                for bi, (t0, n) in enumerate(tok_blocks(T)):
                    pa, pb = pp[0], pp[1]
                    k.mm([(lambda h, kk=kk: h.matmul(pa[:, :n], lhsT=wm[:, kk, :128], rhs=hxT[:, kk, t0:t0 + n],
                                                     start=(kk == 0), stop=(kk == KC - 1))) for kk in range(KC)],
                         [wm, hxT], [pa])
                    k.mm([(lambda h, kk=kk: h.matmul(pb[:, :n], lhsT=ws[:, kk, :128], rhs=hxT[:, kk, t0:t0 + n],
                                                     start=(kk == 0), stop=(kk == KC - 1))) for kk in range(KC)],
                         [ws, hxT], [pb])
                    k.op(k.dve, lambda h: h.tensor_tensor(out=t1[:, :n], in0=pa[:, :n], in1=ropeC[:, t0:t0 + n],
                                                          op=ALU.mult), [pa, ropeC], [t1])
                    k.op(k.dve, lambda h: h.tensor_tensor(out=t2[:, :n], in0=pb[:, :n], in1=ropeS[:, t0:t0 + n],
                                                          op=ALU.mult), [pb, ropeS], [t2])
                    k.op(k.dve, lambda h: h.tensor_tensor(out=dst[:, t0:t0 + n], in0=t1[:, :n], in1=t2[:, :n],
                                                          op=ALU.add), [t1, t2], [dst])
            wv = wl.get(w_in, w_in.t[l, :, C_DA_V + hd * 128:C_DA_V + (hd + 1) * 128], 128)
            for i in range(NT):
                pa = pp[i % 2]
                k.mm([(lambda h, kk=kk: h.matmul(pa[:, :128], lhsT=hxT[:, kk, i * 128:(i + 1) * 128], rhs=wv[:, kk, :128],
                                                 start=(kk == 0), stop=(kk == KC - 1))) for kk in range(KC)],
                     [wv, hxT], [pa])
                k.op(k.act, lambda h: h.copy(out=va[:, i, 0:128], in_=pa[:, :128]), [pa], [va])
            qsrc = qT
            if own:
                h0, h1 = CTX, CTX + SEQ // 2
                k.op(k.dve, lambda h: h.tensor_scalar(out=qTo[:], in0=qT[:, h0:h1], scalar1=selt[:, 0:1], scalar2=None,
                                                      op0=ALU.mult), [qT, selt], [qTo])
                k.op(k.dve, lambda h: h.scalar_tensor_tensor(out=qTo[:], in0=qT[:, h1:T], scalar=selt[:, 1:2], in1=qTo[:],
                                                             op0=ALU.mult, op1=ALU.add), [qT, selt, qTo], [qTo])
                qsrc = qTo
            if qblocks is None:
                qblocks = [(q0 + qb * 512, 512, list(range(NT))) for qb in range(nq // 512)]
            for (qs, nqb, ktiles) in qblocks:
                nqt = nqb // 128
                nkc = len(ktiles)
                glist = [c_ * 4 + q_ for c_ in range(2) for q_ in range(nqt)]

                def emit_S(kci):
                    kc = ktiles[kci]
                    for comp in range(2):
                        sp_ = sT2[comp][kci % 2]
                        r0 = comp * 64
                        k.mm([lambda h: h.matmul(sp_[:, :nqb], lhsT=kT[r0:r0 + 64, kc * 128:(kc + 1) * 128],
                                                 rhs=qsrc[r0:r0 + 64, qs:qs + nqb], start=True, stop=True)],
                             [kT, qsrc], [sp_])

                def emit_exp(kci):
                    for comp in range(2):
                        sp_ = sT2[comp][kci % 2]
                        pt_ = pT[comp][kci % 2]
                        k.op(k.act, lambda h: h.activation(out=pt_[:, :nqb], in_=sp_[:, :nqb], func=AF.Exp, scale=0.125),
                             [sp_], [pt_])

                def emit_PV(kci):
                    kc = ktiles[kci]
                    for comp in range(2):
                        pt_ = pT[comp][kci % 2]
                        fns = []
                        for qt in range(nqt):
                            g = comp * 4 + qt
                            ot = oacc[g // 3]
                            fns.append(lambda h, ot=ot, g=g, qt=qt: h.matmul(
                                ot[:, g % 3, 0:129], lhsT=pt_[:, qt * 128:(qt + 1) * 128], rhs=va[:, kc, 0:129],
                                start=(kci == 0 and g == min(x for x in glist if x // 3 == g // 3)), stop=(kci == nkc - 1),
                                skip_group_check=True))
                        k.mm(fns, [pt_, va], oacc)

                emit_S(0)
                emit_exp(0)
                for kci in range(nkc):
                    if kci + 1 < nkc:
                        emit_S(kci + 1)
                    emit_PV(kci)
                    if kci + 1 < nkc:
                        emit_exp(kci + 1)
                osb = osbs[blk_i[0] % 2]
                blk_i[0] += 1
                for j in range(3):
                    k.op(k.dve, lambda h: h.tensor_copy(out=osb[:, j, :, :], in_=oacc[j][:]), [oacc[j]], [osb])
                for qt in range(nqt):
                    g0, g1 = qt, 4 + qt
                    k.op(k.dve, lambda h: h.reciprocal(out=fin[:, 0:1], in_=osb[:, g0 // 3, g0 % 3, 128:129]), [osb], [fin])
                    k.op(k.dve, lambda h: h.reciprocal(out=fin[:, 1:2], in_=osb[:, g1 // 3, g1 % 3, 128:129]), [osb], [fin])
                    k.op(k.dve, lambda h: h.tensor_tensor(out=fin[:, 1:2], in0=fin[:, 1:2], in1=lam[:, 3:4], op=ALU.mult),
                         [fin, lam], [fin])
                    k.op(k.dve, lambda h: h.tensor_scalar(out=o0[:], in0=osb[:, g0 // 3, g0 % 3, 0:128], scalar1=fin[:, 0:1],
                                                          scalar2=None, op0=ALU.mult), [osb, fin], [o0])
                    k.op(k.dve, lambda h: h.scalar_tensor_tensor(out=o1[:], in0=osb[:, g1 // 3, g1 % 3, 0:128], scalar=fin[:, 1:2],
                                                                 in1=o0[:], op0=ALU.mult, op1=ALU.add),
                         [osb, fin, o0], [o1])
                    k.op(k.dve, lambda h: h.tensor_tensor(out=junk[:], in0=o1[:], in1=o1[:], op=ALU.mult), [o1], [junk])
                    k.op(k.dve, lambda h: h.reduce_sum(out=fin[:, 2:3], in_=junk[:], axis=AX.X), [junk], [fin])
                    k.op(k.dve, lambda h: h.tensor_scalar(out=fin[:, 3:4], in0=fin[:, 2:3], scalar1=1.0 / 128, scalar2=EPS,
                                                          op0=ALU.mult, op1=ALU.add), [fin], [fin])
                    k.op(k.act, lambda h: h.sqrt(out=fin[:, 3:4], in_=fin[:, 3:4]), [fin], [fin])
                    k.op(k.dve, lambda h: h.reciprocal(out=fin[:, 3:4], in_=fin[:, 3:4]), [fin], [fin])
                    k.op(k.dve, lambda h: h.scalar_tensor_tensor(out=ob[:], in0=o1[:], scalar=fin[:, 3:4], in1=dn[:],
                                                                 op0=ALU.mult, op1=ALU.mult), [o1, fin, dn], [ob])
                    k.mm([lambda h: h.transpose(out=ptr[:], in_=ob[:], identity=P.ident[:])], [ob, P.ident], [ptr])
                    c0 = qs + qt * 128
                    if mixT_d is None:
                        k.op(k.dve, lambda h: h.tensor_copy(out=mixT[:, 4 + hd, c0:c0 + 128], in_=ptr[:]), [ptr], [mixT])
                    else:
                        k.op(k.dve, lambda h: h.tensor_copy(out=ost[:, qt * 128:(qt + 1) * 128], in_=ptr[:]), [ptr], [ost])
                if mixT_d is not None:
                    r0_ = 512 + hd * 128
                    k.dma(mixT_d, mixT_d.t[r0_:r0_ + 128, qs:qs + nqb], ost, ost[:, :nqb])


def rope_tables():
    n = np.arange(SEQ)
    row = (n // 64).astype(np.float64)
    col = (n % 64).astype(np.float64)
    inv = 10000.0 ** (-np.arange(16, dtype=np.float64) / 16)
    C = np.ones((64, T), np.float64)
    S = np.zeros((64, T), np.float64)
    for d in range(64):
        pos = row if d < 32 else col
        f = d % 16
        ang = pos * inv[f]
        C[d, CTX:] = np.cos(ang)
        sgn = -1.0 if (d % 32) < 16 else 1.0
        S[d, CTX:] = sgn * np.sin(ang)
    import ml_dtypes
    C2 = np.concatenate([C, C], 0).astype(np.float32).astype(ml_dtypes.bfloat16)
    S2 = np.concatenate([S, S], 0).astype(np.float32).astype(ml_dtypes.bfloat16)
    return C2, S2


def swap_cols(w_in):
    perm64 = np.concatenate([np.arange(16, 32), np.arange(0, 16), np.arange(48, 64), np.arange(32, 48)])
    idx = []
    for base in (C_DA_Q, C_DA_K):
        for u in range(8):
            idx.append(base + u * 64 + perm64)
    idx = np.concatenate(idx)
    return np.ascontiguousarray(w_in[:, :, idx])


def stage_proj_rec(P, l, hxT):
    k = P.k
    w_in = P.w_in
    R = P.rec
    with k.scope():
        wl = WLoader(k, nbuf=2, width=128, name="pw")
        pp = [k.ps("rp%d" % i, [128, 512]) for i in range(3)]
        stf = [k.sb("stf%d" % i, [128, 512], F32) for i in range(2)]
        stb = [k.sb("stb%d" % i, [128, 512], BF16) for i in range(2)]
        gb = k.sb("gbias", [128, 8], F32)
        for ty in range(4):
            for hp in range(2):
                for hh in range(2):
                    c = ty * 4 + hp * 2 + hh
                    k.dma(gb, gb[hh * 64:(hh + 1) * 64, ty * 2 + hp:ty * 2 + hp + 1], P.ml_gate_b,
                          P.ml_gate_b.t[l:l + 1, c:c + 1].to_broadcast([64, 1]))
        cnt = [0]

        def fm_job(wb, dst, row0, func=None, scale=1.0, bias=None, f32=False):
            def evac(p, t0, n):
                i = cnt[0] % 2
                cnt[0] += 1
                st = stf[i] if f32 else stb[i]
                if func is None and bias is None:
                    k.op(k.dve, lambda h: h.tensor_scalar(out=st[:, :n], in0=p[:, :n], scalar1=float(scale), scalar2=None,
                                                          op0=ALU.mult), [p], [st])
                elif func is None:
                    k.op(k.dve, lambda h: h.tensor_scalar(out=st[:, :n], in0=p[:, :n], scalar1=bias, scalar2=None,
                                                          op0=ALU.add), [p, gb], [st])
                else:
                    k.op(k.act, lambda h: h.activation(out=st[:, :n], in_=p[:, :n], func=func), [p], [st])
                k.dma(dst, dst.t[row0:row0 + 128, t0:t0 + n], st, st[:, :n])
            lin_fm(k, wb, 128, hxT, T, evac, pp)

        for c in range(2):
            fm_job(wl.get(w_in, w_in.t[l, :, C_ML_Q + c * 128:C_ML_Q + (c + 1) * 128], 128), R["ml_q"], c * 128)
            fm_job(wl.get(w_in, w_in.t[l, :, C_ML_K + c * 128:C_ML_K + (c + 1) * 128], 128), R["ml_k"], c * 128, scale=0.125)
            fm_job(wl.get(w_in, w_in.t[l, :, C_ML_O + c * 128:C_ML_O + (c + 1) * 128], 128), R["ml_o"], c * 128, func=AF.Sigmoid)
            fm_job(wl.get(w_in, w_in.t[l, :, C_HG_Q + c * 128:C_HG_Q + (c + 1) * 128], 128), R["hg_q"], c * 128, func=AF.Silu)
            fm_job(wl.get(w_in, w_in.t[l, :, C_HG_G + c * 128:C_HG_G + (c + 1) * 128], 128), R["hg_g"], c * 128, func=AF.Silu)
        for c in range(4):
            fm_job(wl.get(w_in, w_in.t[l, :, C_HG_F + c * 128:C_HG_F + (c + 1) * 128], 128), R["hg_f"], c * 128, f32=True)
        g16s = k.sb("g16s", [128, KC, 16], F32)
        k.dma(g16s, g16s[:], w_in, w_in.t[l, :, C_ML_G:C_ML_G + 16].rearrange("(k p) n -> p k n", p=128))
        wrep = [k.sb("wrep%d" % i, [128, KC, 128], BF16) for i in range(2)]
        for ty in range(4):
            for hp in range(2):
                wr = wrep[(ty * 2 + hp) % 2]
                for hh in range(2):
                    c = ty * 4 + hp * 2 + hh
                    k.op(k.dve, lambda h: h.tensor_copy(out=wr[:, :, hh * 64:(hh + 1) * 64],
                                                        in_=g16s[:, :, c:c + 1].to_broadcast([128, KC, 64])), [g16s], [wr])
                fm_job(wr, R["ml_g"], (ty * 2 + hp) * 128, bias=gb[:, ty * 2 + hp:ty * 2 + hp + 1], f32=True)
        for (c0, dst) in ((C_ML_V, R["ml_v"]), (C_HG_I, R["hg_v"])):
            wv = k.sb("wv", [128, KC, 256], BF16)
            for c in range(2):
                wb = wl.get(w_in, w_in.t[l, :, c0 + c * 128:c0 + (c + 1) * 128], 128)
                k.op(k.dve, lambda h: h.tensor_copy(out=wv[:, :, c * 128:(c + 1) * 128], in_=wb[:, :, :128]), [wb], [wv])

            def evac_v(p, i, dst=dst):
                j = cnt[0] % 2
                cnt[0] += 1
                st = stb[j]
                k.op(k.act, lambda h: h.copy(out=st[:, :256], in_=p[:, :256]), [p], [st])
                k.dma(dst, dst.t[i * 128:(i + 1) * 128, :], st, st[:, :256])
            lin_tm(k, wv, 256, hxT, list(range(NT)), evac_v, pp)


NCH = T // 16
SEGS = ((0, CTX), (CTX, T))


def seg_view(ap2d, si, dr):
    a, b = SEGS[si]
    if dr == 0:
        return ap2d[:, a:b]
    if a == 0:
        return ap2d[:, b - 1::-1]
    return ap2d[:, b - 1:a - 1:-1]


def stage_rec(P, l, mixer, mixT_d):
    k = P.k
    R = P.rec
    ml = (mixer == "ml")
    nv = 2 if ml else 1
    row_off = 0 if ml else 256
    with k.scope():
        cst = P.ext
        maskb = k.sb("maskb", [128, 2, 128], BF16)
        k.dma(maskb, maskb[:], cst["cmask"], cst["cmask"].t[:, :, :])
        bm = k.sb("bm", [128, 8], BF16)
        k.dma(bm, bm[:], cst["bmask"], cst["bmask"].t[:, :])
        blk = k.sb("blk", [128, 128], BF16)
        k.dma(blk, blk[:], cst["blk"], cst["blk"].t[:, :])
        bmf = k.sb("bmf", [128, 8], F32)
        k.op(k.dve, lambda h: h.tensor_copy(out=bmf[:], in_=bm[:]), [bm], [bmf])
        if not ml:
            rmask = k.sb("rmask", [128, T], F32)
            k.op(k.dve, lambda h: h.memset(rmask[:], 1.0), [], [rmask])
            k.op(k.dve, lambda h: h.memset(rmask[:].rearrange("p (c i) -> p c i", i=16)[:, :, 0], 0.0), [], [rmask])
        zer = k.sb("zer", [128, 1], F32)
        k.op(k.dve, lambda h: h.memset(zer[:], 0.0), [], [zer])
        onesT = k.sb("onesT", [128, 64], BF16)
        k.op(k.dve, lambda h: h.memset(onesT[:], 1.0), [], [onesT])
        vones = k.sb("vones", [128, 8, 64], BF16)
        k.op(k.dve, lambda h: h.tensor_copy(out=vones[:], in_=bm[:].unsqueeze(2).to_broadcast([128, 8, 64])), [bm], [vones])

        qAs = k.sb("qA", [128, T], BF16) if ml else None
        qAd = [qAs, qAs] if ml else [k.sb("qA%d" % i, [128, T], BF16) for i in range(2)]
        kAd = [k.sb("kA%d" % i, [128, T], BF16) for i in range(2)]
        x3d = [k.sb("x3%d" % i, [128, T], BF16) for i in range(2)]
        cld = [k.sb("cl%d" % i, [128, T], BF16) for i in range(2)] if ml else None
        tA = k.sb("tA", [128, T], F32)
        tB = k.sb("tB", [128, T], F32)
        tC = k.sb("tC", [128, T], F32)
        osum = k.sb("osum", [128, T], F32)
        gate = k.sb("gate", [128, T], BF16)
        vt = k.sb("vt", [128, NT, 128], BF16)
        Rj = k.sb("Rj", [128, NCH], F32)
        Rp = k.sb("Rp", [128, NCH], F32)
        Dcd = [k.sb("Dc%d" % i, [128, NCH], F32) for i in range(2)]
        sc = k.sb("sc", [128, 8], F32)
        nw = k.sb("nw", [128, 1], F32)

        bankA = [k.ps("bkA%d" % i, [128, 512]) for i in range(2)]
        pdSd = [[k.ps("pdS%d_%d" % (i, j), [128, 8, 64]) for j in range(nv)] for i in range(2)]
        pktd = [k.ps("pkt%d" % i, [128, 128], BF16) for i in range(2)]
        AmTd = [[k.sb("AmT%d_%d" % (i, j), [128, 2, 128], BF16) for j in range(2)] for i in range(2)]
        qzd = [[k.sb("qz%d_%d" % (i, j), [128, 2, 128], BF16) for j in range(2)] for i in range(2)]
        for i in range(2):
            for j in range(2):
                k.op(k.dve, lambda h: h.memset(qzd[i][j][:], 0.0), [], [qzd[i][j]])
        kStd = [[k.sb("kSt%d_%d" % (i, j), [128, 128], BF16) for j in range(2)] for i in range(2)]
        Vexpd = [[k.sb("Vexp%d_%d" % (i, j), [128, 2, 8, 64], BF16) for j in range(2)] for i in range(2)]
        Shd = [[k.sb("Sh%d_%d" % (i, j), [128, 9, 64], F32) for j in range(nv)] for i in range(2)]
        Sbd = [[k.sb("Sb%d_%d" % (i, j), [128, 8, 64], BF16) for j in range(nv)] for i in range(2)]
        nsb = [[k.sb("nsb%d_%d" % (i, j), [128, 128], F32) for j in range(2)] for i in range(2)]
        dsb = [[k.sb("dsb%d_%d" % (i, j), [128, 128], F32) for j in range(2)] for i in range(2)] if ml else None
        fq = k.sb("fq", [128, 512], BF16)
        fr_ = k.sb("fr_", [128, 512], F32)
        fy = k.sb("fy", [128, 512], F32)
        fo = [k.sb("fo%d" % i, [128, 512], BF16) for i in range(2)]

        def c16(v):
            return v.rearrange("p (c i) -> p c i", i=16)

        def tile_of(ip, dr):
            if ip < 2:
                return ip if dr == 0 else 1 - ip
            return ip if dr == 0 else (NT - 1) - (ip - 2)

        for hp in range(2):
            rows = slice(hp * 128, (hp + 1) * 128)
            src_v = R["ml_v"] if ml else R["hg_v"]
            for i_ in range(NT):
                k.dma(vt, vt[:, i_, :], src_v, src_v.t[i_ * 128:(i_ + 1) * 128, hp * 128:(hp + 1) * 128])
            src_g = R["ml_o"] if ml else R["hg_g"]
            k.dma(gate, gate[:], src_g, src_g.t[rows, :])
            nsrc = P.ml_norm if ml else P.hg_norm
            k.dma(nw, nw[:], nsrc, nsrc.t[l:l + 1, rows].rearrange("o p -> p o"), allow_slow_non_contiguous=True)
            k.op(k.pool, lambda h: h.memset(osum[:], 0.0), [], [osum])
            if not ml:
                if l == 0:
                    k.op(k.dve, lambda h: h.memset(sc[:, 0:1], 0.0), [], [sc])
                else:
                    lg = P.hg_lb_logits
                    k.dma(sc, sc[:, 4:5], lg, lg.t[0:1, rows].rearrange("o p -> p o"), allow_slow_non_contiguous=True)
                    k.dma(sc, sc[:, 5:6], lg, lg.t[1:2, rows].rearrange("o p -> p o"), allow_slow_non_contiguous=True)
                    k.op(k.dve, lambda h: h.tensor_tensor(out=sc[:, 6:7], in0=sc[:, 5:6], in1=sc[:, 4:5], op=ALU.subtract), [sc], [sc])
                    k.op(k.act, lambda h: h.activation(out=sc[:, 0:1], in_=sc[:, 6:7], func=AF.Sigmoid), [sc], [sc])
                k.op(k.dve, lambda h: h.tensor_scalar(out=sc[:, 1:2], in0=sc[:, 0:1], scalar1=-1.0, scalar2=1.0,
                                                      op0=ALU.mult, op1=ALU.add), [sc], [sc])
                k.op(k.dve, lambda h: h.tensor_scalar(out=sc[:, 2:3], in0=sc[:, 1:2], scalar1=-1.0, scalar2=None,
                                                      op0=ALU.mult), [sc], [sc])
            if ml:
                k.dma(qAs, qAs[:], R["ml_q"], R["ml_q"].t[rows, :])
            for dr in range(2):
                qA, kA, x3, Dc = qAd[dr], kAd[dr], x3d[dr], Dcd[dr]
                if ml:
                    k.dma(kA, kA[:], R["ml_k"], R["ml_k"].t[rows, :])
                    gi0 = ((dr * 2) * 2 + hp) * 128
                    gf0 = ((dr * 2 + 1) * 2 + hp) * 128
                    k.dma(tA, tA[:], R["ml_g"], R["ml_g"].t[gi0:gi0 + 128, :])
                    k.dma(tB, tB[:], R["ml_g"], R["ml_g"].t[gf0:gf0 + 128, :])
                    k.op(k.act, lambda h: h.activation(out=tB[:], in_=tB[:], func=AF.Exp, scale=-1.0), [tB], [tB])
                    k.op(k.act, lambda h: h.activation(out=tB[:], in_=tB[:], func=AF.Ln, bias=1.0), [tB], [tB])
                    for si in range(2):
                        ini = 0.0 if si == 0 else seg_view(tC[:], 0, dr)[:, CTX - 1:CTX]
                        ov, dv = seg_view(tC[:], si, dr), seg_view(tB[:], si, dr)
                        n = SEGS[si][1] - SEGS[si][0]
                        k.op(k.dve, lambda h: h.tensor_tensor_scan(out=ov, data0=dv, data1=zer[:].to_broadcast([128, n]),
                                                                   initial=ini, op0=ALU.add, op1=ALU.add), [tB, tC, zer], [tC])
                    k.op(k.dve, lambda h: h.tensor_tensor(out=tA[:], in0=tA[:], in1=tC[:], op=ALU.add), [tA, tC], [tA])
                    for si in range(2):
                        ini = 0.0 if si == 0 else seg_view(tB[:], 0, dr)[:, CTX - 1:CTX]
                        ov, dv = seg_view(tB[:], si, dr), seg_view(tA[:], si, dr)
                        k.op(k.dve, lambda h: h.tensor_tensor_scan(out=ov, data0=dv, data1=dv, initial=ini,
                                                                   op0=ALU.max, op1=ALU.max), [tA, tB], [tB])
                    for si in range(2):
                        j0, j1 = SEGS[si][0] // 16, SEGS[si][1] // 16
                        k.op(k.dve, lambda h: h.tensor_copy(out=Rj[:, j0:j1], in_=c16(seg_view(tB[:], si, dr))[:, :, 15]),
                             [tB], [Rj])
                    k.op(k.dve, lambda h: h.memset(Rp[:, 0:1], 0.0), [], [Rp])
                    k.op(k.dve, lambda h: h.tensor_copy(out=Rp[:, 1:NCH], in_=Rj[:, 0:NCH - 1]), [Rj], [Rp])
                    k.op(k.dve, lambda h: h.tensor_tensor(out=Dc[:], in0=Rp[:], in1=Rj[:], op=ALU.subtract), [Rp, Rj], [Dc])
                    k.op(k.act, lambda h: h.activation(out=Dc[:], in_=Dc[:], func=AF.Exp), [Dc], [Dc])
                    for si in range(2):
                        j0, j1 = SEGS[si][0] // 16, SEGS[si][1] // 16
                        rb = Rj[:, j0:j1].unsqueeze(2).to_broadcast([128, j1 - j0, 16])
                        db = Dc[:, j0:j1].unsqueeze(2).to_broadcast([128, j1 - j0, 16])
                        av, cv = c16(seg_view(tA[:], si, dr)), c16(seg_view(tC[:], si, dr))
                        k.op(k.dve, lambda h: h.tensor_tensor(out=av, in0=av, in1=rb, op=ALU.subtract), [tA, Rj], [tA])
                        k.op(k.dve, lambda h: h.tensor_tensor(out=cv, in0=cv, in1=rb, op=ALU.subtract), [tC, Rj], [tC])
                        k.op(k.pool, lambda h: h.tensor_tensor(out=c16(seg_view(x3[:], si, dr)), in0=c16(seg_view(qA[:], si, dr)),
                                                               in1=db, op=ALU.mult), [qA, Dc], [x3])
                    k.op(k.act, lambda h: h.activation(out=tA[:], in_=tA[:], func=AF.Exp), [tA], [tA])
                    k.op(k.act, lambda h: h.activation(out=cld[dr][:], in_=tC[:], func=AF.Exp), [tC], [cld[dr]])
                    k.op(k.dve, lambda h: h.tensor_tensor(out=kA[:], in0=kA[:], in1=tA[:], op=ALU.mult), [kA, tA], [kA])
                else:
                    k.dma(qA, qA[:], R["hg_q"], R["hg_q"].t[rows, :])
                    f0 = dr * 256 + hp * 128
                    k.dma(tA, tA[:], R["hg_f"], R["hg_f"].t[f0:f0 + 128, :])
                    k.op(k.act, lambda h: h.activation(out=tB[:], in_=tA[:], func=AF.Sigmoid), [tA], [tB])
                    k.op(k.dve, lambda h: h.tensor_scalar(out=tC[:], in0=tB[:], scalar1=sc[:, 2:3], scalar2=sc[:, 1:2],
                                                          op0=ALU.mult, op1=ALU.add), [tB, sc], [tC])
                    k.op(k.dve, lambda h: h.tensor_scalar(out=tB[:], in0=tB[:], scalar1=sc[:, 1:2], scalar2=sc[:, 0:1],
                                                          op0=ALU.mult, op1=ALU.add), [tB, sc], [tB])
                    k.op(k.act, lambda h: h.activation(out=tB[:], in_=tB[:], func=AF.Ln), [tB], [tB])
                    for si in range(2):
                        j0, j1 = SEGS[si][0] // 16, SEGS[si][1] // 16
                        ov = c16(seg_view(tA[:], si, dr))
                        nseg = SEGS[si][1] - SEGS[si][0]
                        k.op(k.dve, lambda h: h.tensor_tensor_scan(out=seg_view(tA[:], si, dr), data0=rmask[:, 0:nseg],
                                                                   data1=seg_view(tB[:], si, dr), initial=0.0,
                                                                   op0=ALU.mult, op1=ALU.add), [tB, rmask, tA], [tA])
                        k.op(k.dve, lambda h: h.tensor_copy(out=Rj[:, j0:j1], in_=ov[:, :, 15]), [tA], [Rj])
                    k.op(k.act, lambda h: h.activation(out=Dc[:], in_=Rj[:], func=AF.Exp), [Rj], [Dc])
                    k.op(k.act, lambda h: h.activation(out=tB[:], in_=tA[:], func=AF.Exp), [tA], [tB])
                    k.op(k.dve, lambda h: h.tensor_tensor(out=qA[:], in0=qA[:], in1=tB[:], op=ALU.mult), [qA, tB], [qA])
                    k.op(k.act, lambda h: h.activation(out=tB[:], in_=tA[:], func=AF.Exp, scale=-1.0), [tA], [tB])
                    k.op(k.dve, lambda h: h.tensor_tensor(out=kA[:], in0=tC[:], in1=tB[:], op=ALU.mult), [tC, tB], [kA])
                    for si in range(2):
                        j0, j1 = SEGS[si][0] // 16, SEGS[si][1] // 16
                        rb = Rj[:, j0:j1].unsqueeze(2).to_broadcast([128, j1 - j0, 16])
                        av = c16(seg_view(tA[:], si, dr))
                        k.op(k.dve, lambda h: h.tensor_tensor(out=av, in0=rb, in1=av, op=ALU.subtract), [tA, Rj], [tA])
                    k.op(k.act, lambda h: h.activation(out=tA[:], in_=tA[:], func=AF.Exp), [tA], [tA])
                    k.op(k.dve, lambda h: h.tensor_tensor(out=x3[:], in0=tC[:], in1=tA[:], op=ALU.mult), [tC, tA], [x3])

            def qI_of(dr):
                return x3d[dr] if ml else qAd[dr]

            def kS_of(dr):
                return kAd[dr] if ml else x3d[dr]

            def pA_of(dr):
                return bankA[dr][:, 0:256].rearrange("p (h t) -> p h t", h=2)

            def po_of(dr, vs):
                return bankA[dr][:, 256 + vs * 128:256 + (vs + 1) * 128]

            def prep(ip):
                for dr in range(2):
                    it = tile_of(ip, dr)
                    c0 = it * 128
                    qz = qzd[dr][ip % 2]
                    for hh in range(2):
                        rr = slice(hh * 64, (hh + 1) * 64)
                        k.op(k.act, lambda h: h.copy(out=qz[rr, hh, :], in_=qAd[dr][rr, c0:c0 + 128]), [qAd[dr]], [qz])
                    k.mm([lambda h: h.matmul(bankA[dr][:, 0:256], lhsT=kAd[dr][:, c0:c0 + 128],
                                             rhs=qz[:].rearrange("p h t -> p (h t)"), start=True, stop=True)],
                         [kAd[dr], qz], [bankA[dr]])
                    kS = kS_of(dr)
                    k.mm([lambda h: h.transpose(out=pktd[dr][:], in_=kS[:, c0:c0 + 128], identity=P.ident[:])],
                         [kS, P.ident], [pktd[dr]])
                    k.op(k.act, lambda h: h.copy(out=kStd[dr][ip % 2][:], in_=pktd[dr][:]), [pktd[dr]], [kStd[dr][ip % 2]])
                    for c_ in range(8):
                        k.op(k.act, lambda h: h.mul(out=Vexpd[dr][ip % 2][:, :, c_, :], in_=vt[:, it, :].rearrange("p (h e) -> p h e", h=2),
                                                    mul=bmf[:, c_:c_ + 1]), [vt, bmf], [Vexpd[dr][ip % 2]])

            def amt(ip):
                for dr in range(2):
                    AmT = AmTd[dr][ip % 2]
                    k.op(k.dve, lambda h: h.tensor_tensor(out=AmT[:], in0=pA_of(dr),
                                                          in1=maskb[:, dr:dr + 1, :].to_broadcast([128, 2, 128]),
                                                          op=ALU.mult), [bankA[dr], maskb], [AmT])

            def dS_mm(ip):
                for dr in range(2):
                    for vs in range(nv):
                        for hh in range(2):
                            rr = slice(hh * 64, (hh + 1) * 64)
                            rhs = Vexpd[dr][ip % 2][:, hh, :, :] if vs == 0 else vones[:]
                            k.mm([lambda h: h.matmul(pdSd[dr][vs][rr, :, :], lhsT=kStd[dr][ip % 2][:, rr], rhs=rhs, start=True, stop=True)],
                                 [kStd[dr][ip % 2], Vexpd[dr][ip % 2], vones], [pdSd[dr][vs]])

            def steps(ip):
                for dr in range(2):
                    for vs in range(nv):
                        Sh = Shd[dr][vs]
                        k.op(k.pool, lambda h: h.tensor_copy(out=Sh[:, 0, :], in_=Sh[:, 8, :]), [Sh], [Sh])
                for cl in range(8):
                    for dr in range(2):
                        cp = cl if dr == 0 else 7 - cl
                        j = ip * 8 + cl
                        for vs in range(nv):
                            Sh = Shd[dr][vs]
                            k.op(k.dve, lambda h: h.scalar_tensor_tensor(out=Sh[:, cl + 1, :], in0=Sh[:, cl, :],
                                                                         scalar=Dcd[dr][:, j:j + 1], in1=pdSd[dr][vs][:, cp, :],
                                                                         op0=ALU.mult, op1=ALU.add),
                                 [Sh, Dcd[dr], pdSd[dr][vs]], [Sh])
                for dr in range(2):
                    for vs in range(nv):
                        k.op(k.act, lambda h: h.copy(out=Sbd[dr][vs][:], in_=Shd[dr][vs][:, 0:8, :]), [Shd[dr][vs]], [Sbd[dr][vs]])

            def out_mm(ip):
                for dr in range(2):
                    it = tile_of(ip, dr)
                    c0 = it * 128
                    AmT = AmTd[dr][ip % 2]
                    qI = qI_of(dr)
                    for vs in range(nv):
                        fns = []
                        for hh in range(2):
                            rr = slice(hh * 64, (hh + 1) * 64)
                            lh = vt[:, it, hh * 64:(hh + 1) * 64] if vs == 0 else onesT[:]
                            fns.append(lambda h, rr=rr, lh=lh, hh=hh: h.matmul(po_of(dr, vs)[rr, :], lhsT=lh, rhs=AmT[:, hh, :],
                                                                               start=True, stop=False, skip_group_check=True))
                            for cl in range(8):
                                cp = cl if dr == 0 else 7 - cl
                                fns.append(lambda h, rr=rr, cl=cl, cp=cp: h.matmul(
                                    po_of(dr, vs)[rr, cp * 16:(cp + 1) * 16], lhsT=Sbd[dr][vs][rr, cl, :],
                                    rhs=qI[rr, c0 + cp * 16:c0 + (cp + 1) * 16], start=False, stop=(cl == 7),
                                    skip_group_check=True))
                        k.mm(fns, [vt, onesT, AmT, Sbd[dr][vs], qI], [bankA[dr]])

            def evac_a(ip):
                for dr in range(2):
                    n_ = nsb[dr][ip % 2]
                    k.op(k.act, lambda h: h.copy(out=n_[:], in_=po_of(dr, 0)), [bankA[dr]], [n_])
                    if ml:
                        d_ = dsb[dr][ip % 2]
                        k.op(k.act, lambda h: h.activation(out=d_[:], in_=po_of(dr, 1), func=AF.Abs), [bankA[dr]], [d_])

            def evac_b(ip):
                for dr in range(2):
                    c0 = tile_of(ip, dr) * 128
                    n_ = nsb[dr][ip % 2]
                    if ml:
                        d_ = dsb[dr][ip % 2]
                        k.op(k.dve, lambda h: h.tensor_tensor(out=d_[:], in0=d_[:], in1=cld[dr][:, c0:c0 + 128], op=ALU.max),
                             [cld[dr], d_], [d_])
                        k.op(k.dve, lambda h: h.reciprocal(out=d_[:], in_=d_[:]), [d_], [d_])
                        k.op(k.dve, lambda h: h.tensor_tensor(out=n_[:], in0=n_[:], in1=d_[:], op=ALU.mult), [n_, d_], [n_])
                    k.op(k.pool, lambda h: h.tensor_tensor(out=osum[:, c0:c0 + 128], in0=osum[:, c0:c0 + 128], in1=n_[:],
                                                           op=ALU.add), [osum, n_], [osum])

            for dr in range(2):
                for vs in range(nv):
                    k.op(k.dve, lambda h: h.memset(Shd[dr][vs][:, 8, :], 0.0), [], [Shd[dr][vs]])
            prep(0)
            amt(0)
            dS_mm(0)
            prep(1)
            for ip in range(NT):
                if ip + 1 < NT:
                    amt(ip + 1)
                steps(ip)
                if ip > 0:
                    evac_b(ip - 1)
                if ip + 1 < NT:
                    dS_mm(ip + 1)
                out_mm(ip)
                evac_a(ip)
                if ip + 2 < NT:
                    prep(ip + 2)
            evac_b(NT - 1)

            pss = pdSd[0][0]
            for bi, (t0, n) in enumerate(tok_blocks(T)):
                pv = pss[:].rearrange("p c e -> p (c e)")
                k.op(k.act, lambda h: h.activation(out=fq[:, :n], in_=osum[:, t0:t0 + n], func=AF.Square), [osum], [fq])
                k.mm([lambda h: h.matmul(pv[:, :n], lhsT=blk[:], rhs=fq[:, :n], start=True, stop=True)], [blk, fq], [pss])
                k.op(k.dve, lambda h: h.tensor_scalar(out=fr_[:, :n], in0=pv[:, :n], scalar1=1.0 / 64, scalar2=EPS,
                                                      op0=ALU.mult, op1=ALU.add), [pss], [fr_])
                k.op(k.act, lambda h: h.sqrt(out=fr_[:, :n], in_=fr_[:, :n]), [fr_], [fr_])
                k.op(k.dve, lambda h: h.reciprocal(out=fr_[:, :n], in_=fr_[:, :n]), [fr_], [fr_])
                k.op(k.dve, lambda h: h.tensor_tensor(out=fy[:, :n], in0=osum[:, t0:t0 + n], in1=fr_[:, :n], op=ALU.mult),
                     [osum, fr_], [fy])
                ob = fo[bi % 2]
                k.op(k.dve, lambda h: h.scalar_tensor_tensor(out=ob[:, :n], in0=fy[:, :n], scalar=nw[:, 0:1], in1=gate[:, t0:t0 + n],
                                                             op0=ALU.mult, op1=ALU.mult), [fy, nw, gate], [ob])
                r0 = row_off + hp * 128
                k.dma(mixT_d, mixT_d.t[r0:r0 + 128, t0:t0 + n], ob, ob[:, :n])


def rec_consts():
    import ml_dtypes
    s = np.arange(128)[:, None]
    t = np.arange(128)[None, :]
    same = (s // 16) == (t // 16)
    cm = np.stack([(same & (s <= t)), (same & (s >= t))], 1).astype(np.float32)
    bmk = ((s // 16) == np.arange(8)[None, :]).astype(np.float32)
    blk = ((s // 64) == (t // 64)).astype(np.float32)
    b16 = ml_dtypes.bfloat16
    return cm.astype(b16), bmk.astype(b16), blk.astype(b16)


def stage_postmix(P, l, mixT_d, src, dst, tiles, all_latent=False):
    k = P.k
    with k.scope():
        wl = WLoader(k, nbuf=2, width=512, name="ow")
        wo = k.sb("wo", [128, KC, 1024], BF16)
        for hf in range(2):
            wb = wl.get(P.w_out, P.w_out.t[l, :, hf * 512:(hf + 1) * 512], 512)
            k.op(k.dve, lambda h: h.tensor_copy(out=wo[:, :, hf * 512:(hf + 1) * 512], in_=wb[:, :, :512]), [wb], [wo])
        gpb = k.sb("gpb", [128, 2, 1024], F32)
        for kind in range(2):
            k.dma(gpb, gpb[:, kind, :], P.modc[l], P.modc[l].t[kind:kind + 1, 2, :].to_broadcast([128, 1024]))
        mb = [k.sb("mb%d" % i, [128, KC, 128], BF16) for i in range(2)]
        xt = [k.sb("pxt%d" % i, [128, 1024], F32) for i in range(2)]
        pm = [k.ps("pmx%d" % i, [128, 512]) for i in range(4)]
        ep = Epilogue(k)
        for n, i in enumerate(tiles):
            m, x = mb[n % 2], xt[n % 2]
            k.dma(m, m[:], mixT_d, mixT_d.t[:, i * 128:(i + 1) * 128].rearrange("(k p) t -> p k t", p=128))
            k.dma(x, x[:], src, src.t[i * 128:(i + 1) * 128, :])
            ps = []
            for hf in range(2):
                p = pm[(n % 2) * 2 + hf]
                k.mm([(lambda h, kk=kk: h.matmul(p[:], lhsT=m[:, kk, :], rhs=wo[:, kk, hf * 512:(hf + 1) * 512],
                                                 start=(kk == 0), stop=(kk == KC - 1))) for kk in range(KC)], [m, wo], [p])
                ps.append(p)
            ep.run([(p, None) for p in ps], x, gpb, 1 if (i < 2 and not all_latent) else 0, dst, dst.t[i * 128:(i + 1) * 128, :])


class Epilogue:
    def __init__(self, k, need_y=True):
        self.k = k
        self.y = k.sb("ep_y", [128, 1024], F32) if need_y else None
        self.junk = k.sb("ep_j", [128, 1024], BF16)
        self.s = k.sb("ep_s", [128, 2], F32)
        self.o = [k.sb("ep_o%d" % i, [128, 1024], F32) for i in range(2)]
        self.n = 0

    def run(self, halves, x, gpb, kind, dst, dst_ap, ysb=None):
        k = self.k
        y, s = self.y, self.s
        if ysb is None:
            for hf, (p, _) in enumerate(halves):
                eng = k.act if hf == 0 else k.dve
                if hf == 0:
                    k.op(k.act, lambda h: h.copy(out=y[:, 0:512], in_=p[:]), [p], [y])
                else:
                    k.op(k.dve, lambda h: h.tensor_copy(out=y[:, 512:1024], in_=p[:]), [p], [y])
        else:
            ybuf, yap = ysb
        o = self.o[self.n % 2]
        self.n += 1
        k.op(k.dve, lambda h: h.memset(s[:], 0.0), [], [s])
        if ysb is None:
            ybuf, yap = y, y[:]
        k.op(k.act, lambda h: h.activation(out=self.junk[:], in_=yap, func=AF.Square, accum_out=s[:, 0:1]), [ybuf, s], [self.junk, s])
        k.op(k.dve, lambda h: h.tensor_scalar(out=s[:, 1:2], in0=s[:, 0:1], scalar1=1.0 / D, scalar2=EPS,
                                              op0=ALU.mult, op1=ALU.add), [s], [s])
        k.op(k.act, lambda h: h.sqrt(out=s[:, 1:2], in_=s[:, 1:2]), [s], [s])
        k.op(k.dve, lambda h: h.reciprocal(out=s[:, 1:2], in_=s[:, 1:2]), [s], [s])
        k.op(k.dve, lambda h: h.scalar_tensor_tensor(out=o[:], in0=yap, scalar=s[:, 1:2], in1=gpb[:, kind, :],
                                                     op0=ALU.mult, op1=ALU.mult), [ybuf, s, gpb], [o])
        k.op(k.dve, lambda h: h.tensor_tensor(out=o[:], in0=o[:], in1=x[:], op=ALU.add), [o, x], [o])
        k.dma(dst, dst_ap, o, o[:])


def stage_ffn(P, l, src, dst, tiles, dst_row, moe, all_latent=False):
    k = P.k
    nff = (FF_EXP if moe else FF_DENSE) // 128
    nexp = NEXP if moe else 1
    bsz = 8 if moe else 12
    for b0 in range(0, len(tiles), bsz):
        bt = tiles[b0:b0 + bsz]
        nb = len(bt)
        ntok = nb * 128
        with k.scope():
            hxb = k.sb("hxb", [128, KC, bsz * 128], BF16)
            stage_norm(P, src, l, 1, hxb, bt, all_latent=all_latent)
            with k.scope():
                HT = k.sb("HT", [128, nff, bsz * 128], BF16)
                yacc = k.sb("yacc", [128, bsz, 1024], F32)
                wl = WLoader(k, nbuf=2, width=128, name="fw")
                wl2 = WLoader(k, nbuf=2, width=128, name="fu")
                wld = WLoader(k, nbuf=2, width=512, kc=4 if moe else 2, name="fd")
                pg = [k.ps("pg%d" % i, [128, 512]) for i in range(2)]
                pu = [k.ps("pu%d" % i, [128, 512]) for i in range(2)]
                pd = [k.ps("pd%d" % i, [128, 512]) for i in range(2)]
                sg = [k.sb("sg%d" % i, [128, 512], F32) for i in range(2)]
                gates = k.sb("gates", [128, 8, 8], F32)
                gp2 = k.sb("gp2x", [128, 2, 1024], F32)
                for kind_ in range(2):
                    k.dma(gp2, gp2[:, kind_, :], P.modc[l], P.modc[l].t[kind_:kind_ + 1, 5, :].to_broadcast([128, 1024]))
                if moe:
                    rws = k.sb("rws", [128, KC, 8], F32)
                    k.dma(rws, rws[:], P.router_w, P.router_w.t[0, :, :].rearrange("(k p) e -> p k e", p=128))
                    rwb = k.sb("rwb", [128, KC, 8], BF16)
                    k.op(k.dve, lambda h: h.tensor_copy(out=rwb[:], in_=rws[:]), [rws], [rwb])
                    rb = k.sb("rb", [128, 8], F32)
                    k.dma(rb, rb[:], P.router_b, P.router_b.t[0:1, :].to_broadcast([128, 8]))
                    lg = k.sb("lg", [128, 8], F32)
                    l2 = k.sb("l2", [128, 8], F32)
                    mk = k.sb("mk", [128, 2, 8], F32)
                    mx = k.sb("mx", [128, 4], F32)
                    for ti in range(nb):
                        p = pd[ti % 2]
                        k.mm([(lambda h, kk=kk: h.matmul(p[:, 0:8], lhsT=hxb[:, kk, ti * 128:(ti + 1) * 128], rhs=rwb[:, kk, :],
                                                         start=(kk == 0), stop=(kk == KC - 1))) for kk in range(KC)], [hxb, rwb], [p])
                        k.op(k.dve, lambda h: h.tensor_tensor(out=lg[:], in0=p[:, 0:8], in1=rb[:], op=ALU.add), [p, rb], [lg])
                        k.op(k.dve, lambda h: h.reduce_max(out=mx[:, 0:1], in_=lg[:], axis=AX.X), [lg], [mx])
                        k.op(k.dve, lambda h: h.tensor_scalar(out=mk[:, 0, :], in0=lg[:], scalar1=mx[:, 0:1], scalar2=None,
                                                              op0=ALU.is_ge), [lg, mx], [mk])
                        k.op(k.dve, lambda h: h.scalar_tensor_tensor(out=l2[:], in0=mk[:, 0, :], scalar=-1e30, in1=lg[:],
                                                                     op0=ALU.mult, op1=ALU.add), [mk, lg], [l2])
                        k.op(k.dve, lambda h: h.reduce_max(out=mx[:, 1:2], in_=l2[:], axis=AX.X), [l2], [mx])
                        k.op(k.dve, lambda h: h.tensor_scalar(out=mk[:, 1, :], in0=l2[:], scalar1=mx[:, 1:2], scalar2=None,
                                                              op0=ALU.is_ge), [l2, mx], [mk])
                        k.op(k.dve, lambda h: h.tensor_tensor(out=mx[:, 2:3], in0=mx[:, 0:1], in1=mx[:, 1:2], op=ALU.subtract), [mx], [mx])
                        k.op(k.act, lambda h: h.activation(out=mx[:, 2:3], in_=mx[:, 2:3], func=AF.Sigmoid), [mx], [mx])
                        k.op(k.dve, lambda h: h.tensor_scalar(out=mx[:, 3:4], in0=mx[:, 2:3], scalar1=-1.0, scalar2=1.0,
                                                              op0=ALU.mult, op1=ALU.add), [mx], [mx])
                        k.op(k.dve, lambda h: h.tensor_scalar(out=gates[:, ti, :], in0=mk[:, 0, :], scalar1=mx[:, 2:3], scalar2=None,
                                                              op0=ALU.mult), [mk, mx], [gates])
                        k.op(k.dve, lambda h: h.scalar_tensor_tensor(out=gates[:, ti, :], in0=mk[:, 1, :], scalar=mx[:, 3:4],
                                                                     in1=gates[:, ti, :], op0=ALU.mult, op1=ALU.add),
                             [mk, mx, gates], [gates])
                for e in range(nexp):
                    if moe:
                        wg_b, wg_ap = P.moe_w_gate, P.moe_w_gate.t[0, e]
                        wu_b, wu_ap = P.moe_w_up, P.moe_w_up.t[0, e]
                        wd_b, wd_ap = P.moe_w_down, P.moe_w_down.t[0, e]
                    else:
                        wg_b, wg_ap = P.ffn_w_gate, P.ffn_w_gate.t[0]
                        wu_b, wu_ap = P.ffn_w_up, P.ffn_w_up.t[0]
                        wd_b, wd_ap = P.ffn_w_down, P.ffn_w_down.t[0]
                    for c in range(nff):
                        wg = wl.get(wg_b, wg_ap[:, c * 128:(c + 1) * 128], 128)
                        wu = wl2.get(wu_b, wu_ap[:, c * 128:(c + 1) * 128], 128)
                        for bi, (t0, n) in enumerate(tok_blocks(ntok)):
                            a, b_ = pg[bi % 2], pu[bi % 2]
                            k.mm([(lambda h, kk=kk: h.matmul(a[:, :n], lhsT=wg[:, kk, :128], rhs=hxb[:, kk, t0:t0 + n],
                                                             start=(kk == 0), stop=(kk == KC - 1))) for kk in range(KC)], [wg, hxb], [a])
                            k.mm([(lambda h, kk=kk: h.matmul(b_[:, :n], lhsT=wu[:, kk, :128], rhs=hxb[:, kk, t0:t0 + n],
                                                             start=(kk == 0), stop=(kk == KC - 1))) for kk in range(KC)], [wu, hxb], [b_])
                            s_ = sg[bi % 2]
                            k.op(k.act, lambda h: h.activation(out=s_[:, :n], in_=a[:, :n], func=AF.Silu), [a], [s_])
                            k.op(k.dve, lambda h: h.tensor_tensor(out=HT[:, c, t0:t0 + n], in0=s_[:, :n], in1=b_[:, :n], op=ALU.mult),
                                 [s_, b_], [HT])
                    kcd = wld.kc
                    for pc in range(nff // kcd):
                        for hf in range(2):
                            wd = wld.get(wd_b, wd_ap[pc * kcd * 128:(pc + 1) * kcd * 128, hf * 512:(hf + 1) * 512], 512)
                            for ti in range(nb):
                                p = pd[ti % 2]
                                k.mm([(lambda h, kk=kk: h.matmul(p[:], lhsT=HT[:, pc * kcd + kk, ti * 128:(ti + 1) * 128], rhs=wd[:, kk, :],
                                                                 start=(kk == 0), stop=(kk == kcd - 1))) for kk in range(kcd)], [HT, wd], [p])
                                ya = yacc[:, ti, hf * 512:(hf + 1) * 512]
                                first = (e == 0 and pc == 0)
                                if moe:
                                    g_ = gates[:, ti, e:e + 1]
                                    if first:
                                        k.op(k.dve, lambda h: h.tensor_scalar(out=ya, in0=p[:], scalar1=g_, scalar2=None, op0=ALU.mult),
                                             [p, gates], [yacc])
                                    else:
                                        k.op(k.dve, lambda h: h.scalar_tensor_tensor(out=ya, in0=p[:], scalar=g_, in1=ya,
                                                                                     op0=ALU.mult, op1=ALU.add), [p, gates, yacc], [yacc])
                                else:
                                    if first:
                                        k.op(k.act, lambda h: h.copy(out=ya, in_=p[:]), [p], [yacc])
                                    else:
                                        k.op(k.dve, lambda h: h.tensor_tensor(out=ya, in0=ya, in1=p[:], op=ALU.add), [p, yacc], [yacc])
                ep = Epilogue(k, need_y=False)
                xt = [k.sb("fx%d" % i, [128, 1024], F32) for i in range(2)]
                for ti, i in enumerate(bt):
                    x = xt[ti % 2]
                    k.dma(x, x[:], src, src.t[i * 128:(i + 1) * 128, :])
                    r0 = dst_row(i)
                    ep.run(None, x, gp2, 1 if (i < 2 and not all_latent) else 0, dst, dst.t[r0:r0 + 128, :], ysb=(yacc, yacc[:, ti, :]))


def stage_select(P, src, dst):
    k = P.k
    with k.scope():
        sel = k.sb("sel", [128, 2], F32)
        k.dma(sel, sel[:], P.ext["sel"], P.ext["sel"].t[:, :])
        a = [k.sb("sa%d" % i, [128, 1024], F32) for i in range(2)]
        b = [k.sb("sb%d" % i, [128, 1024], F32) for i in range(2)]
        for j in range(16):
            x, y = a[j % 2], b[j % 2]
            i0, i1 = 2 + j, 2 + 16 + j
            k.dma(x, x[:], src, src.t[i0 * 128:(i0 + 1) * 128, :])
            k.dma(y, y[:], src, src.t[i1 * 128:(i1 + 1) * 128, :])
            k.op(k.dve, lambda h: h.tensor_scalar(out=x[:], in0=x[:], scalar1=sel[:, 0:1], scalar2=None, op0=ALU.mult), [x, sel], [x])
            k.op(k.dve, lambda h: h.scalar_tensor_tensor(out=x[:], in0=y[:], scalar=sel[:, 1:2], in1=x[:], op0=ALU.mult, op1=ALU.add),
                 [x, y, sel], [x])
            k.dma(dst, dst.t[j * 128:(j + 1) * 128, :], x, x[:])


def stage_select_mix(P, src, dst):
    k = P.k
    with k.scope():
        sel = k.sb("sel", [128, 2], F32)
        k.dma(sel, sel[:], P.ext["sel"], P.ext["sel"].t[:, :])
        H = SEQ // 2
        a = [k.sb("ma%d" % i, [128, H], BF16) for i in range(2)]
        b = [k.sb("mb%d" % i, [128, H], BF16) for i in range(2)]
        for r in range(4):
            x, y = a[r % 2], b[r % 2]
            k.dma(x, x[:], src, src.t[r * 128:(r + 1) * 128, CTX:CTX + H])
            k.dma(y, y[:], src, src.t[r * 128:(r + 1) * 128, CTX + H:T])
            k.op(k.dve, lambda h: h.tensor_scalar(out=x[:], in0=x[:], scalar1=sel[:, 0:1], scalar2=None, op0=ALU.mult), [x, sel], [x])
            k.op(k.dve, lambda h: h.scalar_tensor_tensor(out=x[:], in0=y[:], scalar=sel[:, 1:2], in1=x[:], op0=ALU.mult, op1=ALU.add),
                 [x, y, sel], [x])
            k.dma(dst, dst.t[r * 128:(r + 1) * 128, :], x, x[:])


def build_program():
    P = Prog()
    k = P.k
    P.xin = P.ext_in("xin", [T, D])
    P.c2 = P.ext_in("c2", [2, D])
    P.w_mod = P.ext_in("w_mod", [2, D, 6 * D])
    P.b_mod = P.ext_in("b_mod", [2, 6 * D])
    for n in ("norm_pre_mix", "norm_post_mix", "norm_pre_ffn", "norm_post_ffn"):
        setattr(P, n, P.ext_in(n, [2, D]))
    P.ext_in("ident", [128, 128])
    P.ext_in("sel", [128, 2])
    P.w_in = P.ext_in("w_in", [2, D, PROJ_W])
    P.ext_in("w_da_sw", [2, D, 1024])
    P.w_out = P.ext_in("w_out", [2, D, D])
    P.ext_in("rope_c", [128, T], BF16)
    P.ext_in("rope_s", [128, T], BF16)
    P.ext_in("cmask", [128, 2, 128], BF16)
    P.ext_in("bmask", [128, 8], BF16)
    P.ext_in("blk", [128, 128], BF16)
    P.ml_gate_b = P.ext_in("ml_gate_b", [2, 16])
    P.ml_norm = P.ext_in("ml_norm", [2, 256])
    P.hg_norm = P.ext_in("hg_norm", [2, 256])
    P.hg_lb_logits = P.ext_in("hg_lb_logits", [2, 256])
    P.da_lambda = P.ext_in("da_lambda", [2, 4, 64])
    P.da_norm = P.ext_in("da_norm", [2, 128])
    P.ffn_w_gate = P.ext_in("ffn_w_gate", [1, D, FF_DENSE])
    P.ffn_w_up = P.ext_in("ffn_w_up", [1, D, FF_DENSE])
    P.ffn_w_down = P.ext_in("ffn_w_down", [1, FF_DENSE, D])
    P.router_w = P.ext_in("router_w", [1, D, NEXP])
    P.router_b = P.ext_in("router_b", [1, NEXP])
    P.moe_w_gate = P.ext_in("moe_w_gate", [1, NEXP, D, FF_EXP])
    P.moe_w_up = P.ext_in("moe_w_up", [1, NEXP, D, FF_EXP])
    P.moe_w_down = P.ext_in("moe_w_down", [1, NEXP, FF_EXP, D])
    out = P.ext_out("out", [SEQ // 2, D])
    P.modc = [k.dram("modc%d" % l, [2, 6, D], F32) for l in range(2)]
    P.rec = dict(ml_q=k.dram("ml_q", [256, T], BF16), ml_k=k.dram("ml_k", [256, T], BF16), ml_o=k.dram("ml_o", [256, T], BF16),
                 ml_g=k.dram("ml_g", [1024, T], F32), hg_q=k.dram("hg_q", [256, T], BF16), hg_g=k.dram("hg_g", [256, T], BF16),
                 hg_f=k.dram("hg_f", [512, T], F32), ml_v=k.dram("ml_v", [T, 256], BF16), hg_v=k.dram("hg_v", [T, 256], BF16))
    mixT_d = k.dram("mixT_d", [1024, T], BF16)
    xmid = [k.dram("xmid%d" % l, [T, D], F32) for l in range(2)]
    x1 = k.dram("x1", [T, D], F32)
    xown = k.dram("xown", [SEQ // 2, D], F32)
    mixown = k.dram("mixown", [1024, SEQ // 2], BF16)
    setup_consts(P)
    stage_mod(P, 0)
    stage_mod(P, 1)
    lat_blocks = [(CTX + qb * 512, 512, list(range(NT))) for qb in range(SEQ // 512)]
    for l in range(2):
        src = P.xin if l == 0 else x1
        lam_init = 0.8 - 0.6 * float(np.exp(-0.3 * l))
        with k.scope():
            hxT = k.sb("hxT", [128, KC, T], BF16)
            stage_norm(P, src, l, 0, hxT, list(range(NT)))
            stage_proj_rec(P, l, hxT)
            if l == 0:
                qbl = [(0, CTX, [0, 1])] + list(lat_blocks)
                stage_attn(P, l, hxT, None, 0, 0, lam_init, qblocks=qbl, mixT_d=mixT_d)
            else:
                qbl = [(qb * 512, 512, list(range(NT))) for qb in range(SEQ // 2 // 512)]
                stage_attn(P, l, hxT, None, 0, 0, lam_init, qblocks=qbl, mixT_d=mixown, own=True)
        stage_rec(P, l, "ml", mixT_d)
        stage_rec(P, l, "hg", mixT_d)
        if l == 0:
            stage_postmix(P, l, mixT_d, src, xmid[0], list(range(NT)))
            stage_ffn(P, 0, xmid[0], x1, list(range(NT)), lambda i: i * 128, moe=False)
        else:
            stage_select_mix(P, mixT_d, mixown)
            stage_select(P, x1, xown)
            stage_postmix(P, l, mixown, xown, xmid[1], list(range(16)), all_latent=True)
            stage_ffn(P, 1, xmid[1], out, list(range(16)), lambda i: i * 128, moe=True, all_latent=True)
    k.finish([out])
    return k.nc


_CACHE = {}


def kernel(x, c, ctx, c_ctx, w_mod, b_mod, norm_pre_mix, norm_post_mix, norm_pre_ffn, norm_post_ffn,
           w_in, w_out, ml_gate_b, ml_norm, hg_lb_logits, hg_norm, da_lambda, da_norm,
           ffn_w_gate, ffn_w_up, ffn_w_down, router_w, router_b, moe_w_gate, moe_w_up, moe_w_down):
    from concourse.bass_utils import run_bass_kernel_spmd
    f = lambda a: np.ascontiguousarray(np.asarray(a, dtype=np.float32))
    x, c, ctx, c_ctx = f(x), f(c), f(ctx), f(c_ctx)
    w_in = f(w_in)
    if "nc" not in _CACHE:
        _CACHE["nc"] = build_program()
    nc = _CACHE["nc"]
    rc, rs = rope_tables()
    cm, bmk, blk = rec_consts()
    shared = dict(w_mod=f(w_mod), b_mod=f(b_mod), norm_pre_mix=f(norm_pre_mix), norm_post_mix=f(norm_post_mix),
                  norm_pre_ffn=f(norm_pre_ffn), norm_post_ffn=f(norm_post_ffn), ident=np.eye(128, dtype=np.float32),
                  w_in=w_in, w_da_sw=swap_cols(w_in), w_out=f(w_out), rope_c=rc, rope_s=rs, cmask=cm, bmask=bmk, blk=blk,
                  ml_gate_b=f(ml_gate_b), ml_norm=f(ml_norm), hg_norm=f(hg_norm), hg_lb_logits=f(hg_lb_logits),
                  da_lambda=f(da_lambda), da_norm=f(da_norm), ffn_w_gate=f(ffn_w_gate), ffn_w_up=f(ffn_w_up),
                  ffn_w_down=f(ffn_w_down), router_w=f(router_w), router_b=f(router_b), moe_w_gate=f(moe_w_gate),
                  moe_w_up=f(moe_w_up), moe_w_down=f(moe_w_down))
    in_maps = []
    for core in range(8):
        b, half = core // 2, core % 2
        m = dict(shared)
        m["xin"] = np.ascontiguousarray(np.concatenate([ctx[b], x[b]], axis=0))
        m["c2"] = np.ascontiguousarray(np.stack([c[b], c_ctx], axis=0))
        sel = np.zeros((128, 2), np.float32)
        sel[:, half] = 1.0
        m["sel"] = sel
        in_maps.append(m)
    res = run_bass_kernel_spmd(nc, in_maps, core_ids=list(range(8)))
    out = np.zeros((4, SEQ, D), np.float32)
    for core in range(8):
        b, half = core // 2, core % 2
        out[b, half * (SEQ // 2):(half + 1) * (SEQ // 2)] = np.asarray(res.results[core]["out"], dtype=np.float32)
    return out
```

```python
import numpy as np
from contextlib import ExitStack
import concourse.bass as bass
import concourse.mybir as mybir

F32 = mybir.dt.float32
BF16 = mybir.dt.bfloat16
AF = mybir.ActivationFunctionType
ALU = mybir.AluOpType
AX = mybir.AxisListType

SAME_ENG_SYNC = True


class Buf:
    def __init__(self, k, t, name, space):
        self.k, self.t, self.name, self.space = k, t, name, space
        self.w = {}
        self.r = {}
        self.dsem = None

    def __getitem__(self, idx):
        return self.t[idx]


class Eng:
    def __init__(self, k, name, h):
        self.k, self.name, self.h = k, name, h
        self.sem = k.es.enter_context(k.nc.semaphore("e_" + name))
        self.cnt = 0
        self.waited = {}


class _Scope:
    def __init__(self, k):
        self.k = k

    def __enter__(self):
        k = self.k
        self.prev = k.es
        self.st = ExitStack()
        k.es = self.st
        k.scopes.append([])
        return self

    def __exit__(self, *a):
        k = self.k
        k.barrier()
        for b in k.scopes.pop():
            if b.dsem is not None and getattr(b, "own_dsem", False):
                k.dpool.append(b.dsem)
        self.st.close()
        k.es = self.prev
        return False


class K:
    def __init__(self):
        self.nc = bass.Bass("TRN2", target_bir_lowering=False)
        self.es = ExitStack()
        nc = self.nc
        self.pe = Eng(self, "pe", nc.tensor)
        self.act = Eng(self, "act", nc.scalar)
        self.dve = Eng(self, "dve", nc.vector)
        self.pool = Eng(self, "pool", nc.gpsimd)
        self.sp = Eng(self, "sp", nc.sync)
        self.sems = {}
        self.totals = {}
        for e in (self.pe, self.act, self.dve, self.pool, self.sp):
            self.sems[e.sem.name] = e.sem
        self.ndsem = 0
        self.dq = 0
        self.dpool = []
        for i in range(88):
            s = self.es.enter_context(self.nc.semaphore("d%d" % i))
            self.sems[s.name] = s
            self.totals[s.name] = 0
            self.dpool.append(s)
        self.scopes = []

    def sb(self, name, shape, dt, dshare=None):
        self.nuniq = getattr(self, "nuniq", 0) + 1
        t = self.es.enter_context(self.nc.sbuf_tensor("%s_%d" % (name, self.nuniq), list(shape), dt))
        b = Buf(self, t, name, "sb")
        if dshare is not None:
            b.dsem = self._dsem(dshare)
        if self.scopes:
            self.scopes[-1].append(b)
        return b

    def ps(self, name, shape, dt=F32):
        self.nuniq = getattr(self, "nuniq", 0) + 1
        t = self.es.enter_context(self.nc.psum_tensor("%s_%d" % (name, self.nuniq), list(shape), dt))
        return Buf(self, t, name, "ps")

    def scope(self):
        return _Scope(self)

    def barrier(self):
        engs = (self.pe, self.act, self.dve, self.pool, self.sp)
        for e in engs:
            for o in engs:
                if o is e or o.cnt == 0:
                    continue
                if e.waited.get(o.sem.name, 0) < o.cnt:
                    e.h.wait_ge(o.sem, o.cnt)
                    e.waited[o.sem.name] = o.cnt
            for s, v in self.totals.items():
                if v and e.waited.get(s, 0) < v:
                    e.h.wait_ge(self.sems[s], v)
                    e.waited[s] = v

    def dram(self, name, shape, dt, kind="Internal"):
        t = self.nc.dram_tensor(name, list(shape), dt, kind=kind)
        return Buf(self, t.ap(), name, "dram")

    def _dsem(self, b):
        if b.dsem is None:
            b.dsem = self.dpool.pop()
            b.own_dsem = True
        return b.dsem

    def _deps(self, reads, writes):
        deps = {}
        self._raw = {}
        for b in reads:
            for s, v in b.w.items():
                deps[s] = max(deps.get(s, 0), v)
                self._raw[s] = max(self._raw.get(s, 0), v)
        for b in writes:
            for s, v in b.w.items():
                deps[s] = max(deps.get(s, 0), v)
            for s, v in b.r.items():
                deps[s] = max(deps.get(s, 0), v)
        return deps

    def _wait(self, eng, deps):
        for s, v in deps.items():
            if s in self.totals:
                v = self.totals[s]
            if s == eng.sem.name and not SAME_ENG_SYNC:
                continue
            if s == eng.sem.name and eng is self.pe:
                continue
            if s == eng.sem.name:
                if s not in self._raw:
                    continue
                v = self._raw[s]
            if eng.waited.get(s, 0) >= v:
                continue
            eng.h.wait_ge(self.sems[s], v)
            eng.waited[s] = v

    def _mark(self, tick, reads, writes):
        s, v = tick
        for b in writes:
            b.w[s] = max(b.w.get(s, 0), v)
            b.r = {}
        for b in reads:
            b.r[s] = max(b.r.get(s, 0), v)

    def op(self, eng, fn, reads=(), writes=()):
        self._wait(eng, self._deps(reads, writes))
        ins = fn(eng.h)
        eng.cnt += 1
        ins.then_inc(eng.sem, 1)
        self._mark((eng.sem.name, eng.cnt), reads, writes)
        return ins

    def mm(self, fns, reads=(), writes=()):
        eng = self.pe
        self._wait(eng, self._deps(reads, writes))
        ins = None
        for fn in fns:
            ins = fn(eng.h)
        eng.cnt += 1
        ins.then_inc(eng.sem, 1)
        self._mark((eng.sem.name, eng.cnt), reads, writes)

    def dma(self, out_b, out_ap, in_b, in_ap, q=None, **kw):
        if q is None:
            q = (self.sp, self.pool)[self.dq % 2]
            self.dq += 1
        sb = out_b if out_b.space != "dram" else in_b
        if sb.space == "dram":
            sb = out_b
        s = self._dsem(sb)
        self._wait(q, self._deps([in_b], [out_b]))
        ins = q.h.dma_start(out=out_ap, in_=in_ap, **kw)
        ins.then_inc(s, 16)
        self.totals[s.name] += 16
        self._mark((s.name, self.totals[s.name]), [in_b], [out_b])
        return ins

    def finish(self, bufs):
        deps = {}
        for b in bufs:
            for s, v in b.w.items():
                deps[s] = max(deps.get(s, 0), v)
        self._wait(self.sp, deps)
        for e in (self.pe, self.act, self.dve, self.pool):
            if e.cnt:
                self.sp.h.wait_ge(e.sem, e.cnt)
        for s, v in self.totals.items():
            if v and self.sp.waited.get(s, 0) < v:
                self.sp.h.wait_ge(self.sems[s], v)
        self.es.close()


D = 1024
KC = 8
SEQ = 4096
CTX = 256
T = SEQ + CTX
NT = T // 128
PROJ_W = 3856
FF_DENSE = 2816
FF_EXP = 3584
NEXP = 8
EPS = 1e-6
C_ML_Q, C_ML_K, C_ML_V, C_ML_O, C_ML_G = 0, 256, 512, 768, 1024
C_HG_Q, C_HG_F, C_HG_I, C_HG_G = 1040, 1296, 1808, 2064
C_DA_Q, C_DA_K, C_DA_V = 2320, 2832, 3344


def tok_blocks(n, bs=512):
    out = []
    o = 0
    while o < n:
        out.append((o, min(bs, n - o)))
        o += bs
    return out


class Prog:
    def __init__(self, dbg=None):
        self.k = K()
        self.dbg = dbg or {}
        self.ext = {}

    def ext_in(self, name, shape, dt=F32):
        b = self.k.dram(name, shape, dt, kind="ExternalInput")
        self.ext[name] = b
        return b

    def ext_out(self, name, shape, dt=F32):
        b = self.k.dram(name, shape, dt, kind="ExternalOutput")
        self.ext[name] = b
        return b


def stage_mod(P, l):
    k = P.k
    with k.scope():
        cT = k.sb("cT", [128, KC, 2], F32)
        for kk in range(KC):
            k.dma(cT, cT[:, kk, :], P.c2, P.c2.t[:, kk * 128:(kk + 1) * 128].rearrange("r p -> p r"),
                  allow_slow_non_contiguous=True)
        k.op(k.act, lambda h: h.activation(out=cT[:], in_=cT[:], func=AF.Silu), [cT], [cT])
        modv = k.sb("modv", [2, 6, 1024], F32)
        bm = k.sb("bm", [2, 6, 1024], F32)
        k.dma(bm, bm[:], P.b_mod,
              P.b_mod.t[l:l + 1, :].rearrange("o (s d) -> o s d", s=6).to_broadcast([2, 6, 1024]))
        nrm = k.sb("nrm", [2, 4, 1024], F32)
        for j, nb in enumerate((P.norm_pre_mix, P.norm_post_mix, P.norm_pre_ffn, P.norm_post_ffn)):
            k.dma(nrm, nrm[:, j, :], nb, nb.t[l:l + 1, :].to_broadcast([2, 1024]))
        wst = [k.sb("wst%d" % i, [128, KC, 512], F32) for i in range(4)]
        pm = [k.ps("pm%d" % i, [2, 512]) for i in range(4)]
        for cb in range(12):
            w = wst[cb % 4]
            p = pm[cb % 4]
            k.dma(w, w[:], P.w_mod, P.w_mod.t[l, :, cb * 512:(cb + 1) * 512].rearrange("(k p) n -> p k n", p=128))
            k.mm([(lambda h, kk=kk: h.matmul(p[:], lhsT=cT[:, kk, :], rhs=w[:, kk, :],
                                             start=(kk == 0), stop=(kk == KC - 1))) for kk in range(KC)],
                 [cT, w], [p])
            s, o = divmod(cb * 512, 1024)
            k.op(k.dve, lambda h: h.tensor_tensor(out=modv[:, s, o:o + 512], in0=p[:], in1=bm[:, s, o:o + 512],
                                                  op=ALU.add), [p, bm], [modv])
        modc = k.sb("modc", [2, 6, 1024], F32)
        for (dst, msc, nj) in ((0, 1, 0), (3, 4, 2)):
            k.op(k.dve, lambda h: h.scalar_tensor_tensor(out=modc[:, dst, :], in0=modv[:, msc, :], scalar=1.0,
                                                         in1=nrm[:, nj, :], op0=ALU.add, op1=ALU.mult),
                 [modv, nrm], [modc])
        for (dst, msh) in ((1, 0), (4, 3)):
            k.op(k.dve, lambda h: h.tensor_copy(out=modc[:, dst, :], in_=modv[:, msh, :]), [modv], [modc])
        for (dst, mg, nj) in ((2, 2, 1), (5, 5, 3)):
            k.op(k.dve, lambda h: h.tensor_tensor(out=modc[:, dst, :], in0=modv[:, mg, :], in1=nrm[:, nj, :],
                                                  op=ALU.mult), [modv, nrm], [modc])
        k.dma(P.modc[l], P.modc[l].t[:, :, :], modc, modc[:])


def stage_norm(P, src, l, which, hxT, tiles, all_latent=False):
    k = P.k
    gi, si = (0, 1) if which == 0 else (3, 4)
    with k.scope():
        gb = k.sb("gb", [128, 2, 1024], F32)
        shb = k.sb("shb", [128, 2, 1024], F32)
        for kind in range(2):
            k.dma(gb, gb[:, kind, :], P.modc[l], P.modc[l].t[kind:kind + 1, gi, :].to_broadcast([128, 1024]))
            k.dma(shb, shb[:, kind, :], P.modc[l], P.modc[l].t[kind:kind + 1, si, :].to_broadcast([128, 1024]))
        xt = [k.sb("xt%d" % i, [128, 1024], F32) for i in range(2)]
        junk = k.sb("junk", [128, 1024], BF16)
        ss = [k.sb("ss%d" % i, [128, 2], F32) for i in range(2)]
        yf = k.sb("yf", [128, 1024], F32)
        yb = [k.sb("yb%d" % i, [128, 1024], BF16) for i in range(2)]
        pt = [k.ps("pt%d" % i, [128, KC, 128], BF16) for i in range(2)]
        for n, i in enumerate(tiles):
            kind = 1 if (i < 2 and not all_latent) else 0
            x, s, y, p = xt[n % 2], ss[n % 2], yb[n % 2], pt[n % 2]
            k.dma(x, x[:], src, src.t[i * 128:(i + 1) * 128, :])
            k.op(k.dve, lambda h: h.memset(s[:], 0.0), [], [s])
            k.op(k.act, lambda h: h.activation(out=junk[:], in_=x[:], func=AF.Square, accum_out=s[:, 0:1]),
                 [x, s], [junk, s])
            k.op(k.dve, lambda h: h.tensor_scalar(out=s[:, 1:2], in0=s[:, 0:1], scalar1=1.0 / D, scalar2=EPS,
                                                  op0=ALU.mult, op1=ALU.add), [s], [s])
            k.op(k.act, lambda h: h.sqrt(out=s[:, 1:2], in_=s[:, 1:2]), [s], [s])
            k.op(k.dve, lambda h: h.reciprocal(out=s[:, 1:2], in_=s[:, 1:2]), [s], [s])
            k.op(k.dve, lambda h: h.scalar_tensor_tensor(out=yf[:], in0=x[:], scalar=s[:, 1:2], in1=gb[:, kind, :],
                                                         op0=ALU.mult, op1=ALU.mult), [x, s, gb], [yf])
            k.op(k.dve, lambda h: h.tensor_tensor(out=y[:], in0=yf[:], in1=shb[:, kind, :], op=ALU.add),
                 [yf, shb], [y])
            k.mm([(lambda h, kk=kk: h.transpose(out=p[:, kk, :], in_=y[:, kk * 128:(kk + 1) * 128],
                                                identity=P.ident[:])) for kk in range(KC)], [y, P.ident], [p])
            k.op(k.act, lambda h: h.copy(out=hxT[:, :, n * 128:(n + 1) * 128], in_=p[:]), [p], [hxT])


def setup_consts(P):
    k = P.k
    idf = k.sb("idf", [128, 128], F32)
    k.dma(idf, idf[:], P.ext["ident"], P.ext["ident"].t[:, :])
    P.ident = k.sb("ident", [128, 128], BF16)
    k.op(k.dve, lambda h: h.tensor_copy(out=P.ident[:], in_=idf[:]), [idf], [P.ident])


class WLoader:
    def __init__(self, k, nbuf=2, width=512, kc=KC, name="wl"):
        self.k = k
        self.kc = kc
        self.wb = [k.sb(name + "b%d" % i, [128, kc, width], BF16) for i in range(nbuf)]
        self.i = 0
        self.nbuf = nbuf

    def get(self, wbuf, w_ap, n):
        k = self.k
        wb = self.wb[self.i % self.nbuf]
        self.i += 1
        k.dma(wb, wb[:, :, :n], wbuf, w_ap.rearrange("(k p) n -> p k n", p=128), q=k.pool)
        return wb


def lin_fm(k, wb, ncol, src, ntok, evac, ps_pool, kc=KC, bs=512):
    for bi, (t0, n) in enumerate(tok_blocks(ntok, bs)):
        p = ps_pool[bi % len(ps_pool)]
        k.mm([(lambda h, kk=kk: h.matmul(p[:ncol, :n], lhsT=wb[:, kk, :ncol], rhs=src[:, kk, t0:t0 + n],
                                         start=(kk == 0), stop=(kk == kc - 1))) for kk in range(kc)],
             [wb, src], [p])
        evac(p, t0, n)


def lin_tm(k, wb, ncol, src, tiles, evac, ps_pool, kc=KC):
    for ti, i in enumerate(tiles):
        p = ps_pool[ti % len(ps_pool)]
        k.mm([(lambda h, kk=kk: h.matmul(p[:, :ncol], lhsT=src[:, kk, i * 128:(i + 1) * 128], rhs=wb[:, kk, :ncol],
                                         start=(kk == 0), stop=(kk == kc - 1))) for kk in range(kc)],
             [wb, src], [p])
        evac(p, i)


def stage_attn(P, l, hxT, mixT, q0, nq, lam_init, heads=(0, 1, 2, 3), qblocks=None, mixT_d=None, own=False):
    k = P.k
    w_in = P.w_in
    with k.scope():
        wl = WLoader(k, nbuf=2, width=128, name="aw")
        ropeC = k.sb("ropeC", [128, T], BF16)
        ropeS = k.sb("ropeS", [128, T], BF16)
        k.dma(ropeC, ropeC[:], P.ext["rope_c"], P.ext["rope_c"].t[:, :])
        k.dma(ropeS, ropeS[:], P.ext["rope_s"], P.ext["rope_s"].t[:, :])
        lp = k.sb("lp", [128, 4, 64], F32)
        k.dma(lp, lp[:], P.da_lambda, P.da_lambda.t[l:l + 1, :, :].to_broadcast([128, 4, 64]))
        lt = k.sb("lt", [128, 2, 64], F32)
        lam = k.sb("lam", [128, 4], F32)
        k.op(k.dve, lambda h: h.tensor_tensor(out=lt[:, 0, :], in0=lp[:, 0, :], in1=lp[:, 1, :], op=ALU.mult), [lp], [lt])
        k.op(k.dve, lambda h: h.tensor_tensor(out=lt[:, 1, :], in0=lp[:, 2, :], in1=lp[:, 3, :], op=ALU.mult), [lp], [lt])
        k.op(k.dve, lambda h: h.reduce_sum(out=lam[:, 0:2], in_=lt[:], axis=AX.X), [lt], [lam])
        k.op(k.act, lambda h: h.activation(out=lam[:, 0:2], in_=lam[:, 0:2], func=AF.Exp), [lam], [lam])
        k.op(k.dve, lambda h: h.tensor_tensor(out=lam[:, 2:3], in0=lam[:, 1:2], in1=lam[:, 0:1], op=ALU.subtract), [lam], [lam])
        k.op(k.dve, lambda h: h.tensor_scalar(out=lam[:, 3:4], in0=lam[:, 2:3], scalar1=-float(lam_init), scalar2=None,
                                              op0=ALU.add), [lam], [lam])
        dn = k.sb("dn", [128, 128], F32)
        k.dma(dn, dn[:], P.da_norm, P.da_norm.t[l:l + 1, :].to_broadcast([128, 128]))
        k.op(k.dve, lambda h: h.tensor_scalar(out=dn[:], in0=dn[:], scalar1=float(1.0 - lam_init), scalar2=None,
                                              op0=ALU.mult), [dn], [dn])

        qT = k.sb("qT", [128, T], BF16)
        kT = k.sb("kT", [128, T], BF16)
        if own:
            qTo = k.sb("qTo", [128, SEQ // 2], BF16)
            selt = k.sb("selt", [128, 2], F32)
            k.dma(selt, selt[:], P.ext["sel"], P.ext["sel"].t[:, :])
        va = k.sb("va", [128, NT, 132], BF16)
        k.op(k.dve, lambda h: h.memset(va[:, :, 128:132], 1.0), [], [va])
        t1 = k.sb("t1", [128, 512], F32)
        t2 = k.sb("t2", [128, 512], F32)
        sTb = [k.ps("sTb%d" % j, [128, 2, 512]) for j in range(2)]
        osbs = [k.sb("osb%d" % i, [128, 3, 3, 132], F32) for i in range(2)]
        blk_i = [0]
        oacc = [k.ps("oacc%d" % i, [128, 3, 132]) for i in range(3)]
        pTb = [k.sb("pTb%d" % j, [128, 2, 512], BF16) for j in range(2)]
        fin = k.sb("fin", [128, 8], F32)
        o0 = k.sb("o0", [128, 128], F32)
        o1 = k.sb("o1", [128, 128], F32)
        junk = k.sb("ajunk", [128, 128], F32)
        ob = k.sb("ob", [128, 128], BF16)
        ptr = k.ps("ptr", [128, 128], BF16)
        ost = k.sb("ost", [128, 512], BF16)
        nkc = NT
        for hd in heads:
            for (dst, c_main, c_sw) in ((qT, C_DA_Q + hd * 128, hd * 128), (kT, C_DA_K + hd * 128, 512 + hd * 128)):
                wm = wl.get(w_in, w_in.t[l, :, c_main:c_main + 128], 128)
                ws = wl.get(P.ext["w_da_sw"], P.ext["w_da_sw"].t[l, :, c_sw:c_sw + 128], 128)
                for bi, (t0, n) in enumerate(tok_blocks(T)):
                    pab = sTb[bi % 2]
                    k.mm([(lambda h, kk=kk: h.matmul(pab[:, 0, :n], lhsT=wm[:, kk, :128], rhs=hxT[:, kk, t0:t0 + n],
                                                     start=(kk == 0), stop=(kk == KC - 1))) for kk in range(KC)],
                         [wm, hxT], [pab])
                    k.mm([(lambda h, kk=kk: h.matmul(pab[:, 1, :n], lhsT=ws[:, kk, :128], rhs=hxT[:, kk, t0:t0 + n],
                                                     start=(kk == 0), stop=(kk == KC - 1))) for kk in range(KC)],
                         [ws, hxT], [pab])
                    k.op(k.dve, lambda h: h.tensor_tensor(out=t1[:, :n], in0=pab[:, 0, :n], in1=ropeC[:, t0:t0 + n],
                                                          op=ALU.mult), [pab, ropeC], [t1])
                    k.op(k.dve, lambda h: h.tensor_tensor(out=t2[:, :n], in0=pab[:, 1, :n], in1=ropeS[:, t0:t0 + n],
                                                          op=ALU.mult), [pab, ropeS], [t2])
                    k.op(k.dve, lambda h: h.tensor_tensor(out=dst[:, t0:t0 + n], in0=t1[:, :n], in1=t2[:, :n],
                                                          op=ALU.add), [t1, t2], [dst])
            wv = wl.get(w_in, w_in.t[l, :, C_DA_V + hd * 128:C_DA_V + (hd + 1) * 128], 128)
            for i in range(NT):
                pa = sTb[i % 2]
                k.mm([(lambda h, kk=kk: h.matmul(pa[:, 0, :128], lhsT=hxT[:, kk, i * 128:(i + 1) * 128], rhs=wv[:, kk, :128],
                                                 start=(kk == 0), stop=(kk == KC - 1))) for kk in range(KC)],
                     [wv, hxT], [pa])
                k.op(k.act, lambda h: h.copy(out=va[:, i, 0:128], in_=pa[:, 0, :128]), [pa], [va])
            qsrc = qT
            if own:
                h0, h1 = CTX, CTX + SEQ // 2
                k.op(k.dve, lambda h: h.tensor_scalar(out=qTo[:], in0=qT[:, h0:h1], scalar1=selt[:, 0:1], scalar2=None,
                                                      op0=ALU.mult), [qT, selt], [qTo])
                k.op(k.dve, lambda h: h.scalar_tensor_tensor(out=qTo[:], in0=qT[:, h1:T], scalar=selt[:, 1:2], in1=qTo[:],
                                                             op0=ALU.mult, op1=ALU.add), [qT, selt, qTo], [qTo])
                qsrc = qTo
            if qblocks is None:
                qblocks = [(q0 + qb * 512, 512, list(range(NT))) for qb in range(nq // 512)]
            for (qs, nqb, ktiles) in qblocks:
                nqt = nqb // 128
                nkc = len(ktiles)
                glist = [c_ * 4 + q_ for c_ in range(2) for q_ in range(nqt)]

                def emit_S(kci):
                    kc = ktiles[kci]
                    sp_ = sTb[kci % 2]
                    fns = []
                    for comp in range(2):
                        r0 = comp * 64
                        fns.append(lambda h, comp=comp, r0=r0: h.matmul(sp_[:, comp, :nqb], lhsT=kT[r0:r0 + 64, kc * 128:(kc + 1) * 128],
                                                                        rhs=qsrc[r0:r0 + 64, qs:qs + nqb], start=True, stop=True))
                    k.mm(fns, [kT, qsrc], [sp_])

                def emit_exp(kci):
                    sp_ = sTb[kci % 2]
                    pt_ = pTb[kci % 2]
                    k.op(k.act, lambda h: h.activation(out=pt_[:, :, :nqb], in_=sp_[:, :, :nqb], func=AF.Exp, scale=0.125),
                         [sp_], [pt_])

                def emit_PV(kci):
                    kc = ktiles[kci]
                    pt_ = pTb[kci % 2]
                    for comp in range(2):
                        fns = []
                        for qt in range(nqt):
                            g = comp * 4 + qt
                            ot = oacc[g // 3]
                            fns.append(lambda h, ot=ot, g=g, qt=qt, comp=comp: h.matmul(
                                ot[:, g % 3, 0:129], lhsT=pt_[:, comp, qt * 128:(qt + 1) * 128], rhs=va[:, kc, 0:129],
                                start=(kci == 0 and g == min(x for x in glist if x // 3 == g // 3)), stop=(kci == nkc - 1),
                                skip_group_check=True))
                        k.mm(fns, [pt_, va], oacc)

                emit_S(0)
                emit_exp(0)
                for kci in range(nkc):
                    if kci + 1 < nkc:
                        emit_S(kci + 1)
                    emit_PV(kci)
                    if kci + 1 < nkc:
                        emit_exp(kci + 1)
                osb = osbs[blk_i[0] % 2]
                blk_i[0] += 1
                for j in range(3):
                    k.op(k.dve, lambda h: h.tensor_copy(out=osb[:, j, :, :], in_=oacc[j][:]), [oacc[j]], [osb])
                for qt in range(nqt):
                    g0, g1 = qt, 4 + qt
                    k.op(k.dve, lambda h: h.reciprocal(out=fin[:, 0:1], in_=osb[:, g0 // 3, g0 % 3, 128:129]), [osb], [fin])
                    k.op(k.dve, lambda h: h.reciprocal(out=fin[:, 1:2], in_=osb[:, g1 // 3, g1 % 3, 128:129]), [osb], [fin])
                    k.op(k.dve, lambda h: h.tensor_tensor(out=fin[:, 1:2], in0=fin[:, 1:2], in1=lam[:, 3:4], op=ALU.mult),
                         [fin, lam], [fin])
                    k.op(k.dve, lambda h: h.tensor_scalar(out=o0[:], in0=osb[:, g0 // 3, g0 % 3, 0:128], scalar1=fin[:, 0:1],
                                                          scalar2=None, op0=ALU.mult), [osb, fin], [o0])
                    k.op(k.dve, lambda h: h.scalar_tensor_tensor(out=o1[:], in0=osb[:, g1 // 3, g1 % 3, 0:128], scalar=fin[:, 1:2],
                                                                 in1=o0[:], op0=ALU.mult, op1=ALU.add),
                         [osb, fin, o0], [o1])
                    k.op(k.dve, lambda h: h.tensor_tensor(out=junk[:], in0=o1[:], in1=o1[:], op=ALU.mult), [o1], [junk])
                    k.op(k.dve, lambda h: h.reduce_sum(out=fin[:, 2:3], in_=junk[:], axis=AX.X), [junk], [fin])
                    k.op(k.dve, lambda h: h.tensor_scalar(out=fin[:, 3:4], in0=fin[:, 2:3], scalar1=1.0 / 128, scalar2=EPS,
                                                          op0=ALU.mult, op1=ALU.add), [fin], [fin])
                    k.op(k.act, lambda h: h.sqrt(out=fin[:, 3:4], in_=fin[:, 3:4]), [fin], [fin])
                    k.op(k.dve, lambda h: h.reciprocal(out=fin[:, 3:4], in_=fin[:, 3:4]), [fin], [fin])
                    k.op(k.dve, lambda h: h.scalar_tensor_tensor(out=ob[:], in0=o1[:], scalar=fin[:, 3:4], in1=dn[:],
                                                                 op0=ALU.mult, op1=ALU.mult), [o1, fin, dn], [ob])
                    k.mm([lambda h: h.transpose(out=ptr[:], in_=ob[:], identity=P.ident[:])], [ob, P.ident], [ptr])
                    c0 = qs + qt * 128
                    if mixT_d is None:
                        k.op(k.dve, lambda h: h.tensor_copy(out=mixT[:, 4 + hd, c0:c0 + 128], in_=ptr[:]), [ptr], [mixT])
                    else:
                        k.op(k.dve, lambda h: h.tensor_copy(out=ost[:, qt * 128:(qt + 1) * 128], in_=ptr[:]), [ptr], [ost])
                if mixT_d is not None:
                    r0_ = 512 + hd * 128
                    k.dma(mixT_d, mixT_d.t[r0_:r0_ + 128, qs:qs + nqb], ost, ost[:, :nqb])


def rope_tables():
    n = np.arange(SEQ)
    row = (n // 64).astype(np.float64)
    col = (n % 64).astype(np.float64)
    inv = 10000.0 ** (-np.arange(16, dtype=np.float64) / 16)
    C = np.ones((64, T), np.float64)
    S = np.zeros((64, T), np.float64)
    for d in range(64):
        pos = row if d < 32 else col
        f = d % 16
        ang = pos * inv[f]
        C[d, CTX:] = np.cos(ang)
        sgn = -1.0 if (d % 32) < 16 else 1.0
        S[d, CTX:] = sgn * np.sin(ang)
    import ml_dtypes
    C2 = np.concatenate([C, C], 0).astype(np.float32).astype(ml_dtypes.bfloat16)
    S2 = np.concatenate([S, S], 0).astype(np.float32).astype(ml_dtypes.bfloat16)
    return C2, S2


def swap_cols(w_in):
    perm64 = np.concatenate([np.arange(16, 32), np.arange(0, 16), np.arange(48, 64), np.arange(32, 48)])
    idx = []
    for base in (C_DA_Q, C_DA_K):
        for u in range(8):
            idx.append(base + u * 64 + perm64)
    idx = np.concatenate(idx)
    return np.ascontiguousarray(w_in[:, :, idx])


def stage_proj_rec(P, l, hxT):
    k = P.k
    w_in = P.w_in
    R = P.rec
    with k.scope():
        wl = WLoader(k, nbuf=2, width=128, name="pw")
        pp = [k.ps("rp%d" % i, [128, 512]) for i in range(3)]
        stf = [k.sb("stf%d" % i, [128, 512], F32) for i in range(2)]
        stb = [k.sb("stb%d" % i, [128, 512], BF16) for i in range(2)]
        gb = k.sb("gbias", [128, 8], F32)
        for ty in range(4):
            for hp in range(2):
                for hh in range(2):
                    c = ty * 4 + hp * 2 + hh
                    k.dma(gb, gb[hh * 64:(hh + 1) * 64, ty * 2 + hp:ty * 2 + hp + 1], P.ml_gate_b,
                          P.ml_gate_b.t[l:l + 1, c:c + 1].to_broadcast([64, 1]))
        cnt = [0]

        def fm_job(wb, dst, row0, func=None, scale=1.0, bias=None, f32=False):
            def evac(p, t0, n):
                i = cnt[0] % 2
                cnt[0] += 1
                st = stf[i] if f32 else stb[i]
                if func is None and bias is None:
                    k.op(k.dve, lambda h: h.tensor_scalar(out=st[:, :n], in0=p[:, :n], scalar1=float(scale), scalar2=None,
                                                          op0=ALU.mult), [p], [st])
                elif func is None:
                    k.op(k.dve, lambda h: h.tensor_scalar(out=st[:, :n], in0=p[:, :n], scalar1=bias, scalar2=None,
                                                          op0=ALU.add), [p, gb], [st])
                else:
                    k.op(k.act, lambda h: h.activation(out=st[:, :n], in_=p[:, :n], func=func), [p], [st])
                k.dma(dst, dst.t[row0:row0 + 128, t0:t0 + n], st, st[:, :n])
            lin_fm(k, wb, 128, hxT, T, evac, pp)

        for c in range(2):
            fm_job(wl.get(w_in, w_in.t[l, :, C_ML_Q + c * 128:C_ML_Q + (c + 1) * 128], 128), R["ml_q"], c * 128)
            fm_job(wl.get(w_in, w_in.t[l, :, C_ML_K + c * 128:C_ML_K + (c + 1) * 128], 128), R["ml_k"], c * 128, scale=0.125)
            fm_job(wl.get(w_in, w_in.t[l, :, C_ML_O + c * 128:C_ML_O + (c + 1) * 128], 128), R["ml_o"], c * 128, func=AF.Sigmoid)
            fm_job(wl.get(w_in, w_in.t[l, :, C_HG_Q + c * 128:C_HG_Q + (c + 1) * 128], 128), R["hg_q"], c * 128, func=AF.Silu)
            fm_job(wl.get(w_in, w_in.t[l, :, C_HG_G + c * 128:C_HG_G + (c + 1) * 128], 128), R["hg_g"], c * 128, func=AF.Silu)
        for c in range(4):
            fm_job(wl.get(w_in, w_in.t[l, :, C_HG_F + c * 128:C_HG_F + (c + 1) * 128], 128), R["hg_f"], c * 128, f32=True)
        g16s = k.sb("g16s", [128, KC, 16], F32)
        k.dma(g16s, g16s[:], w_in, w_in.t[l, :, C_ML_G:C_ML_G + 16].rearrange("(k p) n -> p k n", p=128))
        wrep = [k.sb("wrep%d" % i, [128, KC, 128], BF16) for i in range(2)]
        for ty in range(4):
            for hp in range(2):
                wr = wrep[(ty * 2 + hp) % 2]
                for hh in range(2):
                    c = ty * 4 + hp * 2 + hh
                    k.op(k.dve, lambda h: h.tensor_copy(out=wr[:, :, hh * 64:(hh + 1) * 64],
                                                        in_=g16s[:, :, c:c + 1].to_broadcast([128, KC, 64])), [g16s], [wr])
                fm_job(wr, R["ml_g"], (ty * 2 + hp) * 128, bias=gb[:, ty * 2 + hp:ty * 2 + hp + 1], f32=True)
        for (c0, dst) in ((C_ML_V, R["ml_v"]), (C_HG_I, R["hg_v"])):
            wv = k.sb("wv", [128, KC, 256], BF16)
            for c in range(2):
                wb = wl.get(w_in, w_in.t[l, :, c0 + c * 128:c0 + (c + 1) * 128], 128)
                k.op(k.dve, lambda h: h.tensor_copy(out=wv[:, :, c * 128:(c + 1) * 128], in_=wb[:, :, :128]), [wb], [wv])

            def evac_v(p, i, dst=dst):
                j = cnt[0] % 2
                cnt[0] += 1
                st = stb[j]
                k.op(k.act, lambda h: h.copy(out=st[:, :256], in_=p[:, :256]), [p], [st])
                k.dma(dst, dst.t[i * 128:(i + 1) * 128, :], st, st[:, :256])
            lin_tm(k, wv, 256, hxT, list(range(NT)), evac_v, pp)


NCH = T // 16
SEGS = ((0, CTX), (CTX, T))


def seg_view(ap2d, si, dr):
    a, b = SEGS[si]
    if dr == 0:
        return ap2d[:, a:b]
    if a == 0:
        return ap2d[:, b - 1::-1]
    return ap2d[:, b - 1:a - 1:-1]


def stage_rec(P, l, mixer, mixT_d):
    k = P.k
    R = P.rec
    ml = (mixer == "ml")
    nv = 2 if ml else 1
    row_off = 0 if ml else 256
    with k.scope():
        cst = P.ext
        maskb = k.sb("maskb", [128, 2, 128], BF16)
        k.dma(maskb, maskb[:], cst["cmask"], cst["cmask"].t[:, :, :])
        bm = k.sb("bm", [128, 8], BF16)
        k.dma(bm, bm[:], cst["bmask"], cst["bmask"].t[:, :])
        blk = k.sb("blk", [128, 128], BF16)
        k.dma(blk, blk[:], cst["blk"], cst["blk"].t[:, :])
        if not ml:
            rmask = k.sb("rmask", [128, T], F32)
            k.op(k.dve, lambda h: h.memset(rmask[:], 1.0), [], [rmask])
            k.op(k.dve, lambda h: h.memset(rmask[:].rearrange("p (c i) -> p c i", i=16)[:, :, 0], 0.0), [], [rmask])
        zer = k.sb("zer", [128, 1], F32)
        k.op(k.dve, lambda h: h.memset(zer[:], 0.0), [], [zer])
        onesT = k.sb("onesT", [128, 64], BF16)
        k.op(k.dve, lambda h: h.memset(onesT[:], 1.0), [], [onesT])
        vones = k.sb("vones", [128, 8, 64], BF16)
        k.op(k.dve, lambda h: h.tensor_copy(out=vones[:], in_=bm[:].unsqueeze(2).to_broadcast([128, 8, 64])), [bm], [vones])

        qAs = k.sb("qA", [128, T], BF16) if ml else None
        qAd = [qAs, qAs] if ml else [k.sb("qA%d" % i, [128, T], BF16) for i in range(2)]
        kAd = [k.sb("kA%d" % i, [128, T], BF16) for i in range(2)]
        x3d = [k.sb("x3%d" % i, [128, T], BF16) for i in range(2)]
        cld = [k.sb("cl%d" % i, [128, T], BF16) for i in range(2)] if ml else None
        tA = k.sb("tA", [128, T], F32)
        tB = k.sb("tB", [128, T], F32)
        tC = k.sb("tC", [128, T], F32)
        osum = k.sb("osum", [128, T], F32)
        gate = k.sb("gate", [128, T], BF16)
        vt = k.sb("vt", [128, NT, 128], BF16)
        Rj = k.sb("Rj", [128, NCH], F32)
        Rp = k.sb("Rp", [128, NCH], F32)
        Dcd = [k.sb("Dc%d" % i, [128, NCH], F32) for i in range(2)]
        sc = k.sb("sc", [128, 8], F32)
        nw = k.sb("nw", [128, 1], F32)

        bankA = [k.ps("bkA%d" % i, [128, 512]) for i in range(2)]
        pdSd = [[k.ps("pdS%d_%d" % (i, j), [128, 8, 64]) for j in range(nv)] for i in range(2)]
        pktd = [k.ps("pkt%d" % i, [128, 128], BF16) for i in range(2)]
        AmTd = [[k.sb("AmT%d_%d" % (i, j), [128, 2, 128], BF16) for j in range(2)] for i in range(2)]
        qzd = [[k.sb("qz%d_%d" % (i, j), [128, 2, 128], BF16) for j in range(2)] for i in range(2)]
        for i in range(2):
            for j in range(2):
                k.op(k.dve, lambda h: h.memset(qzd[i][j][:], 0.0), [], [qzd[i][j]])
        kStd = [k.sb("kSt%d" % i, [128, 128], BF16) for i in range(2)]
        Vexpd = [k.sb("Vexp%d" % i, [128, 2, 8, 64], BF16) for i in range(2)]
        Shd = [[k.sb("Sh%d_%d" % (i, j), [128, 9, 64], F32) for j in range(nv)] for i in range(2)]
        Sbd = [[k.sb("Sb%d_%d" % (i, j), [128, 8, 64], BF16) for j in range(nv)] for i in range(2)]
        nsb = [[k.sb("nsb%d_%d" % (i, j), [128, 128], F32) for j in range(2)] for i in range(2)]
        dsb = [[k.sb("dsb%d_%d" % (i, j), [128, 128], F32) for j in range(2)] for i in range(2)] if ml else None
        fq = k.sb("fq", [128, 512], BF16)
        fr_ = k.sb("fr_", [128, 512], F32)
        fy = k.sb("fy", [128, 512], F32)
        fo = [k.sb("fo%d" % i, [128, 512], BF16) for i in range(2)]

        def c16(v):
            return v.rearrange("p (c i) -> p c i", i=16)

        def tile_of(ip, dr):
            if ip < 2:
                return ip if dr == 0 else 1 - ip
            return ip if dr == 0 else (NT - 1) - (ip - 2)

        for hp in range(2):
            rows = slice(hp * 128, (hp + 1) * 128)
            src_v = R["ml_v"] if ml else R["hg_v"]
            for i_ in range(NT):
                k.dma(vt, vt[:, i_, :], src_v, src_v.t[i_ * 128:(i_ + 1) * 128, hp * 128:(hp + 1) * 128])
            src_g = R["ml_o"] if ml else R["hg_g"]
            k.dma(gate, gate[:], src_g, src_g.t[rows, :])
            nsrc = P.ml_norm if ml else P.hg_norm
            k.dma(nw, nw[:], nsrc, nsrc.t[l:l + 1, rows].rearrange("o p -> p o"), allow_slow_non_contiguous=True)
            k.op(k.pool, lambda h: h.memset(osum[:], 0.0), [], [osum])
            if not ml:
                if l == 0:
                    k.op(k.dve, lambda h: h.memset(sc[:, 0:1], 0.0), [], [sc])
                else:
                    lg = P.hg_lb_logits
                    k.dma(sc, sc[:, 4:5], lg, lg.t[0:1, rows].rearrange("o p -> p o"), allow_slow_non_contiguous=True)
                    k.dma(sc, sc[:, 5:6], lg, lg.t[1:2, rows].rearrange("o p -> p o"), allow_slow_non_contiguous=True)
                    k.op(k.dve, lambda h: h.tensor_tensor(out=sc[:, 6:7], in0=sc[:, 5:6], in1=sc[:, 4:5], op=ALU.subtract), [sc], [sc])
                    k.op(k.act, lambda h: h.activation(out=sc[:, 0:1], in_=sc[:, 6:7], func=AF.Sigmoid), [sc], [sc])
                k.op(k.dve, lambda h: h.tensor_scalar(out=sc[:, 1:2], in0=sc[:, 0:1], scalar1=-1.0, scalar2=1.0,
                                                      op0=ALU.mult, op1=ALU.add), [sc], [sc])
                k.op(k.dve, lambda h: h.tensor_scalar(out=sc[:, 2:3], in0=sc[:, 1:2], scalar1=-1.0, scalar2=None,
                                                      op0=ALU.mult), [sc], [sc])
            if ml:
                k.dma(qAs, qAs[:], R["ml_q"], R["ml_q"].t[rows, :])
            for dr in range(2):
                qA, kA, x3, Dc = qAd[dr], kAd[dr], x3d[dr], Dcd[dr]
                if ml:
                    k.dma(kA, kA[:], R["ml_k"], R["ml_k"].t[rows, :])
                    gi0 = ((dr * 2) * 2 + hp) * 128
                    gf0 = ((dr * 2 + 1) * 2 + hp) * 128
                    k.dma(tA, tA[:], R["ml_g"], R["ml_g"].t[gi0:gi0 + 128, :])
                    k.dma(tB, tB[:], R["ml_g"], R["ml_g"].t[gf0:gf0 + 128, :])
                    k.op(k.act, lambda h: h.activation(out=tB[:], in_=tB[:], func=AF.Exp, scale=-1.0), [tB], [tB])
                    k.op(k.act, lambda h: h.activation(out=tB[:], in_=tB[:], func=AF.Ln, bias=1.0), [tB], [tB])
                    for si in range(2):
                        ini = 0.0 if si == 0 else seg_view(tC[:], 0, dr)[:, CTX - 1:CTX]
                        ov, dv = seg_view(tC[:], si, dr), seg_view(tB[:], si, dr)
                        n = SEGS[si][1] - SEGS[si][0]
                        k.op(k.dve, lambda h: h.tensor_tensor_scan(out=ov, data0=dv, data1=zer[:].to_broadcast([128, n]),
                                                                   initial=ini, op0=ALU.add, op1=ALU.add), [tB, tC, zer], [tC])
                    k.op(k.dve, lambda h: h.tensor_tensor(out=tA[:], in0=tA[:], in1=tC[:], op=ALU.add), [tA, tC], [tA])
                    for si in range(2):
                        ini = 0.0 if si == 0 else seg_view(tB[:], 0, dr)[:, CTX - 1:CTX]
                        ov, dv = seg_view(tB[:], si, dr), seg_view(tA[:], si, dr)
                        k.op(k.dve, lambda h: h.tensor_tensor_scan(out=ov, data0=dv, data1=dv, initial=ini,
                                                                   op0=ALU.max, op1=ALU.max), [tA, tB], [tB])
                    for si in range(2):
                        j0, j1 = SEGS[si][0] // 16, SEGS[si][1] // 16
                        k.op(k.dve, lambda h: h.tensor_copy(out=Rj[:, j0:j1], in_=c16(seg_view(tB[:], si, dr))[:, :, 15]),
                             [tB], [Rj])
                    k.op(k.dve, lambda h: h.memset(Rp[:, 0:1], 0.0), [], [Rp])
                    k.op(k.dve, lambda h: h.tensor_copy(out=Rp[:, 1:NCH], in_=Rj[:, 0:NCH - 1]), [Rj], [Rp])
                    k.op(k.dve, lambda h: h.tensor_tensor(out=Dc[:], in0=Rp[:], in1=Rj[:], op=ALU.subtract), [Rp, Rj], [Dc])
                    k.op(k.act, lambda h: h.activation(out=Dc[:], in_=Dc[:], func=AF.Exp), [Dc], [Dc])
                    for si in range(2):
                        j0, j1 = SEGS[si][0] // 16, SEGS[si][1] // 16
                        rb = Rj[:, j0:j1].unsqueeze(2).to_broadcast([128, j1 - j0, 16])
                        db = Dc[:, j0:j1].unsqueeze(2).to_broadcast([128, j1 - j0, 16])
                        av, cv = c16(seg_view(tA[:], si, dr)), c16(seg_view(tC[:], si, dr))
                        k.op(k.dve, lambda h: h.tensor_tensor(out=av, in0=av, in1=rb, op=ALU.subtract), [tA, Rj], [tA])
                        k.op(k.dve, lambda h: h.tensor_tensor(out=cv, in0=cv, in1=rb, op=ALU.subtract), [tC, Rj], [tC])
                        k.op(k.pool, lambda h: h.tensor_tensor(out=c16(seg_view(x3[:], si, dr)), in0=c16(seg_view(qA[:], si, dr)),
                                                               in1=db, op=ALU.mult), [qA, Dc], [x3])
                    k.op(k.act, lambda h: h.activation(out=tA[:], in_=tA[:], func=AF.Exp), [tA], [tA])
                    k.op(k.act, lambda h: h.activation(out=cld[dr][:], in_=tC[:], func=AF.Exp), [tC], [cld[dr]])
                    k.op(k.dve, lambda h: h.tensor_tensor(out=kA[:], in0=kA[:], in1=tA[:], op=ALU.mult), [kA, tA], [kA])
                else:
                    k.dma(qA, qA[:], R["hg_q"], R["hg_q"].t[rows, :])
                    f0 = dr * 256 + hp * 128
                    k.dma(tA, tA[:], R["hg_f"], R["hg_f"].t[f0:f0 + 128, :])
                    k.op(k.act, lambda h: h.activation(out=tB[:], in_=tA[:], func=AF.Sigmoid), [tA], [tB])
                    k.op(k.dve, lambda h: h.tensor_scalar(out=tC[:], in0=tB[:], scalar1=sc[:, 2:3], scalar2=sc[:, 1:2],
                                                          op0=ALU.mult, op1=ALU.add), [tB, sc], [tC])
                    k.op(k.dve, lambda h: h.tensor_scalar(out=tB[:], in0=tB[:], scalar1=sc[:, 1:2], scalar2=sc[:, 0:1],
                                                          op0=ALU.mult, op1=ALU.add), [tB, sc], [tB])
                    k.op(k.act, lambda h: h.activation(out=tB[:], in_=tB[:], func=AF.Ln), [tB], [tB])
                    for si in range(2):
                        j0, j1 = SEGS[si][0] // 16, SEGS[si][1] // 16
                        ov = c16(seg_view(tA[:], si, dr))
                        nseg = SEGS[si][1] - SEGS[si][0]
                        k.op(k.dve, lambda h: h.tensor_tensor_scan(out=seg_view(tA[:], si, dr), data0=rmask[:, 0:nseg],
                                                                   data1=seg_view(tB[:], si, dr), initial=0.0,
                                                                   op0=ALU.mult, op1=ALU.add), [tB, rmask, tA], [tA])
                        k.op(k.dve, lambda h: h.tensor_copy(out=Rj[:, j0:j1], in_=ov[:, :, 15]), [tA], [Rj])
                    k.op(k.act, lambda h: h.activation(out=Dc[:], in_=Rj[:], func=AF.Exp), [Rj], [Dc])
                    k.op(k.act, lambda h: h.activation(out=tB[:], in_=tA[:], func=AF.Exp), [tA], [tB])
                    k.op(k.dve, lambda h: h.tensor_tensor(out=qA[:], in0=qA[:], in1=tB[:], op=ALU.mult), [qA, tB], [qA])
                    k.op(k.act, lambda h: h.activation(out=tB[:], in_=tA[:], func=AF.Exp, scale=-1.0), [tA], [tB])
                    k.op(k.dve, lambda h: h.tensor_tensor(out=kA[:], in0=tC[:], in1=tB[:], op=ALU.mult), [tC, tB], [kA])
                    for si in range(2):
                        j0, j1 = SEGS[si][0] // 16, SEGS[si][1] // 16
                        rb = Rj[:, j0:j1].unsqueeze(2).to_broadcast([128, j1 - j0, 16])
                        av = c16(seg_view(tA[:], si, dr))
                        k.op(k.dve, lambda h: h.tensor_tensor(out=av, in0=rb, in1=av, op=ALU.subtract), [tA, Rj], [tA])
                    k.op(k.act, lambda h: h.activation(out=tA[:], in_=tA[:], func=AF.Exp), [tA], [tA])
                    k.op(k.dve, lambda h: h.tensor_tensor(out=x3[:], in0=tC[:], in1=tA[:], op=ALU.mult), [tC, tA], [x3])

            def qI_of(dr):
                return x3d[dr] if ml else qAd[dr]

            def kS_of(dr):
                return kAd[dr] if ml else x3d[dr]

            def pA_of(dr):
                return bankA[dr][:, 0:256].rearrange("p (h t) -> p h t", h=2)

            def po_of(dr, vs):
                return bankA[dr][:, 256 + vs * 128:256 + (vs + 1) * 128]

            def front_mm(ip):
                for dr in range(2):
                    c0 = tile_of(ip, dr) * 128
                    qz = qzd[dr][ip % 2]
                    for hh in range(2):
                        rr = slice(hh * 64, (hh + 1) * 64)
                        k.op(k.pool, lambda h: h.tensor_copy(out=qz[rr, hh, :], in_=qAd[dr][rr, c0:c0 + 128]), [qAd[dr]], [qz])
                    k.mm([lambda h: h.matmul(bankA[dr][:, 0:256], lhsT=kAd[dr][:, c0:c0 + 128],
                                             rhs=qz[:].rearrange("p h t -> p (h t)"), start=True, stop=True)],
                         [kAd[dr], qz], [bankA[dr]])
                    kS = kS_of(dr)
                    k.mm([lambda h: h.transpose(out=pktd[dr][:], in_=kS[:, c0:c0 + 128], identity=P.ident[:])],
                         [kS, P.ident], [pktd[dr]])

            def front_ev(ip):
                for dr in range(2):
                    it = tile_of(ip, dr)
                    AmT = AmTd[dr][ip % 2]
                    k.op(k.dve, lambda h: h.tensor_tensor(out=AmT[:], in0=pA_of(dr),
                                                          in1=maskb[:, dr:dr + 1, :].to_broadcast([128, 2, 128]),
                                                          op=ALU.mult), [bankA[dr], maskb], [AmT])
                    k.op(k.act, lambda h: h.copy(out=kStd[dr][:], in_=pktd[dr][:]), [pktd[dr]], [kStd[dr]])
                    k.op(k.pool, lambda h: h.tensor_tensor(
                        out=Vexpd[dr][:], in0=vt[:, it, :].rearrange("p (h e) -> p h e", h=2).unsqueeze(2).to_broadcast([128, 2, 8, 64]),
                        in1=bm[:].unsqueeze(1).unsqueeze(3).to_broadcast([128, 2, 8, 64]), op=ALU.mult), [vt, bm], [Vexpd[dr]])

            def dS_mm(ip):
                for dr in range(2):
                    for vs in range(nv):
                        for hh in range(2):
                            rr = slice(hh * 64, (hh + 1) * 64)
                            rhs = Vexpd[dr][:, hh, :, :] if vs == 0 else vones[:]
                            k.mm([lambda h: h.matmul(pdSd[dr][vs][rr, :, :], lhsT=kStd[dr][:, rr], rhs=rhs, start=True, stop=True)],
                                 [kStd[dr], Vexpd[dr], vones], [pdSd[dr][vs]])

            def steps(ip):
                for dr in range(2):
                    for vs in range(nv):
                        Sh = Shd[dr][vs]
                        k.op(k.pool, lambda h: h.tensor_copy(out=Sh[:, 0, :], in_=Sh[:, 8, :]), [Sh], [Sh])
                for cl in range(8):
                    for dr in range(2):
                        cp = cl if dr == 0 else 7 - cl
                        j = ip * 8 + cl
                        for vs in range(nv):
                            Sh = Shd[dr][vs]
                            k.op(k.dve, lambda h: h.scalar_tensor_tensor(out=Sh[:, cl + 1, :], in0=Sh[:, cl, :],
                                                                         scalar=Dcd[dr][:, j:j + 1], in1=pdSd[dr][vs][:, cp, :],
                                                                         op0=ALU.mult, op1=ALU.add),
                                 [Sh, Dcd[dr], pdSd[dr][vs]], [Sh])
                for dr in range(2):
                    for vs in range(nv):
                        k.op(k.act, lambda h: h.copy(out=Sbd[dr][vs][:], in_=Shd[dr][vs][:, 0:8, :]), [Shd[dr][vs]], [Sbd[dr][vs]])

            def out_mm(ip):
                for dr in range(2):
                    it = tile_of(ip, dr)
                    c0 = it * 128
                    AmT = AmTd[dr][ip % 2]
                    qI = qI_of(dr)
                    for vs in range(nv):
                        fns = []
                        for hh in range(2):
                            rr = slice(hh * 64, (hh + 1) * 64)
                            lh = vt[:, it, hh * 64:(hh + 1) * 64] if vs == 0 else onesT[:]
                            fns.append(lambda h, rr=rr, lh=lh, hh=hh: h.matmul(po_of(dr, vs)[rr, :], lhsT=lh, rhs=AmT[:, hh, :],
                                                                               start=True, stop=False, skip_group_check=True))
                            for cl in range(8):
                                cp = cl if dr == 0 else 7 - cl
                                fns.append(lambda h, rr=rr, cl=cl, cp=cp: h.matmul(
                                    po_of(dr, vs)[rr, cp * 16:(cp + 1) * 16], lhsT=Sbd[dr][vs][rr, cl, :],
                                    rhs=qI[rr, c0 + cp * 16:c0 + (cp + 1) * 16], start=False, stop=(cl == 7),
                                    skip_group_check=True))
                        k.mm(fns, [vt, onesT, AmT, Sbd[dr][vs], qI], [bankA[dr]])

            def evac_a(ip):
                for dr in range(2):
                    n_ = nsb[dr][ip % 2]
                    k.op(k.act, lambda h: h.copy(out=n_[:], in_=po_of(dr, 0)), [bankA[dr]], [n_])
                    if ml:
                        d_ = dsb[dr][ip % 2]
                        k.op(k.act, lambda h: h.activation(out=d_[:], in_=po_of(dr, 1), func=AF.Abs), [bankA[dr]], [d_])

            def evac_b(ip):
                for dr in range(2):
                    c0 = tile_of(ip, dr) * 128
                    n_ = nsb[dr][ip % 2]
                    if ml:
                        d_ = dsb[dr][ip % 2]
                        k.op(k.dve, lambda h: h.tensor_tensor(out=d_[:], in0=d_[:], in1=cld[dr][:, c0:c0 + 128], op=ALU.max),
                             [cld[dr], d_], [d_])
                        k.op(k.dve, lambda h: h.reciprocal(out=d_[:], in_=d_[:]), [d_], [d_])
                        k.op(k.dve, lambda h: h.tensor_tensor(out=n_[:], in0=n_[:], in1=d_[:], op=ALU.mult), [n_, d_], [n_])
                    k.op(k.pool, lambda h: h.tensor_tensor(out=osum[:, c0:c0 + 128], in0=osum[:, c0:c0 + 128], in1=n_[:],
                                                           op=ALU.add), [osum, n_], [osum])

            for dr in range(2):
                for vs in range(nv):
                    k.op(k.dve, lambda h: h.memset(Shd[dr][vs][:, 8, :], 0.0), [], [Shd[dr][vs]])
            front_mm(0)
            front_ev(0)
            dS_mm(0)
            for ip in range(NT):
                if ip + 1 < NT:
                    front_mm(ip + 1)
                    front_ev(ip + 1)
                steps(ip)
                if ip > 0:
                    evac_b(ip - 1)
                if ip + 1 < NT:
                    dS_mm(ip + 1)
                out_mm(ip)
                evac_a(ip)
            evac_b(NT - 1)

            pss = pdSd[0][0]
            for bi, (t0, n) in enumerate(tok_blocks(T)):
                pv = pss[:].rearrange("p c e -> p (c e)")
                k.op(k.act, lambda h: h.activation(out=fq[:, :n], in_=osum[:, t0:t0 + n], func=AF.Square), [osum], [fq])
                k.mm([lambda h: h.matmul(pv[:, :n], lhsT=blk[:], rhs=fq[:, :n], start=True, stop=True)], [blk, fq], [pss])
                k.op(k.dve, lambda h: h.tensor_scalar(out=fr_[:, :n], in0=pv[:, :n], scalar1=1.0 / 64, scalar2=EPS,
                                                      op0=ALU.mult, op1=ALU.add), [pss], [fr_])
                k.op(k.act, lambda h: h.sqrt(out=fr_[:, :n], in_=fr_[:, :n]), [fr_], [fr_])
                k.op(k.dve, lambda h: h.reciprocal(out=fr_[:, :n], in_=fr_[:, :n]), [fr_], [fr_])
                k.op(k.dve, lambda h: h.tensor_tensor(out=fy[:, :n], in0=osum[:, t0:t0 + n], in1=fr_[:, :n], op=ALU.mult),
                     [osum, fr_], [fy])
                ob = fo[bi % 2]
                k.op(k.dve, lambda h: h.scalar_tensor_tensor(out=ob[:, :n], in0=fy[:, :n], scalar=nw[:, 0:1], in1=gate[:, t0:t0 + n],
                                                             op0=ALU.mult, op1=ALU.mult), [fy, nw, gate], [ob])
                r0 = row_off + hp * 128
                k.dma(mixT_d, mixT_d.t[r0:r0 + 128, t0:t0 + n], ob, ob[:, :n])


def rec_consts():
    import ml_dtypes
    s = np.arange(128)[:, None]
    t = np.arange(128)[None, :]
    same = (s // 16) == (t // 16)
    cm = np.stack([(same & (s <= t)), (same & (s >= t))], 1).astype(np.float32)
    bmk = ((s // 16) == np.arange(8)[None, :]).astype(np.float32)
    blk = ((s // 64) == (t // 64)).astype(np.float32)
    b16 = ml_dtypes.bfloat16
    return cm.astype(b16), bmk.astype(b16), blk.astype(b16)


def stage_postmix(P, l, mixT_d, src, dst, tiles, all_latent=False):
    k = P.k
    with k.scope():
        wl = WLoader(k, nbuf=2, width=512, name="ow")
        wo = k.sb("wo", [128, KC, 1024], BF16)
        for hf in range(2):
            wb = wl.get(P.w_out, P.w_out.t[l, :, hf * 512:(hf + 1) * 512], 512)
            k.op(k.dve, lambda h: h.tensor_copy(out=wo[:, :, hf * 512:(hf + 1) * 512], in_=wb[:, :, :512]), [wb], [wo])
        gpb = k.sb("gpb", [128, 2, 1024], F32)
        for kind in range(2):
            k.dma(gpb, gpb[:, kind, :], P.modc[l], P.modc[l].t[kind:kind + 1, 2, :].to_broadcast([128, 1024]))
        mb = [k.sb("mb%d" % i, [128, KC, 128], BF16) for i in range(2)]
        xt = [k.sb("pxt%d" % i, [128, 1024], F32) for i in range(2)]
        pm = [k.ps("pmx%d" % i, [128, 512]) for i in range(4)]
        ep = Epilogue(k)
        for n, i in enumerate(tiles):
            m, x = mb[n % 2], xt[n % 2]
            k.dma(m, m[:], mixT_d, mixT_d.t[:, i * 128:(i + 1) * 128].rearrange("(k p) t -> p k t", p=128))
            k.dma(x, x[:], src, src.t[i * 128:(i + 1) * 128, :])
            ps = []
            for hf in range(2):
                p = pm[(n % 2) * 2 + hf]
                k.mm([(lambda h, kk=kk: h.matmul(p[:], lhsT=m[:, kk, :], rhs=wo[:, kk, hf * 512:(hf + 1) * 512],
                                                 start=(kk == 0), stop=(kk == KC - 1))) for kk in range(KC)], [m, wo], [p])
                ps.append(p)
            ep.run([(p, None) for p in ps], x, gpb, 1 if (i < 2 and not all_latent) else 0, dst, dst.t[i * 128:(i + 1) * 128, :])


class Epilogue:
    def __init__(self, k, need_y=True):
        self.k = k
        self.y = k.sb("ep_y", [128, 1024], F32) if need_y else None
        self.junk = k.sb("ep_j", [128, 1024], BF16)
        self.s = k.sb("ep_s", [128, 2], F32)
        self.o = [k.sb("ep_o%d" % i, [128, 1024], F32) for i in range(2)]
        self.n = 0

    def run(self, halves, x, gpb, kind, dst, dst_ap, ysb=None):
        k = self.k
        y, s = self.y, self.s
        if ysb is None:
            for hf, (p, _) in enumerate(halves):
                eng = k.act if hf == 0 else k.dve
                if hf == 0:
                    k.op(k.act, lambda h: h.copy(out=y[:, 0:512], in_=p[:]), [p], [y])
                else:
                    k.op(k.dve, lambda h: h.tensor_copy(out=y[:, 512:1024], in_=p[:]), [p], [y])
        else:
            ybuf, yap = ysb
        o = self.o[self.n % 2]
        self.n += 1
        k.op(k.dve, lambda h: h.memset(s[:], 0.0), [], [s])
        if ysb is None:
            ybuf, yap = y, y[:]
        k.op(k.act, lambda h: h.activation(out=self.junk[:], in_=yap, func=AF.Square, accum_out=s[:, 0:1]), [ybuf, s], [self.junk, s])
        k.op(k.dve, lambda h: h.tensor_scalar(out=s[:, 1:2], in0=s[:, 0:1], scalar1=1.0 / D, scalar2=EPS,
                                              op0=ALU.mult, op1=ALU.add), [s], [s])
        k.op(k.act, lambda h: h.sqrt(out=s[:, 1:2], in_=s[:, 1:2]), [s], [s])
        k.op(k.dve, lambda h: h.reciprocal(out=s[:, 1:2], in_=s[:, 1:2]), [s], [s])
        k.op(k.dve, lambda h: h.scalar_tensor_tensor(out=o[:], in0=yap, scalar=s[:, 1:2], in1=gpb[:, kind, :],
                                                     op0=ALU.mult, op1=ALU.mult), [ybuf, s, gpb], [o])
        k.op(k.dve, lambda h: h.tensor_tensor(out=o[:], in0=o[:], in1=x[:], op=ALU.add), [o, x], [o])
        k.dma(dst, dst_ap, o, o[:])


def stage_ffn(P, l, src, dst, tiles, dst_row, moe, all_latent=False):
    k = P.k
    nff = (FF_EXP if moe else FF_DENSE) // 128
    nexp = NEXP if moe else 1
    bsz = 8 if moe else 12
    for b0 in range(0, len(tiles), bsz):
        bt = tiles[b0:b0 + bsz]
        nb = len(bt)
        ntok = nb * 128
        with k.scope():
            hxb = k.sb("hxb", [128, KC, bsz * 128], BF16)
            stage_norm(P, src, l, 1, hxb, bt, all_latent=all_latent)
            with k.scope():
                HT = k.sb("HT", [128, nff, bsz * 128], BF16)
                yacc = k.sb("yacc", [128, bsz, 1024], F32)
                wl = WLoader(k, nbuf=2, width=128, name="fw")
                wl2 = WLoader(k, nbuf=2, width=128, name="fu")
                wld = WLoader(k, nbuf=2, width=512, kc=4 if moe else 2, name="fd")
                pg = [k.ps("pg%d" % i, [128, 512]) for i in range(2)]
                pu = [k.ps("pu%d" % i, [128, 512]) for i in range(2)]
                pd = [k.ps("pd%d" % i, [128, 512]) for i in range(2)]
                sg = [k.sb("sg%d" % i, [128, 512], F32) for i in range(2)]
                gates = k.sb("gates", [128, 8, 8], F32)
                gp2 = k.sb("gp2x", [128, 2, 1024], F32)
                for kind_ in range(2):
                    k.dma(gp2, gp2[:, kind_, :], P.modc[l], P.modc[l].t[kind_:kind_ + 1, 5, :].to_broadcast([128, 1024]))
                if moe:
                    rws = k.sb("rws", [128, KC, 8], F32)
                    k.dma(rws, rws[:], P.router_w, P.router_w.t[0, :, :].rearrange("(k p) e -> p k e", p=128))
                    rwb = k.sb("rwb", [128, KC, 8], BF16)
                    k.op(k.dve, lambda h: h.tensor_copy(out=rwb[:], in_=rws[:]), [rws], [rwb])
                    rb = k.sb("rb", [128, 8], F32)
                    k.dma(rb, rb[:], P.router_b, P.router_b.t[0:1, :].to_broadcast([128, 8]))
                    lg = k.sb("lg", [128, 8], F32)
                    l2 = k.sb("l2", [128, 8], F32)
                    mk = k.sb("mk", [128, 2, 8], F32)
                    mx = k.sb("mx", [128, 4], F32)
                    for ti in range(nb):
                        p = pd[ti % 2]
                        k.mm([(lambda h, kk=kk: h.matmul(p[:, 0:8], lhsT=hxb[:, kk, ti * 128:(ti + 1) * 128], rhs=rwb[:, kk, :],
                                                         start=(kk == 0), stop=(kk == KC - 1))) for kk in range(KC)], [hxb, rwb], [p])
                        k.op(k.dve, lambda h: h.tensor_tensor(out=lg[:], in0=p[:, 0:8], in1=rb[:], op=ALU.add), [p, rb], [lg])
                        k.op(k.dve, lambda h: h.reduce_max(out=mx[:, 0:1], in_=lg[:], axis=AX.X), [lg], [mx])
                        k.op(k.dve, lambda h: h.tensor_scalar(out=mk[:, 0, :], in0=lg[:], scalar1=mx[:, 0:1], scalar2=None,
                                                              op0=ALU.is_ge), [lg, mx], [mk])
                        k.op(k.dve, lambda h: h.scalar_tensor_tensor(out=l2[:], in0=mk[:, 0, :], scalar=-1e30, in1=lg[:],
                                                                     op0=ALU.mult, op1=ALU.add), [mk, lg], [l2])
                        k.op(k.dve, lambda h: h.reduce_max(out=mx[:, 1:2], in_=l2[:], axis=AX.X), [l2], [mx])
                        k.op(k.dve, lambda h: h.tensor_scalar(out=mk[:, 1, :], in0=l2[:], scalar1=mx[:, 1:2], scalar2=None,
                                                              op0=ALU.is_ge), [l2, mx], [mk])
                        k.op(k.dve, lambda h: h.tensor_tensor(out=mx[:, 2:3], in0=mx[:, 0:1], in1=mx[:, 1:2], op=ALU.subtract), [mx], [mx])
                        k.op(k.act, lambda h: h.activation(out=mx[:, 2:3], in_=mx[:, 2:3], func=AF.Sigmoid), [mx], [mx])
                        k.op(k.dve, lambda h: h.tensor_scalar(out=mx[:, 3:4], in0=mx[:, 2:3], scalar1=-1.0, scalar2=1.0,
                                                              op0=ALU.mult, op1=ALU.add), [mx], [mx])
                        k.op(k.dve, lambda h: h.tensor_scalar(out=gates[:, ti, :], in0=mk[:, 0, :], scalar1=mx[:, 2:3], scalar2=None,
                                                              op0=ALU.mult), [mk, mx], [gates])
                        k.op(k.dve, lambda h: h.scalar_tensor_tensor(out=gates[:, ti, :], in0=mk[:, 1, :], scalar=mx[:, 3:4],
                                                                     in1=gates[:, ti, :], op0=ALU.mult, op1=ALU.add),
                             [mk, mx, gates], [gates])
                for e in range(nexp):
                    if moe:
                        wg_b, wg_ap = P.moe_w_gate, P.moe_w_gate.t[0, e]
                        wu_b, wu_ap = P.moe_w_up, P.moe_w_up.t[0, e]
                        wd_b, wd_ap = P.moe_w_down, P.moe_w_down.t[0, e]
                    else:
                        wg_b, wg_ap = P.ffn_w_gate, P.ffn_w_gate.t[0]
                        wu_b, wu_ap = P.ffn_w_up, P.ffn_w_up.t[0]
                        wd_b, wd_ap = P.ffn_w_down, P.ffn_w_down.t[0]
                    for c in range(nff):
                        wg = wl.get(wg_b, wg_ap[:, c * 128:(c + 1) * 128], 128)
                        wu = wl2.get(wu_b, wu_ap[:, c * 128:(c + 1) * 128], 128)
                        for bi, (t0, n) in enumerate(tok_blocks(ntok)):
                            a, b_ = pg[bi % 2], pu[bi % 2]
                            k.mm([(lambda h, kk=kk: h.matmul(a[:, :n], lhsT=wg[:, kk, :128], rhs=hxb[:, kk, t0:t0 + n],
                                                             start=(kk == 0), stop=(kk == KC - 1))) for kk in range(KC)], [wg, hxb], [a])
                            k.mm([(lambda h, kk=kk: h.matmul(b_[:, :n], lhsT=wu[:, kk, :128], rhs=hxb[:, kk, t0:t0 + n],
                                                             start=(kk == 0), stop=(kk == KC - 1))) for kk in range(KC)], [wu, hxb], [b_])
                            s_ = sg[bi % 2]
                            k.op(k.act, lambda h: h.activation(out=s_[:, :n], in_=a[:, :n], func=AF.Silu), [a], [s_])
                            k.op(k.dve, lambda h: h.tensor_tensor(out=HT[:, c, t0:t0 + n], in0=s_[:, :n], in1=b_[:, :n], op=ALU.mult),
                                 [s_, b_], [HT])
                    kcd = wld.kc
                    for pc in range(nff // kcd):
                        for hf in range(2):
                            wd = wld.get(wd_b, wd_ap[pc * kcd * 128:(pc + 1) * kcd * 128, hf * 512:(hf + 1) * 512], 512)
                            for ti in range(nb):
                                p = pd[ti % 2]
                                k.mm([(lambda h, kk=kk: h.matmul(p[:], lhsT=HT[:, pc * kcd + kk, ti * 128:(ti + 1) * 128], rhs=wd[:, kk, :],
                                                                 start=(kk == 0), stop=(kk == kcd - 1))) for kk in range(kcd)], [HT, wd], [p])
                                ya = yacc[:, ti, hf * 512:(hf + 1) * 512]
                                first = (e == 0 and pc == 0)
                                if moe:
                                    g_ = gates[:, ti, e:e + 1]
                                    if first:
                                        k.op(k.dve, lambda h: h.tensor_scalar(out=ya, in0=p[:], scalar1=g_, scalar2=None, op0=ALU.mult),
                                             [p, gates], [yacc])
                                    else:
                                        k.op(k.dve, lambda h: h.scalar_tensor_tensor(out=ya, in0=p[:], scalar=g_, in1=ya,
                                                                                     op0=ALU.mult, op1=ALU.add), [p, gates, yacc], [yacc])
                                else:
                                    if first:
                                        k.op(k.act, lambda h: h.copy(out=ya, in_=p[:]), [p], [yacc])
                                    else:
                                        k.op(k.dve, lambda h: h.tensor_tensor(out=ya, in0=ya, in1=p[:], op=ALU.add), [p, yacc], [yacc])
                ep = Epilogue(k, need_y=False)
                xt = [k.sb("fx%d" % i, [128, 1024], F32) for i in range(2)]
                for ti, i in enumerate(bt):
                    x = xt[ti % 2]
                    k.dma(x, x[:], src, src.t[i * 128:(i + 1) * 128, :])
                    r0 = dst_row(i)
                    ep.run(None, x, gp2, 1 if (i < 2 and not all_latent) else 0, dst, dst.t[r0:r0 + 128, :], ysb=(yacc, yacc[:, ti, :]))


def stage_select(P, src, dst):
    k = P.k
    with k.scope():
        sel = k.sb("sel", [128, 2], F32)
        k.dma(sel, sel[:], P.ext["sel"], P.ext["sel"].t[:, :])
        a = [k.sb("sa%d" % i, [128, 1024], F32) for i in range(2)]
        b = [k.sb("sb%d" % i, [128, 1024], F32) for i in range(2)]
        for j in range(16):
            x, y = a[j % 2], b[j % 2]
            i0, i1 = 2 + j, 2 + 16 + j
            k.dma(x, x[:], src, src.t[i0 * 128:(i0 + 1) * 128, :])
            k.dma(y, y[:], src, src.t[i1 * 128:(i1 + 1) * 128, :])
            k.op(k.dve, lambda h: h.tensor_scalar(out=x[:], in0=x[:], scalar1=sel[:, 0:1], scalar2=None, op0=ALU.mult), [x, sel], [x])
            k.op(k.dve, lambda h: h.scalar_tensor_tensor(out=x[:], in0=y[:], scalar=sel[:, 1:2], in1=x[:], op0=ALU.mult, op1=ALU.add),
                 [x, y, sel], [x])
            k.dma(dst, dst.t[j * 128:(j + 1) * 128, :], x, x[:])


def stage_select_mix(P, src, dst):
    k = P.k
    with k.scope():
        sel = k.sb("sel", [128, 2], F32)
        k.dma(sel, sel[:], P.ext["sel"], P.ext["sel"].t[:, :])
        H = SEQ // 2
        a = [k.sb("ma%d" % i, [128, H], BF16) for i in range(2)]
        b = [k.sb("mb%d" % i, [128, H], BF16) for i in range(2)]
        for r in range(4):
            x, y = a[r % 2], b[r % 2]
            k.dma(x, x[:], src, src.t[r * 128:(r + 1) * 128, CTX:CTX + H])
            k.dma(y, y[:], src, src.t[r * 128:(r + 1) * 128, CTX + H:T])
            k.op(k.dve, lambda h: h.tensor_scalar(out=x[:], in0=x[:], scalar1=sel[:, 0:1], scalar2=None, op0=ALU.mult), [x, sel], [x])
            k.op(k.dve, lambda h: h.scalar_tensor_tensor(out=x[:], in0=y[:], scalar=sel[:, 1:2], in1=x[:], op0=ALU.mult, op1=ALU.add),
                 [x, y, sel], [x])
            k.dma(dst, dst.t[r * 128:(r + 1) * 128, :], x, x[:])


def build_program():
    P = Prog()
    k = P.k
    P.xin = P.ext_in("xin", [T, D])
    P.c2 = P.ext_in("c2", [2, D])
    P.w_mod = P.ext_in("w_mod", [2, D, 6 * D])
    P.b_mod = P.ext_in("b_mod", [2, 6 * D])
    for n in ("norm_pre_mix", "norm_post_mix", "norm_pre_ffn", "norm_post_ffn"):
        setattr(P, n, P.ext_in(n, [2, D]))
    P.ext_in("ident", [128, 128])
    P.ext_in("sel", [128, 2])
    P.w_in = P.ext_in("w_in", [2, D, PROJ_W])
    P.ext_in("w_da_sw", [2, D, 1024])
    P.w_out = P.ext_in("w_out", [2, D, D])
    P.ext_in("rope_c", [128, T], BF16)
    P.ext_in("rope_s", [128, T], BF16)
    P.ext_in("cmask", [128, 2, 128], BF16)
    P.ext_in("bmask", [128, 8], BF16)
    P.ext_in("blk", [128, 128], BF16)
    P.ml_gate_b = P.ext_in("ml_gate_b", [2, 16])
    P.ml_norm = P.ext_in("ml_norm", [2, 256])
    P.hg_norm = P.ext_in("hg_norm", [2, 256])
    P.hg_lb_logits = P.ext_in("hg_lb_logits", [2, 256])
    P.da_lambda = P.ext_in("da_lambda", [2, 4, 64])
    P.da_norm = P.ext_in("da_norm", [2, 128])
    P.ffn_w_gate = P.ext_in("ffn_w_gate", [1, D, FF_DENSE])
    P.ffn_w_up = P.ext_in("ffn_w_up", [1, D, FF_DENSE])
    P.ffn_w_down = P.ext_in("ffn_w_down", [1, FF_DENSE, D])
    P.router_w = P.ext_in("router_w", [1, D, NEXP])
    P.router_b = P.ext_in("router_b", [1, NEXP])
    P.moe_w_gate = P.ext_in("moe_w_gate", [1, NEXP, D, FF_EXP])
    P.moe_w_up = P.ext_in("moe_w_up", [1, NEXP, D, FF_EXP])
    P.moe_w_down = P.ext_in("moe_w_down", [1, NEXP, FF_EXP, D])
    out = P.ext_out("out", [SEQ // 2, D])
    P.modc = [k.dram("modc%d" % l, [2, 6, D], F32) for l in range(2)]
    P.rec = dict(ml_q=k.dram("ml_q", [256, T], BF16), ml_k=k.dram("ml_k", [256, T], BF16), ml_o=k.dram("ml_o", [256, T], BF16),
                 ml_g=k.dram("ml_g", [1024, T], F32), hg_q=k.dram("hg_q", [256, T], BF16), hg_g=k.dram("hg_g", [256, T], BF16),
                 hg_f=k.dram("hg_f", [512, T], F32), ml_v=k.dram("ml_v", [T, 256], BF16), hg_v=k.dram("hg_v", [T, 256], BF16))
    mixT_d = k.dram("mixT_d", [1024, T], BF16)
    xmid = [k.dram("xmid%d" % l, [T, D], F32) for l in range(2)]
    x1 = k.dram("x1", [T, D], F32)
    xown = k.dram("xown", [SEQ // 2, D], F32)
    mixown = k.dram("mixown", [1024, SEQ // 2], BF16)
    setup_consts(P)
    stage_mod(P, 0)
    stage_mod(P, 1)
    lat_blocks = [(CTX + qb * 512, 512, list(range(NT))) for qb in range(SEQ // 512)]
    for l in range(2):
        src = P.xin if l == 0 else x1
        lam_init = 0.8 - 0.6 * float(np.exp(-0.3 * l))
        with k.scope():
            hxT = k.sb("hxT", [128, KC, T], BF16)
            stage_norm(P, src, l, 0, hxT, list(range(NT)))
            stage_proj_rec(P, l, hxT)
            if l == 0:
                qbl = [(0, CTX, [0, 1])] + list(lat_blocks)
                stage_attn(P, l, hxT, None, 0, 0, lam_init, qblocks=qbl, mixT_d=mixT_d)
            else:
                qbl = [(qb * 512, 512, list(range(NT))) for qb in range(SEQ // 2 // 512)]
                stage_attn(P, l, hxT, None, 0, 0, lam_init, qblocks=qbl, mixT_d=mixown, own=True)
        stage_rec(P, l, "ml", mixT_d)
        stage_rec(P, l, "hg", mixT_d)
        if l == 0:
            stage_postmix(P, l, mixT_d, src, xmid[0], list(range(NT)))
            stage_ffn(P, 0, xmid[0], x1, list(range(NT)), lambda i: i * 128, moe=False)
        else:
            stage_select_mix(P, mixT_d, mixown)
            stage_select(P, x1, xown)
            stage_postmix(P, l, mixown, xown, xmid[1], list(range(16)), all_latent=True)
            stage_ffn(P, 1, xmid[1], out, list(range(16)), lambda i: i * 128, moe=True, all_latent=True)
    k.finish([out])
    return k.nc


_CACHE = {}


def kernel(x, c, ctx, c_ctx, w_mod, b_mod, norm_pre_mix, norm_post_mix, norm_pre_ffn, norm_post_ffn,
           w_in, w_out, ml_gate_b, ml_norm, hg_lb_logits, hg_norm, da_lambda, da_norm,
           ffn_w_gate, ffn_w_up, ffn_w_down, router_w, router_b, moe_w_gate, moe_w_up, moe_w_down):
    from concourse.bass_utils import run_bass_kernel_spmd
    f = lambda a: np.ascontiguousarray(np.asarray(a, dtype=np.float32))
    x, c, ctx, c_ctx = f(x), f(c), f(ctx), f(c_ctx)
    w_in = f(w_in)
    if "nc" not in _CACHE:
        _CACHE["nc"] = build_program()
    nc = _CACHE["nc"]
    rc, rs = rope_tables()
    cm, bmk, blk = rec_consts()
    shared = dict(w_mod=f(w_mod), b_mod=f(b_mod), norm_pre_mix=f(norm_pre_mix), norm_post_mix=f(norm_post_mix),
                  norm_pre_ffn=f(norm_pre_ffn), norm_post_ffn=f(norm_post_ffn), ident=np.eye(128, dtype=np.float32),
                  w_in=w_in, w_da_sw=swap_cols(w_in), w_out=f(w_out), rope_c=rc, rope_s=rs, cmask=cm, bmask=bmk, blk=blk,
                  ml_gate_b=f(ml_gate_b), ml_norm=f(ml_norm), hg_norm=f(hg_norm), hg_lb_logits=f(hg_lb_logits),
                  da_lambda=f(da_lambda), da_norm=f(da_norm), ffn_w_gate=f(ffn_w_gate), ffn_w_up=f(ffn_w_up),
                  ffn_w_down=f(ffn_w_down), router_w=f(router_w), router_b=f(router_b), moe_w_gate=f(moe_w_gate),
                  moe_w_up=f(moe_w_up), moe_w_down=f(moe_w_down))
    in_maps = []
    for core in range(8):
        b, half = core // 2, core % 2
        m = dict(shared)
        m["xin"] = np.ascontiguousarray(np.concatenate([ctx[b], x[b]], axis=0))
        m["c2"] = np.ascontiguousarray(np.stack([c[b], c_ctx], axis=0))
        sel = np.zeros((128, 2), np.float32)
        sel[:, half] = 1.0
        m["sel"] = sel
        in_maps.append(m)
    res = run_bass_kernel_spmd(nc, in_maps, core_ids=list(range(8)))
    out = np.zeros((4, SEQ, D), np.float32)
    for core in range(8):
        b, half = core // 2, core % 2
        out[b, half * (SEQ // 2):(half + 1) * (SEQ // 2)] = np.asarray(res.results[core]["out"], dtype=np.float32)
    return out
```

```python
import numpy as np
from contextlib import ExitStack
import concourse.bass as bass
import concourse.mybir as mybir

F32 = mybir.dt.float32
BF16 = mybir.dt.bfloat16
AF = mybir.ActivationFunctionType
ALU = mybir.AluOpType
AX = mybir.AxisListType

SAME_ENG_SYNC = True


class Buf:
    def __init__(self, k, t, name, space):
        self.k, self.t, self.name, self.space = k, t, name, space
        self.w = {}
        self.r = {}
        self.dsem = None

    def __getitem__(self, idx):
        return self.t[idx]


class Eng:
    def __init__(self, k, name, h):
        self.k, self.name, self.h = k, name, h
        self.sem = k.es.enter_context(k.nc.semaphore("e_" + name))
        self.cnt = 0
        self.waited = {}


class _Scope:
    def __init__(self, k):
        self.k = k

    def __enter__(self):
        k = self.k
        self.prev = k.es
        self.st = ExitStack()
        k.es = self.st
        k.scopes.append([])
        return self

    def __exit__(self, *a):
        k = self.k
        k.barrier()
        for b in k.scopes.pop():
            if b.dsem:
                for s_ in b.dsem.values():
                    k.dpool.append(s_)
        self.st.close()
        k.es = self.prev
        return False


class K:
    def __init__(self):
        self.nc = bass.Bass("TRN2", target_bir_lowering=False)
        self.es = ExitStack()
        nc = self.nc
        self.pe = Eng(self, "pe", nc.tensor)
        self.act = Eng(self, "act", nc.scalar)
        self.dve = Eng(self, "dve", nc.vector)
        self.pool = Eng(self, "pool", nc.gpsimd)
        self.sp = Eng(self, "sp", nc.sync)
        self.sems = {}
        self.totals = {}
        for e in (self.pe, self.act, self.dve, self.pool, self.sp):
            self.sems[e.sem.name] = e.sem
        self.ndsem = 0
        self.dq = 0
        self.dpool = []
        for i in range(88):
            s = self.es.enter_context(self.nc.semaphore("d%d" % i))
            self.sems[s.name] = s
            self.totals[s.name] = 0
            self.dpool.append(s)
        self.scopes = []

    def sb(self, name, shape, dt, dshare=None):
        self.nuniq = getattr(self, "nuniq", 0) + 1
        t = self.es.enter_context(self.nc.sbuf_tensor("%s_%d" % (name, self.nuniq), list(shape), dt))
        b = Buf(self, t, name, "sb")
        if self.scopes:
            self.scopes[-1].append(b)
        return b

    def ps(self, name, shape, dt=F32):
        self.nuniq = getattr(self, "nuniq", 0) + 1
        t = self.es.enter_context(self.nc.psum_tensor("%s_%d" % (name, self.nuniq), list(shape), dt))
        return Buf(self, t, name, "ps")

    def scope(self):
        return _Scope(self)

    def barrier(self):
        engs = (self.pe, self.act, self.dve, self.pool, self.sp)
        for e in engs:
            for o in engs:
                if o is e or o.cnt == 0:
                    continue
                if e.waited.get(o.sem.name, 0) < o.cnt:
                    e.h.wait_ge(o.sem, o.cnt)
                    e.waited[o.sem.name] = o.cnt
            for s, v in self.totals.items():
                if v and e.waited.get(s, 0) < v:
                    e.h.wait_ge(self.sems[s], v)
                    e.waited[s] = v

    def dram(self, name, shape, dt, kind="Internal"):
        t = self.nc.dram_tensor(name, list(shape), dt, kind=kind)
        return Buf(self, t.ap(), name, "dram")

    def _dsem(self, b, qn="sp"):
        if b.dsem is None:
            b.dsem = {}
        if qn not in b.dsem:
            b.dsem[qn] = self.dpool.pop()
        return b.dsem[qn]

    def _deps(self, reads, writes):
        deps = {}
        self._raw = {}
        for b in reads:
            for s, v in b.w.items():
                deps[s] = max(deps.get(s, 0), v)
                self._raw[s] = max(self._raw.get(s, 0), v)
        for b in writes:
            for s, v in b.w.items():
                deps[s] = max(deps.get(s, 0), v)
            for s, v in b.r.items():
                deps[s] = max(deps.get(s, 0), v)
        return deps

    def _wait(self, eng, deps):
        for s, v in deps.items():
            if s in self.totals:
                v = self.totals[s]
            if s == eng.sem.name and not SAME_ENG_SYNC:
                continue
            if s == eng.sem.name and eng is self.pe:
                continue
            if s == eng.sem.name:
                if s not in self._raw:
                    continue
                v = self._raw[s]
            if eng.waited.get(s, 0) >= v:
                continue
            eng.h.wait_ge(self.sems[s], v)
            eng.waited[s] = v

    def _mark(self, tick, reads, writes):
        s, v = tick
        for b in writes:
            b.w[s] = max(b.w.get(s, 0), v)
            b.r = {}
        for b in reads:
            b.r[s] = max(b.r.get(s, 0), v)

    def op(self, eng, fn, reads=(), writes=()):
        self._wait(eng, self._deps(reads, writes))
        ins = fn(eng.h)
        eng.cnt += 1
        ins.then_inc(eng.sem, 1)
        self._mark((eng.sem.name, eng.cnt), reads, writes)
        return ins

    def mm(self, fns, reads=(), writes=()):
        eng = self.pe
        self._wait(eng, self._deps(reads, writes))
        ins = None
        for fn in fns:
            ins = fn(eng.h)
        eng.cnt += 1
        ins.then_inc(eng.sem, 1)
        self._mark((eng.sem.name, eng.cnt), reads, writes)

    def dma(self, out_b, out_ap, in_b, in_ap, q=None, **kw):
        if q is None:
            q = (self.sp, self.pool)[self.dq % 2]
            self.dq += 1
        sb = out_b if out_b.space != "dram" else in_b
        if sb.space == "dram":
            sb = out_b
        s = self._dsem(sb, q.name)
        self._wait(q, self._deps([in_b], [out_b]))
        ins = q.h.dma_start(out=out_ap, in_=in_ap, **kw)
        ins.then_inc(s, 16)
        self.totals[s.name] += 16
        self._mark((s.name, self.totals[s.name]), [in_b], [out_b])
        return ins

    def finish(self, bufs):
        deps = {}
        for b in bufs:
            for s, v in b.w.items():
                deps[s] = max(deps.get(s, 0), v)
        self._wait(self.sp, deps)
        for e in (self.pe, self.act, self.dve, self.pool):
            if e.cnt:
                self.sp.h.wait_ge(e.sem, e.cnt)
        for s, v in self.totals.items():
            if v and self.sp.waited.get(s, 0) < v:
                self.sp.h.wait_ge(self.sems[s], v)
        self.es.close()


D = 1024
KC = 8
SEQ = 4096
CTX = 256
T = SEQ + CTX
NT = T // 128
PROJ_W = 3856
FF_DENSE = 2816
FF_EXP = 3584
NEXP = 8
EPS = 1e-6
C_ML_Q, C_ML_K, C_ML_V, C_ML_O, C_ML_G = 0, 256, 512, 768, 1024
C_HG_Q, C_HG_F, C_HG_I, C_HG_G = 1040, 1296, 1808, 2064
C_DA_Q, C_DA_K, C_DA_V = 2320, 2832, 3344


def tok_blocks(n, bs=512):
    out = []
    o = 0
    while o < n:
        out.append((o, min(bs, n - o)))
        o += bs
    return out


class Prog:
    def __init__(self, dbg=None):
        self.k = K()
        self.dbg = dbg or {}
        self.ext = {}

    def ext_in(self, name, shape, dt=F32):
        b = self.k.dram(name, shape, dt, kind="ExternalInput")
        self.ext[name] = b
        return b

    def ext_out(self, name, shape, dt=F32):
        b = self.k.dram(name, shape, dt, kind="ExternalOutput")
        self.ext[name] = b
        return b


def stage_mod(P, l):
    k = P.k
    with k.scope():
        cT = k.sb("cT", [128, KC, 2], F32)
        for kk in range(KC):
            k.dma(cT, cT[:, kk, :], P.c2, P.c2.t[:, kk * 128:(kk + 1) * 128].rearrange("r p -> p r"),
                  allow_slow_non_contiguous=True)
        k.op(k.act, lambda h: h.activation(out=cT[:], in_=cT[:], func=AF.Silu), [cT], [cT])
        modv = k.sb("modv", [2, 6, 1024], F32)
        bm = k.sb("bm", [2, 6, 1024], F32)
        k.dma(bm, bm[:], P.b_mod,
              P.b_mod.t[l:l + 1, :].rearrange("o (s d) -> o s d", s=6).to_broadcast([2, 6, 1024]))
        nrm = k.sb("nrm", [2, 4, 1024], F32)
        for j, nb in enumerate((P.norm_pre_mix, P.norm_post_mix, P.norm_pre_ffn, P.norm_post_ffn)):
            k.dma(nrm, nrm[:, j, :], nb, nb.t[l:l + 1, :].to_broadcast([2, 1024]))
        wst = [k.sb("wst%d" % i, [128, KC, 512], F32) for i in range(4)]
        pm = [k.ps("pm%d" % i, [2, 512]) for i in range(4)]
        for cb in range(12):
            w = wst[cb % 4]
            p = pm[cb % 4]
            k.dma(w, w[:], P.w_mod, P.w_mod.t[l, :, cb * 512:(cb + 1) * 512].rearrange("(k p) n -> p k n", p=128))
            k.mm([(lambda h, kk=kk: h.matmul(p[:], lhsT=cT[:, kk, :], rhs=w[:, kk, :],
                                             start=(kk == 0), stop=(kk == KC - 1))) for kk in range(KC)],
                 [cT, w], [p])
            s, o = divmod(cb * 512, 1024)
            k.op(k.dve, lambda h: h.tensor_tensor(out=modv[:, s, o:o + 512], in0=p[:], in1=bm[:, s, o:o + 512],
                                                  op=ALU.add), [p, bm], [modv])
        modc = k.sb("modc", [2, 6, 1024], F32)
        for (dst, msc, nj) in ((0, 1, 0), (3, 4, 2)):
            k.op(k.dve, lambda h: h.scalar_tensor_tensor(out=modc[:, dst, :], in0=modv[:, msc, :], scalar=1.0,
                                                         in1=nrm[:, nj, :], op0=ALU.add, op1=ALU.mult),
                 [modv, nrm], [modc])
        for (dst, msh) in ((1, 0), (4, 3)):
            k.op(k.dve, lambda h: h.tensor_copy(out=modc[:, dst, :], in_=modv[:, msh, :]), [modv], [modc])
        for (dst, mg, nj) in ((2, 2, 1), (5, 5, 3)):
            k.op(k.dve, lambda h: h.tensor_tensor(out=modc[:, dst, :], in0=modv[:, mg, :], in1=nrm[:, nj, :],
                                                  op=ALU.mult), [modv, nrm], [modc])
        k.dma(P.modc[l], P.modc[l].t[:, :, :], modc, modc[:])


def stage_norm(P, src, l, which, hxT, tiles, all_latent=False):
    k = P.k
    gi, si = (0, 1) if which == 0 else (3, 4)
    with k.scope():
        gb = k.sb("gb", [128, 2, 1024], F32)
        shb = k.sb("shb", [128, 2, 1024], F32)
        for kind in range(2):
            k.dma(gb, gb[:, kind, :], P.modc[l], P.modc[l].t[kind:kind + 1, gi, :].to_broadcast([128, 1024]))
            k.dma(shb, shb[:, kind, :], P.modc[l], P.modc[l].t[kind:kind + 1, si, :].to_broadcast([128, 1024]))
        xt = [k.sb("xt%d" % i, [128, 1024], F32) for i in range(2)]
        junk = k.sb("junk", [128, 1024], BF16)
        ss = [k.sb("ss%d" % i, [128, 2], F32) for i in range(2)]
        yf = k.sb("yf", [128, 1024], F32)
        yb = [k.sb("yb%d" % i, [128, 1024], BF16) for i in range(2)]
        pt = [k.ps("pt%d" % i, [128, KC, 128], BF16) for i in range(2)]
        for n, i in enumerate(tiles):
            kind = 1 if (i < 2 and not all_latent) else 0
            x, s, y, p = xt[n % 2], ss[n % 2], yb[n % 2], pt[n % 2]
            k.dma(x, x[:], src, src.t[i * 128:(i + 1) * 128, :])
            k.op(k.dve, lambda h: h.memset(s[:], 0.0), [], [s])
            k.op(k.act, lambda h: h.activation(out=junk[:], in_=x[:], func=AF.Square, accum_out=s[:, 0:1]),
                 [x, s], [junk, s])
            k.op(k.dve, lambda h: h.tensor_scalar(out=s[:, 1:2], in0=s[:, 0:1], scalar1=1.0 / D, scalar2=EPS,
                                                  op0=ALU.mult, op1=ALU.add), [s], [s])
            k.op(k.act, lambda h: h.sqrt(out=s[:, 1:2], in_=s[:, 1:2]), [s], [s])
            k.op(k.dve, lambda h: h.reciprocal(out=s[:, 1:2], in_=s[:, 1:2]), [s], [s])
            k.op(k.dve, lambda h: h.scalar_tensor_tensor(out=yf[:], in0=x[:], scalar=s[:, 1:2], in1=gb[:, kind, :],
                                                         op0=ALU.mult, op1=ALU.mult), [x, s, gb], [yf])
            k.op(k.dve, lambda h: h.tensor_tensor(out=y[:], in0=yf[:], in1=shb[:, kind, :], op=ALU.add),
                 [yf, shb], [y])
            k.mm([(lambda h, kk=kk: h.transpose(out=p[:, kk, :], in_=y[:, kk * 128:(kk + 1) * 128],
                                                identity=P.ident[:])) for kk in range(KC)], [y, P.ident], [p])
            k.op(k.act, lambda h: h.copy(out=hxT[:, :, n * 128:(n + 1) * 128], in_=p[:]), [p], [hxT])


def setup_consts(P):
    k = P.k
    idf = k.sb("idf", [128, 128], F32)
    k.dma(idf, idf[:], P.ext["ident"], P.ext["ident"].t[:, :])
    P.ident = k.sb("ident", [128, 128], BF16)
    k.op(k.dve, lambda h: h.tensor_copy(out=P.ident[:], in_=idf[:]), [idf], [P.ident])


class WLoader:
    def __init__(self, k, nbuf=2, width=512, kc=KC, name="wl"):
        self.k = k
        self.kc = kc
        self.wb = [k.sb(name + "b%d" % i, [128, kc, width], BF16) for i in range(nbuf)]
        self.i = 0
        self.nbuf = nbuf

    def get(self, wbuf, w_ap, n):
        k = self.k
        wb = self.wb[self.i % self.nbuf]
        self.i += 1
        k.dma(wb, wb[:, :, :n], wbuf, w_ap.rearrange("(k p) n -> p k n", p=128), q=k.pool)
        return wb


def lin_fm(k, wb, ncol, src, ntok, evac, ps_pool, kc=KC, bs=512):
    for bi, (t0, n) in enumerate(tok_blocks(ntok, bs)):
        p = ps_pool[bi % len(ps_pool)]
        k.mm([(lambda h, kk=kk: h.matmul(p[:ncol, :n], lhsT=wb[:, kk, :ncol], rhs=src[:, kk, t0:t0 + n],
                                         start=(kk == 0), stop=(kk == kc - 1))) for kk in range(kc)],
             [wb, src], [p])
        evac(p, t0, n)


def lin_tm(k, wb, ncol, src, tiles, evac, ps_pool, kc=KC):
    for ti, i in enumerate(tiles):
        p = ps_pool[ti % len(ps_pool)]
        k.mm([(lambda h, kk=kk: h.matmul(p[:, :ncol], lhsT=src[:, kk, i * 128:(i + 1) * 128], rhs=wb[:, kk, :ncol],
                                         start=(kk == 0), stop=(kk == kc - 1))) for kk in range(kc)],
             [wb, src], [p])
        evac(p, i)


def stage_attn(P, l, hxT, mixT, q0, nq, lam_init, heads=(0, 1, 2, 3), qblocks=None, mixT_d=None, own=False):
    k = P.k
    w_in = P.w_in
    with k.scope():
        wl = WLoader(k, nbuf=2, width=128, name="aw")
        ropeC = k.sb("ropeC", [128, T], BF16)
        ropeS = k.sb("ropeS", [128, T], BF16)
        k.dma(ropeC, ropeC[:], P.ext["rope_c"], P.ext["rope_c"].t[:, :])
        k.dma(ropeS, ropeS[:], P.ext["rope_s"], P.ext["rope_s"].t[:, :])
        lp = k.sb("lp", [128, 4, 64], F32)
        k.dma(lp, lp[:], P.da_lambda, P.da_lambda.t[l:l + 1, :, :].to_broadcast([128, 4, 64]))
        lt = k.sb("lt", [128, 2, 64], F32)
        lam = k.sb("lam", [128, 4], F32)
        k.op(k.dve, lambda h: h.tensor_tensor(out=lt[:, 0, :], in0=lp[:, 0, :], in1=lp[:, 1, :], op=ALU.mult), [lp], [lt])
        k.op(k.dve, lambda h: h.tensor_tensor(out=lt[:, 1, :], in0=lp[:, 2, :], in1=lp[:, 3, :], op=ALU.mult), [lp], [lt])
        k.op(k.dve, lambda h: h.reduce_sum(out=lam[:, 0:2], in_=lt[:], axis=AX.X), [lt], [lam])
        k.op(k.act, lambda h: h.activation(out=lam[:, 0:2], in_=lam[:, 0:2], func=AF.Exp), [lam], [lam])
        k.op(k.dve, lambda h: h.tensor_tensor(out=lam[:, 2:3], in0=lam[:, 1:2], in1=lam[:, 0:1], op=ALU.subtract), [lam], [lam])
        k.op(k.dve, lambda h: h.tensor_scalar(out=lam[:, 3:4], in0=lam[:, 2:3], scalar1=-float(lam_init), scalar2=None,
                                              op0=ALU.add), [lam], [lam])
        dn = k.sb("dn", [128, 128], F32)
        k.dma(dn, dn[:], P.da_norm, P.da_norm.t[l:l + 1, :].to_broadcast([128, 128]))
        k.op(k.dve, lambda h: h.tensor_scalar(out=dn[:], in0=dn[:], scalar1=float(1.0 - lam_init), scalar2=None,
                                              op0=ALU.mult), [dn], [dn])

        qT = k.sb("qT", [128, T], BF16)
        kT = k.sb("kT", [128, T], BF16)
        if own:
            qTo = k.sb("qTo", [128, SEQ // 2], BF16)
            selt = k.sb("selt", [128, 2], F32)
            k.dma(selt, selt[:], P.ext["sel"], P.ext["sel"].t[:, :])
        va = k.sb("va", [128, NT, 132], BF16)
        k.op(k.dve, lambda h: h.memset(va[:, :, 128:132], 1.0), [], [va])
        t1 = k.sb("t1", [128, 512], F32)
        t2 = k.sb("t2", [128, 512], F32)
        sT2 = [[k.ps("sT%d_%d" % (i, j), [128, 512]) for j in range(2)] for i in range(2)]
        pp = [sT2[0][0], sT2[0][1]]
        osbs = [k.sb("osb%d" % i, [128, 3, 3, 132], F32) for i in range(2)]
        blk_i = [0]
        oacc = [k.ps("oacc%d" % i, [128, 3, 132]) for i in range(3)]
        pT = [[k.sb("pT%d_%d" % (i, j), [128, 512], BF16) for j in range(2)] for i in range(2)]
        fin = k.sb("fin", [128, 8], F32)
        o0 = k.sb("o0", [128, 128], F32)
        o1 = k.sb("o1", [128, 128], F32)
        junk = k.sb("ajunk", [128, 128], F32)
        ob = k.sb("ob", [128, 128], BF16)
        ptr = k.ps("ptr", [128, 128], BF16)
        ost = k.sb("ost", [128, 512], BF16)
        nkc = NT
        for hd in heads:
            for (dst, c_main, c_sw) in ((qT, C_DA_Q + hd * 128, hd * 128), (kT, C_DA_K + hd * 128, 512 + hd * 128)):
                wm = wl.get(w_in, w_in.t[l, :, c_main:c_main + 128], 128)
                ws = wl.get(P.ext["w_da_sw"], P.ext["w_da_sw"].t[l, :, c_sw:c_sw + 128], 128)
                for bi, (t0, n) in enumerate(tok_blocks(T)):
                    pa, pb = pp[0], pp[1]
                    k.mm([(lambda h, kk=kk: h.matmul(pa[:, :n], lhsT=wm[:, kk, :128], rhs=hxT[:, kk, t0:t0 + n],
                                                     start=(kk == 0), stop=(kk == KC - 1))) for kk in range(KC)],
                         [wm, hxT], [pa])
                    k.mm([(lambda h, kk=kk: h.matmul(pb[:, :n], lhsT=ws[:, kk, :128], rhs=hxT[:, kk, t0:t0 + n],
                                                     start=(kk == 0), stop=(kk == KC - 1))) for kk in range(KC)],
                         [ws, hxT], [pb])
                    k.op(k.dve, lambda h: h.tensor_tensor(out=t1[:, :n], in0=pa[:, :n], in1=ropeC[:, t0:t0 + n],
                                                          op=ALU.mult), [pa, ropeC], [t1])
                    k.op(k.dve, lambda h: h.tensor_tensor(out=t2[:, :n], in0=pb[:, :n], in1=ropeS[:, t0:t0 + n],
                                                          op=ALU.mult), [pb, ropeS], [t2])
                    k.op(k.dve, lambda h: h.tensor_tensor(out=dst[:, t0:t0 + n], in0=t1[:, :n], in1=t2[:, :n],
                                                          op=ALU.add), [t1, t2], [dst])
            wv = wl.get(w_in, w_in.t[l, :, C_DA_V + hd * 128:C_DA_V + (hd + 1) * 128], 128)
            for i in range(NT):
                pa = pp[i % 2]
                k.mm([(lambda h, kk=kk: h.matmul(pa[:, :128], lhsT=hxT[:, kk, i * 128:(i + 1) * 128], rhs=wv[:, kk, :128],
                                                 start=(kk == 0), stop=(kk == KC - 1))) for kk in range(KC)],
                     [wv, hxT], [pa])
                k.op(k.act, lambda h: h.copy(out=va[:, i, 0:128], in_=pa[:, :128]), [pa], [va])
            qsrc = qT
            if own:
                h0, h1 = CTX, CTX + SEQ // 2
                k.op(k.dve, lambda h: h.tensor_scalar(out=qTo[:], in0=qT[:, h0:h1], scalar1=selt[:, 0:1], scalar2=None,
                                                      op0=ALU.mult), [qT, selt], [qTo])
                k.op(k.dve, lambda h: h.scalar_tensor_tensor(out=qTo[:], in0=qT[:, h1:T], scalar=selt[:, 1:2], in1=qTo[:],
                                                             op0=ALU.mult, op1=ALU.add), [qT, selt, qTo], [qTo])
                qsrc = qTo
            if qblocks is None:
                qblocks = [(q0 + qb * 512, 512, list(range(NT))) for qb in range(nq // 512)]
            for (qs, nqb, ktiles) in qblocks:
                nqt = nqb // 128
                nkc = len(ktiles)
                glist = [c_ * 4 + q_ for c_ in range(2) for q_ in range(nqt)]

                def emit_S(kci):
                    kc = ktiles[kci]
                    for comp in range(2):
                        sp_ = sT2[comp][kci % 2]
                        r0 = comp * 64
                        k.mm([lambda h: h.matmul(sp_[:, :nqb], lhsT=kT[r0:r0 + 64, kc * 128:(kc + 1) * 128],
                                                 rhs=qsrc[r0:r0 + 64, qs:qs + nqb], start=True, stop=True)],
                             [kT, qsrc], [sp_])

                def emit_exp(kci):
                    for comp in range(2):
                        sp_ = sT2[comp][kci % 2]
                        pt_ = pT[comp][kci % 2]
                        k.op(k.act, lambda h: h.activation(out=pt_[:, :nqb], in_=sp_[:, :nqb], func=AF.Exp, scale=0.125),
                             [sp_], [pt_])

                def emit_PV(kci):
                    kc = ktiles[kci]
                    for comp in range(2):
                        pt_ = pT[comp][kci % 2]
                        fns = []
                        for qt in range(nqt):
                            g = comp * 4 + qt
                            ot = oacc[g // 3]
                            fns.append(lambda h, ot=ot, g=g, qt=qt: h.matmul(
                                ot[:, g % 3, 0:129], lhsT=pt_[:, qt * 128:(qt + 1) * 128], rhs=va[:, kc, 0:129],
                                start=(kci == 0 and g == min(x for x in glist if x // 3 == g // 3)), stop=(kci == nkc - 1),
                                skip_group_check=True))
                        k.mm(fns, [pt_, va], oacc)

                emit_S(0)
                emit_exp(0)
                for kci in range(nkc):
                    if kci + 1 < nkc:
                        emit_S(kci + 1)
                    emit_PV(kci)
                    if kci + 1 < nkc:
                        emit_exp(kci + 1)
                osb = osbs[blk_i[0] % 2]
                blk_i[0] += 1
                for j in range(3):
                    k.op(k.dve, lambda h: h.tensor_copy(out=osb[:, j, :, :], in_=oacc[j][:]), [oacc[j]], [osb])
                for qt in range(nqt):
                    g0, g1 = qt, 4 + qt
                    k.op(k.dve, lambda h: h.reciprocal(out=fin[:, 0:1], in_=osb[:, g0 // 3, g0 % 3, 128:129]), [osb], [fin])
                    k.op(k.dve, lambda h: h.reciprocal(out=fin[:, 1:2], in_=osb[:, g1 // 3, g1 % 3, 128:129]), [osb], [fin])
                    k.op(k.dve, lambda h: h.tensor_tensor(out=fin[:, 1:2], in0=fin[:, 1:2], in1=lam[:, 3:4], op=ALU.mult),
                         [fin, lam], [fin])
                    k.op(k.dve, lambda h: h.tensor_scalar(out=o0[:], in0=osb[:, g0 // 3, g0 % 3, 0:128], scalar1=fin[:, 0:1],
                                                          scalar2=None, op0=ALU.mult), [osb, fin], [o0])
                    k.op(k.dve, lambda h: h.scalar_tensor_tensor(out=o1[:], in0=osb[:, g1 // 3, g1 % 3, 0:128], scalar=fin[:, 1:2],
                                                                 in1=o0[:], op0=ALU.mult, op1=ALU.add),
                         [osb, fin, o0], [o1])
                    k.op(k.dve, lambda h: h.tensor_tensor(out=junk[:], in0=o1[:], in1=o1[:], op=ALU.mult), [o1], [junk])
                    k.op(k.dve, lambda h: h.reduce_sum(out=fin[:, 2:3], in_=junk[:], axis=AX.X), [junk], [fin])
                    k.op(k.dve, lambda h: h.tensor_scalar(out=fin[:, 3:4], in0=fin[:, 2:3], scalar1=1.0 / 128, scalar2=EPS,
                                                          op0=ALU.mult, op1=ALU.add), [fin], [fin])
                    k.op(k.act, lambda h: h.sqrt(out=fin[:, 3:4], in_=fin[:, 3:4]), [fin], [fin])
                    k.op(k.dve, lambda h: h.reciprocal(out=fin[:, 3:4], in_=fin[:, 3:4]), [fin], [fin])
                    k.op(k.dve, lambda h: h.scalar_tensor_tensor(out=ob[:], in0=o1[:], scalar=fin[:, 3:4], in1=dn[:],
                                                                 op0=ALU.mult, op1=ALU.mult), [o1, fin, dn], [ob])
                    k.mm([lambda h: h.transpose(out=ptr[:], in_=ob[:], identity=P.ident[:])], [ob, P.ident], [ptr])
                    c0 = qs + qt * 128
                    if mixT_d is None:
                        k.op(k.dve, lambda h: h.tensor_copy(out=mixT[:, 4 + hd, c0:c0 + 128], in_=ptr[:]), [ptr], [mixT])
                    else:
                        k.op(k.dve, lambda h: h.tensor_copy(out=ost[:, qt * 128:(qt + 1) * 128], in_=ptr[:]), [ptr], [ost])
                if mixT_d is not None:
                    r0_ = 512 + hd * 128
                    k.dma(mixT_d, mixT_d.t[r0_:r0_ + 128, qs:qs + nqb], ost, ost[:, :nqb])


def rope_tables():
    n = np.arange(SEQ)
    row = (n // 64).astype(np.float64)
    col = (n % 64).astype(np.float64)
    inv = 10000.0 ** (-np.arange(16, dtype=np.float64) / 16)
    C = np.ones((64, T), np.float64)
    S = np.zeros((64, T), np.float64)
    for d in range(64):
        pos = row if d < 32 else col
        f = d % 16
        ang = pos * inv[f]
        C[d, CTX:] = np.cos(ang)
        sgn = -1.0 if (d % 32) < 16 else 1.0
        S[d, CTX:] = sgn * np.sin(ang)
    import ml_dtypes
    C2 = np.concatenate([C, C], 0).astype(np.float32).astype(ml_dtypes.bfloat16)
    S2 = np.concatenate([S, S], 0).astype(np.float32).astype(ml_dtypes.bfloat16)
    return C2, S2


def swap_cols(w_in):
    perm64 = np.concatenate([np.arange(16, 32), np.arange(0, 16), np.arange(48, 64), np.arange(32, 48)])
    idx = []
    for base in (C_DA_Q, C_DA_K):
        for u in range(8):
            idx.append(base + u * 64 + perm64)
    idx = np.concatenate(idx)
    return np.ascontiguousarray(w_in[:, :, idx])


def stage_proj_rec(P, l, hxT):
    k = P.k
    w_in = P.w_in
    R = P.rec
    with k.scope():
        wl = WLoader(k, nbuf=2, width=128, name="pw")
        pp = [k.ps("rp%d" % i, [128, 512]) for i in range(3)]
        stf = [k.sb("stf%d" % i, [128, 512], F32) for i in range(2)]
        stb = [k.sb("stb%d" % i, [128, 512], BF16) for i in range(2)]
        gb = k.sb("gbias", [128, 8], F32)
        for ty in range(4):
            for hp in range(2):
                for hh in range(2):
                    c = ty * 4 + hp * 2 + hh
                    k.dma(gb, gb[hh * 64:(hh + 1) * 64, ty * 2 + hp:ty * 2 + hp + 1], P.ml_gate_b,
                          P.ml_gate_b.t[l:l + 1, c:c + 1].to_broadcast([64, 1]))
        cnt = [0]

        def fm_job(wb, dst, row0, func=None, scale=1.0, bias=None, f32=False):
            def evac(p, t0, n):
                i = cnt[0] % 2
                cnt[0] += 1
                st = stf[i] if f32 else stb[i]
                if func is None and bias is None:
                    k.op(k.dve, lambda h: h.tensor_scalar(out=st[:, :n], in0=p[:, :n], scalar1=float(scale), scalar2=None,
                                                          op0=ALU.mult), [p], [st])
                elif func is None:
                    k.op(k.dve, lambda h: h.tensor_scalar(out=st[:, :n], in0=p[:, :n], scalar1=bias, scalar2=None,
                                                          op0=ALU.add), [p, gb], [st])
                else:
                    k.op(k.act, lambda h: h.activation(out=st[:, :n], in_=p[:, :n], func=func), [p], [st])
                k.dma(dst, dst.t[row0:row0 + 128, t0:t0 + n], st, st[:, :n])
            lin_fm(k, wb, 128, hxT, T, evac, pp)

        for c in range(2):
            fm_job(wl.get(w_in, w_in.t[l, :, C_ML_Q + c * 128:C_ML_Q + (c + 1) * 128], 128), R["ml_q"], c * 128)
            fm_job(wl.get(w_in, w_in.t[l, :, C_ML_K + c * 128:C_ML_K + (c + 1) * 128], 128), R["ml_k"], c * 128, scale=0.125)
            fm_job(wl.get(w_in, w_in.t[l, :, C_ML_O + c * 128:C_ML_O + (c + 1) * 128], 128), R["ml_o"], c * 128, func=AF.Sigmoid)
            fm_job(wl.get(w_in, w_in.t[l, :, C_HG_Q + c * 128:C_HG_Q + (c + 1) * 128], 128), R["hg_q"], c * 128, func=AF.Silu)
            fm_job(wl.get(w_in, w_in.t[l, :, C_HG_G + c * 128:C_HG_G + (c + 1) * 128], 128), R["hg_g"], c * 128, func=AF.Silu)
        for c in range(4):
            fm_job(wl.get(w_in, w_in.t[l, :, C_HG_F + c * 128:C_HG_F + (c + 1) * 128], 128), R["hg_f"], c * 128, f32=True)
        g16s = k.sb("g16s", [128, KC, 16], F32)
        k.dma(g16s, g16s[:], w_in, w_in.t[l, :, C_ML_G:C_ML_G + 16].rearrange("(k p) n -> p k n", p=128))
        wrep = [k.sb("wrep%d" % i, [128, KC, 128], BF16) for i in range(2)]
        for ty in range(4):
            for hp in range(2):
                wr = wrep[(ty * 2 + hp) % 2]
                for hh in range(2):
                    c = ty * 4 + hp * 2 + hh
                    k.op(k.dve, lambda h: h.tensor_copy(out=wr[:, :, hh * 64:(hh + 1) * 64],
                                                        in_=g16s[:, :, c:c + 1].to_broadcast([128, KC, 64])), [g16s], [wr])
                fm_job(wr, R["ml_g"], (ty * 2 + hp) * 128, bias=gb[:, ty * 2 + hp:ty * 2 + hp + 1], f32=True)
        for (c0, dst) in ((C_ML_V, R["ml_v"]), (C_HG_I, R["hg_v"])):
            wv = k.sb("wv", [128, KC, 256], BF16)
            for c in range(2):
                wb = wl.get(w_in, w_in.t[l, :, c0 + c * 128:c0 + (c + 1) * 128], 128)
                k.op(k.dve, lambda h: h.tensor_copy(out=wv[:, :, c * 128:(c + 1) * 128], in_=wb[:, :, :128]), [wb], [wv])

            def evac_v(p, i, dst=dst):
                j = cnt[0] % 2
                cnt[0] += 1
                st = stb[j]
                k.op(k.act, lambda h: h.copy(out=st[:, :256], in_=p[:, :256]), [p], [st])
                k.dma(dst, dst.t[i * 128:(i + 1) * 128, :], st, st[:, :256])
            lin_tm(k, wv, 256, hxT, list(range(NT)), evac_v, pp)


NCH = T // 16
SEGS = ((0, CTX), (CTX, T))


def seg_view(ap2d, si, dr):
    a, b = SEGS[si]
    if dr == 0:
        return ap2d[:, a:b]
    if a == 0:
        return ap2d[:, b - 1::-1]
    return ap2d[:, b - 1:a - 1:-1]


def stage_rec(P, l, mixer, mixT_d):
    k = P.k
    R = P.rec
    ml = (mixer == "ml")
    nv = 2 if ml else 1
    row_off = 0 if ml else 256
    with k.scope():
        cst = P.ext
        maskb = k.sb("maskb", [128, 2, 128], BF16)
        k.dma(maskb, maskb[:], cst["cmask"], cst["cmask"].t[:, :, :])
        bm = k.sb("bm", [128, 8], BF16)
        k.dma(bm, bm[:], cst["bmask"], cst["bmask"].t[:, :])
        blk = k.sb("blk", [128, 128], BF16)
        k.dma(blk, blk[:], cst["blk"], cst["blk"].t[:, :])
        if not ml:
            rmask = k.sb("rmask", [128, T], F32)
            k.op(k.dve, lambda h: h.memset(rmask[:], 1.0), [], [rmask])
            k.op(k.dve, lambda h: h.memset(rmask[:].rearrange("p (c i) -> p c i", i=16)[:, :, 0], 0.0), [], [rmask])
        zer = k.sb("zer", [128, 1], F32)
        k.op(k.dve, lambda h: h.memset(zer[:], 0.0), [], [zer])
        onesT = k.sb("onesT", [128, 64], BF16)
        k.op(k.dve, lambda h: h.memset(onesT[:], 1.0), [], [onesT])
        vones = k.sb("vones", [128, 8, 64], BF16)
        k.op(k.dve, lambda h: h.tensor_copy(out=vones[:], in_=bm[:].unsqueeze(2).to_broadcast([128, 8, 64])), [bm], [vones])

        qAs = k.sb("qA", [128, T], BF16) if ml else None
        qAd = [qAs, qAs] if ml else [k.sb("qA%d" % i, [128, T], BF16) for i in range(2)]
        kAd = [k.sb("kA%d" % i, [128, T], BF16) for i in range(2)]
        x3d = [k.sb("x3%d" % i, [128, T], BF16) for i in range(2)]
        cld = [k.sb("cl%d" % i, [128, T], BF16) for i in range(2)] if ml else None
        tA = k.sb("tA", [128, T], F32)
        tB = k.sb("tB", [128, T], F32)
        tC = k.sb("tC", [128, T], F32)
        osum = k.sb("osum", [128, T], F32)
        gate = k.sb("gate", [128, T], BF16)
        vt = k.sb("vt", [128, NT, 128], BF16)
        Rj = k.sb("Rj", [128, NCH], F32)
        Rp = k.sb("Rp", [128, NCH], F32)
        Dcd = [k.sb("Dc%d" % i, [128, NCH], F32) for i in range(2)]
        sc = k.sb("sc", [128, 8], F32)
        nw = k.sb("nw", [128, 1], F32)

        bankA = [k.ps("bkA%d" % i, [128, 512]) for i in range(2)]
        pdSd = [[k.ps("pdS%d_%d" % (i, j), [128, 8, 64]) for j in range(nv)] for i in range(2)]
        pktd = [k.ps("pkt%d" % i, [128, 128], BF16) for i in range(2)]
        AmTd = [[k.sb("AmT%d_%d" % (i, j), [128, 2, 128], BF16) for j in range(2)] for i in range(2)]
        qzd = [[k.sb("qz%d_%d" % (i, j), [128, 2, 128], BF16) for j in range(2)] for i in range(2)]
        for i in range(2):
            for j in range(2):
                k.op(k.dve, lambda h: h.memset(qzd[i][j][:], 0.0), [], [qzd[i][j]])
        kStd = [k.sb("kSt%d" % i, [128, 128], BF16) for i in range(2)]
        Vexpd = [k.sb("Vexp%d" % i, [128, 2, 8, 64], BF16) for i in range(2)]
        Shd = [[k.sb("Sh%d_%d" % (i, j), [128, 9, 64], F32) for j in range(nv)] for i in range(2)]
        Sbd = [[k.sb("Sb%d_%d" % (i, j), [128, 8, 64], BF16) for j in range(nv)] for i in range(2)]
        nsb = [[k.sb("nsb%d_%d" % (i, j), [128, 128], F32) for j in range(2)] for i in range(2)]
        dsb = [[k.sb("dsb%d_%d" % (i, j), [128, 128], F32) for j in range(2)] for i in range(2)] if ml else None
        fq = k.sb("fq", [128, 512], BF16)
        fr_ = k.sb("fr_", [128, 512], F32)
        fy = k.sb("fy", [128, 512], F32)
        fo = [k.sb("fo%d" % i, [128, 512], BF16) for i in range(2)]

        def c16(v):
            return v.rearrange("p (c i) -> p c i", i=16)

        def tile_of(ip, dr):
            if ip < 2:
                return ip if dr == 0 else 1 - ip
            return ip if dr == 0 else (NT - 1) - (ip - 2)

        for hp in range(2):
            rows = slice(hp * 128, (hp + 1) * 128)
            src_v = R["ml_v"] if ml else R["hg_v"]
            for i_ in range(NT):
                k.dma(vt, vt[:, i_, :], src_v, src_v.t[i_ * 128:(i_ + 1) * 128, hp * 128:(hp + 1) * 128])
            src_g = R["ml_o"] if ml else R["hg_g"]
            k.dma(gate, gate[:], src_g, src_g.t[rows, :])
            nsrc = P.ml_norm if ml else P.hg_norm
            k.dma(nw, nw[:], nsrc, nsrc.t[l:l + 1, rows].rearrange("o p -> p o"), allow_slow_non_contiguous=True)
            k.op(k.pool, lambda h: h.memset(osum[:], 0.0), [], [osum])
            if not ml:
                if l == 0:
                    k.op(k.dve, lambda h: h.memset(sc[:, 0:1], 0.0), [], [sc])
                else:
                    lg = P.hg_lb_logits
                    k.dma(sc, sc[:, 4:5], lg, lg.t[0:1, rows].rearrange("o p -> p o"), allow_slow_non_contiguous=True)
                    k.dma(sc, sc[:, 5:6], lg, lg.t[1:2, rows].rearrange("o p -> p o"), allow_slow_non_contiguous=True)
                    k.op(k.dve, lambda h: h.tensor_tensor(out=sc[:, 6:7], in0=sc[:, 5:6], in1=sc[:, 4:5], op=ALU.subtract), [sc], [sc])
                    k.op(k.act, lambda h: h.activation(out=sc[:, 0:1], in_=sc[:, 6:7], func=AF.Sigmoid), [sc], [sc])
                k.op(k.dve, lambda h: h.tensor_scalar(out=sc[:, 1:2], in0=sc[:, 0:1], scalar1=-1.0, scalar2=1.0,
                                                      op0=ALU.mult, op1=ALU.add), [sc], [sc])
                k.op(k.dve, lambda h: h.tensor_scalar(out=sc[:, 2:3], in0=sc[:, 1:2], scalar1=-1.0, scalar2=None,
                                                      op0=ALU.mult), [sc], [sc])
            if ml:
                k.dma(qAs, qAs[:], R["ml_q"], R["ml_q"].t[rows, :])
            for dr in range(2):
                qA, kA, x3, Dc = qAd[dr], kAd[dr], x3d[dr], Dcd[dr]
                if ml:
                    k.dma(kA, kA[:], R["ml_k"], R["ml_k"].t[rows, :])
                    gi0 = ((dr * 2) * 2 + hp) * 128
                    gf0 = ((dr * 2 + 1) * 2 + hp) * 128
                    k.dma(tA, tA[:], R["ml_g"], R["ml_g"].t[gi0:gi0 + 128, :])
                    k.dma(tB, tB[:], R["ml_g"], R["ml_g"].t[gf0:gf0 + 128, :])
                    k.op(k.act, lambda h: h.activation(out=tB[:], in_=tB[:], func=AF.Exp, scale=-1.0), [tB], [tB])
                    k.op(k.act, lambda h: h.activation(out=tB[:], in_=tB[:], func=AF.Ln, bias=1.0), [tB], [tB])
                    for si in range(2):
                        ini = 0.0 if si == 0 else seg_view(tC[:], 0, dr)[:, CTX - 1:CTX]
                        ov, dv = seg_view(tC[:], si, dr), seg_view(tB[:], si, dr)
                        n = SEGS[si][1] - SEGS[si][0]
                        k.op(k.dve, lambda h: h.tensor_tensor_scan(out=ov, data0=dv, data1=zer[:].to_broadcast([128, n]),
                                                                   initial=ini, op0=ALU.add, op1=ALU.add), [tB, tC, zer], [tC])
                    k.op(k.dve, lambda h: h.tensor_tensor(out=tA[:], in0=tA[:], in1=tC[:], op=ALU.add), [tA, tC], [tA])
                    for si in range(2):
                        ini = 0.0 if si == 0 else seg_view(tB[:], 0, dr)[:, CTX - 1:CTX]
                        ov, dv = seg_view(tB[:], si, dr), seg_view(tA[:], si, dr)
                        k.op(k.dve, lambda h: h.tensor_tensor_scan(out=ov, data0=dv, data1=dv, initial=ini,
                                                                   op0=ALU.max, op1=ALU.max), [tA, tB], [tB])
                    for si in range(2):
                        j0, j1 = SEGS[si][0] // 16, SEGS[si][1] // 16
                        k.op(k.dve, lambda h: h.tensor_copy(out=Rj[:, j0:j1], in_=c16(seg_view(tB[:], si, dr))[:, :, 15]),
                             [tB], [Rj])
                    k.op(k.dve, lambda h: h.memset(Rp[:, 0:1], 0.0), [], [Rp])
                    k.op(k.dve, lambda h: h.tensor_copy(out=Rp[:, 1:NCH], in_=Rj[:, 0:NCH - 1]), [Rj], [Rp])
                    k.op(k.dve, lambda h: h.tensor_tensor(out=Dc[:], in0=Rp[:], in1=Rj[:], op=ALU.subtract), [Rp, Rj], [Dc])
                    k.op(k.act, lambda h: h.activation(out=Dc[:], in_=Dc[:], func=AF.Exp), [Dc], [Dc])
                    for si in range(2):
                        j0, j1 = SEGS[si][0] // 16, SEGS[si][1] // 16
                        rb = Rj[:, j0:j1].unsqueeze(2).to_broadcast([128, j1 - j0, 16])
                        db = Dc[:, j0:j1].unsqueeze(2).to_broadcast([128, j1 - j0, 16])
                        av, cv = c16(seg_view(tA[:], si, dr)), c16(seg_view(tC[:], si, dr))
                        k.op(k.dve, lambda h: h.tensor_tensor(out=av, in0=av, in1=rb, op=ALU.subtract), [tA, Rj], [tA])
                        k.op(k.dve, lambda h: h.tensor_tensor(out=cv, in0=cv, in1=rb, op=ALU.subtract), [tC, Rj], [tC])
                        k.op(k.pool, lambda h: h.tensor_tensor(out=c16(seg_view(x3[:], si, dr)), in0=c16(seg_view(qA[:], si, dr)),
                                                               in1=db, op=ALU.mult), [qA, Dc], [x3])
                    k.op(k.act, lambda h: h.activation(out=tA[:], in_=tA[:], func=AF.Exp), [tA], [tA])
                    k.op(k.act, lambda h: h.activation(out=cld[dr][:], in_=tC[:], func=AF.Exp), [tC], [cld[dr]])
                    k.op(k.dve, lambda h: h.tensor_tensor(out=kA[:], in0=kA[:], in1=tA[:], op=ALU.mult), [kA, tA], [kA])
                else:
                    k.dma(qA, qA[:], R["hg_q"], R["hg_q"].t[rows, :])
                    f0 = dr * 256 + hp * 128
                    k.dma(tA, tA[:], R["hg_f"], R["hg_f"].t[f0:f0 + 128, :])
                    k.op(k.act, lambda h: h.activation(out=tB[:], in_=tA[:], func=AF.Sigmoid), [tA], [tB])
                    k.op(k.dve, lambda h: h.tensor_scalar(out=tC[:], in0=tB[:], scalar1=sc[:, 2:3], scalar2=sc[:, 1:2],
                                                          op0=ALU.mult, op1=ALU.add), [tB, sc], [tC])
                    k.op(k.dve, lambda h: h.tensor_scalar(out=tB[:], in0=tB[:], scalar1=sc[:, 1:2], scalar2=sc[:, 0:1],
                                                          op0=ALU.mult, op1=ALU.add), [tB, sc], [tB])
                    k.op(k.act, lambda h: h.activation(out=tB[:], in_=tB[:], func=AF.Ln), [tB], [tB])
                    for si in range(2):
                        j0, j1 = SEGS[si][0] // 16, SEGS[si][1] // 16
                        ov = c16(seg_view(tA[:], si, dr))
                        nseg = SEGS[si][1] - SEGS[si][0]
                        k.op(k.dve, lambda h: h.tensor_tensor_scan(out=seg_view(tA[:], si, dr), data0=rmask[:, 0:nseg],
                                                                   data1=seg_view(tB[:], si, dr), initial=0.0,
                                                                   op0=ALU.mult, op1=ALU.add), [tB, rmask, tA], [tA])
                        k.op(k.dve, lambda h: h.tensor_copy(out=Rj[:, j0:j1], in_=ov[:, :, 15]), [tA], [Rj])
                    k.op(k.act, lambda h: h.activation(out=Dc[:], in_=Rj[:], func=AF.Exp), [Rj], [Dc])
                    k.op(k.act, lambda h: h.activation(out=tB[:], in_=tA[:], func=AF.Exp), [tA], [tB])
                    k.op(k.dve, lambda h: h.tensor_tensor(out=qA[:], in0=qA[:], in1=tB[:], op=ALU.mult), [qA, tB], [qA])
                    k.op(k.act, lambda h: h.activation(out=tB[:], in_=tA[:], func=AF.Exp, scale=-1.0), [tA], [tB])
                    k.op(k.dve, lambda h: h.tensor_tensor(out=kA[:], in0=tC[:], in1=tB[:], op=ALU.mult), [tC, tB], [kA])
                    for si in range(2):
                        j0, j1 = SEGS[si][0] // 16, SEGS[si][1] // 16
                        rb = Rj[:, j0:j1].unsqueeze(2).to_broadcast([128, j1 - j0, 16])
                        av = c16(seg_view(tA[:], si, dr))
                        k.op(k.dve, lambda h: h.tensor_tensor(out=av, in0=rb, in1=av, op=ALU.subtract), [tA, Rj], [tA])
                    k.op(k.act, lambda h: h.activation(out=tA[:], in_=tA[:], func=AF.Exp), [tA], [tA])
                    k.op(k.dve, lambda h: h.tensor_tensor(out=x3[:], in0=tC[:], in1=tA[:], op=ALU.mult), [tC, tA], [x3])

            def qI_of(dr):
                return x3d[dr] if ml else qAd[dr]

            def kS_of(dr):
                return kAd[dr] if ml else x3d[dr]

            def pA_of(dr):
                return bankA[dr][:, 0:256].rearrange("p (h t) -> p h t", h=2)

            def po_of(dr, vs):
                return bankA[dr][:, 256 + vs * 128:256 + (vs + 1) * 128]

            def front_mm(ip):
                for dr in range(2):
                    c0 = tile_of(ip, dr) * 128
                    qz = qzd[dr][ip % 2]
                    for hh in range(2):
                        rr = slice(hh * 64, (hh + 1) * 64)
                        k.op(k.pool, lambda h: h.tensor_copy(out=qz[rr, hh, :], in_=qAd[dr][rr, c0:c0 + 128]), [qAd[dr]], [qz])
                    k.mm([lambda h: h.matmul(bankA[dr][:, 0:256], lhsT=kAd[dr][:, c0:c0 + 128],
                                             rhs=qz[:].rearrange("p h t -> p (h t)"), start=True, stop=True)],
                         [kAd[dr], qz], [bankA[dr]])
                    kS = kS_of(dr)
                    k.mm([lambda h: h.transpose(out=pktd[dr][:], in_=kS[:, c0:c0 + 128], identity=P.ident[:])],
                         [kS, P.ident], [pktd[dr]])

            def front_ev(ip):
                for dr in range(2):
                    it = tile_of(ip, dr)
                    AmT = AmTd[dr][ip % 2]
                    k.op(k.dve, lambda h: h.tensor_tensor(out=AmT[:], in0=pA_of(dr),
                                                          in1=maskb[:, dr:dr + 1, :].to_broadcast([128, 2, 128]),
                                                          op=ALU.mult), [bankA[dr], maskb], [AmT])
                    k.op(k.act, lambda h: h.copy(out=kStd[dr][:], in_=pktd[dr][:]), [pktd[dr]], [kStd[dr]])
                    k.op(k.pool, lambda h: h.tensor_tensor(
                        out=Vexpd[dr][:], in0=vt[:, it, :].rearrange("p (h e) -> p h e", h=2).unsqueeze(2).to_broadcast([128, 2, 8, 64]),
                        in1=bm[:].unsqueeze(1).unsqueeze(3).to_broadcast([128, 2, 8, 64]), op=ALU.mult), [vt, bm], [Vexpd[dr]])

            def dS_mm(ip):
                for dr in range(2):
                    for vs in range(nv):
                        for hh in range(2):
                            rr = slice(hh * 64, (hh + 1) * 64)
                            rhs = Vexpd[dr][:, hh, :, :] if vs == 0 else vones[:]
                            k.mm([lambda h: h.matmul(pdSd[dr][vs][rr, :, :], lhsT=kStd[dr][:, rr], rhs=rhs, start=True, stop=True)],
                                 [kStd[dr], Vexpd[dr], vones], [pdSd[dr][vs]])

            def steps(ip):
                for dr in range(2):
                    for vs in range(nv):
                        Sh = Shd[dr][vs]
                        k.op(k.pool, lambda h: h.tensor_copy(out=Sh[:, 0, :], in_=Sh[:, 8, :]), [Sh], [Sh])
                for cl in range(8):
                    for dr in range(2):
                        cp = cl if dr == 0 else 7 - cl
                        j = ip * 8 + cl
                        for vs in range(nv):
                            Sh = Shd[dr][vs]
                            k.op(k.dve, lambda h: h.scalar_tensor_tensor(out=Sh[:, cl + 1, :], in0=Sh[:, cl, :],
                                                                         scalar=Dcd[dr][:, j:j + 1], in1=pdSd[dr][vs][:, cp, :],
                                                                         op0=ALU.mult, op1=ALU.add),
                                 [Sh, Dcd[dr], pdSd[dr][vs]], [Sh])
                for dr in range(2):
                    for vs in range(nv):
                        k.op(k.act, lambda h: h.copy(out=Sbd[dr][vs][:], in_=Shd[dr][vs][:, 0:8, :]), [Shd[dr][vs]], [Sbd[dr][vs]])

            def out_mm(ip):
                for dr in range(2):
                    it = tile_of(ip, dr)
                    c0 = it * 128
                    AmT = AmTd[dr][ip % 2]
                    qI = qI_of(dr)
                    for vs in range(nv):
                        fns = []
                        for hh in range(2):
                            rr = slice(hh * 64, (hh + 1) * 64)
                            lh = vt[:, it, hh * 64:(hh + 1) * 64] if vs == 0 else onesT[:]
                            fns.append(lambda h, rr=rr, lh=lh, hh=hh: h.matmul(po_of(dr, vs)[rr, :], lhsT=lh, rhs=AmT[:, hh, :],
                                                                               start=True, stop=False, skip_group_check=True))
                            for cl in range(8):
                                cp = cl if dr == 0 else 7 - cl
                                fns.append(lambda h, rr=rr, cl=cl, cp=cp: h.matmul(
                                    po_of(dr, vs)[rr, cp * 16:(cp + 1) * 16], lhsT=Sbd[dr][vs][rr, cl, :],
                                    rhs=qI[rr, c0 + cp * 16:c0 + (cp + 1) * 16], start=False, stop=(cl == 7),
                                    skip_group_check=True))
                        k.mm(fns, [vt, onesT, AmT, Sbd[dr][vs], qI], [bankA[dr]])

            def evac_a(ip):
                for dr in range(2):
                    n_ = nsb[dr][ip % 2]
                    k.op(k.act, lambda h: h.copy(out=n_[:], in_=po_of(dr, 0)), [bankA[dr]], [n_])
                    if ml:
                        d_ = dsb[dr][ip % 2]
                        k.op(k.act, lambda h: h.activation(out=d_[:], in_=po_of(dr, 1), func=AF.Abs), [bankA[dr]], [d_])

            def evac_b(ip):
                for dr in range(2):
                    c0 = tile_of(ip, dr) * 128
                    n_ = nsb[dr][ip % 2]
                    if ml:
                        d_ = dsb[dr][ip % 2]
                        k.op(k.dve, lambda h: h.tensor_tensor(out=d_[:], in0=d_[:], in1=cld[dr][:, c0:c0 + 128], op=ALU.max),
                             [cld[dr], d_], [d_])
                        k.op(k.dve, lambda h: h.reciprocal(out=d_[:], in_=d_[:]), [d_], [d_])
                        k.op(k.dve, lambda h: h.tensor_tensor(out=n_[:], in0=n_[:], in1=d_[:], op=ALU.mult), [n_, d_], [n_])
                    k.op(k.pool, lambda h: h.tensor_tensor(out=osum[:, c0:c0 + 128], in0=osum[:, c0:c0 + 128], in1=n_[:],
                                                           op=ALU.add), [osum, n_], [osum])

            for dr in range(2):
                for vs in range(nv):
                    k.op(k.dve, lambda h: h.memset(Shd[dr][vs][:, 8, :], 0.0), [], [Shd[dr][vs]])
            front_mm(0)
            front_ev(0)
            dS_mm(0)
            for ip in range(NT):
                if ip + 1 < NT:
                    front_mm(ip + 1)
                    front_ev(ip + 1)
                steps(ip)
                if ip > 0:
                    evac_b(ip - 1)
                if ip + 1 < NT:
                    dS_mm(ip + 1)
                out_mm(ip)
                evac_a(ip)
            evac_b(NT - 1)

            pss = pdSd[0][0]
            for bi, (t0, n) in enumerate(tok_blocks(T)):
                pv = pss[:].rearrange("p c e -> p (c e)")
                k.op(k.act, lambda h: h.activation(out=fq[:, :n], in_=osum[:, t0:t0 + n], func=AF.Square), [osum], [fq])
                k.mm([lambda h: h.matmul(pv[:, :n], lhsT=blk[:], rhs=fq[:, :n], start=True, stop=True)], [blk, fq], [pss])
                k.op(k.dve, lambda h: h.tensor_scalar(out=fr_[:, :n], in0=pv[:, :n], scalar1=1.0 / 64, scalar2=EPS,
                                                      op0=ALU.mult, op1=ALU.add), [pss], [fr_])
                k.op(k.act, lambda h: h.sqrt(out=fr_[:, :n], in_=fr_[:, :n]), [fr_], [fr_])
                k.op(k.dve, lambda h: h.reciprocal(out=fr_[:, :n], in_=fr_[:, :n]), [fr_], [fr_])
                k.op(k.dve, lambda h: h.tensor_tensor(out=fy[:, :n], in0=osum[:, t0:t0 + n], in1=fr_[:, :n], op=ALU.mult),
                     [osum, fr_], [fy])
                ob = fo[bi % 2]
                k.op(k.dve, lambda h: h.scalar_tensor_tensor(out=ob[:, :n], in0=fy[:, :n], scalar=nw[:, 0:1], in1=gate[:, t0:t0 + n],
                                                             op0=ALU.mult, op1=ALU.mult), [fy, nw, gate], [ob])
                r0 = row_off + hp * 128
                k.dma(mixT_d, mixT_d.t[r0:r0 + 128, t0:t0 + n], ob, ob[:, :n])


def rec_consts():
    import ml_dtypes
    s = np.arange(128)[:, None]
    t = np.arange(128)[None, :]
    same = (s // 16) == (t // 16)
    cm = np.stack([(same & (s <= t)), (same & (s >= t))], 1).astype(np.float32)
    bmk = ((s // 16) == np.arange(8)[None, :]).astype(np.float32)
    blk = ((s // 64) == (t // 64)).astype(np.float32)
    b16 = ml_dtypes.bfloat16
    return cm.astype(b16), bmk.astype(b16), blk.astype(b16)


def stage_postmix(P, l, mixT_d, src, dst, tiles, all_latent=False):
    k = P.k
    with k.scope():
        wl = WLoader(k, nbuf=2, width=512, name="ow")
        wo = k.sb("wo", [128, KC, 1024], BF16)
        for hf in range(2):
            wb = wl.get(P.w_out, P.w_out.t[l, :, hf * 512:(hf + 1) * 512], 512)
            k.op(k.dve, lambda h: h.tensor_copy(out=wo[:, :, hf * 512:(hf + 1) * 512], in_=wb[:, :, :512]), [wb], [wo])
        gpb = k.sb("gpb", [128, 2, 1024], F32)
        for kind in range(2):
            k.dma(gpb, gpb[:, kind, :], P.modc[l], P.modc[l].t[kind:kind + 1, 2, :].to_broadcast([128, 1024]))
        mb = [k.sb("mb%d" % i, [128, KC, 128], BF16) for i in range(2)]
        xt = [k.sb("pxt%d" % i, [128, 1024], F32) for i in range(2)]
        pm = [k.ps("pmx%d" % i, [128, 512]) for i in range(4)]
        ep = Epilogue(k)
        for n, i in enumerate(tiles):
            m, x = mb[n % 2], xt[n % 2]
            k.dma(m, m[:], mixT_d, mixT_d.t[:, i * 128:(i + 1) * 128].rearrange("(k p) t -> p k t", p=128))
            k.dma(x, x[:], src, src.t[i * 128:(i + 1) * 128, :])
            ps = []
            for hf in range(2):
                p = pm[(n % 2) * 2 + hf]
                k.mm([(lambda h, kk=kk: h.matmul(p[:], lhsT=m[:, kk, :], rhs=wo[:, kk, hf * 512:(hf + 1) * 512],
                                                 start=(kk == 0), stop=(kk == KC - 1))) for kk in range(KC)], [m, wo], [p])
                ps.append(p)
            ep.run([(p, None) for p in ps], x, gpb, 1 if (i < 2 and not all_latent) else 0, dst, dst.t[i * 128:(i + 1) * 128, :])


class Epilogue:
    def __init__(self, k, need_y=True):
        self.k = k
        self.y = k.sb("ep_y", [128, 1024], F32) if need_y else None
        self.junk = k.sb("ep_j", [128, 1024], BF16)
        self.s = k.sb("ep_s", [128, 2], F32)
        self.o = [k.sb("ep_o%d" % i, [128, 1024], F32) for i in range(2)]
        self.n = 0

    def run(self, halves, x, gpb, kind, dst, dst_ap, ysb=None):
        k = self.k
        y, s = self.y, self.s
        if ysb is None:
            for hf, (p, _) in enumerate(halves):
                eng = k.act if hf == 0 else k.dve
                if hf == 0:
                    k.op(k.act, lambda h: h.copy(out=y[:, 0:512], in_=p[:]), [p], [y])
                else:
                    k.op(k.dve, lambda h: h.tensor_copy(out=y[:, 512:1024], in_=p[:]), [p], [y])
        else:
            ybuf, yap = ysb
        o = self.o[self.n % 2]
        self.n += 1
        k.op(k.dve, lambda h: h.memset(s[:], 0.0), [], [s])
        if ysb is None:
            ybuf, yap = y, y[:]
        k.op(k.act, lambda h: h.activation(out=self.junk[:], in_=yap, func=AF.Square, accum_out=s[:, 0:1]), [ybuf, s], [self.junk, s])
        k.op(k.dve, lambda h: h.tensor_scalar(out=s[:, 1:2], in0=s[:, 0:1], scalar1=1.0 / D, scalar2=EPS,
                                              op0=ALU.mult, op1=ALU.add), [s], [s])
        k.op(k.act, lambda h: h.sqrt(out=s[:, 1:2], in_=s[:, 1:2]), [s], [s])
        k.op(k.dve, lambda h: h.reciprocal(out=s[:, 1:2], in_=s[:, 1:2]), [s], [s])
        k.op(k.dve, lambda h: h.scalar_tensor_tensor(out=o[:], in0=yap, scalar=s[:, 1:2], in1=gpb[:, kind, :],
                                                     op0=ALU.mult, op1=ALU.mult), [ybuf, s, gpb], [o])
        k.op(k.dve, lambda h: h.tensor_tensor(out=o[:], in0=o[:], in1=x[:], op=ALU.add), [o, x], [o])
        k.dma(dst, dst_ap, o, o[:])


def stage_ffn(P, l, src, dst, tiles, dst_row, moe, all_latent=False):
    k = P.k
    nff = (FF_EXP if moe else FF_DENSE) // 128
    nexp = NEXP if moe else 1
    bsz = 8 if moe else 12
    for b0 in range(0, len(tiles), bsz):
        bt = tiles[b0:b0 + bsz]
        nb = len(bt)
        ntok = nb * 128
        with k.scope():
            hxb = k.sb("hxb", [128, KC, bsz * 128], BF16)
            stage_norm(P, src, l, 1, hxb, bt, all_latent=all_latent)
            with k.scope():
                HT = k.sb("HT", [128, nff, bsz * 128], BF16)
                yacc = k.sb("yacc", [128, bsz, 1024], F32)
                wl = WLoader(k, nbuf=2, width=128, name="fw")
                wl2 = WLoader(k, nbuf=2, width=128, name="fu")
                wld = WLoader(k, nbuf=2, width=512, kc=4 if moe else 2, name="fd")
                pg = [k.ps("pg%d" % i, [128, 512]) for i in range(2)]
                pu = [k.ps("pu%d" % i, [128, 512]) for i in range(2)]
                pd = [k.ps("pd%d" % i, [128, 512]) for i in range(2)]
                sg = [k.sb("sg%d" % i, [128, 512], F32) for i in range(2)]
                gates = k.sb("gates", [128, 8, 8], F32)
                gp2 = k.sb("gp2x", [128, 2, 1024], F32)
                for kind_ in range(2):
                    k.dma(gp2, gp2[:, kind_, :], P.modc[l], P.modc[l].t[kind_:kind_ + 1, 5, :].to_broadcast([128, 1024]))
                if moe:
                    rws = k.sb("rws", [128, KC, 8], F32)
                    k.dma(rws, rws[:], P.router_w, P.router_w.t[0, :, :].rearrange("(k p) e -> p k e", p=128))
                    rwb = k.sb("rwb", [128, KC, 8], BF16)
                    k.op(k.dve, lambda h: h.tensor_copy(out=rwb[:], in_=rws[:]), [rws], [rwb])
                    rb = k.sb("rb", [128, 8], F32)
                    k.dma(rb, rb[:], P.router_b, P.router_b.t[0:1, :].to_broadcast([128, 8]))
                    lg = k.sb("lg", [128, 8], F32)
                    l2 = k.sb("l2", [128, 8], F32)
                    mk = k.sb("mk", [128, 2, 8], F32)
                    mx = k.sb("mx", [128, 4], F32)
                    for ti in range(nb):
                        p = pd[ti % 2]
                        k.mm([(lambda h, kk=kk: h.matmul(p[:, 0:8], lhsT=hxb[:, kk, ti * 128:(ti + 1) * 128], rhs=rwb[:, kk, :],
                                                         start=(kk == 0), stop=(kk == KC - 1))) for kk in range(KC)], [hxb, rwb], [p])
                        k.op(k.dve, lambda h: h.tensor_tensor(out=lg[:], in0=p[:, 0:8], in1=rb[:], op=ALU.add), [p, rb], [lg])
                        k.op(k.dve, lambda h: h.reduce_max(out=mx[:, 0:1], in_=lg[:], axis=AX.X), [lg], [mx])
                        k.op(k.dve, lambda h: h.tensor_scalar(out=mk[:, 0, :], in0=lg[:], scalar1=mx[:, 0:1], scalar2=None,
                                                              op0=ALU.is_ge), [lg, mx], [mk])
                        k.op(k.dve, lambda h: h.scalar_tensor_tensor(out=l2[:], in0=mk[:, 0, :], scalar=-1e30, in1=lg[:],
                                                                     op0=ALU.mult, op1=ALU.add), [mk, lg], [l2])
                        k.op(k.dve, lambda h: h.reduce_max(out=mx[:, 1:2], in_=l2[:], axis=AX.X), [l2], [mx])
                        k.op(k.dve, lambda h: h.tensor_scalar(out=mk[:, 1, :], in0=l2[:], scalar1=mx[:, 1:2], scalar2=None,
                                                              op0=ALU.is_ge), [l2, mx], [mk])
                        k.op(k.dve, lambda h: h.tensor_tensor(out=mx[:, 2:3], in0=mx[:, 0:1], in1=mx[:, 1:2], op=ALU.subtract), [mx], [mx])
                        k.op(k.act, lambda h: h.activation(out=mx[:, 2:3], in_=mx[:, 2:3], func=AF.Sigmoid), [mx], [mx])
                        k.op(k.dve, lambda h: h.tensor_scalar(out=mx[:, 3:4], in0=mx[:, 2:3], scalar1=-1.0, scalar2=1.0,
                                                              op0=ALU.mult, op1=ALU.add), [mx], [mx])
                        k.op(k.dve, lambda h: h.tensor_scalar(out=gates[:, ti, :], in0=mk[:, 0, :], scalar1=mx[:, 2:3], scalar2=None,
                                                              op0=ALU.mult), [mk, mx], [gates])
                        k.op(k.dve, lambda h: h.scalar_tensor_tensor(out=gates[:, ti, :], in0=mk[:, 1, :], scalar=mx[:, 3:4],
                                                                     in1=gates[:, ti, :], op0=ALU.mult, op1=ALU.add),
                             [mk, mx, gates], [gates])
                for e in range(nexp):
                    if moe:
                        wg_b, wg_ap = P.moe_w_gate, P.moe_w_gate.t[0, e]
                        wu_b, wu_ap = P.moe_w_up, P.moe_w_up.t[0, e]
                        wd_b, wd_ap = P.moe_w_down, P.moe_w_down.t[0, e]
                    else:
                        wg_b, wg_ap = P.ffn_w_gate, P.ffn_w_gate.t[0]
                        wu_b, wu_ap = P.ffn_w_up, P.ffn_w_up.t[0]
                        wd_b, wd_ap = P.ffn_w_down, P.ffn_w_down.t[0]
                    for c in range(nff):
                        wg = wl.get(wg_b, wg_ap[:, c * 128:(c + 1) * 128], 128)
                        wu = wl2.get(wu_b, wu_ap[:, c * 128:(c + 1) * 128], 128)
                        for bi, (t0, n) in enumerate(tok_blocks(ntok)):
                            a, b_ = pg[bi % 2], pu[bi % 2]
                            k.mm([(lambda h, kk=kk: h.matmul(a[:, :n], lhsT=wg[:, kk, :128], rhs=hxb[:, kk, t0:t0 + n],
                                                             start=(kk == 0), stop=(kk == KC - 1))) for kk in range(KC)], [wg, hxb], [a])
                            k.mm([(lambda h, kk=kk: h.matmul(b_[:, :n], lhsT=wu[:, kk, :128], rhs=hxb[:, kk, t0:t0 + n],
                                                             start=(kk == 0), stop=(kk == KC - 1))) for kk in range(KC)], [wu, hxb], [b_])
                            s_ = sg[bi % 2]
                            k.op(k.act, lambda h: h.activation(out=s_[:, :n], in_=a[:, :n], func=AF.Silu), [a], [s_])
                            k.op(k.dve, lambda h: h.tensor_tensor(out=HT[:, c, t0:t0 + n], in0=s_[:, :n], in1=b_[:, :n], op=ALU.mult),
                                 [s_, b_], [HT])
                    kcd = wld.kc
                    for pc in range(nff // kcd):
                        for hf in range(2):
                            wd = wld.get(wd_b, wd_ap[pc * kcd * 128:(pc + 1) * kcd * 128, hf * 512:(hf + 1) * 512], 512)
                            for ti in range(nb):
                                p = pd[ti % 2]
                                k.mm([(lambda h, kk=kk: h.matmul(p[:], lhsT=HT[:, pc * kcd + kk, ti * 128:(ti + 1) * 128], rhs=wd[:, kk, :],
                                                                 start=(kk == 0), stop=(kk == kcd - 1))) for kk in range(kcd)], [HT, wd], [p])
                                ya = yacc[:, ti, hf * 512:(hf + 1) * 512]
                                first = (e == 0 and pc == 0)
                                if moe:
                                    g_ = gates[:, ti, e:e + 1]
                                    if first:
                                        k.op(k.dve, lambda h: h.tensor_scalar(out=ya, in0=p[:], scalar1=g_, scalar2=None, op0=ALU.mult),
                                             [p, gates], [yacc])
                                    else:
                                        k.op(k.dve, lambda h: h.scalar_tensor_tensor(out=ya, in0=p[:], scalar=g_, in1=ya,
                                                                                     op0=ALU.mult, op1=ALU.add), [p, gates, yacc], [yacc])
                                else:
                                    if first:
                                        k.op(k.act, lambda h: h.copy(out=ya, in_=p[:]), [p], [yacc])
                                    else:
                                        k.op(k.dve, lambda h: h.tensor_tensor(out=ya, in0=ya, in1=p[:], op=ALU.add), [p, yacc], [yacc])
                ep = Epilogue(k, need_y=False)
                xt = [k.sb("fx%d" % i, [128, 1024], F32) for i in range(2)]
                for ti, i in enumerate(bt):
                    x = xt[ti % 2]
                    k.dma(x, x[:], src, src.t[i * 128:(i + 1) * 128, :])
                    r0 = dst_row(i)
                    ep.run(None, x, gp2, 1 if (i < 2 and not all_latent) else 0, dst, dst.t[r0:r0 + 128, :], ysb=(yacc, yacc[:, ti, :]))


def stage_select(P, src, dst):
    k = P.k
    with k.scope():
        sel = k.sb("sel", [128, 2], F32)
        k.dma(sel, sel[:], P.ext["sel"], P.ext["sel"].t[:, :])
        a = [k.sb("sa%d" % i, [128, 1024], F32) for i in range(2)]
        b = [k.sb("sb%d" % i, [128, 1024], F32) for i in range(2)]
        for j in range(16):
            x, y = a[j % 2], b[j % 2]
            i0, i1 = 2 + j, 2 + 16 + j
            k.dma(x, x[:], src, src.t[i0 * 128:(i0 + 1) * 128, :])
            k.dma(y, y[:], src, src.t[i1 * 128:(i1 + 1) * 128, :])
            k.op(k.dve, lambda h: h.tensor_scalar(out=x[:], in0=x[:], scalar1=sel[:, 0:1], scalar2=None, op0=ALU.mult), [x, sel], [x])
            k.op(k.dve, lambda h: h.scalar_tensor_tensor(out=x[:], in0=y[:], scalar=sel[:, 1:2], in1=x[:], op0=ALU.mult, op1=ALU.add),
                 [x, y, sel], [x])
            k.dma(dst, dst.t[j * 128:(j + 1) * 128, :], x, x[:])


def stage_select_mix(P, src, dst):
    k = P.k
    with k.scope():
        sel = k.sb("sel", [128, 2], F32)
        k.dma(sel, sel[:], P.ext["sel"], P.ext["sel"].t[:, :])
        H = SEQ // 2
        a = [k.sb("ma%d" % i, [128, H], BF16) for i in range(2)]
        b = [k.sb("mb%d" % i, [128, H], BF16) for i in range(2)]
        for r in range(4):
            x, y = a[r % 2], b[r % 2]
            k.dma(x, x[:], src, src.t[r * 128:(r + 1) * 128, CTX:CTX + H])
            k.dma(y, y[:], src, src.t[r * 128:(r + 1) * 128, CTX + H:T])
            k.op(k.dve, lambda h: h.tensor_scalar(out=x[:], in0=x[:], scalar1=sel[:, 0:1], scalar2=None, op0=ALU.mult), [x, sel], [x])
            k.op(k.dve, lambda h: h.scalar_tensor_tensor(out=x[:], in0=y[:], scalar=sel[:, 1:2], in1=x[:], op0=ALU.mult, op1=ALU.add),
                 [x, y, sel], [x])
            k.dma(dst, dst.t[r * 128:(r + 1) * 128, :], x, x[:])


def build_program():
    P = Prog()
    k = P.k
    P.xin = P.ext_in("xin", [T, D])
    P.c2 = P.ext_in("c2", [2, D])
    P.w_mod = P.ext_in("w_mod", [2, D, 6 * D])
    P.b_mod = P.ext_in("b_mod", [2, 6 * D])
    for n in ("norm_pre_mix", "norm_post_mix", "norm_pre_ffn", "norm_post_ffn"):
        setattr(P, n, P.ext_in(n, [2, D]))
    P.ext_in("ident", [128, 128])
    P.ext_in("sel", [128, 2])
    P.w_in = P.ext_in("w_in", [2, D, PROJ_W])
    P.ext_in("w_da_sw", [2, D, 1024])
    P.w_out = P.ext_in("w_out", [2, D, D])
    P.ext_in("rope_c", [128, T], BF16)
    P.ext_in("rope_s", [128, T], BF16)
    P.ext_in("cmask", [128, 2, 128], BF16)
    P.ext_in("bmask", [128, 8], BF16)
    P.ext_in("blk", [128, 128], BF16)
    P.ml_gate_b = P.ext_in("ml_gate_b", [2, 16])
    P.ml_norm = P.ext_in("ml_norm", [2, 256])
    P.hg_norm = P.ext_in("hg_norm", [2, 256])
    P.hg_lb_logits = P.ext_in("hg_lb_logits", [2, 256])
    P.da_lambda = P.ext_in("da_lambda", [2, 4, 64])
    P.da_norm = P.ext_in("da_norm", [2, 128])
    P.ffn_w_gate = P.ext_in("ffn_w_gate", [1, D, FF_DENSE])
    P.ffn_w_up = P.ext_in("ffn_w_up", [1, D, FF_DENSE])
    P.ffn_w_down = P.ext_in("ffn_w_down", [1, FF_DENSE, D])
    P.router_w = P.ext_in("router_w", [1, D, NEXP])
    P.router_b = P.ext_in("router_b", [1, NEXP])
    P.moe_w_gate = P.ext_in("moe_w_gate", [1, NEXP, D, FF_EXP])
    P.moe_w_up = P.ext_in("moe_w_up", [1, NEXP, D, FF_EXP])
    P.moe_w_down = P.ext_in("moe_w_down", [1, NEXP, FF_EXP, D])
    out = P.ext_out("out", [SEQ // 2, D])
    P.modc = [k.dram("modc%d" % l, [2, 6, D], F32) for l in range(2)]
    P.rec = dict(ml_q=k.dram("ml_q", [256, T], BF16), ml_k=k.dram("ml_k", [256, T], BF16), ml_o=k.dram("ml_o", [256, T], BF16),
                 ml_g=k.dram("ml_g", [1024, T], F32), hg_q=k.dram("hg_q", [256, T], BF16), hg_g=k.dram("hg_g", [256, T], BF16),
                 hg_f=k.dram("hg_f", [512, T], F32), ml_v=k.dram("ml_v", [T, 256], BF16), hg_v=k.dram("hg_v", [T, 256], BF16))
    mixT_d = k.dram("mixT_d", [1024, T], BF16)
    xmid = [k.dram("xmid%d" % l, [T, D], F32) for l in range(2)]
    x1 = k.dram("x1", [T, D], F32)
    xown = k.dram("xown", [SEQ // 2, D], F32)
    mixown = k.dram("mixown", [1024, SEQ // 2], BF16)
    setup_consts(P)
    stage_mod(P, 0)
    stage_mod(P, 1)
    lat_blocks = [(CTX + qb * 512, 512, list(range(NT))) for qb in range(SEQ // 512)]
    for l in range(2):
        src = P.xin if l == 0 else x1
        lam_init = 0.8 - 0.6 * float(np.exp(-0.3 * l))
        with k.scope():
            hxT = k.sb("hxT", [128, KC, T], BF16)
            stage_norm(P, src, l, 0, hxT, list(range(NT)))
            stage_proj_rec(P, l, hxT)
            if l == 0:
                qbl = [(0, CTX, [0, 1])] + list(lat_blocks)
                stage_attn(P, l, hxT, None, 0, 0, lam_init, qblocks=qbl, mixT_d=mixT_d)
            else:
                qbl = [(qb * 512, 512, list(range(NT))) for qb in range(SEQ // 2 // 512)]
                stage_attn(P, l, hxT, None, 0, 0, lam_init, qblocks=qbl, mixT_d=mixown, own=True)
        stage_rec(P, l, "ml", mixT_d)
        stage_rec(P, l, "hg", mixT_d)
        if l == 0:
            stage_postmix(P, l, mixT_d, src, xmid[0], list(range(NT)))
            stage_ffn(P, 0, xmid[0], x1, list(range(NT)), lambda i: i * 128, moe=False)
        else:
            stage_select_mix(P, mixT_d, mixown)
            stage_select(P, x1, xown)
            stage_postmix(P, l, mixown, xown, xmid[1], list(range(16)), all_latent=True)
            stage_ffn(P, 1, xmid[1], out, list(range(16)), lambda i: i * 128, moe=True, all_latent=True)
    k.finish([out])
    return k.nc


_CACHE = {}


def kernel(x, c, ctx, c_ctx, w_mod, b_mod, norm_pre_mix, norm_post_mix, norm_pre_ffn, norm_post_ffn,
           w_in, w_out, ml_gate_b, ml_norm, hg_lb_logits, hg_norm, da_lambda, da_norm,
           ffn_w_gate, ffn_w_up, ffn_w_down, router_w, router_b, moe_w_gate, moe_w_up, moe_w_down):
    from concourse.bass_utils import run_bass_kernel_spmd
    f = lambda a: np.ascontiguousarray(np.asarray(a, dtype=np.float32))
    x, c, ctx, c_ctx = f(x), f(c), f(ctx), f(c_ctx)
    w_in = f(w_in)
    if "nc" not in _CACHE:
        _CACHE["nc"] = build_program()
    nc = _CACHE["nc"]
    rc, rs = rope_tables()
    cm, bmk, blk = rec_consts()
    shared = dict(w_mod=f(w_mod), b_mod=f(b_mod), norm_pre_mix=f(norm_pre_mix), norm_post_mix=f(norm_post_mix),
                  norm_pre_ffn=f(norm_pre_ffn), norm_post_ffn=f(norm_post_ffn), ident=np.eye(128, dtype=np.float32),
                  w_in=w_in, w_da_sw=swap_cols(w_in), w_out=f(w_out), rope_c=rc, rope_s=rs, cmask=cm, bmask=bmk, blk=blk,
                  ml_gate_b=f(ml_gate_b), ml_norm=f(ml_norm), hg_norm=f(hg_norm), hg_lb_logits=f(hg_lb_logits),
                  da_lambda=f(da_lambda), da_norm=f(da_norm), ffn_w_gate=f(ffn_w_gate), ffn_w_up=f(ffn_w_up),
                  ffn_w_down=f(ffn_w_down), router_w=f(router_w), router_b=f(router_b), moe_w_gate=f(moe_w_gate),
                  moe_w_up=f(moe_w_up), moe_w_down=f(moe_w_down))
    in_maps = []
    for core in range(8):
        b, half = core // 2, core % 2
        m = dict(shared)
        m["xin"] = np.ascontiguousarray(np.concatenate([ctx[b], x[b]], axis=0))
        m["c2"] = np.ascontiguousarray(np.stack([c[b], c_ctx], axis=0))
        sel = np.zeros((128, 2), np.float32)
        sel[:, half] = 1.0
        m["sel"] = sel
        in_maps.append(m)
    res = run_bass_kernel_spmd(nc, in_maps, core_ids=list(range(8)))
    out = np.zeros((4, SEQ, D), np.float32)
    for core in range(8):
        b, half = core // 2, core % 2
        out[b, half * (SEQ // 2):(half + 1) * (SEQ // 2)] = np.asarray(res.results[core]["out"], dtype=np.float32)
    return out
```

```python
import numpy as np
from contextlib import ExitStack
import concourse.bass as bass
import concourse.mybir as mybir

F32 = mybir.dt.float32
BF16 = mybir.dt.bfloat16
AF = mybir.ActivationFunctionType
ALU = mybir.AluOpType
AX = mybir.AxisListType

SAME_ENG_SYNC = True


class Buf:
    def __init__(self, k, t, name, space):
        self.k, self.t, self.name, self.space = k, t, name, space
        self.w = {}
        self.r = {}
        self.dsem = None

    def __getitem__(self, idx):
        return self.t[idx]


class Eng:
    def __init__(self, k, name, h):
        self.k, self.name, self.h = k, name, h
        self.sem = k.es.enter_context(k.nc.semaphore("e_" + name))
        self.cnt = 0
        self.waited = {}


class _Scope:
    def __init__(self, k):
        self.k = k

    def __enter__(self):
        k = self.k
        self.prev = k.es
        self.st = ExitStack()
        k.es = self.st
        k.scopes.append([])
        return self

    def __exit__(self, *a):
        k = self.k
        k.barrier()
        for b in k.scopes.pop():
            if b.dsem:
                for qn_, s_ in b.dsem.items():
                    (k.dpool_sw if qn_ == "pool" else k.dpool).append(s_)
        self.st.close()
        k.es = self.prev
        return False


class K:
    def __init__(self):
        self.nc = bass.Bass("TRN2", target_bir_lowering=False)
        self.es = ExitStack()
        nc = self.nc
        self.pe = Eng(self, "pe", nc.tensor)
        self.act = Eng(self, "act", nc.scalar)
        self.dve = Eng(self, "dve", nc.vector)
        self.pool = Eng(self, "pool", nc.gpsimd)
        self.sp = Eng(self, "sp", nc.sync)
        self.sems = {}
        self.totals = {}
        for e in (self.pe, self.act, self.dve, self.pool, self.sp):
            self.sems[e.sem.name] = e.sem
        self.ndsem = 0
        self.dq = 0
        self.dpool = []
        self.dpool_sw = []
        for i in range(88):
            s = self.es.enter_context(self.nc.semaphore("d%d" % i))
            self.sems[s.name] = s
            self.totals[s.name] = 0
            (self.dpool if i < 44 else self.dpool_sw).append(s)
        self.scopes = []

    def sb(self, name, shape, dt, dshare=None):
        self.nuniq = getattr(self, "nuniq", 0) + 1
        t = self.es.enter_context(self.nc.sbuf_tensor("%s_%d" % (name, self.nuniq), list(shape), dt))
        b = Buf(self, t, name, "sb")
        if self.scopes:
            self.scopes[-1].append(b)
        return b

    def ps(self, name, shape, dt=F32):
        self.nuniq = getattr(self, "nuniq", 0) + 1
        t = self.es.enter_context(self.nc.psum_tensor("%s_%d" % (name, self.nuniq), list(shape), dt))
        return Buf(self, t, name, "ps")

    def scope(self):
        return _Scope(self)

    def barrier(self):
        engs = (self.pe, self.act, self.dve, self.pool, self.sp)
        for e in engs:
            for o in engs:
                if o is e or o.cnt == 0:
                    continue
                if e.waited.get(o.sem.name, 0) < o.cnt:
                    e.h.wait_ge(o.sem, o.cnt)
                    e.waited[o.sem.name] = o.cnt
            for s, v in self.totals.items():
                if v and e.waited.get(s, 0) < v:
                    e.h.wait_ge(self.sems[s], v)
                    e.waited[s] = v

    def dram(self, name, shape, dt, kind="Internal"):
        t = self.nc.dram_tensor(name, list(shape), dt, kind=kind)
        return Buf(self, t.ap(), name, "dram")

    def _dsem(self, b, qn="sp"):
        if b.dsem is None:
            b.dsem = {}
        if qn not in b.dsem:
            pool = self.dpool_sw if qn == "pool" else self.dpool
            b.dsem[qn] = pool.pop()
        return b.dsem[qn]

    def _deps(self, reads, writes):
        deps = {}
        self._raw = {}
        for b in reads:
            for s, v in b.w.items():
                deps[s] = max(deps.get(s, 0), v)
                self._raw[s] = max(self._raw.get(s, 0), v)
        for b in writes:
            for s, v in b.w.items():
                deps[s] = max(deps.get(s, 0), v)
            for s, v in b.r.items():
                deps[s] = max(deps.get(s, 0), v)
        return deps

    def _wait(self, eng, deps):
        for s, v in deps.items():
            if s in self.totals:
                v = self.totals[s]
            if s == eng.sem.name and not SAME_ENG_SYNC:
                continue
            if s == eng.sem.name and eng is self.pe:
                continue
            if s == eng.sem.name:
                if s not in self._raw:
                    continue
                v = self._raw[s]
            if eng.waited.get(s, 0) >= v:
                continue
            eng.h.wait_ge(self.sems[s], v)
            eng.waited[s] = v

    def _mark(self, tick, reads, writes):
        s, v = tick
        for b in writes:
            b.w[s] = max(b.w.get(s, 0), v)
            b.r = {}
        for b in reads:
            b.r[s] = max(b.r.get(s, 0), v)

    def op(self, eng, fn, reads=(), writes=()):
        self._wait(eng, self._deps(reads, writes))
        ins = fn(eng.h)
        eng.cnt += 1
        ins.then_inc(eng.sem, 1)
        self._mark((eng.sem.name, eng.cnt), reads, writes)
        return ins

    def mm(self, fns, reads=(), writes=()):
        eng = self.pe
        self._wait(eng, self._deps(reads, writes))
        ins = None
        for fn in fns:
            ins = fn(eng.h)
        eng.cnt += 1
        ins.then_inc(eng.sem, 1)
        self._mark((eng.sem.name, eng.cnt), reads, writes)

    def dma(self, out_b, out_ap, in_b, in_ap, q=None, **kw):
        if q is None:
            q = (self.sp, self.pool)[self.dq % 2]
            self.dq += 1
        sb = out_b if out_b.space != "dram" else in_b
        if sb.space == "dram":
            sb = out_b
        s = self._dsem(sb, q.name)
        self._wait(q, self._deps([in_b], [out_b]))
        ins = q.h.dma_start(out=out_ap, in_=in_ap, **kw)
        ins.then_inc(s, 16)
        self.totals[s.name] += 16
        self._mark((s.name, self.totals[s.name]), [in_b], [out_b])
        return ins

    def finish(self, bufs):
        deps = {}
        for b in bufs:
            for s, v in b.w.items():
                deps[s] = max(deps.get(s, 0), v)
        self._wait(self.sp, deps)
        for e in (self.pe, self.act, self.dve, self.pool):
            if e.cnt:
                self.sp.h.wait_ge(e.sem, e.cnt)
        for s, v in self.totals.items():
            if v and self.sp.waited.get(s, 0) < v:
                self.sp.h.wait_ge(self.sems[s], v)
        self.es.close()


D = 1024
KC = 8
SEQ = 4096
CTX = 256
T = SEQ + CTX
NT = T // 128
PROJ_W = 3856
FF_DENSE = 2816
FF_EXP = 3584
NEXP = 8
EPS = 1e-6
C_ML_Q, C_ML_K, C_ML_V, C_ML_O, C_ML_G = 0, 256, 512, 768, 1024
C_HG_Q, C_HG_F, C_HG_I, C_HG_G = 1040, 1296, 1808, 2064
C_DA_Q, C_DA_K, C_DA_V = 2320, 2832, 3344


def tok_blocks(n, bs=512):
    out = []
    o = 0
    while o < n:
        out.append((o, min(bs, n - o)))
        o += bs
    return out


class Prog:
    def __init__(self, dbg=None):
        self.k = K()
        self.dbg = dbg or {}
        self.ext = {}

    def ext_in(self, name, shape, dt=F32):
        b = self.k.dram(name, shape, dt, kind="ExternalInput")
        self.ext[name] = b
        return b

    def ext_out(self, name, shape, dt=F32):
        b = self.k.dram(name, shape, dt, kind="ExternalOutput")
        self.ext[name] = b
        return b


def stage_mod(P, l):
    k = P.k
    with k.scope():
        cT = k.sb("cT", [128, KC, 2], F32)
        for kk in range(KC):
            k.dma(cT, cT[:, kk, :], P.c2, P.c2.t[:, kk * 128:(kk + 1) * 128].rearrange("r p -> p r"),
                  allow_slow_non_contiguous=True)
        k.op(k.act, lambda h: h.activation(out=cT[:], in_=cT[:], func=AF.Silu), [cT], [cT])
        modv = k.sb("modv", [2, 6, 1024], F32)
        bm = k.sb("bm", [2, 6, 1024], F32)
        k.dma(bm, bm[:], P.b_mod,
              P.b_mod.t[l:l + 1, :].rearrange("o (s d) -> o s d", s=6).to_broadcast([2, 6, 1024]))
        nrm = k.sb("nrm", [2, 4, 1024], F32)
        for j, nb in enumerate((P.norm_pre_mix, P.norm_post_mix, P.norm_pre_ffn, P.norm_post_ffn)):
            k.dma(nrm, nrm[:, j, :], nb, nb.t[l:l + 1, :].to_broadcast([2, 1024]))
        wst = [k.sb("wst%d" % i, [128, KC, 512], F32) for i in range(4)]
        pm = [k.ps("pm%d" % i, [2, 512]) for i in range(4)]
        for cb in range(12):
            w = wst[cb % 4]
            p = pm[cb % 4]
            k.dma(w, w[:], P.w_mod, P.w_mod.t[l, :, cb * 512:(cb + 1) * 512].rearrange("(k p) n -> p k n", p=128))
            k.mm([(lambda h, kk=kk: h.matmul(p[:], lhsT=cT[:, kk, :], rhs=w[:, kk, :],
                                             start=(kk == 0), stop=(kk == KC - 1))) for kk in range(KC)],
                 [cT, w], [p])
            s, o = divmod(cb * 512, 1024)
            k.op(k.dve, lambda h: h.tensor_tensor(out=modv[:, s, o:o + 512], in0=p[:], in1=bm[:, s, o:o + 512],
                                                  op=ALU.add), [p, bm], [modv])
        modc = k.sb("modc", [2, 6, 1024], F32)
        for (dst, msc, nj) in ((0, 1, 0), (3, 4, 2)):
            k.op(k.dve, lambda h: h.scalar_tensor_tensor(out=modc[:, dst, :], in0=modv[:, msc, :], scalar=1.0,
                                                         in1=nrm[:, nj, :], op0=ALU.add, op1=ALU.mult),
                 [modv, nrm], [modc])
        for (dst, msh) in ((1, 0), (4, 3)):
            k.op(k.dve, lambda h: h.tensor_copy(out=modc[:, dst, :], in_=modv[:, msh, :]), [modv], [modc])
        for (dst, mg, nj) in ((2, 2, 1), (5, 5, 3)):
            k.op(k.dve, lambda h: h.tensor_tensor(out=modc[:, dst, :], in0=modv[:, mg, :], in1=nrm[:, nj, :],
                                                  op=ALU.mult), [modv, nrm], [modc])
        k.dma(P.modc[l], P.modc[l].t[:, :, :], modc, modc[:])


def stage_norm(P, src, l, which, hxT, tiles, all_latent=False):
    k = P.k
    gi, si = (0, 1) if which == 0 else (3, 4)
    with k.scope():
        gb = k.sb("gb", [128, 2, 1024], F32)
        shb = k.sb("shb", [128, 2, 1024], F32)
        for kind in range(2):
            k.dma(gb, gb[:, kind, :], P.modc[l], P.modc[l].t[kind:kind + 1, gi, :].to_broadcast([128, 1024]))
            k.dma(shb, shb[:, kind, :], P.modc[l], P.modc[l].t[kind:kind + 1, si, :].to_broadcast([128, 1024]))
        xt = [k.sb("xt%d" % i, [128, 1024], F32) for i in range(2)]
        junk = k.sb("junk", [128, 1024], BF16)
        ss = [k.sb("ss%d" % i, [128, 2], F32) for i in range(2)]
        yf = k.sb("yf", [128, 1024], F32)
        yb = [k.sb("yb%d" % i, [128, 1024], BF16) for i in range(2)]
        pt = [k.ps("pt%d" % i, [128, KC, 128], BF16) for i in range(2)]
        for n, i in enumerate(tiles):
            kind = 1 if (i < 2 and not all_latent) else 0
            x, s, y, p = xt[n % 2], ss[n % 2], yb[n % 2], pt[n % 2]
            k.dma(x, x[:], src, src.t[i * 128:(i + 1) * 128, :])
            k.op(k.dve, lambda h: h.memset(s[:], 0.0), [], [s])
            k.op(k.act, lambda h: h.activation(out=junk[:], in_=x[:], func=AF.Square, accum_out=s[:, 0:1]),
                 [x, s], [junk, s])
            k.op(k.dve, lambda h: h.tensor_scalar(out=s[:, 1:2], in0=s[:, 0:1], scalar1=1.0 / D, scalar2=EPS,
                                                  op0=ALU.mult, op1=ALU.add), [s], [s])
            k.op(k.act, lambda h: h.sqrt(out=s[:, 1:2], in_=s[:, 1:2]), [s], [s])
            k.op(k.dve, lambda h: h.reciprocal(out=s[:, 1:2], in_=s[:, 1:2]), [s], [s])
            k.op(k.dve, lambda h: h.scalar_tensor_tensor(out=yf[:], in0=x[:], scalar=s[:, 1:2], in1=gb[:, kind, :],
                                                         op0=ALU.mult, op1=ALU.mult), [x, s, gb], [yf])
            k.op(k.dve, lambda h: h.tensor_tensor(out=y[:], in0=yf[:], in1=shb[:, kind, :], op=ALU.add),
                 [yf, shb], [y])
            k.mm([(lambda h, kk=kk: h.transpose(out=p[:, kk, :], in_=y[:, kk * 128:(kk + 1) * 128],
                                                identity=P.ident[:])) for kk in range(KC)], [y, P.ident], [p])
            k.op(k.act, lambda h: h.copy(out=hxT[:, :, n * 128:(n + 1) * 128], in_=p[:]), [p], [hxT])


def setup_consts(P):
    k = P.k
    idf = k.sb("idf", [128, 128], F32)
    k.dma(idf, idf[:], P.ext["ident"], P.ext["ident"].t[:, :])
    P.ident = k.sb("ident", [128, 128], BF16)
    k.op(k.dve, lambda h: h.tensor_copy(out=P.ident[:], in_=idf[:]), [idf], [P.ident])


class WLoader:
    def __init__(self, k, nbuf=2, width=512, kc=KC, name="wl"):
        self.k = k
        self.kc = kc
        self.wb = [k.sb(name + "b%d" % i, [128, kc, width], BF16) for i in range(nbuf)]
        self.i = 0
        self.nbuf = nbuf

    def get(self, wbuf, w_ap, n):
        k = self.k
        wb = self.wb[self.i % self.nbuf]
        self.i += 1
        k.dma(wb, wb[:, :, :n], wbuf, w_ap.rearrange("(k p) n -> p k n", p=128), q=k.pool)
        return wb


def lin_fm(k, wb, ncol, src, ntok, evac, ps_pool, kc=KC, bs=512):
    for bi, (t0, n) in enumerate(tok_blocks(ntok, bs)):
        p = ps_pool[bi % len(ps_pool)]
        k.mm([(lambda h, kk=kk: h.matmul(p[:ncol, :n], lhsT=wb[:, kk, :ncol], rhs=src[:, kk, t0:t0 + n],
                                         start=(kk == 0), stop=(kk == kc - 1))) for kk in range(kc)],
             [wb, src], [p])
        evac(p, t0, n)


def lin_tm(k, wb, ncol, src, tiles, evac, ps_pool, kc=KC):
    for ti, i in enumerate(tiles):
        p = ps_pool[ti % len(ps_pool)]
        k.mm([(lambda h, kk=kk: h.matmul(p[:, :ncol], lhsT=src[:, kk, i * 128:(i + 1) * 128], rhs=wb[:, kk, :ncol],
                                         start=(kk == 0), stop=(kk == kc - 1))) for kk in range(kc)],
             [wb, src], [p])
        evac(p, i)


def stage_attn(P, l, hxT, mixT, q0, nq, lam_init, heads=(0, 1, 2, 3), qblocks=None, mixT_d=None, own=False):
    k = P.k
    w_in = P.w_in
    with k.scope():
        wl = WLoader(k, nbuf=2, width=128, name="aw")
        ropeC = k.sb("ropeC", [128, T], BF16)
        ropeS = k.sb("ropeS", [128, T], BF16)
        k.dma(ropeC, ropeC[:], P.ext["rope_c"], P.ext["rope_c"].t[:, :])
        k.dma(ropeS, ropeS[:], P.ext["rope_s"], P.ext["rope_s"].t[:, :])
        lp = k.sb("lp", [128, 4, 64], F32)
        k.dma(lp, lp[:], P.da_lambda, P.da_lambda.t[l:l + 1, :, :].to_broadcast([128, 4, 64]))
        lt = k.sb("lt", [128, 2, 64], F32)
        lam = k.sb("lam", [128, 4], F32)
        k.op(k.dve, lambda h: h.tensor_tensor(out=lt[:, 0, :], in0=lp[:, 0, :], in1=lp[:, 1, :], op=ALU.mult), [lp], [lt])
        k.op(k.dve, lambda h: h.tensor_tensor(out=lt[:, 1, :], in0=lp[:, 2, :], in1=lp[:, 3, :], op=ALU.mult), [lp], [lt])
        k.op(k.dve, lambda h: h.reduce_sum(out=lam[:, 0:2], in_=lt[:], axis=AX.X), [lt], [lam])
        k.op(k.act, lambda h: h.activation(out=lam[:, 0:2], in_=lam[:, 0:2], func=AF.Exp), [lam], [lam])
        k.op(k.dve, lambda h: h.tensor_tensor(out=lam[:, 2:3], in0=lam[:, 1:2], in1=lam[:, 0:1], op=ALU.subtract), [lam], [lam])
        k.op(k.dve, lambda h: h.tensor_scalar(out=lam[:, 3:4], in0=lam[:, 2:3], scalar1=-float(lam_init), scalar2=None,
                                              op0=ALU.add), [lam], [lam])
        dn = k.sb("dn", [128, 128], F32)
        k.dma(dn, dn[:], P.da_norm, P.da_norm.t[l:l + 1, :].to_broadcast([128, 128]))
        k.op(k.dve, lambda h: h.tensor_scalar(out=dn[:], in0=dn[:], scalar1=float(1.0 - lam_init), scalar2=None,
                                              op0=ALU.mult), [dn], [dn])

        qT = k.sb("qT", [128, T], BF16)
        kT = k.sb("kT", [128, T], BF16)
        if own:
            qTo = k.sb("qTo", [128, SEQ // 2], BF16)
            selt = k.sb("selt", [128, 2], F32)
            k.dma(selt, selt[:], P.ext["sel"], P.ext["sel"].t[:, :])
        va = k.sb("va", [128, NT, 132], BF16)
        k.op(k.dve, lambda h: h.memset(va[:, :, 128:132], 1.0), [], [va])
        t1 = k.sb("t1", [128, 512], F32)
        t2 = k.sb("t2", [128, 512], F32)
        sT2 = [[k.ps("sT%d_%d" % (i, j), [128, 512]) for j in range(2)] for i in range(2)]
        pp = [sT2[0][0], sT2[0][1]]
        osbs = [k.sb("osb%d" % i, [128, 3, 3, 132], F32) for i in range(2)]
        blk_i = [0]
        oacc = [k.ps("oacc%d" % i, [128, 3, 132]) for i in range(3)]
        pT = [[k.sb("pT%d_%d" % (i, j), [128, 512], BF16) for j in range(2)] for i in range(2)]
        fin = k.sb("fin", [128, 8], F32)
        o0 = k.sb("o0", [128, 128], F32)
        o1 = k.sb("o1", [128, 128], F32)
        junk = k.sb("ajunk", [128, 128], F32)
        ob = k.sb("ob", [128, 128], BF16)
        ptr = k.ps("ptr", [128, 128], BF16)
        ost = k.sb("ost", [128, 512], BF16)
        nkc = NT
        for hd in heads:
            for (dst, c_main, c_sw) in ((qT, C_DA_Q + hd * 128, hd * 128), (kT, C_DA_K + hd * 128, 512 + hd * 128)):
                wm = wl.get(w_in, w_in.t[l, :, c_main:c_main + 128], 128)
                ws = wl.get(P.ext["w_da_sw"], P.ext["w_da_sw"].t[l, :, c_sw:c_sw + 128], 128)
                for bi, (t0, n) in enumerate(tok_blocks(T)):
                    pa, pb = pp[0], pp[1]
                    k.mm([(lambda h, kk=kk: h.matmul(pa[:, :n], lhsT=wm[:, kk, :128], rhs=hxT[:, kk, t0:t0 + n],
                                                     start=(kk == 0), stop=(kk == KC - 1))) for kk in range(KC)],
                         [wm, hxT], [pa])
                    k.mm([(lambda h, kk=kk: h.matmul(pb[:, :n], lhsT=ws[:, kk, :128], rhs=hxT[:, kk, t0:t0 + n],
                                                     start=(kk == 0), stop=(kk == KC - 1))) for kk in range(KC)],
                         [ws, hxT], [pb])
                    k.op(k.dve, lambda h: h.tensor_tensor(out=t1[:, :n], in0=pa[:, :n], in1=ropeC[:, t0:t0 + n],
                                                          op=ALU.mult), [pa, ropeC], [t1])
                    k.op(k.dve, lambda h: h.tensor_tensor(out=t2[:, :n], in0=pb[:, :n], in1=ropeS[:, t0:t0 + n],
                                                          op=ALU.mult), [pb, ropeS], [t2])
                    k.op(k.dve, lambda h: h.tensor_tensor(out=dst[:, t0:t0 + n], in0=t1[:, :n], in1=t2[:, :n],
                                                          op=ALU.add), [t1, t2], [dst])
            wv = wl.get(w_in, w_in.t[l, :, C_DA_V + hd * 128:C_DA_V + (hd + 1) * 128], 128)
            for i in range(NT):
                pa = pp[i % 2]
                k.mm([(lambda h, kk=kk: h.matmul(pa[:, :128], lhsT=hxT[:, kk, i * 128:(i + 1) * 128], rhs=wv[:, kk, :128],
                                                 start=(kk == 0), stop=(kk == KC - 1))) for kk in range(KC)],
                     [wv, hxT], [pa])
                k.op(k.act, lambda h: h.copy(out=va[:, i, 0:128], in_=pa[:, :128]), [pa], [va])
            qsrc = qT
            if own:
                h0, h1 = CTX, CTX + SEQ // 2
                k.op(k.dve, lambda h: h.tensor_scalar(out=qTo[:], in0=qT[:, h0:h1], scalar1=selt[:, 0:1], scalar2=None,
                                                      op0=ALU.mult), [qT, selt], [qTo])
                k.op(k.dve, lambda h: h.scalar_tensor_tensor(out=qTo[:], in0=qT[:, h1:T], scalar=selt[:, 1:2], in1=qTo[:],
                                                             op0=ALU.mult, op1=ALU.add), [qT, selt, qTo], [qTo])
                qsrc = qTo
            if qblocks is None:
                qblocks = [(q0 + qb * 512, 512, list(range(NT))) for qb in range(nq // 512)]
            for (qs, nqb, ktiles) in qblocks:
                nqt = nqb // 128
                nkc = len(ktiles)
                glist = [c_ * 4 + q_ for c_ in range(2) for q_ in range(nqt)]

                def emit_S(kci):
                    kc = ktiles[kci]
                    for comp in range(2):
                        sp_ = sT2[comp][kci % 2]
                        r0 = comp * 64
                        k.mm([lambda h: h.matmul(sp_[:, :nqb], lhsT=kT[r0:r0 + 64, kc * 128:(kc + 1) * 128],
                                                 rhs=qsrc[r0:r0 + 64, qs:qs + nqb], start=True, stop=True)],
                             [kT, qsrc], [sp_])

                def emit_exp(kci):
                    for comp in range(2):
                        sp_ = sT2[comp][kci % 2]
                        pt_ = pT[comp][kci % 2]
                        k.op(k.act, lambda h: h.activation(out=pt_[:, :nqb], in_=sp_[:, :nqb], func=AF.Exp, scale=0.125),
                             [sp_], [pt_])

                def emit_PV(kci):
                    kc = ktiles[kci]
                    for comp in range(2):
                        pt_ = pT[comp][kci % 2]
                        fns = []
                        for qt in range(nqt):
                            g = comp * 4 + qt
                            ot = oacc[g // 3]
                            fns.append(lambda h, ot=ot, g=g, qt=qt: h.matmul(
                                ot[:, g % 3, 0:129], lhsT=pt_[:, qt * 128:(qt + 1) * 128], rhs=va[:, kc, 0:129],
                                start=(kci == 0 and g == min(x for x in glist if x // 3 == g // 3)), stop=(kci == nkc - 1),
                                skip_group_check=True))
                        k.mm(fns, [pt_, va], oacc)

                emit_S(0)
                emit_exp(0)
                for kci in range(nkc):
                    if kci + 1 < nkc:
                        emit_S(kci + 1)
                    emit_PV(kci)
                    if kci + 1 < nkc:
                        emit_exp(kci + 1)
                osb = osbs[blk_i[0] % 2]
                blk_i[0] += 1
                for j in range(3):
                    k.op(k.dve, lambda h: h.tensor_copy(out=osb[:, j, :, :], in_=oacc[j][:]), [oacc[j]], [osb])
                for qt in range(nqt):
                    g0, g1 = qt, 4 + qt
                    k.op(k.dve, lambda h: h.reciprocal(out=fin[:, 0:1], in_=osb[:, g0 // 3, g0 % 3, 128:129]), [osb], [fin])
                    k.op(k.dve, lambda h: h.reciprocal(out=fin[:, 1:2], in_=osb[:, g1 // 3, g1 % 3, 128:129]), [osb], [fin])
                    k.op(k.dve, lambda h: h.tensor_tensor(out=fin[:, 1:2], in0=fin[:, 1:2], in1=lam[:, 3:4], op=ALU.mult),
                         [fin, lam], [fin])
                    k.op(k.dve, lambda h: h.tensor_scalar(out=o0[:], in0=osb[:, g0 // 3, g0 % 3, 0:128], scalar1=fin[:, 0:1],
                                                          scalar2=None, op0=ALU.mult), [osb, fin], [o0])
                    k.op(k.dve, lambda h: h.scalar_tensor_tensor(out=o1[:], in0=osb[:, g1 // 3, g1 % 3, 0:128], scalar=fin[:, 1:2],
                                                                 in1=o0[:], op0=ALU.mult, op1=ALU.add),
                         [osb, fin, o0], [o1])
                    k.op(k.dve, lambda h: h.tensor_tensor(out=junk[:], in0=o1[:], in1=o1[:], op=ALU.mult), [o1], [junk])
                    k.op(k.dve, lambda h: h.reduce_sum(out=fin[:, 2:3], in_=junk[:], axis=AX.X), [junk], [fin])
                    k.op(k.dve, lambda h: h.tensor_scalar(out=fin[:, 3:4], in0=fin[:, 2:3], scalar1=1.0 / 128, scalar2=EPS,
                                                          op0=ALU.mult, op1=ALU.add), [fin], [fin])
                    k.op(k.act, lambda h: h.sqrt(out=fin[:, 3:4], in_=fin[:, 3:4]), [fin], [fin])
                    k.op(k.dve, lambda h: h.reciprocal(out=fin[:, 3:4], in_=fin[:, 3:4]), [fin], [fin])
                    k.op(k.dve, lambda h: h.scalar_tensor_tensor(out=ob[:], in0=o1[:], scalar=fin[:, 3:4], in1=dn[:],
                                                                 op0=ALU.mult, op1=ALU.mult), [o1, fin, dn], [ob])
                    k.mm([lambda h: h.transpose(out=ptr[:], in_=ob[:], identity=P.ident[:])], [ob, P.ident], [ptr])
                    c0 = qs + qt * 128
                    if mixT_d is None:
                        k.op(k.dve, lambda h: h.tensor_copy(out=mixT[:, 4 + hd, c0:c0 + 128], in_=ptr[:]), [ptr], [mixT])
                    else:
                        k.op(k.dve, lambda h: h.tensor_copy(out=ost[:, qt * 128:(qt + 1) * 128], in_=ptr[:]), [ptr], [ost])
                if mixT_d is not None:
                    r0_ = 512 + hd * 128
                    k.dma(mixT_d, mixT_d.t[r0_:r0_ + 128, qs:qs + nqb], ost, ost[:, :nqb])


def rope_tables():
    n = np.arange(SEQ)
    row = (n // 64).astype(np.float64)
    col = (n % 64).astype(np.float64)
    inv = 10000.0 ** (-np.arange(16, dtype=np.float64) / 16)
    C = np.ones((64, T), np.float64)
    S = np.zeros((64, T), np.float64)
    for d in range(64):
        pos = row if d < 32 else col
        f = d % 16
        ang = pos * inv[f]
        C[d, CTX:] = np.cos(ang)
        sgn = -1.0 if (d % 32) < 16 else 1.0
        S[d, CTX:] = sgn * np.sin(ang)
    import ml_dtypes
    C2 = np.concatenate([C, C], 0).astype(np.float32).astype(ml_dtypes.bfloat16)
    S2 = np.concatenate([S, S], 0).astype(np.float32).astype(ml_dtypes.bfloat16)
    return C2, S2


def swap_cols(w_in):
    perm64 = np.concatenate([np.arange(16, 32), np.arange(0, 16), np.arange(48, 64), np.arange(32, 48)])
    idx = []
    for base in (C_DA_Q, C_DA_K):
        for u in range(8):
            idx.append(base + u * 64 + perm64)
    idx = np.concatenate(idx)
    return np.ascontiguousarray(w_in[:, :, idx])


def stage_proj_rec(P, l, hxT):
    k = P.k
    w_in = P.w_in
    R = P.rec
    with k.scope():
        wl = WLoader(k, nbuf=2, width=128, name="pw")
        pp = [k.ps("rp%d" % i, [128, 512]) for i in range(3)]
        stf = [k.sb("stf%d" % i, [128, 512], F32) for i in range(2)]
        stb = [k.sb("stb%d" % i, [128, 512], BF16) for i in range(2)]
        gb = k.sb("gbias", [128, 8], F32)
        for ty in range(4):
            for hp in range(2):
                for hh in range(2):
                    c = ty * 4 + hp * 2 + hh
                    k.dma(gb, gb[hh * 64:(hh + 1) * 64, ty * 2 + hp:ty * 2 + hp + 1], P.ml_gate_b,
                          P.ml_gate_b.t[l:l + 1, c:c + 1].to_broadcast([64, 1]))
        cnt = [0]

        def fm_job(wb, dst, row0, func=None, scale=1.0, bias=None, f32=False):
            def evac(p, t0, n):
                i = cnt[0] % 2
                cnt[0] += 1
                st = stf[i] if f32 else stb[i]
                if func is None and bias is None:
                    k.op(k.dve, lambda h: h.tensor_scalar(out=st[:, :n], in0=p[:, :n], scalar1=float(scale), scalar2=None,
                                                          op0=ALU.mult), [p], [st])
                elif func is None:
                    k.op(k.dve, lambda h: h.tensor_scalar(out=st[:, :n], in0=p[:, :n], scalar1=bias, scalar2=None,
                                                          op0=ALU.add), [p, gb], [st])
                else:
                    k.op(k.act, lambda h: h.activation(out=st[:, :n], in_=p[:, :n], func=func), [p], [st])
                k.dma(dst, dst.t[row0:row0 + 128, t0:t0 + n], st, st[:, :n])
            lin_fm(k, wb, 128, hxT, T, evac, pp)

        for c in range(2):
            fm_job(wl.get(w_in, w_in.t[l, :, C_ML_Q + c * 128:C_ML_Q + (c + 1) * 128], 128), R["ml_q"], c * 128)
            fm_job(wl.get(w_in, w_in.t[l, :, C_ML_K + c * 128:C_ML_K + (c + 1) * 128], 128), R["ml_k"], c * 128, scale=0.125)
            fm_job(wl.get(w_in, w_in.t[l, :, C_ML_O + c * 128:C_ML_O + (c + 1) * 128], 128), R["ml_o"], c * 128, func=AF.Sigmoid)
            fm_job(wl.get(w_in, w_in.t[l, :, C_HG_Q + c * 128:C_HG_Q + (c + 1) * 128], 128), R["hg_q"], c * 128, func=AF.Silu)
            fm_job(wl.get(w_in, w_in.t[l, :, C_HG_G + c * 128:C_HG_G + (c + 1) * 128], 128), R["hg_g"], c * 128, func=AF.Silu)
        for c in range(4):
            fm_job(wl.get(w_in, w_in.t[l, :, C_HG_F + c * 128:C_HG_F + (c + 1) * 128], 128), R["hg_f"], c * 128, f32=True)
        g16s = k.sb("g16s", [128, KC, 16], F32)
        k.dma(g16s, g16s[:], w_in, w_in.t[l, :, C_ML_G:C_ML_G + 16].rearrange("(k p) n -> p k n", p=128))
        wrep = [k.sb("wrep%d" % i, [128, KC, 128], BF16) for i in range(2)]
        for ty in range(4):
            for hp in range(2):
                wr = wrep[(ty * 2 + hp) % 2]
                for hh in range(2):
                    c = ty * 4 + hp * 2 + hh
                    k.op(k.dve, lambda h: h.tensor_copy(out=wr[:, :, hh * 64:(hh + 1) * 64],
                                                        in_=g16s[:, :, c:c + 1].to_broadcast([128, KC, 64])), [g16s], [wr])
                fm_job(wr, R["ml_g"], (ty * 2 + hp) * 128, bias=gb[:, ty * 2 + hp:ty * 2 + hp + 1], f32=True)
        for (c0, dst) in ((C_ML_V, R["ml_v"]), (C_HG_I, R["hg_v"])):
            wv = k.sb("wv", [128, KC, 256], BF16)
            for c in range(2):
                wb = wl.get(w_in, w_in.t[l, :, c0 + c * 128:c0 + (c + 1) * 128], 128)
                k.op(k.dve, lambda h: h.tensor_copy(out=wv[:, :, c * 128:(c + 1) * 128], in_=wb[:, :, :128]), [wb], [wv])

            def evac_v(p, i, dst=dst):
                j = cnt[0] % 2
                cnt[0] += 1
                st = stb[j]
                k.op(k.act, lambda h: h.copy(out=st[:, :256], in_=p[:, :256]), [p], [st])
                k.dma(dst, dst.t[i * 128:(i + 1) * 128, :], st, st[:, :256])
            lin_tm(k, wv, 256, hxT, list(range(NT)), evac_v, pp)


NCH = T // 16
SEGS = ((0, CTX), (CTX, T))


def seg_view(ap2d, si, dr):
    a, b = SEGS[si]
    if dr == 0:
        return ap2d[:, a:b]
    if a == 0:
        return ap2d[:, b - 1::-1]
    return ap2d[:, b - 1:a - 1:-1]


def stage_rec(P, l, mixer, mixT_d):
    k = P.k
    R = P.rec
    ml = (mixer == "ml")
    nv = 2 if ml else 1
    row_off = 0 if ml else 256
    with k.scope():
        cst = P.ext
        maskb = k.sb("maskb", [128, 2, 128], BF16)
        k.dma(maskb, maskb[:], cst["cmask"], cst["cmask"].t[:, :, :])
        bm = k.sb("bm", [128, 8], BF16)
        k.dma(bm, bm[:], cst["bmask"], cst["bmask"].t[:, :])
        blk = k.sb("blk", [128, 128], BF16)
        k.dma(blk, blk[:], cst["blk"], cst["blk"].t[:, :])
        if not ml:
            rmask = k.sb("rmask", [128, T], F32)
            k.op(k.dve, lambda h: h.memset(rmask[:], 1.0), [], [rmask])
            k.op(k.dve, lambda h: h.memset(rmask[:].rearrange("p (c i) -> p c i", i=16)[:, :, 0], 0.0), [], [rmask])
        zer = k.sb("zer", [128, 1], F32)
        k.op(k.dve, lambda h: h.memset(zer[:], 0.0), [], [zer])
        onesT = k.sb("onesT", [128, 64], BF16)
        k.op(k.dve, lambda h: h.memset(onesT[:], 1.0), [], [onesT])
        vones = k.sb("vones", [128, 8, 64], BF16)
        k.op(k.dve, lambda h: h.tensor_copy(out=vones[:], in_=bm[:].unsqueeze(2).to_broadcast([128, 8, 64])), [bm], [vones])

        qAs = k.sb("qA", [128, T], BF16) if ml else None
        qAd = [qAs, qAs] if ml else [k.sb("qA%d" % i, [128, T], BF16) for i in range(2)]
        kAd = [k.sb("kA%d" % i, [128, T], BF16) for i in range(2)]
        x3d = [k.sb("x3%d" % i, [128, T], BF16) for i in range(2)]
        cld = [k.sb("cl%d" % i, [128, T], BF16) for i in range(2)] if ml else None
        tA = k.sb("tA", [128, T], F32)
        tB = k.sb("tB", [128, T], F32)
        tC = k.sb("tC", [128, T], F32)
        osum = k.sb("osum", [128, T], F32)
        gate = k.sb("gate", [128, T], BF16)
        vt = k.sb("vt", [128, NT, 128], BF16)
        Rj = k.sb("Rj", [128, NCH], F32)
        Rp = k.sb("Rp", [128, NCH], F32)
        Dcd = [k.sb("Dc%d" % i, [128, NCH], F32) for i in range(2)]
        sc = k.sb("sc", [128, 8], F32)
        nw = k.sb("nw", [128, 1], F32)

        bankA = [k.ps("bkA%d" % i, [128, 512]) for i in range(2)]
        pdSd = [[k.ps("pdS%d_%d" % (i, j), [128, 8, 64]) for j in range(nv)] for i in range(2)]
        pktd = [k.ps("pkt%d" % i, [128, 128], BF16) for i in range(2)]
        AmTd = [[k.sb("AmT%d_%d" % (i, j), [128, 2, 128], BF16) for j in range(2)] for i in range(2)]
        qzd = [[k.sb("qz%d_%d" % (i, j), [128, 2, 128], BF16) for j in range(2)] for i in range(2)]
        for i in range(2):
            for j in range(2):
                k.op(k.dve, lambda h: h.memset(qzd[i][j][:], 0.0), [], [qzd[i][j]])
        kStd = [k.sb("kSt%d" % i, [128, 128], BF16) for i in range(2)]
        Vexpd = [k.sb("Vexp%d" % i, [128, 2, 8, 64], BF16) for i in range(2)]
        Shd = [[k.sb("Sh%d_%d" % (i, j), [128, 9, 64], F32) for j in range(nv)] for i in range(2)]
        Sbd = [[k.sb("Sb%d_%d" % (i, j), [128, 8, 64], BF16) for j in range(nv)] for i in range(2)]
        nsb = [[k.sb("nsb%d_%d" % (i, j), [128, 128], F32) for j in range(2)] for i in range(2)]
        dsb = [[k.sb("dsb%d_%d" % (i, j), [128, 128], F32) for j in range(2)] for i in range(2)] if ml else None
        fq = k.sb("fq", [128, 512], BF16)
        fr_ = k.sb("fr_", [128, 512], F32)
        fy = k.sb("fy", [128, 512], F32)
        fo = [k.sb("fo%d" % i, [128, 512], BF16) for i in range(2)]

        def c16(v):
            return v.rearrange("p (c i) -> p c i", i=16)

        def tile_of(ip, dr):
            if ip < 2:
                return ip if dr == 0 else 1 - ip
            return ip if dr == 0 else (NT - 1) - (ip - 2)

        for hp in range(2):
            rows = slice(hp * 128, (hp + 1) * 128)
            src_v = R["ml_v"] if ml else R["hg_v"]
            for i_ in range(NT):
                k.dma(vt, vt[:, i_, :], src_v, src_v.t[i_ * 128:(i_ + 1) * 128, hp * 128:(hp + 1) * 128])
            src_g = R["ml_o"] if ml else R["hg_g"]
            k.dma(gate, gate[:], src_g, src_g.t[rows, :])
            nsrc = P.ml_norm if ml else P.hg_norm
            k.dma(nw, nw[:], nsrc, nsrc.t[l:l + 1, rows].rearrange("o p -> p o"), allow_slow_non_contiguous=True)
            k.op(k.pool, lambda h: h.memset(osum[:], 0.0), [], [osum])
            if not ml:
                if l == 0:
                    k.op(k.dve, lambda h: h.memset(sc[:, 0:1], 0.0), [], [sc])
                else:
                    lg = P.hg_lb_logits
                    k.dma(sc, sc[:, 4:5], lg, lg.t[0:1, rows].rearrange("o p -> p o"), allow_slow_non_contiguous=True)
                    k.dma(sc, sc[:, 5:6], lg, lg.t[1:2, rows].rearrange("o p -> p o"), allow_slow_non_contiguous=True)
                    k.op(k.dve, lambda h: h.tensor_tensor(out=sc[:, 6:7], in0=sc[:, 5:6], in1=sc[:, 4:5], op=ALU.subtract), [sc], [sc])
                    k.op(k.act, lambda h: h.activation(out=sc[:, 0:1], in_=sc[:, 6:7], func=AF.Sigmoid), [sc], [sc])
                k.op(k.dve, lambda h: h.tensor_scalar(out=sc[:, 1:2], in0=sc[:, 0:1], scalar1=-1.0, scalar2=1.0,
                                                      op0=ALU.mult, op1=ALU.add), [sc], [sc])
                k.op(k.dve, lambda h: h.tensor_scalar(out=sc[:, 2:3], in0=sc[:, 1:2], scalar1=-1.0, scalar2=None,
                                                      op0=ALU.mult), [sc], [sc])
            if ml:
                k.dma(qAs, qAs[:], R["ml_q"], R["ml_q"].t[rows, :])
            for dr in range(2):
                qA, kA, x3, Dc = qAd[dr], kAd[dr], x3d[dr], Dcd[dr]
                if ml:
                    k.dma(kA, kA[:], R["ml_k"], R["ml_k"].t[rows, :])
                    gi0 = ((dr * 2) * 2 + hp) * 128
                    gf0 = ((dr * 2 + 1) * 2 + hp) * 128
                    k.dma(tA, tA[:], R["ml_g"], R["ml_g"].t[gi0:gi0 + 128, :])
                    k.dma(tB, tB[:], R["ml_g"], R["ml_g"].t[gf0:gf0 + 128, :])
                    k.op(k.act, lambda h: h.activation(out=tB[:], in_=tB[:], func=AF.Exp, scale=-1.0), [tB], [tB])
                    k.op(k.act, lambda h: h.activation(out=tB[:], in_=tB[:], func=AF.Ln, bias=1.0), [tB], [tB])
                    for si in range(2):
                        ini = 0.0 if si == 0 else seg_view(tC[:], 0, dr)[:, CTX - 1:CTX]
                        ov, dv = seg_view(tC[:], si, dr), seg_view(tB[:], si, dr)
                        n = SEGS[si][1] - SEGS[si][0]
                        k.op(k.dve, lambda h: h.tensor_tensor_scan(out=ov, data0=dv, data1=zer[:].to_broadcast([128, n]),
                                                                   initial=ini, op0=ALU.add, op1=ALU.add), [tB, tC, zer], [tC])
                    k.op(k.dve, lambda h: h.tensor_tensor(out=tA[:], in0=tA[:], in1=tC[:], op=ALU.add), [tA, tC], [tA])
                    for si in range(2):
                        ini = 0.0 if si == 0 else seg_view(tB[:], 0, dr)[:, CTX - 1:CTX]
                        ov, dv = seg_view(tB[:], si, dr), seg_view(tA[:], si, dr)
                        k.op(k.dve, lambda h: h.tensor_tensor_scan(out=ov, data0=dv, data1=dv, initial=ini,
                                                                   op0=ALU.max, op1=ALU.max), [tA, tB], [tB])
                    for si in range(2):
                        j0, j1 = SEGS[si][0] // 16, SEGS[si][1] // 16
                        k.op(k.dve, lambda h: h.tensor_copy(out=Rj[:, j0:j1], in_=c16(seg_view(tB[:], si, dr))[:, :, 15]),
                             [tB], [Rj])
                    k.op(k.dve, lambda h: h.memset(Rp[:, 0:1], 0.0), [], [Rp])
                    k.op(k.dve, lambda h: h.tensor_copy(out=Rp[:, 1:NCH], in_=Rj[:, 0:NCH - 1]), [Rj], [Rp])
                    k.op(k.dve, lambda h: h.tensor_tensor(out=Dc[:], in0=Rp[:], in1=Rj[:], op=ALU.subtract), [Rp, Rj], [Dc])
                    k.op(k.act, lambda h: h.activation(out=Dc[:], in_=Dc[:], func=AF.Exp), [Dc], [Dc])
                    for si in range(2):
                        j0, j1 = SEGS[si][0] // 16, SEGS[si][1] // 16
                        rb = Rj[:, j0:j1].unsqueeze(2).to_broadcast([128, j1 - j0, 16])
                        db = Dc[:, j0:j1].unsqueeze(2).to_broadcast([128, j1 - j0, 16])
                        av, cv = c16(seg_view(tA[:], si, dr)), c16(seg_view(tC[:], si, dr))
                        k.op(k.dve, lambda h: h.tensor_tensor(out=av, in0=av, in1=rb, op=ALU.subtract), [tA, Rj], [tA])
                        k.op(k.dve, lambda h: h.tensor_tensor(out=cv, in0=cv, in1=rb, op=ALU.subtract), [tC, Rj], [tC])
                        k.op(k.pool, lambda h: h.tensor_tensor(out=c16(seg_view(x3[:], si, dr)), in0=c16(seg_view(qA[:], si, dr)),
                                                               in1=db, op=ALU.mult), [qA, Dc], [x3])
                    k.op(k.act, lambda h: h.activation(out=tA[:], in_=tA[:], func=AF.Exp), [tA], [tA])
                    k.op(k.act, lambda h: h.activation(out=cld[dr][:], in_=tC[:], func=AF.Exp), [tC], [cld[dr]])
                    k.op(k.dve, lambda h: h.tensor_tensor(out=kA[:], in0=kA[:], in1=tA[:], op=ALU.mult), [kA, tA], [kA])
                else:
                    k.dma(qA, qA[:], R["hg_q"], R["hg_q"].t[rows, :])
                    f0 = dr * 256 + hp * 128
                    k.dma(tA, tA[:], R["hg_f"], R["hg_f"].t[f0:f0 + 128, :])
                    k.op(k.act, lambda h: h.activation(out=tB[:], in_=tA[:], func=AF.Sigmoid), [tA], [tB])
                    k.op(k.dve, lambda h: h.tensor_scalar(out=tC[:], in0=tB[:], scalar1=sc[:, 2:3], scalar2=sc[:, 1:2],
                                                          op0=ALU.mult, op1=ALU.add), [tB, sc], [tC])
                    k.op(k.dve, lambda h: h.tensor_scalar(out=tB[:], in0=tB[:], scalar1=sc[:, 1:2], scalar2=sc[:, 0:1],
                                                          op0=ALU.mult, op1=ALU.add), [tB, sc], [tB])
                    k.op(k.act, lambda h: h.activation(out=tB[:], in_=tB[:], func=AF.Ln), [tB], [tB])
                    for si in range(2):
                        j0, j1 = SEGS[si][0] // 16, SEGS[si][1] // 16
                        ov = c16(seg_view(tA[:], si, dr))
                        nseg = SEGS[si][1] - SEGS[si][0]
                        k.op(k.dve, lambda h: h.tensor_tensor_scan(out=seg_view(tA[:], si, dr), data0=rmask[:, 0:nseg],
                                                                   data1=seg_view(tB[:], si, dr), initial=0.0,
                                                                   op0=ALU.mult, op1=ALU.add), [tB, rmask, tA], [tA])
                        k.op(k.dve, lambda h: h.tensor_copy(out=Rj[:, j0:j1], in_=ov[:, :, 15]), [tA], [Rj])
                    k.op(k.act, lambda h: h.activation(out=Dc[:], in_=Rj[:], func=AF.Exp), [Rj], [Dc])
                    k.op(k.act, lambda h: h.activation(out=tB[:], in_=tA[:], func=AF.Exp), [tA], [tB])
                    k.op(k.dve, lambda h: h.tensor_tensor(out=qA[:], in0=qA[:], in1=tB[:], op=ALU.mult), [qA, tB], [qA])
                    k.op(k.act, lambda h: h.activation(out=tB[:], in_=tA[:], func=AF.Exp, scale=-1.0), [tA], [tB])
                    k.op(k.dve, lambda h: h.tensor_tensor(out=kA[:], in0=tC[:], in1=tB[:], op=ALU.mult), [tC, tB], [kA])
                    for si in range(2):
                        j0, j1 = SEGS[si][0] // 16, SEGS[si][1] // 16
                        rb = Rj[:, j0:j1].unsqueeze(2).to_broadcast([128, j1 - j0, 16])
                        av = c16(seg_view(tA[:], si, dr))
                        k.op(k.dve, lambda h: h.tensor_tensor(out=av, in0=rb, in1=av, op=ALU.subtract), [tA, Rj], [tA])
                    k.op(k.act, lambda h: h.activation(out=tA[:], in_=tA[:], func=AF.Exp), [tA], [tA])
                    k.op(k.dve, lambda h: h.tensor_tensor(out=x3[:], in0=tC[:], in1=tA[:], op=ALU.mult), [tC, tA], [x3])

            def qI_of(dr):
                return x3d[dr] if ml else qAd[dr]

            def kS_of(dr):
                return kAd[dr] if ml else x3d[dr]

            def pA_of(dr):
                return bankA[dr][:, 0:256].rearrange("p (h t) -> p h t", h=2)

            def po_of(dr, vs):
                return bankA[dr][:, 256 + vs * 128:256 + (vs + 1) * 128]

            def front_mm(ip):
                for dr in range(2):
                    c0 = tile_of(ip, dr) * 128
                    qz = qzd[dr][ip % 2]
                    for hh in range(2):
                        rr = slice(hh * 64, (hh + 1) * 64)
                        k.op(k.pool, lambda h: h.tensor_copy(out=qz[rr, hh, :], in_=qAd[dr][rr, c0:c0 + 128]), [qAd[dr]], [qz])
                    k.mm([lambda h: h.matmul(bankA[dr][:, 0:256], lhsT=kAd[dr][:, c0:c0 + 128],
                                             rhs=qz[:].rearrange("p h t -> p (h t)"), start=True, stop=True)],
                         [kAd[dr], qz], [bankA[dr]])
                    kS = kS_of(dr)
                    k.mm([lambda h: h.transpose(out=pktd[dr][:], in_=kS[:, c0:c0 + 128], identity=P.ident[:])],
                         [kS, P.ident], [pktd[dr]])

            def front_ev(ip):
                for dr in range(2):
                    it = tile_of(ip, dr)
                    AmT = AmTd[dr][ip % 2]
                    k.op(k.dve, lambda h: h.tensor_tensor(out=AmT[:], in0=pA_of(dr),
                                                          in1=maskb[:, dr:dr + 1, :].to_broadcast([128, 2, 128]),
                                                          op=ALU.mult), [bankA[dr], maskb], [AmT])
                    k.op(k.act, lambda h: h.copy(out=kStd[dr][:], in_=pktd[dr][:]), [pktd[dr]], [kStd[dr]])
                    k.op(k.pool, lambda h: h.tensor_tensor(
                        out=Vexpd[dr][:], in0=vt[:, it, :].rearrange("p (h e) -> p h e", h=2).unsqueeze(2).to_broadcast([128, 2, 8, 64]),
                        in1=bm[:].unsqueeze(1).unsqueeze(3).to_broadcast([128, 2, 8, 64]), op=ALU.mult), [vt, bm], [Vexpd[dr]])

            def dS_mm(ip):
                for dr in range(2):
                    for vs in range(nv):
                        for hh in range(2):
                            rr = slice(hh * 64, (hh + 1) * 64)
                            rhs = Vexpd[dr][:, hh, :, :] if vs == 0 else vones[:]
                            k.mm([lambda h: h.matmul(pdSd[dr][vs][rr, :, :], lhsT=kStd[dr][:, rr], rhs=rhs, start=True, stop=True)],
                                 [kStd[dr], Vexpd[dr], vones], [pdSd[dr][vs]])

            def steps(ip):
                for dr in range(2):
                    for vs in range(nv):
                        Sh = Shd[dr][vs]
                        k.op(k.pool, lambda h: h.tensor_copy(out=Sh[:, 0, :], in_=Sh[:, 8, :]), [Sh], [Sh])
                for cl in range(8):
                    for dr in range(2):
                        cp = cl if dr == 0 else 7 - cl
                        j = ip * 8 + cl
                        for vs in range(nv):
                            Sh = Shd[dr][vs]
                            k.op(k.dve, lambda h: h.scalar_tensor_tensor(out=Sh[:, cl + 1, :], in0=Sh[:, cl, :],
                                                                         scalar=Dcd[dr][:, j:j + 1], in1=pdSd[dr][vs][:, cp, :],
                                                                         op0=ALU.mult, op1=ALU.add),
                                 [Sh, Dcd[dr], pdSd[dr][vs]], [Sh])
                for dr in range(2):
                    for vs in range(nv):
                        k.op(k.act, lambda h: h.copy(out=Sbd[dr][vs][:], in_=Shd[dr][vs][:, 0:8, :]), [Shd[dr][vs]], [Sbd[dr][vs]])

            def out_mm(ip):
                for dr in range(2):
                    it = tile_of(ip, dr)
                    c0 = it * 128
                    AmT = AmTd[dr][ip % 2]
                    qI = qI_of(dr)
                    for vs in range(nv):
                        fns = []
                        for hh in range(2):
                            rr = slice(hh * 64, (hh + 1) * 64)
                            lh = vt[:, it, hh * 64:(hh + 1) * 64] if vs == 0 else onesT[:]
                            fns.append(lambda h, rr=rr, lh=lh, hh=hh: h.matmul(po_of(dr, vs)[rr, :], lhsT=lh, rhs=AmT[:, hh, :],
                                                                               start=True, stop=False, skip_group_check=True))
                            for cl in range(8):
                                cp = cl if dr == 0 else 7 - cl
                                fns.append(lambda h, rr=rr, cl=cl, cp=cp: h.matmul(
                                    po_of(dr, vs)[rr, cp * 16:(cp + 1) * 16], lhsT=Sbd[dr][vs][rr, cl, :],
                                    rhs=qI[rr, c0 + cp * 16:c0 + (cp + 1) * 16], start=False, stop=(cl == 7),
                                    skip_group_check=True))
                        k.mm(fns, [vt, onesT, AmT, Sbd[dr][vs], qI], [bankA[dr]])

            def evac_a(ip):
                for dr in range(2):
                    n_ = nsb[dr][ip % 2]
                    k.op(k.act, lambda h: h.copy(out=n_[:], in_=po_of(dr, 0)), [bankA[dr]], [n_])
                    if ml:
                        d_ = dsb[dr][ip % 2]
                        k.op(k.act, lambda h: h.activation(out=d_[:], in_=po_of(dr, 1), func=AF.Abs), [bankA[dr]], [d_])

            def evac_b(ip):
                for dr in range(2):
                    c0 = tile_of(ip, dr) * 128
                    n_ = nsb[dr][ip % 2]
                    if ml:
                        d_ = dsb[dr][ip % 2]
                        k.op(k.dve, lambda h: h.tensor_tensor(out=d_[:], in0=d_[:], in1=cld[dr][:, c0:c0 + 128], op=ALU.max),
                             [cld[dr], d_], [d_])
                        k.op(k.dve, lambda h: h.reciprocal(out=d_[:], in_=d_[:]), [d_], [d_])
                        k.op(k.dve, lambda h: h.tensor_tensor(out=n_[:], in0=n_[:], in1=d_[:], op=ALU.mult), [n_, d_], [n_])
                    k.op(k.pool, lambda h: h.tensor_tensor(out=osum[:, c0:c0 + 128], in0=osum[:, c0:c0 + 128], in1=n_[:],
                                                           op=ALU.add), [osum, n_], [osum])

            for dr in range(2):
                for vs in range(nv):
                    k.op(k.dve, lambda h: h.memset(Shd[dr][vs][:, 8, :], 0.0), [], [Shd[dr][vs]])
            front_mm(0)
            front_ev(0)
            dS_mm(0)
            for ip in range(NT):
                if ip + 1 < NT:
                    front_mm(ip + 1)
                    front_ev(ip + 1)
                steps(ip)
                if ip > 0:
                    evac_b(ip - 1)
                if ip + 1 < NT:
                    dS_mm(ip + 1)
                out_mm(ip)
                evac_a(ip)
            evac_b(NT - 1)

            pss = pdSd[0][0]
            for bi, (t0, n) in enumerate(tok_blocks(T)):
                pv = pss[:].rearrange("p c e -> p (c e)")
                k.op(k.act, lambda h: h.activation(out=fq[:, :n], in_=osum[:, t0:t0 + n], func=AF.Square), [osum], [fq])
                k.mm([lambda h: h.matmul(pv[:, :n], lhsT=blk[:], rhs=fq[:, :n], start=True, stop=True)], [blk, fq], [pss])
                k.op(k.dve, lambda h: h.tensor_scalar(out=fr_[:, :n], in0=pv[:, :n], scalar1=1.0 / 64, scalar2=EPS,
                                                      op0=ALU.mult, op1=ALU.add), [pss], [fr_])
                k.op(k.act, lambda h: h.sqrt(out=fr_[:, :n], in_=fr_[:, :n]), [fr_], [fr_])
                k.op(k.dve, lambda h: h.reciprocal(out=fr_[:, :n], in_=fr_[:, :n]), [fr_], [fr_])
                k.op(k.dve, lambda h: h.tensor_tensor(out=fy[:, :n], in0=osum[:, t0:t0 + n], in1=fr_[:, :n], op=ALU.mult),
                     [osum, fr_], [fy])
                ob = fo[bi % 2]
                k.op(k.dve, lambda h: h.scalar_tensor_tensor(out=ob[:, :n], in0=fy[:, :n], scalar=nw[:, 0:1], in1=gate[:, t0:t0 + n],
                                                             op0=ALU.mult, op1=ALU.mult), [fy, nw, gate], [ob])
                r0 = row_off + hp * 128
                k.dma(mixT_d, mixT_d.t[r0:r0 + 128, t0:t0 + n], ob, ob[:, :n])


def rec_consts():
    import ml_dtypes
    s = np.arange(128)[:, None]
    t = np.arange(128)[None, :]
    same = (s // 16) == (t // 16)
    cm = np.stack([(same & (s <= t)), (same & (s >= t))], 1).astype(np.float32)
    bmk = ((s // 16) == np.arange(8)[None, :]).astype(np.float32)
    blk = ((s // 64) == (t // 64)).astype(np.float32)
    b16 = ml_dtypes.bfloat16
    return cm.astype(b16), bmk.astype(b16), blk.astype(b16)


def stage_postmix(P, l, mixT_d, src, dst, tiles, all_latent=False):
    k = P.k
    with k.scope():
        wl = WLoader(k, nbuf=2, width=512, name="ow")
        wo = k.sb("wo", [128, KC, 1024], BF16)
        for hf in range(2):
            wb = wl.get(P.w_out, P.w_out.t[l, :, hf * 512:(hf + 1) * 512], 512)
            k.op(k.dve, lambda h: h.tensor_copy(out=wo[:, :, hf * 512:(hf + 1) * 512], in_=wb[:, :, :512]), [wb], [wo])
        gpb = k.sb("gpb", [128, 2, 1024], F32)
        for kind in range(2):
            k.dma(gpb, gpb[:, kind, :], P.modc[l], P.modc[l].t[kind:kind + 1, 2, :].to_broadcast([128, 1024]))
        mb = [k.sb("mb%d" % i, [128, KC, 128], BF16) for i in range(2)]
        xt = [k.sb("pxt%d" % i, [128, 1024], F32) for i in range(2)]
        pm = [k.ps("pmx%d" % i, [128, 512]) for i in range(4)]
        ep = Epilogue(k)
        for n, i in enumerate(tiles):
            m, x = mb[n % 2], xt[n % 2]
            k.dma(m, m[:], mixT_d, mixT_d.t[:, i * 128:(i + 1) * 128].rearrange("(k p) t -> p k t", p=128))
            k.dma(x, x[:], src, src.t[i * 128:(i + 1) * 128, :])
            ps = []
            for hf in range(2):
                p = pm[(n % 2) * 2 + hf]
                k.mm([(lambda h, kk=kk: h.matmul(p[:], lhsT=m[:, kk, :], rhs=wo[:, kk, hf * 512:(hf + 1) * 512],
                                                 start=(kk == 0), stop=(kk == KC - 1))) for kk in range(KC)], [m, wo], [p])
                ps.append(p)
            ep.run([(p, None) for p in ps], x, gpb, 1 if (i < 2 and not all_latent) else 0, dst, dst.t[i * 128:(i + 1) * 128, :])


class Epilogue:
    def __init__(self, k, need_y=True):
        self.k = k
        self.y = k.sb("ep_y", [128, 1024], F32) if need_y else None
        self.junk = k.sb("ep_j", [128, 1024], BF16)
        self.s = k.sb("ep_s", [128, 2], F32)
        self.o = [k.sb("ep_o%d" % i, [128, 1024], F32) for i in range(2)]
        self.n = 0

    def run(self, halves, x, gpb, kind, dst, dst_ap, ysb=None):
        k = self.k
        y, s = self.y, self.s
        if ysb is None:
            for hf, (p, _) in enumerate(halves):
                eng = k.act if hf == 0 else k.dve
                if hf == 0:
                    k.op(k.act, lambda h: h.copy(out=y[:, 0:512], in_=p[:]), [p], [y])
                else:
                    k.op(k.dve, lambda h: h.tensor_copy(out=y[:, 512:1024], in_=p[:]), [p], [y])
        else:
            ybuf, yap = ysb
        o = self.o[self.n % 2]
        self.n += 1
        k.op(k.dve, lambda h: h.memset(s[:], 0.0), [], [s])
        if ysb is None:
            ybuf, yap = y, y[:]
        k.op(k.act, lambda h: h.activation(out=self.junk[:], in_=yap, func=AF.Square, accum_out=s[:, 0:1]), [ybuf, s], [self.junk, s])
        k.op(k.dve, lambda h: h.tensor_scalar(out=s[:, 1:2], in0=s[:, 0:1], scalar1=1.0 / D, scalar2=EPS,
                                              op0=ALU.mult, op1=ALU.add), [s], [s])
        k.op(k.act, lambda h: h.sqrt(out=s[:, 1:2], in_=s[:, 1:2]), [s], [s])
        k.op(k.dve, lambda h: h.reciprocal(out=s[:, 1:2], in_=s[:, 1:2]), [s], [s])
        k.op(k.dve, lambda h: h.scalar_tensor_tensor(out=o[:], in0=yap, scalar=s[:, 1:2], in1=gpb[:, kind, :],
                                                     op0=ALU.mult, op1=ALU.mult), [ybuf, s, gpb], [o])
        k.op(k.dve, lambda h: h.tensor_tensor(out=o[:], in0=o[:], in1=x[:], op=ALU.add), [o, x], [o])
        k.dma(dst, dst_ap, o, o[:])


def stage_ffn(P, l, src, dst, tiles, dst_row, moe, all_latent=False):
    k = P.k
    nff = (FF_EXP if moe else FF_DENSE) // 128
    nexp = NEXP if moe else 1
    bsz = 8 if moe else 12
    for b0 in range(0, len(tiles), bsz):
        bt = tiles[b0:b0 + bsz]
        nb = len(bt)
        ntok = nb * 128
        with k.scope():
            hxb = k.sb("hxb", [128, KC, bsz * 128], BF16)
            stage_norm(P, src, l, 1, hxb, bt, all_latent=all_latent)
            with k.scope():
                HT = k.sb("HT", [128, nff, bsz * 128], BF16)
                yacc = k.sb("yacc", [128, bsz, 1024], F32)
                wl = WLoader(k, nbuf=2, width=128, name="fw")
                wl2 = WLoader(k, nbuf=2, width=128, name="fu")
                wld = WLoader(k, nbuf=2, width=512, kc=4 if moe else 2, name="fd")
                pg = [k.ps("pg%d" % i, [128, 512]) for i in range(2)]
                pu = [k.ps("pu%d" % i, [128, 512]) for i in range(2)]
                pd = [k.ps("pd%d" % i, [128, 512]) for i in range(2)]
                sg = [k.sb("sg%d" % i, [128, 512], F32) for i in range(2)]
                gates = k.sb("gates", [128, 8, 8], F32)
                gp2 = k.sb("gp2x", [128, 2, 1024], F32)
                for kind_ in range(2):
                    k.dma(gp2, gp2[:, kind_, :], P.modc[l], P.modc[l].t[kind_:kind_ + 1, 5, :].to_broadcast([128, 1024]))
                if moe:
                    rws = k.sb("rws", [128, KC, 8], F32)
                    k.dma(rws, rws[:], P.router_w, P.router_w.t[0, :, :].rearrange("(k p) e -> p k e", p=128))
                    rwb = k.sb("rwb", [128, KC, 8], BF16)
                    k.op(k.dve, lambda h: h.tensor_copy(out=rwb[:], in_=rws[:]), [rws], [rwb])
                    rb = k.sb("rb", [128, 8], F32)
                    k.dma(rb, rb[:], P.router_b, P.router_b.t[0:1, :].to_broadcast([128, 8]))
                    lg = k.sb("lg", [128, 8], F32)
                    l2 = k.sb("l2", [128, 8], F32)
                    mk = k.sb("mk", [128, 2, 8], F32)
                    mx = k.sb("mx", [128, 4], F32)
                    for ti in range(nb):
                        p = pd[ti % 2]
                        k.mm([(lambda h, kk=kk: h.matmul(p[:, 0:8], lhsT=hxb[:, kk, ti * 128:(ti + 1) * 128], rhs=rwb[:, kk, :],
                                                         start=(kk == 0), stop=(kk == KC - 1))) for kk in range(KC)], [hxb, rwb], [p])
                        k.op(k.dve, lambda h: h.tensor_tensor(out=lg[:], in0=p[:, 0:8], in1=rb[:], op=ALU.add), [p, rb], [lg])
                        k.op(k.dve, lambda h: h.reduce_max(out=mx[:, 0:1], in_=lg[:], axis=AX.X), [lg], [mx])
                        k.op(k.dve, lambda h: h.tensor_scalar(out=mk[:, 0, :], in0=lg[:], scalar1=mx[:, 0:1], scalar2=None,
                                                              op0=ALU.is_ge), [lg, mx], [mk])
                        k.op(k.dve, lambda h: h.scalar_tensor_tensor(out=l2[:], in0=mk[:, 0, :], scalar=-1e30, in1=lg[:],
                                                                     op0=ALU.mult, op1=ALU.add), [mk, lg], [l2])
                        k.op(k.dve, lambda h: h.reduce_max(out=mx[:, 1:2], in_=l2[:], axis=AX.X), [l2], [mx])
                        k.op(k.dve, lambda h: h.tensor_scalar(out=mk[:, 1, :], in0=l2[:], scalar1=mx[:, 1:2], scalar2=None,
                                                              op0=ALU.is_ge), [l2, mx], [mk])
                        k.op(k.dve, lambda h: h.tensor_tensor(out=mx[:, 2:3], in0=mx[:, 0:1], in1=mx[:, 1:2], op=ALU.subtract), [mx], [mx])
                        k.op(k.act, lambda h: h.activation(out=mx[:, 2:3], in_=mx[:, 2:3], func=AF.Sigmoid), [mx], [mx])
                        k.op(k.dve, lambda h: h.tensor_scalar(out=mx[:, 3:4], in0=mx[:, 2:3], scalar1=-1.0, scalar2=1.0,
                                                              op0=ALU.mult, op1=ALU.add), [mx], [mx])
                        k.op(k.dve, lambda h: h.tensor_scalar(out=gates[:, ti, :], in0=mk[:, 0, :], scalar1=mx[:, 2:3], scalar2=None,
                                                              op0=ALU.mult), [mk, mx], [gates])
                        k.op(k.dve, lambda h: h.scalar_tensor_tensor(out=gates[:, ti, :], in0=mk[:, 1, :], scalar=mx[:, 3:4],
                                                                     in1=gates[:, ti, :], op0=ALU.mult, op1=ALU.add),
                             [mk, mx, gates], [gates])
                for e in range(nexp):
                    if moe:
                        wg_b, wg_ap = P.moe_w_gate, P.moe_w_gate.t[0, e]
                        wu_b, wu_ap = P.moe_w_up, P.moe_w_up.t[0, e]
                        wd_b, wd_ap = P.moe_w_down, P.moe_w_down.t[0, e]
                    else:
                        wg_b, wg_ap = P.ffn_w_gate, P.ffn_w_gate.t[0]
                        wu_b, wu_ap = P.ffn_w_up, P.ffn_w_up.t[0]
                        wd_b, wd_ap = P.ffn_w_down, P.ffn_w_down.t[0]
                    for c in range(nff):
                        wg = wl.get(wg_b, wg_ap[:, c * 128:(c + 1) * 128], 128)
                        wu = wl2.get(wu_b, wu_ap[:, c * 128:(c + 1) * 128], 128)
                        for bi, (t0, n) in enumerate(tok_blocks(ntok)):
                            a, b_ = pg[bi % 2], pu[bi % 2]
                            k.mm([(lambda h, kk=kk: h.matmul(a[:, :n], lhsT=wg[:, kk, :128], rhs=hxb[:, kk, t0:t0 + n],
                                                             start=(kk == 0), stop=(kk == KC - 1))) for kk in range(KC)], [wg, hxb], [a])
                            k.mm([(lambda h, kk=kk: h.matmul(b_[:, :n], lhsT=wu[:, kk, :128], rhs=hxb[:, kk, t0:t0 + n],
                                                             start=(kk == 0), stop=(kk == KC - 1))) for kk in range(KC)], [wu, hxb], [b_])
                            s_ = sg[bi % 2]
                            k.op(k.act, lambda h: h.activation(out=s_[:, :n], in_=a[:, :n], func=AF.Silu), [a], [s_])
                            k.op(k.dve, lambda h: h.tensor_tensor(out=HT[:, c, t0:t0 + n], in0=s_[:, :n], in1=b_[:, :n], op=ALU.mult),
                                 [s_, b_], [HT])
                    kcd = wld.kc
                    for pc in range(nff // kcd):
                        for hf in range(2):
                            wd = wld.get(wd_b, wd_ap[pc * kcd * 128:(pc + 1) * kcd * 128, hf * 512:(hf + 1) * 512], 512)
                            for ti in range(nb):
                                p = pd[ti % 2]
                                k.mm([(lambda h, kk=kk: h.matmul(p[:], lhsT=HT[:, pc * kcd + kk, ti * 128:(ti + 1) * 128], rhs=wd[:, kk, :],
                                                                 start=(kk == 0), stop=(kk == kcd - 1))) for kk in range(kcd)], [HT, wd], [p])
                                ya = yacc[:, ti, hf * 512:(hf + 1) * 512]
                                first = (e == 0 and pc == 0)
                                if moe:
                                    g_ = gates[:, ti, e:e + 1]
                                    if first:
                                        k.op(k.dve, lambda h: h.tensor_scalar(out=ya, in0=p[:], scalar1=g_, scalar2=None, op0=ALU.mult),
                                             [p, gates], [yacc])
                                    else:
                                        k.op(k.dve, lambda h: h.scalar_tensor_tensor(out=ya, in0=p[:], scalar=g_, in1=ya,
                                                                                     op0=ALU.mult, op1=ALU.add), [p, gates, yacc], [yacc])
                                else:
                                    if first:
                                        k.op(k.act, lambda h: h.copy(out=ya, in_=p[:]), [p], [yacc])
                                    else:
                                        k.op(k.dve, lambda h: h.tensor_tensor(out=ya, in0=ya, in1=p[:], op=ALU.add), [p, yacc], [yacc])
                ep = Epilogue(k, need_y=False)
                xt = [k.sb("fx%d" % i, [128, 1024], F32) for i in range(2)]
                for ti, i in enumerate(bt):
                    x = xt[ti % 2]
                    k.dma(x, x[:], src, src.t[i * 128:(i + 1) * 128, :])
                    r0 = dst_row(i)
                    ep.run(None, x, gp2, 1 if (i < 2 and not all_latent) else 0, dst, dst.t[r0:r0 + 128, :], ysb=(yacc, yacc[:, ti, :]))


def stage_select(P, src, dst):
    k = P.k
    with k.scope():
        sel = k.sb("sel", [128, 2], F32)
        k.dma(sel, sel[:], P.ext["sel"], P.ext["sel"].t[:, :])
        a = [k.sb("sa%d" % i, [128, 1024], F32) for i in range(2)]
        b = [k.sb("sb%d" % i, [128, 1024], F32) for i in range(2)]
        for j in range(16):
            x, y = a[j % 2], b[j % 2]
            i0, i1 = 2 + j, 2 + 16 + j
            k.dma(x, x[:], src, src.t[i0 * 128:(i0 + 1) * 128, :])
            k.dma(y, y[:], src, src.t[i1 * 128:(i1 + 1) * 128, :])
            k.op(k.dve, lambda h: h.tensor_scalar(out=x[:], in0=x[:], scalar1=sel[:, 0:1], scalar2=None, op0=ALU.mult), [x, sel], [x])
            k.op(k.dve, lambda h: h.scalar_tensor_tensor(out=x[:], in0=y[:], scalar=sel[:, 1:2], in1=x[:], op0=ALU.mult, op1=ALU.add),
                 [x, y, sel], [x])
            k.dma(dst, dst.t[j * 128:(j + 1) * 128, :], x, x[:])


def stage_select_mix(P, src, dst):
    k = P.k
    with k.scope():
        sel = k.sb("sel", [128, 2], F32)
        k.dma(sel, sel[:], P.ext["sel"], P.ext["sel"].t[:, :])
        H = SEQ // 2
        a = [k.sb("ma%d" % i, [128, H], BF16) for i in range(2)]
        b = [k.sb("mb%d" % i, [128, H], BF16) for i in range(2)]
        for r in range(4):
            x, y = a[r % 2], b[r % 2]
            k.dma(x, x[:], src, src.t[r * 128:(r + 1) * 128, CTX:CTX + H])
            k.dma(y, y[:], src, src.t[r * 128:(r + 1) * 128, CTX + H:T])
            k.op(k.dve, lambda h: h.tensor_scalar(out=x[:], in0=x[:], scalar1=sel[:, 0:1], scalar2=None, op0=ALU.mult), [x, sel], [x])
            k.op(k.dve, lambda h: h.scalar_tensor_tensor(out=x[:], in0=y[:], scalar=sel[:, 1:2], in1=x[:], op0=ALU.mult, op1=ALU.add),
                 [x, y, sel], [x])
            k.dma(dst, dst.t[r * 128:(r + 1) * 128, :], x, x[:])


def build_program():
    P = Prog()
    k = P.k
    P.xin = P.ext_in("xin", [T, D])
    P.c2 = P.ext_in("c2", [2, D])
    P.w_mod = P.ext_in("w_mod", [2, D, 6 * D])
    P.b_mod = P.ext_in("b_mod", [2, 6 * D])
    for n in ("norm_pre_mix", "norm_post_mix", "norm_pre_ffn", "norm_post_ffn"):
        setattr(P, n, P.ext_in(n, [2, D]))
    P.ext_in("ident", [128, 128])
    P.ext_in("sel", [128, 2])
    P.w_in = P.ext_in("w_in", [2, D, PROJ_W])
    P.ext_in("w_da_sw", [2, D, 1024])
    P.w_out = P.ext_in("w_out", [2, D, D])
    P.ext_in("rope_c", [128, T], BF16)
    P.ext_in("rope_s", [128, T], BF16)
    P.ext_in("cmask", [128, 2, 128], BF16)
    P.ext_in("bmask", [128, 8], BF16)
    P.ext_in("blk", [128, 128], BF16)
    P.ml_gate_b = P.ext_in("ml_gate_b", [2, 16])
    P.ml_norm = P.ext_in("ml_norm", [2, 256])
    P.hg_norm = P.ext_in("hg_norm", [2, 256])
    P.hg_lb_logits = P.ext_in("hg_lb_logits", [2, 256])
    P.da_lambda = P.ext_in("da_lambda", [2, 4, 64])
    P.da_norm = P.ext_in("da_norm", [2, 128])
    P.ffn_w_gate = P.ext_in("ffn_w_gate", [1, D, FF_DENSE])
    P.ffn_w_up = P.ext_in("ffn_w_up", [1, D, FF_DENSE])
    P.ffn_w_down = P.ext_in("ffn_w_down", [1, FF_DENSE, D])
    P.router_w = P.ext_in("router_w", [1, D, NEXP])
    P.router_b = P.ext_in("router_b", [1, NEXP])
    P.moe_w_gate = P.ext_in("moe_w_gate", [1, NEXP, D, FF_EXP])
    P.moe_w_up = P.ext_in("moe_w_up", [1, NEXP, D, FF_EXP])
    P.moe_w_down = P.ext_in("moe_w_down", [1, NEXP, FF_EXP, D])
    out = P.ext_out("out", [SEQ // 2, D])
    P.modc = [k.dram("modc%d" % l, [2, 6, D], F32) for l in range(2)]
    P.rec = dict(ml_q=k.dram("ml_q", [256, T], BF16), ml_k=k.dram("ml_k", [256, T], BF16), ml_o=k.dram("ml_o", [256, T], BF16),
                 ml_g=k.dram("ml_g", [1024, T], F32), hg_q=k.dram("hg_q", [256, T], BF16), hg_g=k.dram("hg_g", [256, T], BF16),
                 hg_f=k.dram("hg_f", [512, T], F32), ml_v=k.dram("ml_v", [T, 256], BF16), hg_v=k.dram("hg_v", [T, 256], BF16))
    mixT_d = k.dram("mixT_d", [1024, T], BF16)
    xmid = [k.dram("xmid%d" % l, [T, D], F32) for l in range(2)]
    x1 = k.dram("x1", [T, D], F32)
    xown = k.dram("xown", [SEQ // 2, D], F32)
    mixown = k.dram("mixown", [1024, SEQ // 2], BF16)
    setup_consts(P)
    stage_mod(P, 0)
    stage_mod(P, 1)
    lat_blocks = [(CTX + qb * 512, 512, list(range(NT))) for qb in range(SEQ // 512)]
    for l in range(2):
        src = P.xin if l == 0 else x1
        lam_init = 0.8 - 0.6 * float(np.exp(-0.3 * l))
        with k.scope():
            hxT = k.sb("hxT", [128, KC, T], BF16)
            stage_norm(P, src, l, 0, hxT, list(range(NT)))
            stage_proj_rec(P, l, hxT)
            if l == 0:
                qbl = [(0, CTX, [0, 1])] + list(lat_blocks)
                stage_attn(P, l, hxT, None, 0, 0, lam_init, qblocks=qbl, mixT_d=mixT_d)
            else:
                qbl = [(qb * 512, 512, list(range(NT))) for qb in range(SEQ // 2 // 512)]
                stage_attn(P, l, hxT, None, 0, 0, lam_init, qblocks=qbl, mixT_d=mixown, own=True)
        stage_rec(P, l, "ml", mixT_d)
        stage_rec(P, l, "hg", mixT_d)
        if l == 0:
            stage_postmix(P, l, mixT_d, src, xmid[0], list(range(NT)))
            stage_ffn(P, 0, xmid[0], x1, list(range(NT)), lambda i: i * 128, moe=False)
        else:
            stage_select_mix(P, mixT_d, mixown)
            stage_select(P, x1, xown)
            stage_postmix(P, l, mixown, xown, xmid[1], list(range(16)), all_latent=True)
            stage_ffn(P, 1, xmid[1], out, list(range(16)), lambda i: i * 128, moe=True, all_latent=True)
    k.finish([out])
    return k.nc


_CACHE = {}


def kernel(x, c, ctx, c_ctx, w_mod, b_mod, norm_pre_mix, norm_post_mix, norm_pre_ffn, norm_post_ffn,
           w_in, w_out, ml_gate_b, ml_norm, hg_lb_logits, hg_norm, da_lambda, da_norm,
           ffn_w_gate, ffn_w_up, ffn_w_down, router_w, router_b, moe_w_gate, moe_w_up, moe_w_down):
    from concourse.bass_utils import run_bass_kernel_spmd
    f = lambda a: np.ascontiguousarray(np.asarray(a, dtype=np.float32))
    x, c, ctx, c_ctx = f(x), f(c), f(ctx), f(c_ctx)
    w_in = f(w_in)
    if "nc" not in _CACHE:
        _CACHE["nc"] = build_program()
    nc = _CACHE["nc"]
    rc, rs = rope_tables()
    cm, bmk, blk = rec_consts()
    shared = dict(w_mod=f(w_mod), b_mod=f(b_mod), norm_pre_mix=f(norm_pre_mix), norm_post_mix=f(norm_post_mix),
                  norm_pre_ffn=f(norm_pre_ffn), norm_post_ffn=f(norm_post_ffn), ident=np.eye(128, dtype=np.float32),
                  w_in=w_in, w_da_sw=swap_cols(w_in), w_out=f(w_out), rope_c=rc, rope_s=rs, cmask=cm, bmask=bmk, blk=blk,
                  ml_gate_b=f(ml_gate_b), ml_norm=f(ml_norm), hg_norm=f(hg_norm), hg_lb_logits=f(hg_lb_logits),
                  da_lambda=f(da_lambda), da_norm=f(da_norm), ffn_w_gate=f(ffn_w_gate), ffn_w_up=f(ffn_w_up),
                  ffn_w_down=f(ffn_w_down), router_w=f(router_w), router_b=f(router_b), moe_w_gate=f(moe_w_gate),
                  moe_w_up=f(moe_w_up), moe_w_down=f(moe_w_down))
    in_maps = []
    for core in range(8):
        b, half = core // 2, core % 2
        m = dict(shared)
        m["xin"] = np.ascontiguousarray(np.concatenate([ctx[b], x[b]], axis=0))
        m["c2"] = np.ascontiguousarray(np.stack([c[b], c_ctx], axis=0))
        sel = np.zeros((128, 2), np.float32)
        sel[:, half] = 1.0
        m["sel"] = sel
        in_maps.append(m)
    res = run_bass_kernel_spmd(nc, in_maps, core_ids=list(range(8)))
    out = np.zeros((4, SEQ, D), np.float32)
    for core in range(8):
        b, half = core // 2, core % 2
        out[b, half * (SEQ // 2):(half + 1) * (SEQ // 2)] = np.asarray(res.results[core]["out"], dtype=np.float32)
    return out
```

```python
import numpy as np
from contextlib import ExitStack
import concourse.bass as bass
import concourse.mybir as mybir

F32 = mybir.dt.float32
BF16 = mybir.dt.bfloat16
AF = mybir.ActivationFunctionType
ALU = mybir.AluOpType
AX = mybir.AxisListType

SAME_ENG_SYNC = True
STRICT_SAME_ENGINE = True


class Buf:
    def __init__(self, k, t, name, space):
        self.k, self.t, self.name, self.space = k, t, name, space
        self.w = {}
        self.r = {}
        self.dsem = None

    def __getitem__(self, idx):
        return self.t[idx]


class Eng:
    def __init__(self, k, name, h):
        self.k, self.name, self.h = k, name, h
        self.sem = k.es.enter_context(k.nc.semaphore("e_" + name))
        self.cnt = 0
        self.waited = {}


class _Scope:
    def __init__(self, k):
        self.k = k

    def __enter__(self):
        k = self.k
        self.prev = k.es
        self.st = ExitStack()
        k.es = self.st
        k.scopes.append([])
        return self

    def __exit__(self, *a):
        k = self.k
        k.barrier()
        for b in k.scopes.pop():
            if b.dsem:
                for qn_, s_ in b.dsem.items():
                    (k.dpool_sw if qn_ == "pool" else k.dpool).append(s_)
        self.st.close()
        k.es = self.prev
        return False


class K:
    def __init__(self):
        self.nc = bass.Bass("TRN2", target_bir_lowering=False)
        self.es = ExitStack()
        nc = self.nc
        self.pe = Eng(self, "pe", nc.tensor)
        self.act = Eng(self, "act", nc.scalar)
        self.dve = Eng(self, "dve", nc.vector)
        self.pool = Eng(self, "pool", nc.gpsimd)
        self.sp = Eng(self, "sp", nc.sync)
        self.sems = {}
        self.totals = {}
        for e in (self.pe, self.act, self.dve, self.pool, self.sp):
            self.sems[e.sem.name] = e.sem
        self.ndsem = 0
        self.dq = 0
        self.dpool = []
        self.dpool_sw = []
        for i in range(88):
            s = self.es.enter_context(self.nc.semaphore("d%d" % i))
            self.sems[s.name] = s
            self.totals[s.name] = 0
            (self.dpool if i < 44 else self.dpool_sw).append(s)
        self.scopes = []

    def sb(self, name, shape, dt, dshare=None):
        self.nuniq = getattr(self, "nuniq", 0) + 1
        t = self.es.enter_context(self.nc.sbuf_tensor("%s_%d" % (name, self.nuniq), list(shape), dt))
        b = Buf(self, t, name, "sb")
        if self.scopes:
            self.scopes[-1].append(b)
        return b

    def ps(self, name, shape, dt=F32):
        self.nuniq = getattr(self, "nuniq", 0) + 1
        t = self.es.enter_context(self.nc.psum_tensor("%s_%d" % (name, self.nuniq), list(shape), dt))
        return Buf(self, t, name, "ps")

    def scope(self):
        return _Scope(self)

    def barrier(self):
        engs = (self.pe, self.act, self.dve, self.pool, self.sp)
        for e in engs:
            for o in engs:
                if o is e or o.cnt == 0:
                    continue
                if e.waited.get(o.sem.name, 0) < o.cnt:
                    e.h.wait_ge(o.sem, o.cnt)
                    e.waited[o.sem.name] = o.cnt
            for s, v in self.totals.items():
                if v and e.waited.get(s, 0) < v:
                    e.h.wait_ge(self.sems[s], v)
                    e.waited[s] = v

    def dram(self, name, shape, dt, kind="Internal"):
        t = self.nc.dram_tensor(name, list(shape), dt, kind=kind)
        return Buf(self, t.ap(), name, "dram")

    def _dsem(self, b, qn="sp"):
        if b.dsem is None:
            b.dsem = {}
        if qn not in b.dsem:
            pool = self.dpool_sw if qn == "pool" else self.dpool
            b.dsem[qn] = pool.pop()
        return b.dsem[qn]

    def _deps(self, reads, writes):
        deps = {}
        self._raw = {}
        for b in reads:
            for s, v in b.w.items():
                deps[s] = max(deps.get(s, 0), v)
                self._raw[s] = max(self._raw.get(s, 0), v)
        for b in writes:
            for s, v in b.w.items():
                deps[s] = max(deps.get(s, 0), v)
            for s, v in b.r.items():
                deps[s] = max(deps.get(s, 0), v)
        return deps

    def _wait(self, eng, deps):
        for s, v in deps.items():
            if s in self.totals:
                v = self.totals[s]
            if s == eng.sem.name and not SAME_ENG_SYNC:
                continue
            if s == eng.sem.name and eng is self.pe:
                continue
            if s == eng.sem.name and not STRICT_SAME_ENGINE:
                if s not in self._raw:
                    continue
                v = self._raw[s]
            if eng.waited.get(s, 0) >= v:
                continue
            eng.h.wait_ge(self.sems[s], v)
            eng.waited[s] = v

    def _mark(self, tick, reads, writes):
        s, v = tick
        for b in writes:
            b.w[s] = max(b.w.get(s, 0), v)
            b.r = {}
        for b in reads:
            b.r[s] = max(b.r.get(s, 0), v)

    def op(self, eng, fn, reads=(), writes=()):
        self._wait(eng, self._deps(reads, writes))
        ins = fn(eng.h)
        eng.cnt += 1
        ins.then_inc(eng.sem, 1)
        self._mark((eng.sem.name, eng.cnt), reads, writes)
        return ins

    def mm(self, fns, reads=(), writes=()):
        eng = self.pe
        self._wait(eng, self._deps(reads, writes))
        ins = None
        for fn in fns:
            ins = fn(eng.h)
        eng.cnt += 1
        ins.then_inc(eng.sem, 1)
        self._mark((eng.sem.name, eng.cnt), reads, writes)

    def dma(self, out_b, out_ap, in_b, in_ap, q=None, **kw):
        if q is None:
            q = (self.sp, self.pool)[self.dq % 2]
            self.dq += 1
        sb = out_b if out_b.space != "dram" else in_b
        if sb.space == "dram":
            sb = out_b
        s = self._dsem(sb, q.name)
        self._wait(q, self._deps([in_b], [out_b]))
        ins = q.h.dma_start(out=out_ap, in_=in_ap, **kw)
        ins.then_inc(s, 16)
        self.totals[s.name] += 16
        self._mark((s.name, self.totals[s.name]), [in_b], [out_b])
        return ins

    def finish(self, bufs):
        deps = {}
        for b in bufs:
            for s, v in b.w.items():
                deps[s] = max(deps.get(s, 0), v)
        self._wait(self.sp, deps)
        for e in (self.pe, self.act, self.dve, self.pool):
            if e.cnt:
                self.sp.h.wait_ge(e.sem, e.cnt)
        for s, v in self.totals.items():
            if v and self.sp.waited.get(s, 0) < v:
                self.sp.h.wait_ge(self.sems[s], v)
        self.es.close()


D = 1024
KC = 8
SEQ = 4096
CTX = 256
T = SEQ + CTX
NT = T // 128
PROJ_W = 3856
FF_DENSE = 2816
FF_EXP = 3584
NEXP = 8
EPS = 1e-6
C_ML_Q, C_ML_K, C_ML_V, C_ML_O, C_ML_G = 0, 256, 512, 768, 1024
C_HG_Q, C_HG_F, C_HG_I, C_HG_G = 1040, 1296, 1808, 2064
C_DA_Q, C_DA_K, C_DA_V = 2320, 2832, 3344


def tok_blocks(n, bs=512):
    out = []
    o = 0
    while o < n:
        out.append((o, min(bs, n - o)))
        o += bs
    return out


class Prog:
    def __init__(self, dbg=None):
        self.k = K()
        self.dbg = dbg or {}
        self.ext = {}

    def ext_in(self, name, shape, dt=F32):
        b = self.k.dram(name, shape, dt, kind="ExternalInput")
        self.ext[name] = b
        return b

    def ext_out(self, name, shape, dt=F32):
        b = self.k.dram(name, shape, dt, kind="ExternalOutput")
        self.ext[name] = b
        return b


def stage_mod(P, l):
    k = P.k
    with k.scope():
        cT = k.sb("cT", [128, KC, 2], F32)
        for kk in range(KC):
            k.dma(cT, cT[:, kk, :], P.c2, P.c2.t[:, kk * 128:(kk + 1) * 128].rearrange("r p -> p r"),
                  allow_slow_non_contiguous=True)
        k.op(k.act, lambda h: h.activation(out=cT[:], in_=cT[:], func=AF.Silu), [cT], [cT])
        modv = k.sb("modv", [2, 6, 1024], F32)
        bm = k.sb("bm", [2, 6, 1024], F32)
        k.dma(bm, bm[:], P.b_mod,
              P.b_mod.t[l:l + 1, :].rearrange("o (s d) -> o s d", s=6).to_broadcast([2, 6, 1024]))
        nrm = k.sb("nrm", [2, 4, 1024], F32)
        for j, nb in enumerate((P.norm_pre_mix, P.norm_post_mix, P.norm_pre_ffn, P.norm_post_ffn)):
            k.dma(nrm, nrm[:, j, :], nb, nb.t[l:l + 1, :].to_broadcast([2, 1024]))
        wst = [k.sb("wst%d" % i, [128, KC, 512], F32) for i in range(4)]
        pm = [k.ps("pm%d" % i, [2, 512]) for i in range(4)]
        for cb in range(12):
            w = wst[cb % 4]
            p = pm[cb % 4]
            k.dma(w, w[:], P.w_mod, P.w_mod.t[l, :, cb * 512:(cb + 1) * 512].rearrange("(k p) n -> p k n", p=128))
            k.mm([(lambda h, kk=kk: h.matmul(p[:], lhsT=cT[:, kk, :], rhs=w[:, kk, :],
                                             start=(kk == 0), stop=(kk == KC - 1))) for kk in range(KC)],
                 [cT, w], [p])
            s, o = divmod(cb * 512, 1024)
            k.op(k.dve, lambda h: h.tensor_tensor(out=modv[:, s, o:o + 512], in0=p[:], in1=bm[:, s, o:o + 512],
                                                  op=ALU.add), [p, bm], [modv])
        modc = k.sb("modc", [2, 6, 1024], F32)
        for (dst, msc, nj) in ((0, 1, 0), (3, 4, 2)):
            k.op(k.dve, lambda h: h.scalar_tensor_tensor(out=modc[:, dst, :], in0=modv[:, msc, :], scalar=1.0,
                                                         in1=nrm[:, nj, :], op0=ALU.add, op1=ALU.mult),
                 [modv, nrm], [modc])
        for (dst, msh) in ((1, 0), (4, 3)):
            k.op(k.dve, lambda h: h.tensor_copy(out=modc[:, dst, :], in_=modv[:, msh, :]), [modv], [modc])
        for (dst, mg, nj) in ((2, 2, 1), (5, 5, 3)):
            k.op(k.dve, lambda h: h.tensor_tensor(out=modc[:, dst, :], in0=modv[:, mg, :], in1=nrm[:, nj, :],
                                                  op=ALU.mult), [modv, nrm], [modc])
        k.dma(P.modc[l], P.modc[l].t[:, :, :], modc, modc[:])


def stage_norm(P, src, l, which, hxT, tiles, all_latent=False):
    k = P.k
    gi, si = (0, 1) if which == 0 else (3, 4)
    with k.scope():
        gb = k.sb("gb", [128, 2, 1024], F32)
        shb = k.sb("shb", [128, 2, 1024], F32)
        for kind in range(2):
            k.dma(gb, gb[:, kind, :], P.modc[l], P.modc[l].t[kind:kind + 1, gi, :].to_broadcast([128, 1024]))
            k.dma(shb, shb[:, kind, :], P.modc[l], P.modc[l].t[kind:kind + 1, si, :].to_broadcast([128, 1024]))
        xt = [k.sb("xt%d" % i, [128, 1024], F32) for i in range(2)]
        junk = k.sb("junk", [128, 1024], BF16)
        ss = [k.sb("ss%d" % i, [128, 2], F32) for i in range(2)]
        yf = k.sb("yf", [128, 1024], F32)
        yb = [k.sb("yb%d" % i, [128, 1024], BF16) for i in range(2)]
        pt = [k.ps("pt%d" % i, [128, KC, 128], BF16) for i in range(2)]
        for n, i in enumerate(tiles):
            kind = 1 if (i < 2 and not all_latent) else 0
            x, s, y, p = xt[n % 2], ss[n % 2], yb[n % 2], pt[n % 2]
            k.dma(x, x[:], src, src.t[i * 128:(i + 1) * 128, :])
            k.op(k.dve, lambda h: h.memset(s[:], 0.0), [], [s])
            k.op(k.act, lambda h: h.activation(out=junk[:], in_=x[:], func=AF.Square, accum_out=s[:, 0:1]),
                 [x, s], [junk, s])
            k.op(k.dve, lambda h: h.tensor_scalar(out=s[:, 1:2], in0=s[:, 0:1], scalar1=1.0 / D, scalar2=EPS,
                                                  op0=ALU.mult, op1=ALU.add), [s], [s])
            k.op(k.act, lambda h: h.sqrt(out=s[:, 1:2], in_=s[:, 1:2]), [s], [s])
            k.op(k.dve, lambda h: h.reciprocal(out=s[:, 1:2], in_=s[:, 1:2]), [s], [s])
            k.op(k.dve, lambda h: h.scalar_tensor_tensor(out=yf[:], in0=x[:], scalar=s[:, 1:2], in1=gb[:, kind, :],
                                                         op0=ALU.mult, op1=ALU.mult), [x, s, gb], [yf])
            k.op(k.dve, lambda h: h.tensor_tensor(out=y[:], in0=yf[:], in1=shb[:, kind, :], op=ALU.add),
                 [yf, shb], [y])
            k.mm([(lambda h, kk=kk: h.transpose(out=p[:, kk, :], in_=y[:, kk * 128:(kk + 1) * 128],
                                                identity=P.ident[:])) for kk in range(KC)], [y, P.ident], [p])
            k.op(k.act, lambda h: h.copy(out=hxT[:, :, n * 128:(n + 1) * 128], in_=p[:]), [p], [hxT])


def setup_consts(P):
    k = P.k
    idf = k.sb("idf", [128, 128], F32)
    k.dma(idf, idf[:], P.ext["ident"], P.ext["ident"].t[:, :])
    P.ident = k.sb("ident", [128, 128], BF16)
    k.op(k.dve, lambda h: h.tensor_copy(out=P.ident[:], in_=idf[:]), [idf], [P.ident])


class WLoader:
    def __init__(self, k, nbuf=2, width=512, kc=KC, name="wl"):
        self.k = k
        self.kc = kc
        self.wb = [k.sb(name + "b%d" % i, [128, kc, width], BF16) for i in range(nbuf)]
        self.i = 0
        self.nbuf = nbuf

    def get(self, wbuf, w_ap, n):
        k = self.k
        wb = self.wb[self.i % self.nbuf]
        self.i += 1
        k.dma(wb, wb[:, :, :n], wbuf, w_ap.rearrange("(k p) n -> p k n", p=128), q=k.pool)
        return wb


def lin_fm(k, wb, ncol, src, ntok, evac, ps_pool, kc=KC, bs=512):
    for bi, (t0, n) in enumerate(tok_blocks(ntok, bs)):
        p = ps_pool[bi % len(ps_pool)]
        k.mm([(lambda h, kk=kk: h.matmul(p[:ncol, :n], lhsT=wb[:, kk, :ncol], rhs=src[:, kk, t0:t0 + n],
                                         start=(kk == 0), stop=(kk == kc - 1))) for kk in range(kc)],
             [wb, src], [p])
        evac(p, t0, n)


def lin_tm(k, wb, ncol, src, tiles, evac, ps_pool, kc=KC):
    for ti, i in enumerate(tiles):
        p = ps_pool[ti % len(ps_pool)]
        k.mm([(lambda h, kk=kk: h.matmul(p[:, :ncol], lhsT=src[:, kk, i * 128:(i + 1) * 128], rhs=wb[:, kk, :ncol],
                                         start=(kk == 0), stop=(kk == kc - 1))) for kk in range(kc)],
             [wb, src], [p])
        evac(p, i)


def stage_attn(P, l, hxT, mixT, q0, nq, lam_init, heads=(0, 1, 2, 3), qblocks=None, mixT_d=None, own=False):
    k = P.k
    w_in = P.w_in
    with k.scope():
        wl = WLoader(k, nbuf=2, width=128, name="aw")
        ropeC = k.sb("ropeC", [128, T], BF16)
        ropeS = k.sb("ropeS", [128, T], BF16)
        k.dma(ropeC, ropeC[:], P.ext["rope_c"], P.ext["rope_c"].t[:, :])
        k.dma(ropeS, ropeS[:], P.ext["rope_s"], P.ext["rope_s"].t[:, :])
        lp = k.sb("lp", [128, 4, 64], F32)
        k.dma(lp, lp[:], P.da_lambda, P.da_lambda.t[l:l + 1, :, :].to_broadcast([128, 4, 64]))
        lt = k.sb("lt", [128, 2, 64], F32)
        lam = k.sb("lam", [128, 4], F32)
        k.op(k.dve, lambda h: h.tensor_tensor(out=lt[:, 0, :], in0=lp[:, 0, :], in1=lp[:, 1, :], op=ALU.mult), [lp], [lt])
        k.op(k.dve, lambda h: h.tensor_tensor(out=lt[:, 1, :], in0=lp[:, 2, :], in1=lp[:, 3, :], op=ALU.mult), [lp], [lt])
        k.op(k.dve, lambda h: h.reduce_sum(out=lam[:, 0:2], in_=lt[:], axis=AX.X), [lt], [lam])
        k.op(k.act, lambda h: h.activation(out=lam[:, 0:2], in_=lam[:, 0:2], func=AF.Exp), [lam], [lam])
        k.op(k.dve, lambda h: h.tensor_tensor(out=lam[:, 2:3], in0=lam[:, 1:2], in1=lam[:, 0:1], op=ALU.subtract), [lam], [lam])
        k.op(k.dve, lambda h: h.tensor_scalar(out=lam[:, 3:4], in0=lam[:, 2:3], scalar1=-float(lam_init), scalar2=None,
                                              op0=ALU.add), [lam], [lam])
        dn = k.sb("dn", [128, 128], F32)
        k.dma(dn, dn[:], P.da_norm, P.da_norm.t[l:l + 1, :].to_broadcast([128, 128]))
        k.op(k.dve, lambda h: h.tensor_scalar(out=dn[:], in0=dn[:], scalar1=float(1.0 - lam_init), scalar2=None,
                                              op0=ALU.mult), [dn], [dn])

        qT = k.sb("qT", [128, T], BF16)
        kT = k.sb("kT", [128, T], BF16)
        if own:
            qTo = k.sb("qTo", [128, SEQ // 2], BF16)
            selt = k.sb("selt", [128, 2], F32)
            k.dma(selt, selt[:], P.ext["sel"], P.ext["sel"].t[:, :])
        va = k.sb("va", [128, NT, 132], BF16)
        k.op(k.dve, lambda h: h.memset(va[:, :, 128:132], 1.0), [], [va])
        t1 = k.sb("t1", [128, 512], F32)
        t2 = k.sb("t2", [128, 512], F32)
        sT2 = [[k.ps("sT%d_%d" % (i, j), [128, 512]) for j in range(2)] for i in range(2)]
        pp = [sT2[0][0], sT2[0][1]]
        osbs = [k.sb("osb%d" % i, [128, 3, 3, 132], F32) for i in range(2)]
        blk_i = [0]
        oacc = [k.ps("oacc%d" % i, [128, 3, 132]) for i in range(3)]
        pT = [[k.sb("pT%d_%d" % (i, j), [128, 512], BF16) for j in range(2)] for i in range(2)]
        fin = k.sb("fin", [128, 8], F32)
        o0 = k.sb("o0", [128, 128], F32)
        o1 = k.sb("o1", [128, 128], F32)
        junk = k.sb("ajunk", [128, 128], F32)
        ob = k.sb("ob", [128, 128], BF16)
        ptr = k.ps("ptr", [128, 128], BF16)
        ost = k.sb("ost", [128, 512], BF16)
        nkc = NT
        for hd in heads:
            for (dst, c_main, c_sw) in ((qT, C_DA_Q + hd * 128, hd * 128), (kT, C_DA_K + hd * 128, 512 + hd * 128)):
                wm = wl.get(w_in, w_in.t[l, :, c_main:c_main + 128], 128)
                ws = wl.get(P.ext["w_da_sw"], P.ext["w_da_sw"].t[l, :, c_sw:c_sw + 128], 128)
                for bi, (t0, n) in enumerate(tok_blocks(T)):
                    pa, pb = pp[0], pp[1]
                    k.mm([(lambda h, kk=kk: h.matmul(pa[:, :n], lhsT=wm[:, kk, :128], rhs=hxT[:, kk, t0:t0 + n],
                                                     start=(kk == 0), stop=(kk == KC - 1))) for kk in range(KC)],
                         [wm, hxT], [pa])
                    k.mm([(lambda h, kk=kk: h.matmul(pb[:, :n], lhsT=ws[:, kk, :128], rhs=hxT[:, kk, t0:t0 + n],
                                                     start=(kk == 0), stop=(kk == KC - 1))) for kk in range(KC)],
                         [ws, hxT], [pb])
                    k.op(k.dve, lambda h: h.tensor_tensor(out=t1[:, :n], in0=pa[:, :n], in1=ropeC[:, t0:t0 + n],
                                                          op=ALU.mult), [pa, ropeC], [t1])
                    k.op(k.dve, lambda h: h.tensor_tensor(out=t2[:, :n], in0=pb[:, :n], in1=ropeS[:, t0:t0 + n],
                                                          op=ALU.mult), [pb, ropeS], [t2])
                    k.op(k.dve, lambda h: h.tensor_tensor(out=dst[:, t0:t0 + n], in0=t1[:, :n], in1=t2[:, :n],
                                                          op=ALU.add), [t1, t2], [dst])
            wv = wl.get(w_in, w_in.t[l, :, C_DA_V + hd * 128:C_DA_V + (hd + 1) * 128], 128)
            for i in range(NT):
                pa = pp[i % 2]
                k.mm([(lambda h, kk=kk: h.matmul(pa[:, :128], lhsT=hxT[:, kk, i * 128:(i + 1) * 128], rhs=wv[:, kk, :128],
                                                 start=(kk == 0), stop=(kk == KC - 1))) for kk in range(KC)],
                     [wv, hxT], [pa])
                k.op(k.act, lambda h: h.copy(out=va[:, i, 0:128], in_=pa[:, :128]), [pa], [va])
            qsrc = qT
            if own:
                h0, h1 = CTX, CTX + SEQ // 2
                k.op(k.dve, lambda h: h.tensor_scalar(out=qTo[:], in0=qT[:, h0:h1], scalar1=selt[:, 0:1], scalar2=None,
                                                      op0=ALU.mult), [qT, selt], [qTo])
                k.op(k.dve, lambda h: h.scalar_tensor_tensor(out=qTo[:], in0=qT[:, h1:T], scalar=selt[:, 1:2], in1=qTo[:],
                                                             op0=ALU.mult, op1=ALU.add), [qT, selt, qTo], [qTo])
                qsrc = qTo
            if qblocks is None:
                qblocks = [(q0 + qb * 512, 512, list(range(NT))) for qb in range(nq // 512)]
            for (qs, nqb, ktiles) in qblocks:
                nqt = nqb // 128
                nkc = len(ktiles)
                glist = [c_ * 4 + q_ for c_ in range(2) for q_ in range(nqt)]

                def emit_S(kci):
                    kc = ktiles[kci]
                    for comp in range(2):
                        sp_ = sT2[comp][kci % 2]
                        r0 = comp * 64
                        k.mm([lambda h: h.matmul(sp_[:, :nqb], lhsT=kT[r0:r0 + 64, kc * 128:(kc + 1) * 128],
                                                 rhs=qsrc[r0:r0 + 64, qs:qs + nqb], start=True, stop=True)],
                             [kT, qsrc], [sp_])

                def emit_exp(kci):
                    for comp in range(2):
                        sp_ = sT2[comp][kci % 2]
                        pt_ = pT[comp][kci % 2]
                        k.op(k.act, lambda h: h.activation(out=pt_[:, :nqb], in_=sp_[:, :nqb], func=AF.Exp, scale=0.125),
                             [sp_], [pt_])

                def emit_PV(kci):
                    kc = ktiles[kci]
                    for comp in range(2):
                        pt_ = pT[comp][kci % 2]
                        fns = []
                        for qt in range(nqt):
                            g = comp * 4 + qt
                            ot = oacc[g // 3]
                            fns.append(lambda h, ot=ot, g=g, qt=qt: h.matmul(
                                ot[:, g % 3, 0:129], lhsT=pt_[:, qt * 128:(qt + 1) * 128], rhs=va[:, kc, 0:129],
                                start=(kci == 0 and g == min(x for x in glist if x // 3 == g // 3)), stop=(kci == nkc - 1),
                                skip_group_check=True))
                        k.mm(fns, [pt_, va], oacc)

                emit_S(0)
                emit_exp(0)
                for kci in range(nkc):
                    if kci + 1 < nkc:
                        emit_S(kci + 1)
                    emit_PV(kci)
                    if kci + 1 < nkc:
                        emit_exp(kci + 1)
                osb = osbs[blk_i[0] % 2]
                blk_i[0] += 1
                for j in range(3):
                    k.op(k.dve, lambda h: h.tensor_copy(out=osb[:, j, :, :], in_=oacc[j][:]), [oacc[j]], [osb])
                for qt in range(nqt):
                    g0, g1 = qt, 4 + qt
                    k.op(k.dve, lambda h: h.reciprocal(out=fin[:, 0:1], in_=osb[:, g0 // 3, g0 % 3, 128:129]), [osb], [fin])
                    k.op(k.dve, lambda h: h.reciprocal(out=fin[:, 1:2], in_=osb[:, g1 // 3, g1 % 3, 128:129]), [osb], [fin])
                    k.op(k.dve, lambda h: h.tensor_tensor(out=fin[:, 1:2], in0=fin[:, 1:2], in1=lam[:, 3:4], op=ALU.mult),
                         [fin, lam], [fin])
                    k.op(k.dve, lambda h: h.tensor_scalar(out=o0[:], in0=osb[:, g0 // 3, g0 % 3, 0:128], scalar1=fin[:, 0:1],
                                                          scalar2=None, op0=ALU.mult), [osb, fin], [o0])
                    k.op(k.dve, lambda h: h.scalar_tensor_tensor(out=o1[:], in0=osb[:, g1 // 3, g1 % 3, 0:128], scalar=fin[:, 1:2],
                                                                 in1=o0[:], op0=ALU.mult, op1=ALU.add),
                         [osb, fin, o0], [o1])
                    k.op(k.dve, lambda h: h.tensor_tensor(out=junk[:], in0=o1[:], in1=o1[:], op=ALU.mult), [o1], [junk])
                    k.op(k.dve, lambda h: h.reduce_sum(out=fin[:, 2:3], in_=junk[:], axis=AX.X), [junk], [fin])
                    k.op(k.dve, lambda h: h.tensor_scalar(out=fin[:, 3:4], in0=fin[:, 2:3], scalar1=1.0 / 128, scalar2=EPS,
                                                          op0=ALU.mult, op1=ALU.add), [fin], [fin])
                    k.op(k.act, lambda h: h.sqrt(out=fin[:, 3:4], in_=fin[:, 3:4]), [fin], [fin])
                    k.op(k.dve, lambda h: h.reciprocal(out=fin[:, 3:4], in_=fin[:, 3:4]), [fin], [fin])
                    k.op(k.dve, lambda h: h.scalar_tensor_tensor(out=ob[:], in0=o1[:], scalar=fin[:, 3:4], in1=dn[:],
                                                                 op0=ALU.mult, op1=ALU.mult), [o1, fin, dn], [ob])
                    k.mm([lambda h: h.transpose(out=ptr[:], in_=ob[:], identity=P.ident[:])], [ob, P.ident], [ptr])
                    c0 = qs + qt * 128
                    if mixT_d is None:
                        k.op(k.dve, lambda h: h.tensor_copy(out=mixT[:, 4 + hd, c0:c0 + 128], in_=ptr[:]), [ptr], [mixT])
                    else:
                        k.op(k.dve, lambda h: h.tensor_copy(out=ost[:, qt * 128:(qt + 1) * 128], in_=ptr[:]), [ptr], [ost])
                if mixT_d is not None:
                    r0_ = 512 + hd * 128
                    k.dma(mixT_d, mixT_d.t[r0_:r0_ + 128, qs:qs + nqb], ost, ost[:, :nqb])


def rope_tables():
    n = np.arange(SEQ)
    row = (n // 64).astype(np.float64)
    col = (n % 64).astype(np.float64)
    inv = 10000.0 ** (-np.arange(16, dtype=np.float64) / 16)
    C = np.ones((64, T), np.float64)
    S = np.zeros((64, T), np.float64)
    for d in range(64):
        pos = row if d < 32 else col
        f = d % 16
        ang = pos * inv[f]
        C[d, CTX:] = np.cos(ang)
        sgn = -1.0 if (d % 32) < 16 else 1.0
        S[d, CTX:] = sgn * np.sin(ang)
    import ml_dtypes
    C2 = np.concatenate([C, C], 0).astype(np.float32).astype(ml_dtypes.bfloat16)
    S2 = np.concatenate([S, S], 0).astype(np.float32).astype(ml_dtypes.bfloat16)
    return C2, S2


def swap_cols(w_in):
    perm64 = np.concatenate([np.arange(16, 32), np.arange(0, 16), np.arange(48, 64), np.arange(32, 48)])
    idx = []
    for base in (C_DA_Q, C_DA_K):
        for u in range(8):
            idx.append(base + u * 64 + perm64)
    idx = np.concatenate(idx)
    return np.ascontiguousarray(w_in[:, :, idx])


def stage_proj_rec(P, l, hxT):
    k = P.k
    w_in = P.w_in
    R = P.rec
    with k.scope():
        wl = WLoader(k, nbuf=2, width=128, name="pw")
        pp = [k.ps("rp%d" % i, [128, 512]) for i in range(3)]
        stf = [k.sb("stf%d" % i, [128, 512], F32) for i in range(2)]
        stb = [k.sb("stb%d" % i, [128, 512], BF16) for i in range(2)]
        gb = k.sb("gbias", [128, 8], F32)
        for ty in range(4):
            for hp in range(2):
                for hh in range(2):
                    c = ty * 4 + hp * 2 + hh
                    k.dma(gb, gb[hh * 64:(hh + 1) * 64, ty * 2 + hp:ty * 2 + hp + 1], P.ml_gate_b,
                          P.ml_gate_b.t[l:l + 1, c:c + 1].to_broadcast([64, 1]))
        cnt = [0]

        def fm_job(wb, dst, row0, func=None, scale=1.0, bias=None, f32=False):
            def evac(p, t0, n):
                i = cnt[0] % 2
                cnt[0] += 1
                st = stf[i] if f32 else stb[i]
                if func is None and bias is None:
                    k.op(k.dve, lambda h: h.tensor_scalar(out=st[:, :n], in0=p[:, :n], scalar1=float(scale), scalar2=None,
                                                          op0=ALU.mult), [p], [st])
                elif func is None:
                    k.op(k.dve, lambda h: h.tensor_scalar(out=st[:, :n], in0=p[:, :n], scalar1=bias, scalar2=None,
                                                          op0=ALU.add), [p, gb], [st])
                else:
                    k.op(k.act, lambda h: h.activation(out=st[:, :n], in_=p[:, :n], func=func), [p], [st])
                k.dma(dst, dst.t[row0:row0 + 128, t0:t0 + n], st, st[:, :n])
            lin_fm(k, wb, 128, hxT, T, evac, pp)

        for c in range(2):
            fm_job(wl.get(w_in, w_in.t[l, :, C_ML_Q + c * 128:C_ML_Q + (c + 1) * 128], 128), R["ml_q"], c * 128)
            fm_job(wl.get(w_in, w_in.t[l, :, C_ML_K + c * 128:C_ML_K + (c + 1) * 128], 128), R["ml_k"], c * 128, scale=0.125)
            fm_job(wl.get(w_in, w_in.t[l, :, C_ML_O + c * 128:C_ML_O + (c + 1) * 128], 128), R["ml_o"], c * 128, func=AF.Sigmoid)
            fm_job(wl.get(w_in, w_in.t[l, :, C_HG_Q + c * 128:C_HG_Q + (c + 1) * 128], 128), R["hg_q"], c * 128, func=AF.Silu)
            fm_job(wl.get(w_in, w_in.t[l, :, C_HG_G + c * 128:C_HG_G + (c + 1) * 128], 128), R["hg_g"], c * 128, func=AF.Silu)
        for c in range(4):
            fm_job(wl.get(w_in, w_in.t[l, :, C_HG_F + c * 128:C_HG_F + (c + 1) * 128], 128), R["hg_f"], c * 128, f32=True)
        g16s = k.sb("g16s", [128, KC, 16], F32)
        k.dma(g16s, g16s[:], w_in, w_in.t[l, :, C_ML_G:C_ML_G + 16].rearrange("(k p) n -> p k n", p=128))
        wrep = [k.sb("wrep%d" % i, [128, KC, 128], BF16) for i in range(2)]
        for ty in range(4):
            for hp in range(2):
                wr = wrep[(ty * 2 + hp) % 2]
                for hh in range(2):
                    c = ty * 4 + hp * 2 + hh
                    k.op(k.dve, lambda h: h.tensor_copy(out=wr[:, :, hh * 64:(hh + 1) * 64],
                                                        in_=g16s[:, :, c:c + 1].to_broadcast([128, KC, 64])), [g16s], [wr])
                fm_job(wr, R["ml_g"], (ty * 2 + hp) * 128, bias=gb[:, ty * 2 + hp:ty * 2 + hp + 1], f32=True)
        for (c0, dst) in ((C_ML_V, R["ml_v"]), (C_HG_I, R["hg_v"])):
            wv = k.sb("wv", [128, KC, 256], BF16)
            for c in range(2):
                wb = wl.get(w_in, w_in.t[l, :, c0 + c * 128:c0 + (c + 1) * 128], 128)
                k.op(k.dve, lambda h: h.tensor_copy(out=wv[:, :, c * 128:(c + 1) * 128], in_=wb[:, :, :128]), [wb], [wv])

            def evac_v(p, i, dst=dst):
                j = cnt[0] % 2
                cnt[0] += 1
                st = stb[j]
                k.op(k.act, lambda h: h.copy(out=st[:, :256], in_=p[:, :256]), [p], [st])
                k.dma(dst, dst.t[i * 128:(i + 1) * 128, :], st, st[:, :256])
            lin_tm(k, wv, 256, hxT, list(range(NT)), evac_v, pp)


NCH = T // 16
SEGS = ((0, CTX), (CTX, T))


def seg_view(ap2d, si, dr):
    a, b = SEGS[si]
    if dr == 0:
        return ap2d[:, a:b]
    if a == 0:
        return ap2d[:, b - 1::-1]
    return ap2d[:, b - 1:a - 1:-1]


def stage_rec(P, l, mixer, mixT_d):
    k = P.k
    R = P.rec
    ml = (mixer == "ml")
    nv = 2 if ml else 1
    row_off = 0 if ml else 256
    with k.scope():
        cst = P.ext
        maskb = k.sb("maskb", [128, 2, 128], BF16)
        k.dma(maskb, maskb[:], cst["cmask"], cst["cmask"].t[:, :, :])
        bm = k.sb("bm", [128, 8], BF16)
        k.dma(bm, bm[:], cst["bmask"], cst["bmask"].t[:, :])
        blk = k.sb("blk", [128, 128], BF16)
        k.dma(blk, blk[:], cst["blk"], cst["blk"].t[:, :])
        if not ml:
            rmask = k.sb("rmask", [128, T], F32)
            k.op(k.dve, lambda h: h.memset(rmask[:], 1.0), [], [rmask])
            k.op(k.dve, lambda h: h.memset(rmask[:].rearrange("p (c i) -> p c i", i=16)[:, :, 0], 0.0), [], [rmask])
        zer = k.sb("zer", [128, 1], F32)
        k.op(k.dve, lambda h: h.memset(zer[:], 0.0), [], [zer])
        onesT = k.sb("onesT", [128, 64], BF16)
        k.op(k.dve, lambda h: h.memset(onesT[:], 1.0), [], [onesT])
        vones = k.sb("vones", [128, 8, 64], BF16)
        k.op(k.dve, lambda h: h.tensor_copy(out=vones[:], in_=bm[:].unsqueeze(2).to_broadcast([128, 8, 64])), [bm], [vones])

        qAs = k.sb("qA", [128, T], BF16) if ml else None
        qAd = [qAs, qAs] if ml else [k.sb("qA%d" % i, [128, T], BF16) for i in range(2)]
        kAd = [k.sb("kA%d" % i, [128, T], BF16) for i in range(2)]
        x3d = [k.sb("x3%d" % i, [128, T], BF16) for i in range(2)]
        cld = [k.sb("cl%d" % i, [128, T], BF16) for i in range(2)] if ml else None
        tA = k.sb("tA", [128, T], F32)
        tB = k.sb("tB", [128, T], F32)
        tC = k.sb("tC", [128, T], F32)
        osum = k.sb("osum", [128, T], F32)
        gate = k.sb("gate", [128, T], BF16)
        vt = k.sb("vt", [128, NT, 128], BF16)
        Rj = k.sb("Rj", [128, NCH], F32)
        Rp = k.sb("Rp", [128, NCH], F32)
        Dcd = [k.sb("Dc%d" % i, [128, NCH], F32) for i in range(2)]
        sc = k.sb("sc", [128, 8], F32)
        nw = k.sb("nw", [128, 1], F32)

        bankA = [k.ps("bkA%d" % i, [128, 512]) for i in range(2)]
        pdSd = [[k.ps("pdS%d_%d" % (i, j), [128, 8, 64]) for j in range(nv)] for i in range(2)]
        pktd = [k.ps("pkt%d" % i, [128, 128], BF16) for i in range(2)]
        AmTd = [[k.sb("AmT%d_%d" % (i, j), [128, 2, 128], BF16) for j in range(2)] for i in range(2)]
        qzd = [[k.sb("qz%d_%d" % (i, j), [128, 2, 128], BF16) for j in range(2)] for i in range(2)]
        for i in range(2):
            for j in range(2):
                k.op(k.dve, lambda h: h.memset(qzd[i][j][:], 0.0), [], [qzd[i][j]])
        kStd = [k.sb("kSt%d" % i, [128, 128], BF16) for i in range(2)]
        Vexpd = [k.sb("Vexp%d" % i, [128, 2, 8, 64], BF16) for i in range(2)]
        Shd = [[k.sb("Sh%d_%d" % (i, j), [128, 9, 64], F32) for j in range(nv)] for i in range(2)]
        Sbd = [[k.sb("Sb%d_%d" % (i, j), [128, 8, 64], BF16) for j in range(nv)] for i in range(2)]
        nsb = [[k.sb("nsb%d_%d" % (i, j), [128, 128], F32) for j in range(2)] for i in range(2)]
        dsb = [[k.sb("dsb%d_%d" % (i, j), [128, 128], F32) for j in range(2)] for i in range(2)] if ml else None
        fq = k.sb("fq", [128, 512], BF16)
        fr_ = k.sb("fr_", [128, 512], F32)
        fy = k.sb("fy", [128, 512], F32)
        fo = [k.sb("fo%d" % i, [128, 512], BF16) for i in range(2)]

        def c16(v):
            return v.rearrange("p (c i) -> p c i", i=16)

        def tile_of(ip, dr):
            if ip < 2:
                return ip if dr == 0 else 1 - ip
            return ip if dr == 0 else (NT - 1) - (ip - 2)

        for hp in range(2):
            rows = slice(hp * 128, (hp + 1) * 128)
            src_v = R["ml_v"] if ml else R["hg_v"]
            for i_ in range(NT):
                k.dma(vt, vt[:, i_, :], src_v, src_v.t[i_ * 128:(i_ + 1) * 128, hp * 128:(hp + 1) * 128])
            src_g = R["ml_o"] if ml else R["hg_g"]
            k.dma(gate, gate[:], src_g, src_g.t[rows, :])
            nsrc = P.ml_norm if ml else P.hg_norm
            k.dma(nw, nw[:], nsrc, nsrc.t[l:l + 1, rows].rearrange("o p -> p o"), allow_slow_non_contiguous=True)
            k.op(k.pool, lambda h: h.memset(osum[:], 0.0), [], [osum])
            if not ml:
                if l == 0:
                    k.op(k.dve, lambda h: h.memset(sc[:, 0:1], 0.0), [], [sc])
                else:
                    lg = P.hg_lb_logits
                    k.dma(sc, sc[:, 4:5], lg, lg.t[0:1, rows].rearrange("o p -> p o"), allow_slow_non_contiguous=True)
                    k.dma(sc, sc[:, 5:6], lg, lg.t[1:2, rows].rearrange("o p -> p o"), allow_slow_non_contiguous=True)
                    k.op(k.dve, lambda h: h.tensor_tensor(out=sc[:, 6:7], in0=sc[:, 5:6], in1=sc[:, 4:5], op=ALU.subtract), [sc], [sc])
                    k.op(k.act, lambda h: h.activation(out=sc[:, 0:1], in_=sc[:, 6:7], func=AF.Sigmoid), [sc], [sc])
                k.op(k.dve, lambda h: h.tensor_scalar(out=sc[:, 1:2], in0=sc[:, 0:1], scalar1=-1.0, scalar2=1.0,
                                                      op0=ALU.mult, op1=ALU.add), [sc], [sc])
                k.op(k.dve, lambda h: h.tensor_scalar(out=sc[:, 2:3], in0=sc[:, 1:2], scalar1=-1.0, scalar2=None,
                                                      op0=ALU.mult), [sc], [sc])
            if ml:
                k.dma(qAs, qAs[:], R["ml_q"], R["ml_q"].t[rows, :])
            for dr in range(2):
                qA, kA, x3, Dc = qAd[dr], kAd[dr], x3d[dr], Dcd[dr]
                if ml:
                    k.dma(kA, kA[:], R["ml_k"], R["ml_k"].t[rows, :])
                    gi0 = ((dr * 2) * 2 + hp) * 128
                    gf0 = ((dr * 2 + 1) * 2 + hp) * 128
                    k.dma(tA, tA[:], R["ml_g"], R["ml_g"].t[gi0:gi0 + 128, :])
                    k.dma(tB, tB[:], R["ml_g"], R["ml_g"].t[gf0:gf0 + 128, :])
                    k.op(k.act, lambda h: h.activation(out=tB[:], in_=tB[:], func=AF.Exp, scale=-1.0), [tB], [tB])
                    k.op(k.act, lambda h: h.activation(out=tB[:], in_=tB[:], func=AF.Ln, bias=1.0), [tB], [tB])
                    for si in range(2):
                        ini = 0.0 if si == 0 else seg_view(tC[:], 0, dr)[:, CTX - 1:CTX]
                        ov, dv = seg_view(tC[:], si, dr), seg_view(tB[:], si, dr)
                        n = SEGS[si][1] - SEGS[si][0]
                        k.op(k.dve, lambda h: h.tensor_tensor_scan(out=ov, data0=dv, data1=zer[:].to_broadcast([128, n]),
                                                                   initial=ini, op0=ALU.add, op1=ALU.add), [tB, tC, zer], [tC])
                    k.op(k.dve, lambda h: h.tensor_tensor(out=tA[:], in0=tA[:], in1=tC[:], op=ALU.add), [tA, tC], [tA])
                    for si in range(2):
                        ini = 0.0 if si == 0 else seg_view(tB[:], 0, dr)[:, CTX - 1:CTX]
                        ov, dv = seg_view(tB[:], si, dr), seg_view(tA[:], si, dr)
                        k.op(k.dve, lambda h: h.tensor_tensor_scan(out=ov, data0=dv, data1=dv, initial=ini,
                                                                   op0=ALU.max, op1=ALU.max), [tA, tB], [tB])
                    for si in range(2):
                        j0, j1 = SEGS[si][0] // 16, SEGS[si][1] // 16
                        k.op(k.dve, lambda h: h.tensor_copy(out=Rj[:, j0:j1], in_=c16(seg_view(tB[:], si, dr))[:, :, 15]),
                             [tB], [Rj])
                    k.op(k.dve, lambda h: h.memset(Rp[:, 0:1], 0.0), [], [Rp])
                    k.op(k.dve, lambda h: h.tensor_copy(out=Rp[:, 1:NCH], in_=Rj[:, 0:NCH - 1]), [Rj], [Rp])
                    k.op(k.dve, lambda h: h.tensor_tensor(out=Dc[:], in0=Rp[:], in1=Rj[:], op=ALU.subtract), [Rp, Rj], [Dc])
                    k.op(k.act, lambda h: h.activation(out=Dc[:], in_=Dc[:], func=AF.Exp), [Dc], [Dc])
                    for si in range(2):
                        j0, j1 = SEGS[si][0] // 16, SEGS[si][1] // 16
                        rb = Rj[:, j0:j1].unsqueeze(2).to_broadcast([128, j1 - j0, 16])
                        db = Dc[:, j0:j1].unsqueeze(2).to_broadcast([128, j1 - j0, 16])
                        av, cv = c16(seg_view(tA[:], si, dr)), c16(seg_view(tC[:], si, dr))
                        k.op(k.dve, lambda h: h.tensor_tensor(out=av, in0=av, in1=rb, op=ALU.subtract), [tA, Rj], [tA])
                        k.op(k.dve, lambda h: h.tensor_tensor(out=cv, in0=cv, in1=rb, op=ALU.subtract), [tC, Rj], [tC])
                        k.op(k.pool, lambda h: h.tensor_tensor(out=c16(seg_view(x3[:], si, dr)), in0=c16(seg_view(qA[:], si, dr)),
                                                               in1=db, op=ALU.mult), [qA, Dc], [x3])
                    k.op(k.act, lambda h: h.activation(out=tA[:], in_=tA[:], func=AF.Exp), [tA], [tA])
                    k.op(k.act, lambda h: h.activation(out=cld[dr][:], in_=tC[:], func=AF.Exp), [tC], [cld[dr]])
                    k.op(k.dve, lambda h: h.tensor_tensor(out=kA[:], in0=kA[:], in1=tA[:], op=ALU.mult), [kA, tA], [kA])
                else:
                    k.dma(qA, qA[:], R["hg_q"], R["hg_q"].t[rows, :])
                    f0 = dr * 256 + hp * 128
                    k.dma(tA, tA[:], R["hg_f"], R["hg_f"].t[f0:f0 + 128, :])
                    k.op(k.act, lambda h: h.activation(out=tB[:], in_=tA[:], func=AF.Sigmoid), [tA], [tB])
                    k.op(k.dve, lambda h: h.tensor_scalar(out=tC[:], in0=tB[:], scalar1=sc[:, 2:3], scalar2=sc[:, 1:2],
                                                          op0=ALU.mult, op1=ALU.add), [tB, sc], [tC])
                    k.op(k.dve, lambda h: h.tensor_scalar(out=tB[:], in0=tB[:], scalar1=sc[:, 1:2], scalar2=sc[:, 0:1],
                                                          op0=ALU.mult, op1=ALU.add), [tB, sc], [tB])
                    k.op(k.act, lambda h: h.activation(out=tB[:], in_=tB[:], func=AF.Ln), [tB], [tB])
                    for si in range(2):
                        j0, j1 = SEGS[si][0] // 16, SEGS[si][1] // 16
                        ov = c16(seg_view(tA[:], si, dr))
                        nseg = SEGS[si][1] - SEGS[si][0]
                        k.op(k.dve, lambda h: h.tensor_tensor_scan(out=seg_view(tA[:], si, dr), data0=rmask[:, 0:nseg],
                                                                   data1=seg_view(tB[:], si, dr), initial=0.0,
                                                                   op0=ALU.mult, op1=ALU.add), [tB, rmask, tA], [tA])
                        k.op(k.dve, lambda h: h.tensor_copy(out=Rj[:, j0:j1], in_=ov[:, :, 15]), [tA], [Rj])
                    k.op(k.act, lambda h: h.activation(out=Dc[:], in_=Rj[:], func=AF.Exp), [Rj], [Dc])
                    k.op(k.act, lambda h: h.activation(out=tB[:], in_=tA[:], func=AF.Exp), [tA], [tB])
                    k.op(k.dve, lambda h: h.tensor_tensor(out=qA[:], in0=qA[:], in1=tB[:], op=ALU.mult), [qA, tB], [qA])
                    k.op(k.act, lambda h: h.activation(out=tB[:], in_=tA[:], func=AF.Exp, scale=-1.0), [tA], [tB])
                    k.op(k.dve, lambda h: h.tensor_tensor(out=kA[:], in0=tC[:], in1=tB[:], op=ALU.mult), [tC, tB], [kA])
                    for si in range(2):
                        j0, j1 = SEGS[si][0] // 16, SEGS[si][1] // 16
                        rb = Rj[:, j0:j1].unsqueeze(2).to_broadcast([128, j1 - j0, 16])
                        av = c16(seg_view(tA[:], si, dr))
                        k.op(k.dve, lambda h: h.tensor_tensor(out=av, in0=rb, in1=av, op=ALU.subtract), [tA, Rj], [tA])
                    k.op(k.act, lambda h: h.activation(out=tA[:], in_=tA[:], func=AF.Exp), [tA], [tA])
                    k.op(k.dve, lambda h: h.tensor_tensor(out=x3[:], in0=tC[:], in1=tA[:], op=ALU.mult), [tC, tA], [x3])

            def qI_of(dr):
                return x3d[dr] if ml else qAd[dr]

            def kS_of(dr):
                return kAd[dr] if ml else x3d[dr]

            def pA_of(dr):
                return bankA[dr][:, 0:256].rearrange("p (h t) -> p h t", h=2)

            def po_of(dr, vs):
                return bankA[dr][:, 256 + vs * 128:256 + (vs + 1) * 128]

            def front_mm(ip):
                for dr in range(2):
                    c0 = tile_of(ip, dr) * 128
                    qz = qzd[dr][ip % 2]
                    for hh in range(2):
                        rr = slice(hh * 64, (hh + 1) * 64)
                        k.op(k.pool, lambda h: h.tensor_copy(out=qz[rr, hh, :], in_=qAd[dr][rr, c0:c0 + 128]), [qAd[dr]], [qz])
                    k.mm([lambda h: h.matmul(bankA[dr][:, 0:256], lhsT=kAd[dr][:, c0:c0 + 128],
                                             rhs=qz[:].rearrange("p h t -> p (h t)"), start=True, stop=True)],
                         [kAd[dr], qz], [bankA[dr]])
                    kS = kS_of(dr)
                    k.mm([lambda h: h.transpose(out=pktd[dr][:], in_=kS[:, c0:c0 + 128], identity=P.ident[:])],
                         [kS, P.ident], [pktd[dr]])

            def front_ev(ip):
                for dr in range(2):
                    it = tile_of(ip, dr)
                    AmT = AmTd[dr][ip % 2]
                    k.op(k.dve, lambda h: h.tensor_tensor(out=AmT[:], in0=pA_of(dr),
                                                          in1=maskb[:, dr:dr + 1, :].to_broadcast([128, 2, 128]),
                                                          op=ALU.mult), [bankA[dr], maskb], [AmT])
                    k.op(k.act, lambda h: h.copy(out=kStd[dr][:], in_=pktd[dr][:]), [pktd[dr]], [kStd[dr]])
                    k.op(k.pool, lambda h: h.tensor_tensor(
                        out=Vexpd[dr][:], in0=vt[:, it, :].rearrange("p (h e) -> p h e", h=2).unsqueeze(2).to_broadcast([128, 2, 8, 64]),
                        in1=bm[:].unsqueeze(1).unsqueeze(3).to_broadcast([128, 2, 8, 64]), op=ALU.mult), [vt, bm], [Vexpd[dr]])

            def dS_mm(ip):
                for dr in range(2):
                    for vs in range(nv):
                        for hh in range(2):
                            rr = slice(hh * 64, (hh + 1) * 64)
                            rhs = Vexpd[dr][:, hh, :, :] if vs == 0 else vones[:]
                            k.mm([lambda h: h.matmul(pdSd[dr][vs][rr, :, :], lhsT=kStd[dr][:, rr], rhs=rhs, start=True, stop=True)],
                                 [kStd[dr], Vexpd[dr], vones], [pdSd[dr][vs]])

            def steps(ip):
                for dr in range(2):
                    for vs in range(nv):
                        Sh = Shd[dr][vs]
                        k.op(k.pool, lambda h: h.tensor_copy(out=Sh[:, 0, :], in_=Sh[:, 8, :]), [Sh], [Sh])
                for cl in range(8):
                    for dr in range(2):
                        cp = cl if dr == 0 else 7 - cl
                        j = ip * 8 + cl
                        for vs in range(nv):
                            Sh = Shd[dr][vs]
                            k.op(k.dve, lambda h: h.scalar_tensor_tensor(out=Sh[:, cl + 1, :], in0=Sh[:, cl, :],
                                                                         scalar=Dcd[dr][:, j:j + 1], in1=pdSd[dr][vs][:, cp, :],
                                                                         op0=ALU.mult, op1=ALU.add),
                                 [Sh, Dcd[dr], pdSd[dr][vs]], [Sh])
                for dr in range(2):
                    for vs in range(nv):
                        k.op(k.act, lambda h: h.copy(out=Sbd[dr][vs][:], in_=Shd[dr][vs][:, 0:8, :]), [Shd[dr][vs]], [Sbd[dr][vs]])

            def out_mm(ip):
                for dr in range(2):
                    it = tile_of(ip, dr)
                    c0 = it * 128
                    AmT = AmTd[dr][ip % 2]
                    qI = qI_of(dr)
                    for vs in range(nv):
                        fns = []
                        for hh in range(2):
                            rr = slice(hh * 64, (hh + 1) * 64)
                            lh = vt[:, it, hh * 64:(hh + 1) * 64] if vs == 0 else onesT[:]
                            fns.append(lambda h, rr=rr, lh=lh, hh=hh: h.matmul(po_of(dr, vs)[rr, :], lhsT=lh, rhs=AmT[:, hh, :],
                                                                               start=True, stop=False, skip_group_check=True))
                            for cl in range(8):
                                cp = cl if dr == 0 else 7 - cl
                                fns.append(lambda h, rr=rr, cl=cl, cp=cp: h.matmul(
                                    po_of(dr, vs)[rr, cp * 16:(cp + 1) * 16], lhsT=Sbd[dr][vs][rr, cl, :],
                                    rhs=qI[rr, c0 + cp * 16:c0 + (cp + 1) * 16], start=False, stop=(cl == 7),
                                    skip_group_check=True))
                        k.mm(fns, [vt, onesT, AmT, Sbd[dr][vs], qI], [bankA[dr]])

            def evac_a(ip):
                for dr in range(2):
                    n_ = nsb[dr][ip % 2]
                    k.op(k.act, lambda h: h.copy(out=n_[:], in_=po_of(dr, 0)), [bankA[dr]], [n_])
                    if ml:
                        d_ = dsb[dr][ip % 2]
                        k.op(k.act, lambda h: h.activation(out=d_[:], in_=po_of(dr, 1), func=AF.Abs), [bankA[dr]], [d_])

            def evac_b(ip):
                for dr in range(2):
                    c0 = tile_of(ip, dr) * 128
                    n_ = nsb[dr][ip % 2]
                    if ml:
                        d_ = dsb[dr][ip % 2]
                        k.op(k.dve, lambda h: h.tensor_tensor(out=d_[:], in0=d_[:], in1=cld[dr][:, c0:c0 + 128], op=ALU.max),
                             [cld[dr], d_], [d_])
                        k.op(k.dve, lambda h: h.reciprocal(out=d_[:], in_=d_[:]), [d_], [d_])
                        k.op(k.dve, lambda h: h.tensor_tensor(out=n_[:], in0=n_[:], in1=d_[:], op=ALU.mult), [n_, d_], [n_])
                    k.op(k.pool, lambda h: h.tensor_tensor(out=osum[:, c0:c0 + 128], in0=osum[:, c0:c0 + 128], in1=n_[:],
                                                           op=ALU.add), [osum, n_], [osum])

            for dr in range(2):
                for vs in range(nv):
                    k.op(k.dve, lambda h: h.memset(Shd[dr][vs][:, 8, :], 0.0), [], [Shd[dr][vs]])
            front_mm(0)
            front_ev(0)
            dS_mm(0)
            for ip in range(NT):
                if ip + 1 < NT:
                    front_mm(ip + 1)
                    front_ev(ip + 1)
                steps(ip)
                if ip > 0:
                    evac_b(ip - 1)
                if ip + 1 < NT:
                    dS_mm(ip + 1)
                out_mm(ip)
                evac_a(ip)
            evac_b(NT - 1)

            pss = pdSd[0][0]
            for bi, (t0, n) in enumerate(tok_blocks(T)):
                pv = pss[:].rearrange("p c e -> p (c e)")
                k.op(k.act, lambda h: h.activation(out=fq[:, :n], in_=osum[:, t0:t0 + n], func=AF.Square), [osum], [fq])
                k.mm([lambda h: h.matmul(pv[:, :n], lhsT=blk[:], rhs=fq[:, :n], start=True, stop=True)], [blk, fq], [pss])
                k.op(k.dve, lambda h: h.tensor_scalar(out=fr_[:, :n], in0=pv[:, :n], scalar1=1.0 / 64, scalar2=EPS,
                                                      op0=ALU.mult, op1=ALU.add), [pss], [fr_])
                k.op(k.act, lambda h: h.sqrt(out=fr_[:, :n], in_=fr_[:, :n]), [fr_], [fr_])
                k.op(k.dve, lambda h: h.reciprocal(out=fr_[:, :n], in_=fr_[:, :n]), [fr_], [fr_])
                k.op(k.dve, lambda h: h.tensor_tensor(out=fy[:, :n], in0=osum[:, t0:t0 + n], in1=fr_[:, :n], op=ALU.mult),
                     [osum, fr_], [fy])
                ob = fo[bi % 2]
                k.op(k.dve, lambda h: h.scalar_tensor_tensor(out=ob[:, :n], in0=fy[:, :n], scalar=nw[:, 0:1], in1=gate[:, t0:t0 + n],
                                                             op0=ALU.mult, op1=ALU.mult), [fy, nw, gate], [ob])
                r0 = row_off + hp * 128
                k.dma(mixT_d, mixT_d.t[r0:r0 + 128, t0:t0 + n], ob, ob[:, :n])


def rec_consts():
    import ml_dtypes
    s = np.arange(128)[:, None]
    t = np.arange(128)[None, :]
    same = (s // 16) == (t // 16)
    cm = np.stack([(same & (s <= t)), (same & (s >= t))], 1).astype(np.float32)
    bmk = ((s // 16) == np.arange(8)[None, :]).astype(np.float32)
    blk = ((s // 64) == (t // 64)).astype(np.float32)
    b16 = ml_dtypes.bfloat16
    return cm.astype(b16), bmk.astype(b16), blk.astype(b16)


def stage_postmix(P, l, mixT_d, src, dst, tiles, all_latent=False):
    k = P.k
    with k.scope():
        wl = WLoader(k, nbuf=2, width=512, name="ow")
        wo = k.sb("wo", [128, KC, 1024], BF16)
        for hf in range(2):
            wb = wl.get(P.w_out, P.w_out.t[l, :, hf * 512:(hf + 1) * 512], 512)
            k.op(k.dve, lambda h: h.tensor_copy(out=wo[:, :, hf * 512:(hf + 1) * 512], in_=wb[:, :, :512]), [wb], [wo])
        gpb = k.sb("gpb", [128, 2, 1024], F32)
        for kind in range(2):
            k.dma(gpb, gpb[:, kind, :], P.modc[l], P.modc[l].t[kind:kind + 1, 2, :].to_broadcast([128, 1024]))
        mb = [k.sb("mb%d" % i, [128, KC, 128], BF16) for i in range(2)]
        xt = [k.sb("pxt%d" % i, [128, 1024], F32) for i in range(2)]
        pm = [k.ps("pmx%d" % i, [128, 512]) for i in range(4)]
        ep = Epilogue(k)
        for n, i in enumerate(tiles):
            m, x = mb[n % 2], xt[n % 2]
            k.dma(m, m[:], mixT_d, mixT_d.t[:, i * 128:(i + 1) * 128].rearrange("(k p) t -> p k t", p=128))
            k.dma(x, x[:], src, src.t[i * 128:(i + 1) * 128, :])
            ps = []
            for hf in range(2):
                p = pm[(n % 2) * 2 + hf]
                k.mm([(lambda h, kk=kk: h.matmul(p[:], lhsT=m[:, kk, :], rhs=wo[:, kk, hf * 512:(hf + 1) * 512],
                                                 start=(kk == 0), stop=(kk == KC - 1))) for kk in range(KC)], [m, wo], [p])
                ps.append(p)
            ep.run([(p, None) for p in ps], x, gpb, 1 if (i < 2 and not all_latent) else 0, dst, dst.t[i * 128:(i + 1) * 128, :])


class Epilogue:
    def __init__(self, k, need_y=True):
        self.k = k
        self.y = k.sb("ep_y", [128, 1024], F32) if need_y else None
        self.junk = k.sb("ep_j", [128, 1024], BF16)
        self.s = k.sb("ep_s", [128, 2], F32)
        self.o = [k.sb("ep_o%d" % i, [128, 1024], F32) for i in range(2)]
        self.n = 0

    def run(self, halves, x, gpb, kind, dst, dst_ap, ysb=None):
        k = self.k
        y, s = self.y, self.s
        if ysb is None:
            for hf, (p, _) in enumerate(halves):
                eng = k.act if hf == 0 else k.dve
                if hf == 0:
                    k.op(k.act, lambda h: h.copy(out=y[:, 0:512], in_=p[:]), [p], [y])
                else:
                    k.op(k.dve, lambda h: h.tensor_copy(out=y[:, 512:1024], in_=p[:]), [p], [y])
        else:
            ybuf, yap = ysb
        o = self.o[self.n % 2]
        self.n += 1
        k.op(k.dve, lambda h: h.memset(s[:], 0.0), [], [s])
        if ysb is None:
            ybuf, yap = y, y[:]
        k.op(k.act, lambda h: h.activation(out=self.junk[:], in_=yap, func=AF.Square, accum_out=s[:, 0:1]), [ybuf, s], [self.junk, s])
        k.op(k.dve, lambda h: h.tensor_scalar(out=s[:, 1:2], in0=s[:, 0:1], scalar1=1.0 / D, scalar2=EPS,
                                              op0=ALU.mult, op1=ALU.add), [s], [s])
        k.op(k.act, lambda h: h.sqrt(out=s[:, 1:2], in_=s[:, 1:2]), [s], [s])
        k.op(k.dve, lambda h: h.reciprocal(out=s[:, 1:2], in_=s[:, 1:2]), [s], [s])
        k.op(k.dve, lambda h: h.scalar_tensor_tensor(out=o[:], in0=yap, scalar=s[:, 1:2], in1=gpb[:, kind, :],
                                                     op0=ALU.mult, op1=ALU.mult), [ybuf, s, gpb], [o])
        k.op(k.dve, lambda h: h.tensor_tensor(out=o[:], in0=o[:], in1=x[:], op=ALU.add), [o, x], [o])
        k.dma(dst, dst_ap, o, o[:])


def stage_ffn(P, l, src, dst, tiles, dst_row, moe, all_latent=False):
    k = P.k
    nff = (FF_EXP if moe else FF_DENSE) // 128
    nexp = NEXP if moe else 1
    bsz = 8 if moe else 12
    for b0 in range(0, len(tiles), bsz):
        bt = tiles[b0:b0 + bsz]
        nb = len(bt)
        ntok = nb * 128
        with k.scope():
            hxb = k.sb("hxb", [128, KC, bsz * 128], BF16)
            stage_norm(P, src, l, 1, hxb, bt, all_latent=all_latent)
            with k.scope():
                HT = k.sb("HT", [128, nff, bsz * 128], BF16)
                yacc = k.sb("yacc", [128, bsz, 1024], F32)
                wl = WLoader(k, nbuf=2, width=128, name="fw")
                wl2 = WLoader(k, nbuf=2, width=128, name="fu")
                wld = WLoader(k, nbuf=2, width=512, kc=4 if moe else 2, name="fd")
                pg = [k.ps("pg%d" % i, [128, 512]) for i in range(2)]
                pu = [k.ps("pu%d" % i, [128, 512]) for i in range(2)]
                pd = [k.ps("pd%d" % i, [128, 512]) for i in range(2)]
                sg = [k.sb("sg%d" % i, [128, 512], F32) for i in range(2)]
                gates = k.sb("gates", [128, 8, 8], F32)
                gp2 = k.sb("gp2x", [128, 2, 1024], F32)
                for kind_ in range(2):
                    k.dma(gp2, gp2[:, kind_, :], P.modc[l], P.modc[l].t[kind_:kind_ + 1, 5, :].to_broadcast([128, 1024]))
                if moe:
                    rws = k.sb("rws", [128, KC, 8], F32)
                    k.dma(rws, rws[:], P.router_w, P.router_w.t[0, :, :].rearrange("(k p) e -> p k e", p=128))
                    rwb = k.sb("rwb", [128, KC, 8], BF16)
                    k.op(k.dve, lambda h: h.tensor_copy(out=rwb[:], in_=rws[:]), [rws], [rwb])
                    rb = k.sb("rb", [128, 8], F32)
                    k.dma(rb, rb[:], P.router_b, P.router_b.t[0:1, :].to_broadcast([128, 8]))
                    lg = k.sb("lg", [128, 8], F32)
                    l2 = k.sb("l2", [128, 8], F32)
                    mk = k.sb("mk", [128, 2, 8], F32)
                    mx = k.sb("mx", [128, 4], F32)
                    for ti in range(nb):
                        p = pd[ti % 2]
                        k.mm([(lambda h, kk=kk: h.matmul(p[:, 0:8], lhsT=hxb[:, kk, ti * 128:(ti + 1) * 128], rhs=rwb[:, kk, :],
                                                         start=(kk == 0), stop=(kk == KC - 1))) for kk in range(KC)], [hxb, rwb], [p])
                        k.op(k.dve, lambda h: h.tensor_tensor(out=lg[:], in0=p[:, 0:8], in1=rb[:], op=ALU.add), [p, rb], [lg])
                        k.op(k.dve, lambda h: h.reduce_max(out=mx[:, 0:1], in_=lg[:], axis=AX.X), [lg], [mx])
                        k.op(k.dve, lambda h: h.tensor_scalar(out=mk[:, 0, :], in0=lg[:], scalar1=mx[:, 0:1], scalar2=None,
                                                              op0=ALU.is_ge), [lg, mx], [mk])
                        k.op(k.dve, lambda h: h.scalar_tensor_tensor(out=l2[:], in0=mk[:, 0, :], scalar=-1e30, in1=lg[:],
                                                                     op0=ALU.mult, op1=ALU.add), [mk, lg], [l2])
                        k.op(k.dve, lambda h: h.reduce_max(out=mx[:, 1:2], in_=l2[:], axis=AX.X), [l2], [mx])
                        k.op(k.dve, lambda h: h.tensor_scalar(out=mk[:, 1, :], in0=l2[:], scalar1=mx[:, 1:2], scalar2=None,
                                                              op0=ALU.is_ge), [l2, mx], [mk])
                        k.op(k.dve, lambda h: h.tensor_tensor(out=mx[:, 2:3], in0=mx[:, 0:1], in1=mx[:, 1:2], op=ALU.subtract), [mx], [mx])
                        k.op(k.act, lambda h: h.activation(out=mx[:, 2:3], in_=mx[:, 2:3], func=AF.Sigmoid), [mx], [mx])
                        k.op(k.dve, lambda h: h.tensor_scalar(out=mx[:, 3:4], in0=mx[:, 2:3], scalar1=-1.0, scalar2=1.0,
                                                              op0=ALU.mult, op1=ALU.add), [mx], [mx])
                        k.op(k.dve, lambda h: h.tensor_scalar(out=gates[:, ti, :], in0=mk[:, 0, :], scalar1=mx[:, 2:3], scalar2=None,
                                                              op0=ALU.mult), [mk, mx], [gates])
                        k.op(k.dve, lambda h: h.scalar_tensor_tensor(out=gates[:, ti, :], in0=mk[:, 1, :], scalar=mx[:, 3:4],
                                                                     in1=gates[:, ti, :], op0=ALU.mult, op1=ALU.add),
                             [mk, mx, gates], [gates])
                for e in range(nexp):
                    if moe:
                        wg_b, wg_ap = P.moe_w_gate, P.moe_w_gate.t[0, e]
                        wu_b, wu_ap = P.moe_w_up, P.moe_w_up.t[0, e]
                        wd_b, wd_ap = P.moe_w_down, P.moe_w_down.t[0, e]
                    else:
                        wg_b, wg_ap = P.ffn_w_gate, P.ffn_w_gate.t[0]
                        wu_b, wu_ap = P.ffn_w_up, P.ffn_w_up.t[0]
                        wd_b, wd_ap = P.ffn_w_down, P.ffn_w_down.t[0]
                    for c in range(nff):
                        wg = wl.get(wg_b, wg_ap[:, c * 128:(c + 1) * 128], 128)
                        wu = wl2.get(wu_b, wu_ap[:, c * 128:(c + 1) * 128], 128)
                        for bi, (t0, n) in enumerate(tok_blocks(ntok)):
                            a, b_ = pg[bi % 2], pu[bi % 2]
                            k.mm([(lambda h, kk=kk: h.matmul(a[:, :n], lhsT=wg[:, kk, :128], rhs=hxb[:, kk, t0:t0 + n],
                                                             start=(kk == 0), stop=(kk == KC - 1))) for kk in range(KC)], [wg, hxb], [a])
                            k.mm([(lambda h, kk=kk: h.matmul(b_[:, :n], lhsT=wu[:, kk, :128], rhs=hxb[:, kk, t0:t0 + n],
                                                             start=(kk == 0), stop=(kk == KC - 1))) for kk in range(KC)], [wu, hxb], [b_])
                            s_ = sg[bi % 2]
                            k.op(k.act, lambda h: h.activation(out=s_[:, :n], in_=a[:, :n], func=AF.Silu), [a], [s_])
                            k.op(k.dve, lambda h: h.tensor_tensor(out=HT[:, c, t0:t0 + n], in0=s_[:, :n], in1=b_[:, :n], op=ALU.mult),
                                 [s_, b_], [HT])
                    kcd = wld.kc
                    for pc in range(nff // kcd):
                        for hf in range(2):
                            wd = wld.get(wd_b, wd_ap[pc * kcd * 128:(pc + 1) * kcd * 128, hf * 512:(hf + 1) * 512], 512)
                            for ti in range(nb):
                                p = pd[ti % 2]
                                k.mm([(lambda h, kk=kk: h.matmul(p[:], lhsT=HT[:, pc * kcd + kk, ti * 128:(ti + 1) * 128], rhs=wd[:, kk, :],
                                                                 start=(kk == 0), stop=(kk == kcd - 1))) for kk in range(kcd)], [HT, wd], [p])
                                ya = yacc[:, ti, hf * 512:(hf + 1) * 512]
                                first = (e == 0 and pc == 0)
                                if moe:
                                    g_ = gates[:, ti, e:e + 1]
                                    if first:
                                        k.op(k.dve, lambda h: h.tensor_scalar(out=ya, in0=p[:], scalar1=g_, scalar2=None, op0=ALU.mult),
                                             [p, gates], [yacc])
                                    else:
                                        k.op(k.dve, lambda h: h.scalar_tensor_tensor(out=ya, in0=p[:], scalar=g_, in1=ya,
                                                                                     op0=ALU.mult, op1=ALU.add), [p, gates, yacc], [yacc])
                                else:
                                    if first:
                                        k.op(k.act, lambda h: h.copy(out=ya, in_=p[:]), [p], [yacc])
                                    else:
                                        k.op(k.dve, lambda h: h.tensor_tensor(out=ya, in0=ya, in1=p[:], op=ALU.add), [p, yacc], [yacc])
                ep = Epilogue(k, need_y=False)
                xt = [k.sb("fx%d" % i, [128, 1024], F32) for i in range(2)]
                for ti, i in enumerate(bt):
                    x = xt[ti % 2]
                    k.dma(x, x[:], src, src.t[i * 128:(i + 1) * 128, :])
                    r0 = dst_row(i)
                    ep.run(None, x, gp2, 1 if (i < 2 and not all_latent) else 0, dst, dst.t[r0:r0 + 128, :], ysb=(yacc, yacc[:, ti, :]))


def stage_select(P, src, dst):
    k = P.k
    with k.scope():
        sel = k.sb("sel", [128, 2], F32)
        k.dma(sel, sel[:], P.ext["sel"], P.ext["sel"].t[:, :])
        a = [k.sb("sa%d" % i, [128, 1024], F32) for i in range(2)]
        b = [k.sb("sb%d" % i, [128, 1024], F32) for i in range(2)]
        for j in range(16):
            x, y = a[j % 2], b[j % 2]
            i0, i1 = 2 + j, 2 + 16 + j
            k.dma(x, x[:], src, src.t[i0 * 128:(i0 + 1) * 128, :])
            k.dma(y, y[:], src, src.t[i1 * 128:(i1 + 1) * 128, :])
            k.op(k.dve, lambda h: h.tensor_scalar(out=x[:], in0=x[:], scalar1=sel[:, 0:1], scalar2=None, op0=ALU.mult), [x, sel], [x])
            k.op(k.dve, lambda h: h.scalar_tensor_tensor(out=x[:], in0=y[:], scalar=sel[:, 1:2], in1=x[:], op0=ALU.mult, op1=ALU.add),
                 [x, y, sel], [x])
            k.dma(dst, dst.t[j * 128:(j + 1) * 128, :], x, x[:])


def stage_select_mix(P, src, dst):
    k = P.k
    with k.scope():
        sel = k.sb("sel", [128, 2], F32)
        k.dma(sel, sel[:], P.ext["sel"], P.ext["sel"].t[:, :])
        H = SEQ // 2
        a = [k.sb("ma%d" % i, [128, H], BF16) for i in range(2)]
        b = [k.sb("mb%d" % i, [128, H], BF16) for i in range(2)]
        for r in range(4):
            x, y = a[r % 2], b[r % 2]
            k.dma(x, x[:], src, src.t[r * 128:(r + 1) * 128, CTX:CTX + H])
            k.dma(y, y[:], src, src.t[r * 128:(r + 1) * 128, CTX + H:T])
            k.op(k.dve, lambda h: h.tensor_scalar(out=x[:], in0=x[:], scalar1=sel[:, 0:1], scalar2=None, op0=ALU.mult), [x, sel], [x])
            k.op(k.dve, lambda h: h.scalar_tensor_tensor(out=x[:], in0=y[:], scalar=sel[:, 1:2], in1=x[:], op0=ALU.mult, op1=ALU.add),
                 [x, y, sel], [x])
            k.dma(dst, dst.t[r * 128:(r + 1) * 128, :], x, x[:])


def build_program():
    P = Prog()
    k = P.k
    P.xin = P.ext_in("xin", [T, D])
    P.c2 = P.ext_in("c2", [2, D])
    P.w_mod = P.ext_in("w_mod", [2, D, 6 * D])
    P.b_mod = P.ext_in("b_mod", [2, 6 * D])
    for n in ("norm_pre_mix", "norm_post_mix", "norm_pre_ffn", "norm_post_ffn"):
        setattr(P, n, P.ext_in(n, [2, D]))
    P.ext_in("ident", [128, 128])
    P.ext_in("sel", [128, 2])
    P.w_in = P.ext_in("w_in", [2, D, PROJ_W])
    P.ext_in("w_da_sw", [2, D, 1024])
    P.w_out = P.ext_in("w_out", [2, D, D])
    P.ext_in("rope_c", [128, T], BF16)
    P.ext_in("rope_s", [128, T], BF16)
    P.ext_in("cmask", [128, 2, 128], BF16)
    P.ext_in("bmask", [128, 8], BF16)
    P.ext_in("blk", [128, 128], BF16)
    P.ml_gate_b = P.ext_in("ml_gate_b", [2, 16])
    P.ml_norm = P.ext_in("ml_norm", [2, 256])
    P.hg_norm = P.ext_in("hg_norm", [2, 256])
    P.hg_lb_logits = P.ext_in("hg_lb_logits", [2, 256])
    P.da_lambda = P.ext_in("da_lambda", [2, 4, 64])
    P.da_norm = P.ext_in("da_norm", [2, 128])
    P.ffn_w_gate = P.ext_in("ffn_w_gate", [1, D, FF_DENSE])
    P.ffn_w_up = P.ext_in("ffn_w_up", [1, D, FF_DENSE])
    P.ffn_w_down = P.ext_in("ffn_w_down", [1, FF_DENSE, D])
    P.router_w = P.ext_in("router_w", [1, D, NEXP])
    P.router_b = P.ext_in("router_b", [1, NEXP])
    P.moe_w_gate = P.ext_in("moe_w_gate", [1, NEXP, D, FF_EXP])
    P.moe_w_up = P.ext_in("moe_w_up", [1, NEXP, D, FF_EXP])
    P.moe_w_down = P.ext_in("moe_w_down", [1, NEXP, FF_EXP, D])
    out = P.ext_out("out", [SEQ // 2, D])
    P.modc = [k.dram("modc%d" % l, [2, 6, D], F32) for l in range(2)]
    P.rec = dict(ml_q=k.dram("ml_q", [256, T], BF16), ml_k=k.dram("ml_k", [256, T], BF16), ml_o=k.dram("ml_o", [256, T], BF16),
                 ml_g=k.dram("ml_g", [1024, T], F32), hg_q=k.dram("hg_q", [256, T], BF16), hg_g=k.dram("hg_g", [256, T], BF16),
                 hg_f=k.dram("hg_f", [512, T], F32), ml_v=k.dram("ml_v", [T, 256], BF16), hg_v=k.dram("hg_v", [T, 256], BF16))
    mixT_d = k.dram("mixT_d", [1024, T], BF16)
    xmid = [k.dram("xmid%d" % l, [T, D], F32) for l in range(2)]
    x1 = k.dram("x1", [T, D], F32)
    xown = k.dram("xown", [SEQ // 2, D], F32)
    mixown = k.dram("mixown", [1024, SEQ // 2], BF16)
    setup_consts(P)
    stage_mod(P, 0)
    stage_mod(P, 1)
    lat_blocks = [(CTX + qb * 512, 512, list(range(NT))) for qb in range(SEQ // 512)]
    for l in range(2):
        src = P.xin if l == 0 else x1
        lam_init = 0.8 - 0.6 * float(np.exp(-0.3 * l))
        with k.scope():
            hxT = k.sb("hxT", [128, KC, T], BF16)
            stage_norm(P, src, l, 0, hxT, list(range(NT)))
            stage_proj_rec(P, l, hxT)
            if l == 0:
                qbl = [(0, CTX, [0, 1])] + list(lat_blocks)
                stage_attn(P, l, hxT, None, 0, 0, lam_init, qblocks=qbl, mixT_d=mixT_d)
            else:
                qbl = [(qb * 512, 512, list(range(NT))) for qb in range(SEQ // 2 // 512)]
                stage_attn(P, l, hxT, None, 0, 0, lam_init, qblocks=qbl, mixT_d=mixown, own=True)
        stage_rec(P, l, "ml", mixT_d)
        stage_rec(P, l, "hg", mixT_d)
        if l == 0:
            stage_postmix(P, l, mixT_d, src, xmid[0], list(range(NT)))
            stage_ffn(P, 0, xmid[0], x1, list(range(NT)), lambda i: i * 128, moe=False)
        else:
            stage_select_mix(P, mixT_d, mixown)
            stage_select(P, x1, xown)
            stage_postmix(P, l, mixown, xown, xmid[1], list(range(16)), all_latent=True)
            stage_ffn(P, 1, xmid[1], out, list(range(16)), lambda i: i * 128, moe=True, all_latent=True)
    k.finish([out])
    return k.nc


_CACHE = {}


def kernel(x, c, ctx, c_ctx, w_mod, b_mod, norm_pre_mix, norm_post_mix, norm_pre_ffn, norm_post_ffn,
           w_in, w_out, ml_gate_b, ml_norm, hg_lb_logits, hg_norm, da_lambda, da_norm,
           ffn_w_gate, ffn_w_up, ffn_w_down, router_w, router_b, moe_w_gate, moe_w_up, moe_w_down):
    from concourse.bass_utils import run_bass_kernel_spmd
    f = lambda a: np.ascontiguousarray(np.asarray(a, dtype=np.float32))
    x, c, ctx, c_ctx = f(x), f(c), f(ctx), f(c_ctx)
    w_in = f(w_in)
    if "nc" not in _CACHE:
        _CACHE["nc"] = build_program()
    nc = _CACHE["nc"]
    rc, rs = rope_tables()
    cm, bmk, blk = rec_consts()
    shared = dict(w_mod=f(w_mod), b_mod=f(b_mod), norm_pre_mix=f(norm_pre_mix), norm_post_mix=f(norm_post_mix),
                  norm_pre_ffn=f(norm_pre_ffn), norm_post_ffn=f(norm_post_ffn), ident=np.eye(128, dtype=np.float32),
                  w_in=w_in, w_da_sw=swap_cols(w_in), w_out=f(w_out), rope_c=rc, rope_s=rs, cmask=cm, bmask=bmk, blk=blk,
                  ml_gate_b=f(ml_gate_b), ml_norm=f(ml_norm), hg_norm=f(hg_norm), hg_lb_logits=f(hg_lb_logits),
                  da_lambda=f(da_lambda), da_norm=f(da_norm), ffn_w_gate=f(ffn_w_gate), ffn_w_up=f(ffn_w_up),
                  ffn_w_down=f(ffn_w_down), router_w=f(router_w), router_b=f(router_b), moe_w_gate=f(moe_w_gate),
                  moe_w_up=f(moe_w_up), moe_w_down=f(moe_w_down))
    in_maps = []
    for core in range(8):
        b, half = core // 2, core % 2
        m = dict(shared)
        m["xin"] = np.ascontiguousarray(np.concatenate([ctx[b], x[b]], axis=0))
        m["c2"] = np.ascontiguousarray(np.stack([c[b], c_ctx], axis=0))
        sel = np.zeros((128, 2), np.float32)
        sel[:, half] = 1.0
        m["sel"] = sel
        in_maps.append(m)
    res = run_bass_kernel_spmd(nc, in_maps, core_ids=list(range(8)))
    out = np.zeros((4, SEQ, D), np.float32)
    for core in range(8):
        b, half = core // 2, core % 2
        out[b, half * (SEQ // 2):(half + 1) * (SEQ // 2)] = np.asarray(res.results[core]["out"], dtype=np.float32)
    return out
```
